# Optimizing a Trainium2 kernel written in Bass

```python
import math
import jax, jax.numpy as jnp
from jax import lax
import numpy as np

D_MODEL = 1024
BATCH = 8
SEQ = 4096
DEPTH = 4

N_MEM = 256
N_MIXERS = 3
HEAD_DIM = 64
MIX_HEADS = 12
MIX_WIDTH = MIX_HEADS * HEAD_DIM
XA_HEADS = 4
XA_WIDTH = XA_HEADS * HEAD_DIM
OUT_IN = MIX_WIDTH + XA_WIDTH
ROPE_THETA = 500000.0
ROT_DIM = HEAD_DIM // 4
LN_EPS = 1e-5
RMS_EPS = 1e-6
DEEPNORM_ALPHA = (2.0 * DEPTH) ** 0.25
DEEPNORM_BETA = (8.0 * DEPTH) ** -0.25

NSA_KV_HEADS = 4
NSA_GROUP = MIX_HEADS // NSA_KV_HEADS
NSA_KV_WIDTH = NSA_KV_HEADS * HEAD_DIM
CMP_LEN = 32
CMP_STRIDE = 16
CMP_HIDDEN = 128
SEL_BLOCK = 64
SEL_TOPK = 16
WINDOW = 512
NSA_QBLOCK = 32
NSA_IN = MIX_WIDTH + 6 * NSA_KV_WIDTH + 3 * MIX_HEADS + XA_WIDTH

MLA_Q_RANK = 256
MLA_KV_RANK = 128
MLA_NOPE = 64
MLA_ROPE = 32
MLA_V = 64
MLA_THETA = 10000.0
MLA_QBLOCK = 128
MLA_IN = MLA_Q_RANK + MLA_KV_RANK + MLA_ROPE + XA_WIDTH

CONV_CH = MIX_WIDTH
CONV_WIDTH = 31
CONV_IN = 2 * CONV_CH + XA_WIDTH

N_GROUPS = 4
EXPERTS_PER_GROUP = 4
N_EXPERTS = N_GROUPS * EXPERTS_PER_GROUP
EXPERT_FF = 512
TOPK_IN_GROUP = 2

kernel_name = 'hybrid_nsa_mla_conformer_hmoe_trunk'


def layer_norm(x, g, b):
    xf = x.astype(jnp.float32)
    mu = jnp.mean(xf, -1, keepdims=True)
    var = jnp.mean(jnp.square(xf - mu), -1, keepdims=True)
    y = (xf - mu) * lax.rsqrt(var + LN_EPS) * g.astype(jnp.float32) + b.astype(jnp.float32)
    return y.astype(x.dtype)


def rms_norm(x, g):
    xf = x.astype(jnp.float32)
    y = xf * lax.rsqrt(jnp.mean(xf * xf, -1, keepdims=True) + RMS_EPS) * g.astype(jnp.float32)
    return y.astype(x.dtype)


def rope_tables(positions, dim, theta):
    inv = theta ** (-jnp.arange(0, dim, 2, dtype=jnp.float32) / dim)
    ang = positions.astype(jnp.float32)[..., None] * inv
    return jnp.cos(ang), jnp.sin(ang)


def apply_rope(x, cos, sin):
    r = cos.shape[-1]
    x1, x2 = x[..., :r], x[..., r:]
    c, s = cos[:, :, None, :], sin[:, :, None, :]
    return jnp.concatenate([x1 * c - x2 * s, x1 * s + x2 * c], -1).astype(x.dtype)


def partial_rope(x, cos, sin):
    return jnp.concatenate([apply_rope(x[..., :ROT_DIM], cos, sin), x[..., ROT_DIM:]], -1)


def masked_softmax(s, mask):
    s = jnp.where(mask, s.astype(jnp.float32), -jnp.inf)
    m = jnp.max(s, -1, keepdims=True)
    m = jnp.where(jnp.isfinite(m), m, 0.0)
    e = jnp.where(mask, jnp.exp(s - m), 0.0)
    return e / jnp.maximum(jnp.sum(e, -1, keepdims=True), 1e-30)


def nsa_mixer(mix_cols, cmp_pe, cmp_w1, cmp_w2, cos_n, sin_n):
    B, S, _ = mix_cols.shape
    H, K, G, Dh = MIX_HEADS, NSA_KV_HEADS, NSA_GROUP, HEAD_DIM
    offs = np.cumsum([MIX_WIDTH] + [NSA_KV_WIDTH] * 6).tolist()
    q, kc, vc, ks, vs, kw, vw, gate_logits = jnp.split(mix_cols, offs, axis=-1)
    dt = mix_cols.dtype
    q = partial_rope(q.reshape(B, S, H, Dh), cos_n, sin_n)
    ks = partial_rope(ks.reshape(B, S, K, Dh), cos_n, sin_n)
    kw = partial_rope(kw.reshape(B, S, K, Dh), cos_n, sin_n)
    vs, vw = vs.reshape(B, S, K, Dh), vw.reshape(B, S, K, Dh)
    kc, vc = kc.reshape(B, S, K, Dh), vc.reshape(B, S, K, Dh)
    gates = jax.nn.sigmoid(gate_logits.astype(jnp.float32)).astype(dt).reshape(B, S, H, 3)

    n_cmp = (S - CMP_LEN) // CMP_STRIDE + 1
    starts = np.arange(n_cmp) * CMP_STRIDE
    tok_idx = starts[:, None] + np.arange(CMP_LEN)[None, :]
    end_idx = starts + CMP_LEN - 1

    def compress(t, pe, w1, w2):
        blk = t[:, tok_idx] + pe[None, None, :, None, :]
        blk = blk.transpose(0, 1, 3, 2, 4).reshape(B, n_cmp, K, CMP_LEN * Dh)
        return jax.nn.silu(blk @ w1) @ w2

    k_cmp = compress(kc, cmp_pe[0], cmp_w1[0], cmp_w2[0])
    v_cmp = compress(vc, cmp_pe[1], cmp_w1[1], cmp_w2[1])
    k_cmp = partial_rope(k_cmp, cos_n[:, end_idx], sin_n[:, end_idx])

    n_sel = S // SEL_BLOCK
    top_n = min(SEL_TOPK, n_sel)
    sel_start = np.arange(n_sel) * SEL_BLOCK
    overlap = ((starts[:, None] < sel_start[None, :] + SEL_BLOCK)
               & (starts[:, None] + CMP_LEN > sel_start[None, :])).astype(np.float32)
    overlap = jnp.asarray(overlap)
    ksb = ks.reshape(B, n_sel, SEL_BLOCK, K, Dh).transpose(0, 3, 1, 2, 4)
    vsb = vs.reshape(B, n_sel, SEL_BLOCK, K, Dh).transpose(0, 3, 1, 2, 4)
    kwp = jnp.pad(kw, ((0, 0), (WINDOW, 0), (0, 0), (0, 0)))
    vwp = jnp.pad(vw, ((0, 0), (WINDOW, 0), (0, 0), (0, 0)))
    qg = q.reshape(B, S, K, G, Dh)
    scale = 1.0 / math.sqrt(Dh)
    C = NSA_QBLOCK
    bi = jnp.arange(B)[:, None, None, None]
    ki = jnp.arange(K)[None, :, None, None]
    blk_id = jnp.arange(n_sel)

    def block(i):
        t0 = i * C
        qb = lax.dynamic_slice_in_dim(qg, t0, C, axis=1)
        t = t0 + jnp.arange(C)
        s_c = jnp.einsum('bckgd,bnkd->bkgcn', qb, k_cmp) * scale
        p_c = masked_softmax(s_c, end_idx[None, :] <= t[:, None])
        o_c = jnp.einsum('bkgcn,bnkd->bckgd', p_c.astype(dt), v_cmp)
        imp = jnp.einsum('bkgcn,nj->bkcj', p_c, overlap)
        cur = t // SEL_BLOCK
        valid = blk_id[None, :] <= cur[:, None]
        forced = (blk_id[None, :] == 0) | (blk_id[None, :] == cur[:, None]) | (blk_id[None, :] == cur[:, None] - 1)
        imp = jnp.where(valid & forced, jnp.inf, jnp.where(valid, imp, -jnp.inf))
        _, sel = lax.top_k(imp, top_n)
        k_sel = ksb[bi, ki, sel]
        v_sel = vsb[bi, ki, sel]
        s_s = jnp.einsum('bckgd,bkcnld->bkgcnl', qb, k_sel) * scale
        key_pos = sel[..., None] * SEL_BLOCK + jnp.arange(SEL_BLOCK)
        mask_s = (key_pos <= t[None, None, :, None, None]).reshape(B, K, 1, C, top_n * SEL_BLOCK)
        p_s = masked_softmax(s_s.reshape(B, K, G, C, top_n * SEL_BLOCK), mask_s)
        o_s = jnp.einsum('bkgcm,bkcmd->bckgd', p_s.astype(dt),
                         v_sel.reshape(B, K, C, top_n * SEL_BLOCK, Dh))
        kwb = lax.dynamic_slice_in_dim(kwp, t0, WINDOW + C, axis=1)
        vwb = lax.dynamic_slice_in_dim(vwp, t0, WINDOW + C, axis=1)
        s_w = jnp.einsum('bckgd,bjkd->bkgcj', qb, kwb) * scale
        kp = t0 - WINDOW + jnp.arange(WINDOW + C)
        diff = t[:, None] - kp[None, :]
        p_w = masked_softmax(s_w, (diff >= 0) & (diff < WINDOW) & (kp[None, :] >= 0))
        o_w = jnp.einsum('bkgcj,bjkd->bckgd', p_w.astype(dt), vwb)
        gb = lax.dynamic_slice_in_dim(gates, t0, C, axis=1).reshape(B, C, K, G, 3)
        o = gb[..., 0:1] * o_c + gb[..., 1:2] * o_s + gb[..., 2:3] * o_w
        return o.reshape(B, C, H * Dh)

    out = lax.map(block, jnp.arange(S // C))
    return out.transpose(1, 0, 2, 3).reshape(B, S, MIX_WIDTH)


def mla_mixer(mix_cols, q_norm, w_uq, kv_norm, w_ukv, cos_m, sin_m):
    B, S, _ = mix_cols.shape
    H = MIX_HEADS
    dt = mix_cols.dtype
    c_q = mix_cols[..., :MLA_Q_RANK]
    c_kv = mix_cols[..., MLA_Q_RANK:MLA_Q_RANK + MLA_KV_RANK]
    k_rope = mix_cols[..., MLA_Q_RANK + MLA_KV_RANK:]
    q = (rms_norm(c_q, q_norm) @ w_uq).reshape(B, S, H, MLA_NOPE + MLA_ROPE)
    q = jnp.concatenate([q[..., :MLA_NOPE], apply_rope(q[..., MLA_NOPE:], cos_m, sin_m)], -1)
    kv = (rms_norm(c_kv, kv_norm) @ w_ukv).reshape(B, S, H, MLA_NOPE + MLA_V)
    k_nope, v = kv[..., :MLA_NOPE], kv[..., MLA_NOPE:]
    k_rope = apply_rope(k_rope[:, :, None, :], cos_m, sin_m)
    k = jnp.concatenate([k_nope, jnp.broadcast_to(k_rope, (B, S, H, MLA_ROPE))], -1)
    scale = 1.0 / math.sqrt(MLA_NOPE + MLA_ROPE)
    key_idx = jnp.arange(S)

    def block(i):
        t0 = i * MLA_QBLOCK
        qb = lax.dynamic_slice_in_dim(q, t0, MLA_QBLOCK, axis=1)
        s = jnp.einsum('bqhd,bkhd->bhqk', qb, k) * scale
        mask = (t0 + jnp.arange(MLA_QBLOCK))[:, None] >= key_idx[None, :]
        p = masked_softmax(s, mask)
        return jnp.einsum('bhqk,bkhd->bqhd', p.astype(dt), v).reshape(B, MLA_QBLOCK, H * MLA_V)

    out = lax.map(block, jnp.arange(S // MLA_QBLOCK))
    return out.transpose(1, 0, 2, 3).reshape(B, S, H * MLA_V)


def conv_mixer(mix_cols, b_in, dw_w, dw_b, ln_g, ln_b):
    a = mix_cols + b_in
    u = a[..., :CONV_CH] * jax.nn.sigmoid(a[..., CONV_CH:])
    y = lax.conv_general_dilated(u, dw_w[:, None, :], window_strides=(1,),
                                 padding=[(CONV_WIDTH - 1, 0)],
                                 dimension_numbers=('NWC', 'WIO', 'NWC'),
                                 feature_group_count=CONV_CH) + dw_b
    return jax.nn.silu(layer_norm(y, ln_g, ln_b))


def memory_cross_attention(q, mem_k, mem_v):
    s = jnp.einsum('bshd,bmhd->bhsm', q, mem_k) * (1.0 / math.sqrt(HEAD_DIM))
    p = jax.nn.softmax(s.astype(jnp.float32), -1).astype(q.dtype)
    return jnp.einsum('bhsm,bmhd->bshd', p, mem_v)


def hier_moe(x, w_grp, b_grp, w_exp, b_exp, w_gate, w_up, w_down):
    def per_seq(xs):
        S = xs.shape[0]
        pg = jax.nn.softmax((xs @ w_grp + b_grp).astype(jnp.float32), -1)
        g_sel = jnp.argmax(pg, -1)
        g_prob = jnp.max(pg, -1)
        le = (xs @ w_exp + b_exp).astype(jnp.float32).reshape(S, N_GROUPS, EXPERTS_PER_GROUP)
        le = le[jnp.arange(S), g_sel]
        pe = jax.nn.softmax(le, -1)
        top_p, top_i = lax.top_k(pe, TOPK_IN_GROUP)
        top_p = top_p / jnp.sum(top_p, -1, keepdims=True)
        eid = g_sel[:, None] * EXPERTS_PER_GROUP + top_i
        gate = jnp.sum(jax.nn.one_hot(eid, N_EXPERTS, dtype=jnp.float32)
                       * (g_prob[:, None] * top_p)[..., None], axis=1)
        hdn = jax.nn.silu(jnp.einsum('sd,edf->sef', xs, w_gate)) * jnp.einsum('sd,edf->sef', xs, w_up)
        hdn = hdn * gate[..., None].astype(hdn.dtype)
        return jnp.einsum('sef,efd->sd', hdn, w_down)
    return lax.map(per_seq, x)


def setup_inputs(seed: int = 0) -> dict:
    key = jax.random.key(seed)
    ks = iter(jax.random.split(key, 40))
    nA = len(range(0, DEPTH, N_MIXERS))
    nB = len(range(1, DEPTH, N_MIXERS))
    nC = len(range(2, DEPTH, N_MIXERS))
    f32 = jnp.float32

    def nrm(shape, scale):
        return jax.random.normal(next(ks), shape, f32) * scale

    def gain(shape):
        return 1.0 + nrm(shape, 0.02)

    positions = (jax.random.randint(next(ks), (BATCH, 1), 0, 2048, dtype=jnp.int32)
                 + jnp.arange(SEQ, dtype=jnp.int32)[None, :])
    return {
        'x': nrm((BATCH, SEQ, D_MODEL), 1.0),
        'mem': nrm((BATCH, N_MEM, D_MODEL), 1.0),
        'positions': positions,
        'nsa_w_in': nrm((nA, D_MODEL, NSA_IN), D_MODEL ** -0.5),
        'nsa_cmp_pe': nrm((nA, 2, CMP_LEN, HEAD_DIM), 0.1),
        'nsa_cmp_w1': nrm((nA, 2, CMP_LEN * HEAD_DIM, CMP_HIDDEN), (CMP_LEN * HEAD_DIM) ** -0.5),
        'nsa_cmp_w2': nrm((nA, 2, CMP_HIDDEN, HEAD_DIM), CMP_HIDDEN ** -0.5),
        'mla_w_in': nrm((nB, D_MODEL, MLA_IN), D_MODEL ** -0.5),
        'mla_q_norm': gain((nB, MLA_Q_RANK)),
        'mla_w_uq': nrm((nB, MLA_Q_RANK, MIX_HEADS * (MLA_NOPE + MLA_ROPE)), MLA_Q_RANK ** -0.5),
        'mla_kv_norm': gain((nB, MLA_KV_RANK)),
        'mla_w_ukv': nrm((nB, MLA_KV_RANK, MIX_HEADS * (MLA_NOPE + MLA_V)), MLA_KV_RANK ** -0.5),
        'conv_w_in': nrm((nC, D_MODEL, CONV_IN), D_MODEL ** -0.5),
        'conv_b_in': nrm((nC, 2 * CONV_CH), 0.02),
        'conv_dw_w': nrm((nC, CONV_WIDTH, CONV_CH), CONV_WIDTH ** -0.5),
        'conv_dw_b': nrm((nC, CONV_CH), 0.02),
        'conv_ln_g': gain((nC, CONV_CH)),
        'conv_ln_b': nrm((nC, CONV_CH), 0.02),
        'mem_w_kv': nrm((DEPTH, D_MODEL, 2 * XA_WIDTH), D_MODEL ** -0.5),
        'w_out': nrm((DEPTH, OUT_IN, D_MODEL), OUT_IN ** -0.5 * DEEPNORM_BETA),
        'ln_g': gain((DEPTH, 2, D_MODEL)),
        'ln_b': nrm((DEPTH, 2, D_MODEL), 0.02),
        'moe_w_grp': nrm((DEPTH, D_MODEL, N_GROUPS), D_MODEL ** -0.5),
        'moe_b_grp': nrm((DEPTH, N_GROUPS), 0.01),
        'moe_w_exp': nrm((DEPTH, D_MODEL, N_EXPERTS), D_MODEL ** -0.5),
        'moe_b_exp': nrm((DEPTH, N_EXPERTS), 0.01),
        'moe_w_gate': nrm((DEPTH, N_EXPERTS, D_MODEL, EXPERT_FF), D_MODEL ** -0.5),
        'moe_w_up': nrm((DEPTH, N_EXPERTS, D_MODEL, EXPERT_FF), D_MODEL ** -0.5),
        'moe_w_down': nrm((DEPTH, N_EXPERTS, EXPERT_FF, D_MODEL), EXPERT_FF ** -0.5 * DEEPNORM_BETA),
    }


def reference(x, mem, positions, nsa_w_in, nsa_cmp_pe, nsa_cmp_w1, nsa_cmp_w2,
              mla_w_in, mla_q_norm, mla_w_uq, mla_kv_norm, mla_w_ukv,
              conv_w_in, conv_b_in, conv_dw_w, conv_dw_b, conv_ln_g, conv_ln_b,
              mem_w_kv, w_out, ln_g, ln_b,
              moe_w_grp, moe_b_grp, moe_w_exp, moe_b_exp, moe_w_gate, moe_w_up, moe_w_down):
    B, S, _ = x.shape
    cos_n, sin_n = rope_tables(positions, ROT_DIM, ROPE_THETA)
    cos_m, sin_m = rope_tables(positions, MLA_ROPE, MLA_THETA)
    for i in range(DEPTH):
        kind = i % N_MIXERS
        j = i // N_MIXERS
        if kind == 0:
            proj = x @ nsa_w_in[j]
            mix = nsa_mixer(proj[..., :-XA_WIDTH], nsa_cmp_pe[j], nsa_cmp_w1[j], nsa_cmp_w2[j], cos_n, sin_n)
        elif kind == 1:
            proj = x @ mla_w_in[j]
            mix = mla_mixer(proj[..., :-XA_WIDTH], mla_q_norm[j], mla_w_uq[j],
                            mla_kv_norm[j], mla_w_ukv[j], cos_m, sin_m)
        else:
            proj = x @ conv_w_in[j]
            mix = conv_mixer(proj[..., :-XA_WIDTH], conv_b_in[j], conv_dw_w[j], conv_dw_b[j],
                             conv_ln_g[j], conv_ln_b[j])
        xq = proj[..., -XA_WIDTH:].reshape(B, S, XA_HEADS, HEAD_DIM)
        mkv = mem @ mem_w_kv[i]
        mk = mkv[..., :XA_WIDTH].reshape(B, N_MEM, XA_HEADS, HEAD_DIM)
        mv = mkv[..., XA_WIDTH:].reshape(B, N_MEM, XA_HEADS, HEAD_DIM)
        xa = memory_cross_attention(xq, mk, mv).reshape(B, S, XA_WIDTH)
        y = jnp.concatenate([mix, xa], -1) @ w_out[i]
        x = layer_norm(DEEPNORM_ALPHA * x + y, ln_g[i, 0], ln_b[i, 0])
        f = hier_moe(x, moe_w_grp[i], moe_b_grp[i], moe_w_exp[i], moe_b_exp[i],
                     moe_w_gate[i], moe_w_up[i], moe_w_down[i])
        x = layer_norm(DEEPNORM_ALPHA * x + f, ln_g[i, 1], ln_b[i, 1])
    return x
```

```python
import contextlib
import math
import numpy as np
import concourse.bass as bass
import concourse.mybir as mybir
from concourse.bass_utils import run_bass_kernel_spmd

F32 = mybir.dt.float32
BF16 = mybir.dt.bfloat16
I32 = mybir.dt.int32
AF = mybir.ActivationFunctionType
ALU = mybir.AluOpType
AX = mybir.AxisListType

D = 1024
NCORES = 8
N_MEM = 256
ALPHA = (2.0 * 4) ** 0.25
LN_EPS = 1e-5
RMS_EPS = 1e-6
NEG = -30000.0
EPOCH = 16000
ENGS = ("pe", "act", "dve", "pool", "sp")


class Buf:
    __slots__ = ("name", "w", "r", "excl")

    def __init__(self, name="", excl=False):
        self.name = name
        self.w = None
        self.r = {}
        self.excl = excl


class Prog:
    def __init__(self, nc, stack, n_dma_slots=(("sp", 16), ("pool", 16), ("act", 4))):
        self.nc = nc
        self.stack = stack
        self.streams = {e: [] for e in ENGS}
        self.cnt = {e: 0 for e in ENGS}
        self.sems = []
        self.eng_sems = {e: [] for e in ENGS}
        self.waited = {e: {} for e in ENGS}
        self.dma_slots = {}
        for q, n in n_dma_slots:
            self.dma_slots[q] = [[self._new_sem(f"d{q}{i}"), 0] for i in range(n)]
        self.dma_rr = {q: 0 for q, _ in n_dma_slots}
        self.ninstr = 0

    def _new_sem(self, name):
        h = self.stack.enter_context(self.nc.semaphore(name))
        self.sems.append(h)
        return len(self.sems) - 1

    def _eng_sem(self, eng, epoch):
        lst = self.eng_sems[eng]
        while len(lst) <= epoch:
            lst.append(self._new_sem(f"s{eng}{len(lst)}"))
        return lst[epoch]

    def _need(self, eng, deps):
        w = self.waited[eng]
        for si, val in deps.items():
            if w.get(si, 0) >= val:
                continue
            w[si] = val
            h = self.sems[si]
            self.streams[eng].append(lambda e, h=h, val=val: e.wait_ge(h, val))

    def _collect(self, reads, writes, eng=None):
        deps = {}
        own = self.eng_sems.get(eng, ()) if eng else ()
        for b in reads:
            ev = b.w
            if ev is not None and deps.get(ev[0], 0) < ev[1]:
                deps[ev[0]] = ev[1]
            if b.excl:
                for si, v in b.r.items():
                    if si not in own and deps.get(si, 0) < v:
                        deps[si] = v
        for b in writes:
            ev = b.w
            if ev is not None and deps.get(ev[0], 0) < ev[1]:
                deps[ev[0]] = ev[1]
            for si, v in b.r.items():
                if deps.get(si, 0) < v:
                    deps[si] = v
        return deps

    @staticmethod
    def _mark(ev, reads, writes):
        si, v = ev
        for b in writes:
            b.w = ev
            b.r = {}
        for b in reads:
            if b.r.get(si, 0) < v:
                b.r[si] = v

    def op(self, eng, fn, reads=(), writes=()):
        deps = self._collect(reads, writes, eng)
        n = self.cnt[eng]
        epoch, within = divmod(n, EPOCH)
        si = self._eng_sem(eng, epoch)
        if eng == "pe":
            for s in self.eng_sems["pe"]:
                deps.pop(s, None)
        self._need(eng, deps)
        h = self.sems[si]
        self.streams[eng].append(lambda e, fn=fn, h=h: fn(e).then_inc(h, 1))
        self.cnt[eng] = n + 1
        ev = (si, within + 1)
        self._mark(ev, reads, writes)
        self.ninstr += 1
        return ev

    def dma(self, q, out, in_, reads=(), writes=(), **kw):
        deps = self._collect(reads, writes)
        slots = self.dma_slots[q]
        k = self.dma_rr[q]
        self.dma_rr[q] = (k + 1) % len(slots)
        slot = slots[k]
        si = slot[0]
        if slot[1] > 0:
            deps[si] = max(deps.get(si, 0), slot[1])
        self._need(q, deps)
        slot[1] += 16
        val = slot[1]
        h = self.sems[si]
        self.streams[q].append(
            lambda e, out=out, in_=in_, h=h, kw=kw: e.dma_start(out=out, in_=in_, **kw).then_inc(h, 16))
        ev = (si, val)
        self._mark(ev, reads, writes)
        self.ninstr += 1
        return ev

    def barrier(self):
        deps = {}
        for e in ENGS:
            n = self.cnt[e]
            if n == 0:
                continue
            epoch, within = divmod(n - 1, EPOCH)
            deps[self.eng_sems[e][epoch]] = within + 1
            for ep in range(epoch):
                deps[self.eng_sems[e][ep]] = EPOCH
        for q in self.dma_slots:
            for si, v in self.dma_slots[q]:
                if v:
                    deps[si] = v
        for e in ENGS:
            self._need(e, dict(deps))

    def emit(self):
        nc = self.nc
        with nc.Block() as block:
            @block.tensor
            def _(e):
                for f in self.streams["pe"]:
                    f(e)

            @block.scalar
            def _(e):
                for f in self.streams["act"]:
                    f(e)

            @block.vector
            def _(e):
                for f in self.streams["dve"]:
                    f(e)

            @block.gpsimd
            def _(e):
                for f in self.streams["pool"]:
                    f(e)

            @block.sync
            def _(e):
                for f in self.streams["sp"]:
                    f(e)


class Arena:
    def __init__(self, ap, nbf16):
        self.ap = ap
        self.n = nbf16
        self.off = 0

    def reset(self):
        self.off = 0

    def bf(self, parts, *shape):
        self.off = (self.off + 31) // 32 * 32
        n = int(np.prod(shape))
        n2 = (n + 1) // 2 * 2
        assert self.off + n2 <= self.n, ("arena overflow", self.off, n2, self.n)
        v = self.ap[0:parts, self.off:self.off + n]
        self.off += n2
        if len(shape) == 2:
            v = v.rearrange("p (a b) -> p a b", b=shape[1])
        elif len(shape) == 3:
            v = v.rearrange("p (a b c) -> p a b c", b=shape[1], c=shape[2])
        return v

    def f32(self, parts, *shape, dt=F32):
        self.off = (self.off + 31) // 32 * 32
        n = int(np.prod(shape))
        assert self.off + 2 * n <= self.n, ("arena overflow", self.off, 2 * n, self.n)
        v = self.ap[0:parts, self.off:self.off + 2 * n].bitcast(dt)
        self.off += 2 * n
        if len(shape) == 2:
            v = v.rearrange("p (a b) -> p a b", b=shape[1])
        elif len(shape) == 3:
            v = v.rearrange("p (a b c) -> p a b c", b=shape[1], c=shape[2])
        return v


NSA_IN = 2596
MLA_IN = 672
CONV_IN = 1792


class Builder:
    def __init__(self, S, kinds, last_only_out=True):
        self.S = S
        self.kinds = list(kinds)
        self.NT = S // 128
        self.NQ = S // 512
        self.SC = min(2048, S // 2)

    def mm(self, out, lhsT, rhs, start, stop, reads, writes):
        return self.P.op("pe", lambda e: e.matmul(out, lhsT, rhs, start=start, stop=stop), reads, writes)

    def tr(self, out, in_, ident, reads, writes):
        return self.P.op("pe", lambda e: e.transpose(out, in_, ident), reads, writes)

    def act(self, out, in_, func, reads, writes, **kw):
        return self.P.op("act", lambda e: e.activation(out, in_, func, **kw), reads, writes)

    def dbg_dump(self, name, ap, shape, dt, reads):
        import os
        if not os.environ.get("DBG_NSA"):
            return
        self.dbg_names = getattr(self, "dbg_names", [])
        d = self.dram(name, shape, dt, kind="ExternalOutput")
        self.P.dma("sp", d, ap, reads=reads)
        self.dbg_names.append(name)

    def dram(self, name, shape, dt, kind="Internal"):
        return self.nc.dram_tensor(name, list(shape), dt, kind=kind).ap()

    def build(self):
        S = self.S
        nc = self.nc = bass.Bass("TRN2", target_bir_lowering=False)
        I = self.I = {}

        def inp(name, shape, dt=F32):
            I[name] = self.dram(name, shape, dt, kind="ExternalInput")
            return I[name]

        inp("x", [S, D]); inp("mem", [N_MEM, D]); inp("pos", [1, S], I32); inp("pos_end", [1, 256], I32)
        inp("c_ident", [128, 128]); inp("c_rope", [128, 4 * 128 + 4])
        inp("c_ebig", [64, S]); inp("c_ov", [256, 64]); inp("c_fb", [S, 64]); inp("c_sel", [3, 3 * 64])
        for l, kind in enumerate(self.kinds):
            nin = (NSA_IN, MLA_IN, CONV_IN)[kind]
            inp(f"win{l}", [D, nin]); inp(f"wkv{l}", [D, 512]); inp(f"wo{l}", [D, D])
            inp(f"lng{l}", [2, D]); inp(f"lnb{l}", [2, D])
            inp(f"wr{l}", [D, 20]); inp(f"br{l}", [1, 20])
            inp(f"wg{l}", [16, D, 512]); inp(f"wu{l}", [16, D, 512]); inp(f"wd{l}", [16, 512, D])
            if kind == 0:
                inp(f"cpe{l}", [2, 64, 32]); inp(f"cw1{l}", [2, 64, 32, 128]); inp(f"cw2{l}", [2, 128, 64])
            elif kind == 1:
                inp(f"qn{l}", [128, 2]); inp(f"kvn{l}", [128, 1]); inp(f"wuq{l}", [256, 1152]); inp(f"wukv{l}", [128, 1536])
            else:
                inp(f"cbin{l}", [128, 12]); inp(f"cdw{l}", [128, 6 * 31]); inp(f"cdb{l}", [128, 6])
                inp(f"clg{l}", [128, 6]); inp(f"clb{l}", [128, 6])
        self.out = self.dram("out", [S, D], F32, kind="ExternalOutput")
        self.xres = self.dram("xres", [S, D], F32)
        self.pT = self.dram("pT", [2304, S], BF16)
        self.vtok = self.dram("vtok", [S, 768], BF16)
        import os
        self.glT = self.dram("glT", [36, S], F32, kind=("ExternalOutput" if os.environ.get("DBG_GL") else "Internal"))

        with contextlib.ExitStack() as st:
            P = self.P = Prog(nc, st)
            sb = lambda name, shape, dt: st.enter_context(nc.sbuf_tensor(name, shape, dt))
            self.A_t = sb("arenaA", [128, max(8 * S, 186 * 128)], BF16)
            self.B_t = sb("arenaB", [128, 8 * S], BF16)
            ZN = 4608
            self.Z = Arena(sb("arenaZ", [128, ZN], BF16)[:], ZN)
            rem = int(nc.sbuf_bytes_remaining) - 1024
            CN = min(rem // 2, 37000)
            self.C = Arena(sb("arenaC", [128, CN], BF16)[:], CN)
            self.PS = [st.enter_context(nc.psum_tensor(f"ps{i}", [128, 512], F32)) for i in range(8)]
            self.PB = [Buf(f"ps{i}", excl=True) for i in range(8)]
            self.XT = self.A_t[:, 0:8 * S].rearrange("p (k s) -> p k s", s=S)
            self.CT = self.B_t[:].rearrange("p (k s) -> p k s", s=S)
            self.XTb = [Buf(f"xt{q}") for q in range(self.NQ)]
            self.CTb = [[Buf(f"ct{k}_{q}") for q in range(self.NQ)] for k in range(8)]
            self.xres_b = [Buf(f"xr{t}") for t in range(self.NT)]
            self.pT_b = Buf("pT"); self.vtok_b = Buf("vtok"); self.glT_b = Buf("glT")
            self.setup_consts()
            for l, kind in enumerate(self.kinds):
                self.layer(l, kind)
            P.barrier()
            P.emit()
        return nc

    def setup_consts(self):
        P, Z, I = self.P, self.Z, self.I
        self.identF = Z.f32(128, 128); self.identB = Z.bf(128, 128)
        self.onesB = Z.bf(128, 128)
        self.onesF = Z.f32(128, 128)
        self.eps_t = Z.f32(128, 2)
        self.RnB = Z.bf(128, 128); self.RmB = Z.bf(128, 128); self.invF = Z.f32(128, 4)
        self.memT = Z.bf(128, 8, 256)
        self.gates = Z.f32(128, self.NT, 16)
        self.cB = Buf("consts"); self.memT_b = Buf("memT"); self.gates_b = [Buf() for _ in range(self.NT)]
        P.dma("sp", self.identF, I["c_ident"], writes=[self.cB])
        P.dma("pool", self.identB, I["c_ident"], writes=[self.cB])
        P.op("dve", lambda e: e.memset(self.onesB, 1.0), writes=[self.cB])
        P.op("dve", lambda e: e.memset(self.onesF, 1.0), writes=[self.cB])
        P.op("dve", lambda e: e.memset(self.eps_t[:, 0:1], LN_EPS), writes=[self.cB])
        P.op("dve", lambda e: e.memset(self.eps_t[:, 1:2], RMS_EPS), writes=[self.cB])
        P.dma("pool", self.RnB, I["c_rope"][:, 0:128], writes=[self.cB])
        P.dma("pool", self.RmB, I["c_rope"][:, 128:256], writes=[self.cB])
        P.dma("sp", self.invF, I["c_rope"][:, 512:516], writes=[self.cB])
        C = self.C
        C.reset()
        m = C.f32(128, 2, D)
        mb = Buf()
        P.dma("sp", m, I["mem"].rearrange("(t p) d -> p t d", p=128), writes=[mb])
        for t in range(2):
            for half in range(2):
                pi = t * 2 + half
                for k4 in range(4):
                    k = half * 4 + k4
                    self.tr(self.PS[pi][:, k4 * 128:(k4 + 1) * 128], m[:, t, k * 128:(k + 1) * 128], self.identF,
                            [mb, self.cB], [self.PB[pi]])
                src = self.PS[pi][:].rearrange("p (k s) -> p k s", s=128)
                dst = self.memT[:, half * 4:half * 4 + 4, t * 128:(t + 1) * 128]
                P.op("act", lambda e, dst=dst, src=src: e.copy(dst, src), [self.PB[pi]], [self.memT_b])
        P.barrier()

    def layer(self, l, kind):
        first = (l == 0)
        last = (l == len(self.kinds) - 1)
        self.xsrc = self.I["x"] if first else self.xres
        if first:
            self.load_xt_from_x()
        if kind == 2:
            self.mixer_conv(l)
        elif kind == 1:
            self.mixer_mla(l)
        else:
            self.mixer_nsa(l)
        import os
        if os.environ.get("DBG_CT") and l == 0:
            dbg = self.dram("dbg", [128, 8 * self.S], BF16, kind="ExternalOutput")
            self.P.dma("sp", dbg, self.B_t[:, 0:8 * self.S])
            self.P.barrier()
        self.phase_out(l)
        self.phase_moe(l, last)

    def load_xt_from_x(self):
        P, C = self.P, self.C
        C.reset()
        xt = [C.f32(128, D) for _ in range(3)]
        xb = [Buf() for _ in range(3)]
        x = self.I["x"]
        for t in range(self.NT):
            j = t % 3
            P.dma("sp", xt[j], x[t * 128:(t + 1) * 128, :], writes=[xb[j]])
            self.transpose_to_xt(xt[j], xb[j], t, bank0=(t % 2) * 2)
        P.barrier()

    def transpose_to_xt(self, src, srcb, t, bank0, scale=None):
        P = self.P
        q = t // 4
        for half in range(2):
            pi = bank0 + half
            for k4 in range(4):
                k = half * 4 + k4
                self.tr(self.PS[pi][:, k4 * 128:(k4 + 1) * 128], src[:, k * 128:(k + 1) * 128], self.identF,
                        [srcb, self.cB], [self.PB[pi]])
            s_ = self.PS[pi][:].rearrange("p (k s) -> p k s", s=128)
            d_ = self.XT[:, half * 4:half * 4 + 4, t * 128:(t + 1) * 128]
            P.op("act", lambda e, d_=d_, s_=s_: e.copy(d_, s_), [self.PB[pi]], [self.XTb[q]])

    def ln_tile(self, v, vb, gbc, bbc, gbb, scr, j):
        P = self.P
        st_, mv, rs = scr["st"][j], scr["mv"][j], scr["rs"][j][:, 0:1]
        sb_ = scr["b"][j]
        P.op("dve", lambda e: e.bn_stats(st_[:, 0, :], v[:, 0:512]), [vb], [sb_])
        P.op("dve", lambda e: e.bn_stats(st_[:, 1, :], v[:, 512:1024]), [vb], [sb_])
        P.op("dve", lambda e: e.bn_aggr(mv, st_[:].rearrange("p a b -> p (a b)")), [sb_], [sb_])
        P.op("act", lambda e: e.activation(rs, mv[:, 1:2], AF.Sqrt, bias=self.eps_t[:, 0:1], scale=1.0), [sb_, self.cB], [sb_])
        P.op("dve", lambda e: e.reciprocal(rs, rs), [sb_], [sb_])
        P.op("dve", lambda e: e.tensor_scalar(v, v, mv[:, 0:1], rs, ALU.subtract, ALU.mult), [vb, sb_], [vb])
        P.op("pool", lambda e: e.tensor_tensor(v, v, gbc, ALU.mult), [vb, gbb], [vb])
        P.op("pool", lambda e: e.tensor_tensor(v, v, bbc, ALU.add), [vb, gbb], [vb])

    def ln_scratch(self, n=2):
        C = self.C
        return {"st": [C.f32(128, 2, 6) for _ in range(n)], "mv": [C.f32(128, 2) for _ in range(n)],
                "rs": [C.f32(128, 2) for _ in range(n)], "b": [Buf() for _ in range(n)]}

    def phase_out(self, l):
        P, C, I = self.P, self.C, self.I
        C.reset()
        Wo = C.bf(128, 8, D); wob = Buf()
        P.dma("pool", Wo, I[f"wo{l}"].rearrange("(k p) n -> p k n", p=128), writes=[wob])
        gbc = C.f32(128, D); bbc = C.f32(128, D); gbb = Buf()
        P.dma("sp", gbc, I[f"lng{l}"][0:1, :].broadcast_to([128, D]), writes=[gbb])
        P.dma("sp", bbc, I[f"lnb{l}"][0:1, :].broadcast_to([128, D]), writes=[gbb])
        scr = self.ln_scratch()
        NB = 3
        xo = [C.f32(128, D) for _ in range(NB)]
        xb = [Buf() for _ in range(NB)]

        def load(t):
            P.dma("sp", xo[t % NB], self.xsrc[t * 128:(t + 1) * 128, :], reads=[self.xres_b[t]], writes=[xb[t % NB]])
        load(0)
        for t in range(self.NT):
            if t + 1 < self.NT:
                load(t + 1)
            j = t % NB
            q = t // 4
            for half in range(2):
                pi = (t % 2) * 2 + half
                for k in range(8):
                    self.mm(self.PS[pi][:], self.CT[:, k, t * 128:(t + 1) * 128], Wo[:, k, half * 512:(half + 1) * 512],
                            k == 0, k == 7, [self.CTb[k][q], wob], [self.PB[pi]])
                xs = xo[j][:, half * 512:(half + 1) * 512]
                P.op("dve", lambda e, xs=xs, pi=pi: e.scalar_tensor_tensor(xs, xs, ALPHA, self.PS[pi][:], ALU.mult, ALU.add),
                     [xb[j], self.PB[pi]], [xb[j]])
            self.ln_tile(xo[j], xb[j], gbc, bbc, gbb, scr, t % 2)
            P.dma("pool", self.xres[t * 128:(t + 1) * 128, :], xo[j], reads=[xb[j]], writes=[self.xres_b[t]])
            self.transpose_to_xt(xo[j], xb[j], t, bank0=4 + (t % 2) * 2)
        P.barrier()

    def phase_moe(self, l, last):
        P, C, I, S = self.P, self.C, self.I, self.S
        C.reset()
        NT, SC = self.NT, self.SC
        nsc = S // SC
        tps = SC // 128
        cps = SC // 512
        self.router(l)
        C.reset()
        gbc = C.f32(128, D); bbc = C.f32(128, D); gbb = Buf()
        P.dma("sp", gbc, I[f"lng{l}"][1:2, :].broadcast_to([128, D]), writes=[gbb])
        P.dma("sp", bbc, I[f"lnb{l}"][1:2, :].broadcast_to([128, D]), writes=[gbb])
        scr = self.ln_scratch()
        wg = [C.bf(128, 8, 512) for _ in range(2)]; wgb = [Buf() for _ in range(2)]
        wu = [C.bf(128, 8, 512) for _ in range(2)]; wub = [Buf() for _ in range(2)]
        wd = [C.bf(128, 4, D) for _ in range(1)]; wdb = [Buf() for _ in range(1)]
        hd = [C.bf(128, 4, 512) for _ in range(2)]; hdb = [Buf() for _ in range(2)]
        sg = [C.bf(128, 512) for _ in range(2)]; sgb = [Buf() for _ in range(2)]
        xo = [C.f32(128, D) for _ in range(2)]; xb = [Buf() for _ in range(2)]
        facc = self.B_t[:].bitcast(F32).rearrange("p (t d) -> p t d", d=D)
        fab = [Buf() for _ in range(tps)]

        def fa_bufs(tl):
            lo, hi = 2048 * tl, 2048 * tl + 2048
            res = [fab[tl]]
            for k in range(lo // S, (hi - 1) // S + 1):
                s0 = max(lo, k * S) - k * S
                s1 = min(hi, (k + 1) * S) - k * S
                for q in range(s0 // 512, (s1 - 1) // 512 + 1):
                    res.append(self.CTb[k][q])
            return res

        wgv = I[f"wg{l}"]; wuv = I[f"wu{l}"]; wdv = I[f"wd{l}"]

        def load_w(e):
            P.dma("pool", wg[e % 2], wgv[e].rearrange("(k p) n -> p k n", p=128), writes=[wgb[e % 2]])
            P.dma("pool", wu[e % 2], wuv[e].rearrange("(k p) n -> p k n", p=128), writes=[wub[e % 2]])

        def load_wd(e):
            P.dma("pool", wd[0], wdv[e].rearrange("(k p) n -> p k n", p=128), writes=[wdb[0]])

        for sc in range(nsc):
            tok0 = sc * SC
            load_w(0)
            load_wd(0)
            for e in range(16):
                if e + 1 < 16:
                    load_w(e + 1)
                for c in range(cps):
                    q = (tok0 // 512) + c
                    hb = hd[c % 2]
                    for f in range(4):
                        pg, pu = (f % 2) * 2, (f % 2) * 2 + 1
                        for k in range(8):
                            self.mm(self.PS[pg][:], wg[e % 2][:, k, f * 128:(f + 1) * 128], self.XT[:, k, q * 512:(q + 1) * 512],
                                    k == 0, k == 7, [wgb[e % 2], self.XTb[q]], [self.PB[pg]])
                        for k in range(8):
                            self.mm(self.PS[pu][:], wu[e % 2][:, k, f * 128:(f + 1) * 128], self.XT[:, k, q * 512:(q + 1) * 512],
                                    k == 0, k == 7, [wub[e % 2], self.XTb[q]], [self.PB[pu]])
                        s_ = sg[f % 2]
                        self.act(s_, self.PS[pg][:], AF.Silu, [self.PB[pg]], [sgb[f % 2]])
                        P.op("dve", lambda e_, f=f, s_=s_, pu=pu, hb=hb: e_.tensor_tensor(hb[:, f, :], s_, self.PS[pu][:], ALU.mult),
                             [sgb[f % 2], self.PB[pu]], [hdb[c % 2]])
                    for ts in range(4):
                        tl = c * 4 + ts
                        tg = tok0 // 128 + tl
                        for half in range(2):
                            po = 4 + ((ts * 2 + half) % 4)
                            for f in range(4):
                                self.mm(self.PS[po][:], hb[:, f, ts * 128:(ts + 1) * 128], wd[0][:, f, half * 512:(half + 1) * 512],
                                        f == 0, f == 3, [hdb[c % 2], wdb[0]], [self.PB[po]])
                            fa = facc[:, tl, half * 512:(half + 1) * 512]
                            gsc = self.gates[:, tg, e:e + 1]
                            if e == 0:
                                P.op("dve", lambda e_, fa=fa, po=po, gsc=gsc: e_.tensor_scalar(fa, self.PS[po][:], gsc, None, ALU.mult),
                                     [self.PB[po], self.gates_b[tg]], fa_bufs(tl))
                            else:
                                P.op("dve", lambda e_, fa=fa, po=po, gsc=gsc: e_.scalar_tensor_tensor(fa, self.PS[po][:], gsc, fa, ALU.mult, ALU.add),
                                     [self.PB[po], self.gates_b[tg]], fa_bufs(tl))
                if e + 1 < 16:
                    load_wd(e + 1)
            dst = self.out if last else self.xres

            def load(tl):
                tg = tok0 // 128 + tl
                P.dma("sp", xo[tl % 2], self.xres[tg * 128:(tg + 1) * 128, :], reads=[self.xres_b[tg]], writes=[xb[tl % 2]])
            load(0)
            for tl in range(tps):
                if tl + 1 < tps:
                    load(tl + 1)
                tg = tok0 // 128 + tl
                j = tl % 2
                P.op("dve", lambda e_, j=j, tl=tl: e_.scalar_tensor_tensor(xo[j], xo[j], ALPHA, facc[:, tl, :], ALU.mult, ALU.add),
                     [xb[j]] + fa_bufs(tl), [xb[j]])
                self.ln_tile(xo[j], xb[j], gbc, bbc, gbb, scr, j)
                P.dma("pool", dst[tg * 128:(tg + 1) * 128, :], xo[j], reads=[xb[j]], writes=[self.xres_b[tg]])
                if not last:
                    self.transpose_to_xt(xo[j], xb[j], tg, bank0=(tl % 2) * 2)
        P.barrier()

    def router(self, l):
        P, C, I = self.P, self.C, self.I
        NT = self.NT
        C.reset()
        Wr = C.bf(128, 8, 20); wrb = Buf()
        P.dma("pool", Wr, I[f"wr{l}"].rearrange("(k p) n -> p k n", p=128), writes=[wrb])
        brc = C.f32(128, 20)
        P.dma("sp", brc, I[f"br{l}"][0:1, :].broadcast_to([128, 20]), writes=[wrb])
        lg = C.f32(128, NT, 20); rb = Buf()
        for t in range(NT):
            q = t // 4
            pi = t % 4
            lgp = self.PS[pi][:, 0:20]
            for k in range(8):
                self.mm(lgp, self.XT[:, k, t * 128:(t + 1) * 128], Wr[:, k, :], k == 0, k == 7,
                        [self.XTb[q], wrb], [self.PB[pi]])
            P.op("dve", lambda e, t=t, lgp=lgp: e.tensor_tensor(lg[:, t, :], lgp, brc, ALU.add), [self.PB[pi], wrb], [rb])
        lgg = lg[:, :, 0:4]
        le = lg[:, :, 4:20].rearrange("p t (g j) -> p t g j", j=4)
        m = C.f32(128, NT); oh = C.f32(128, NT, 4); eg = C.f32(128, NT, 4); gs = C.f32(128, NT)
        m1 = C.f32(128, NT, 4); is1 = C.f32(128, NT, 4, 4); le2 = C.f32(128, NT, 4, 4); m2 = C.f32(128, NT, 4)
        sel2 = C.f32(128, NT, 4, 4); ee = C.f32(128, NT, 4, 4); ss = C.f32(128, NT, 4); w = C.f32(128, NT, 4)
        bc3 = lambda a: a.unsqueeze(2).broadcast_to([128, NT, 4])
        bc4 = lambda a: a.unsqueeze(3).broadcast_to([128, NT, 4, 4])
        R = [rb]
        dv = lambda fn: P.op("dve", fn, R, R)
        dv(lambda e: e.tensor_reduce(m, lgg, AX.X, ALU.max))
        dv(lambda e: e.tensor_tensor(oh, lgg, bc3(m), ALU.is_equal))
        dv(lambda e: e.tensor_tensor(eg, lgg, bc3(m), ALU.subtract))
        P.op("act", lambda e: e.activation(eg, eg, AF.Exp), R, R)
        dv(lambda e: e.tensor_reduce(gs, eg, AX.X, ALU.add))
        dv(lambda e: e.reciprocal(gs, gs))
        dv(lambda e: e.tensor_reduce(m1, le, AX.X, ALU.max))
        dv(lambda e: e.tensor_tensor(is1, le, bc4(m1), ALU.is_equal))
        dv(lambda e: e.scalar_tensor_tensor(le2, is1, -1.0e9, le, ALU.mult, ALU.add))
        dv(lambda e: e.tensor_reduce(m2, le2, AX.X, ALU.max))
        dv(lambda e: e.tensor_tensor(sel2, le, bc4(m2), ALU.is_ge))
        dv(lambda e: e.tensor_tensor(ee, le, bc4(m1), ALU.subtract))
        P.op("act", lambda e: e.activation(ee, ee, AF.Exp), R, R)
        dv(lambda e: e.tensor_tensor(ee, ee, sel2, ALU.mult))
        dv(lambda e: e.tensor_reduce(ss, ee, AX.X, ALU.add))
        dv(lambda e: e.reciprocal(ss, ss))
        dv(lambda e: e.tensor_tensor(w, ss, oh, ALU.mult))
        dv(lambda e: e.tensor_tensor(w, w, bc3(gs), ALU.mult))
        gv = self.gates[:].rearrange("p t (g j) -> p t g j", j=4)
        P.op("dve", lambda e: e.tensor_tensor(gv, ee, bc4(w), ALU.mult), R, R + self.gates_b)
        P.barrier()

    def proj_chunk(self, pi, M, Wb, wbuf, col0, tq):
        for k in range(8):
            self.mm(self.PS[pi][0:M, :], Wb[:, k, col0:col0 + M], self.XT[:, k, tq * 512:(tq + 1) * 512],
                    k == 0, k == 7, [wbuf, self.XTb[tq]], [self.PB[pi]])

    def proj_xq(self, win, col0):
        P, C = self.P, self.C
        C.reset()
        stg = [C.bf(128, 512) for _ in range(2)]; stgb = [Buf() for _ in range(2)]
        Wx = C.bf(128, 8, 256); Wxb = Buf()
        P.dma("pool", Wx, win[:, :, col0:col0 + 256], writes=[Wxb])
        ns = 0
        for hh in range(2):
            for tq in range(self.NQ):
                pi = ns % 2
                self.proj_chunk(pi, 128, Wx, Wxb, hh * 128, tq)
                sg_ = stg[ns % 2]; sb_ = stgb[ns % 2]; ns += 1
                P.op("act", lambda e, sg_=sg_, pi=pi: e.copy(sg_, self.PS[pi][:]), [self.PB[pi]], [sb_])
                P.dma("sp", self.pT[2048 + hh * 128:2048 + (hh + 1) * 128, tq * 512:(tq + 1) * 512], sg_, reads=[sb_], writes=[self.pT_b])
        P.barrier()
        C.reset()

    def rope_tables(self, tq, npart, inv, posb_t, posf, kf, ki, Ct, St, tb, pos_src=None, width=512):
        P = self.P
        src = pos_src if pos_src is not None else self.I["pos"][0:1, tq * 512:(tq + 1) * 512]
        P.dma("sp", posb_t, src.broadcast_to([npart, width]), writes=[tb])
        P.op("dve", lambda e: e.tensor_copy(posf, posb_t), [tb], [tb])
        P.op("dve", lambda e: e.tensor_scalar(posf, posf, inv, None, ALU.mult), [tb, self.cB], [tb])
        TWO_PI = 2.0 * math.pi
        C1 = 6.28125
        C2 = TWO_PI - C1
        for out_t, shift in ((St, 0.0), (Ct, math.pi / 2)):
            P.op("dve", lambda e, shift=shift: e.tensor_scalar(kf, posf, shift, 1.0 / TWO_PI, ALU.add, ALU.mult), [tb], [tb])
            P.op("dve", lambda e: e.tensor_copy(ki, kf), [tb], [tb])
            P.op("dve", lambda e: e.tensor_copy(kf, ki), [tb], [tb])
            P.op("dve", lambda e, out_t=out_t, shift=shift: e.tensor_scalar(out_t, posf, shift, None, ALU.add), [tb], [tb])
            P.op("dve", lambda e, out_t=out_t: e.scalar_tensor_tensor(out_t, kf, -C1, out_t, ALU.mult, ALU.add), [tb], [tb])
            P.op("dve", lambda e, out_t=out_t: e.scalar_tensor_tensor(out_t, kf, -C2, out_t, ALU.mult, ALU.add), [tb], [tb])
            P.op("dve", lambda e, out_t=out_t: e.tensor_scalar(kf, out_t, math.pi, -TWO_PI, ALU.is_gt, ALU.mult), [tb], [tb])
            P.op("dve", lambda e, out_t=out_t: e.tensor_tensor(out_t, out_t, kf, ALU.add), [tb], [tb])
            P.op("dve", lambda e, out_t=out_t: e.tensor_scalar(kf, out_t, -math.pi, TWO_PI, ALU.is_lt, ALU.mult), [tb], [tb])
            P.op("dve", lambda e, out_t=out_t: e.tensor_tensor(out_t, out_t, kf, ALU.add), [tb], [tb])
            P.op("dve", lambda e, out_t=out_t: e.tensor_scalar(out_t, out_t, 3.1415925, -3.1415925, ALU.min, ALU.max), [tb], [tb])
            P.op("act", lambda e, out_t=out_t: e.activation(out_t, out_t, AF.Sin), [tb], [tb])

    def xattn(self, l):
        P, C, I, S = self.P, self.C, self.I, self.S
        C.reset()
        Wkv = C.bf(128, 8, 512); wb = Buf()
        P.dma("pool", Wkv, I[f"wkv{l}"].rearrange("(k p) n -> p k n", p=128), writes=[wb])
        mkT = C.bf(64, 4, 256); mkb = Buf()
        mv = C.bf(128, 2, 4, 128); mvb = Buf()
        xq = [C.bf(64, S) for _ in range(2)]; xqb = [Buf() for _ in range(2)]
        E = [C.bf(128, 512) for _ in range(3)]; Eb = [Buf() for _ in range(3)]
        rd = [C.f32(64, 512) for _ in range(2)]; rdb = [Buf() for _ in range(2)]
        P.op("pool", lambda e: e.memset(mv, 1.0), [], [mvb])
        for h in range(4):
            pi = h % 2
            for k in range(8):
                self.mm(self.PS[pi][0:64, 0:256], Wkv[:, k, h * 64:(h + 1) * 64], self.memT[:, k, :], k == 0, k == 7,
                        [wb, self.memT_b], [self.PB[pi]])
            P.op("act", lambda e, h=h, pi=pi: e.copy(mkT[:, h, :], self.PS[pi][0:64, 0:256]), [self.PB[pi]], [mkb])
        for mt in range(2):
            pi = 2 + mt
            for k in range(8):
                self.mm(self.PS[pi][:, 0:256], self.memT[:, k, mt * 128:(mt + 1) * 128], Wkv[:, k, 256:512], k == 0, k == 7,
                        [wb, self.memT_b], [self.PB[pi]])
            src = self.PS[pi][:, 0:256].rearrange("p (h d) -> p h d", d=64)
            P.op("act", lambda e, mt=mt, src=src: e.copy(mv[:, mt, :, 0:64], src), [self.PB[pi]], [mvb])
        ne = 0
        for h in range(4):
            xh = xq[h % 2]; xhb = xqb[h % 2]
            P.dma("sp", xh, self.pT[2048 + 64 * h:2048 + 64 * (h + 1), :], reads=[self.pT_b], writes=[xhb])
            for tq in range(self.NQ):
                po = 4 + (tq % 2)
                for mt in range(2):
                    ps = mt + 2 * (tq % 2)
                    self.mm(self.PS[ps][:], mkT[:, h, mt * 128:(mt + 1) * 128], xh[:, tq * 512:(tq + 1) * 512], True, True,
                            [mkb, xhb], [self.PB[ps]])
                    Ei = E[ne % 3]; Ebi = Eb[ne % 3]; ne += 1
                    self.act(Ei, self.PS[ps][:], AF.Exp, [self.PB[ps]], [Ebi], scale=0.125)
                    self.mm(self.PS[po][:], mv[:, mt, h, :], Ei, mt == 0, mt == 1, [mvb, Ebi], [self.PB[po]])
                r = rd[tq % 2]; rb = rdb[tq % 2]
                P.op("dve", lambda e, r=r, po=po: e.reciprocal(r, self.PS[po][64:128, :]), [self.PB[po]], [rb])
                dst = self.CT[64 * (h % 2):64 * (h % 2) + 64, 6 + h // 2, tq * 512:(tq + 1) * 512]
                P.op("dve", lambda e, r=r, po=po, dst=dst: e.tensor_tensor(dst, self.PS[po][0:64, :], r, ALU.mult),
                     [self.PB[po], rb], [self.CTb[6 + h // 2][tq]])
        P.barrier()

    def mixer_conv(self, l):
        P, C, I, S = self.P, self.C, self.I, self.S
        NQ = self.NQ
        win = I[f"win{l}"].rearrange("(k p) n -> p k n", p=128)
        self.proj_xq(win, 1536)
        UW = 30 + S
        U = C.bf(128, 6, UW); Ub = [Buf() for _ in range(6)]
        cb = C.f32(128, 12); cdw = C.f32(128, 186); cdb = C.f32(128, 6); clg = C.f32(128, 6); clb = C.f32(128, 6)
        pb = Buf()
        for t_, n_ in ((cb, "cbin"), (cdw, "cdw"), (cdb, "cdb"), (clg, "clg"), (clb, "clb")):
            P.dma("sp", t_, I[f"{n_}{l}"], writes=[pb])
        mark = C.off
        Wc = [C.bf(128, 8, 256) for _ in range(2)]; Wcb = [Buf() for _ in range(2)]
        sig = [C.f32(128, 512) for _ in range(2)]; sigb = [Buf() for _ in range(2)]
        for c in range(6):
            P.op("pool", lambda e, c=c: e.memset(U[:, c, 0:30], 0.0), [], [Ub[c]])
        for c in range(6):
            W_ = Wc[c % 2]; Wb_ = Wcb[c % 2]
            P.dma("pool", W_[:, :, 0:128], win[:, :, c * 128:(c + 1) * 128], writes=[Wb_])
            P.dma("pool", W_[:, :, 128:256], win[:, :, 768 + c * 128:768 + (c + 1) * 128], writes=[Wb_])
            for tq in range(NQ):
                p1, p2 = 2 + (tq % 2) * 2, 3 + (tq % 2) * 2
                self.proj_chunk(p1, 128, W_, Wb_, 0, tq)
                self.proj_chunk(p2, 128, W_, Wb_, 128, tq)
                sg_ = sig[tq % 2]; sb_ = sigb[tq % 2]
                self.act(sg_, self.PS[p2][:], AF.Sigmoid, [self.PB[p2], pb], [sb_], bias=cb[:, 6 + c:7 + c], scale=1.0)
                dst = U[:, c, 30 + tq * 512:30 + (tq + 1) * 512]
                P.op("dve", lambda e, dst=dst, p1=p1, c=c, sg_=sg_: e.scalar_tensor_tensor(dst, self.PS[p1][:], cb[:, c:c + 1], sg_, ALU.add, ALU.mult),
                     [self.PB[p1], sb_, pb], [Ub[c]])
        P.barrier()
        C.off = mark
        diag = self.A_t[:, 0:186 * 128].rearrange("p (j m) -> p j m", m=128)
        dgb = Buf()
        for idx in range(186):
            eng = "dve" if idx % 2 == 0 else "pool"
            P.op(eng, lambda e, idx=idx: e.tensor_scalar(diag[:, idx, :], self.identB, cdw[:, idx:idx + 1], None, ALU.mult),
                 [self.cB, pb] , [dgb] + self.XTb)
        ysb = [C.f32(128, 512) for _ in range(2)]; ysbb = [Buf() for _ in range(2)]
        ysq = [C.f32(128, 512) for _ in range(2)]; ysqb = [Buf() for _ in range(2)]
        mean = C.f32(128, 512); rstd = C.f32(128, 512); msq = C.f32(128, 512); stb = Buf()
        tmp = [C.f32(128, 512) for _ in range(2)]; tmpb = [Buf() for _ in range(2)]
        n2 = 0
        for tq in range(NQ):
            for c in range(6):
                for j in range(31):
                    self.mm(self.PS[c][:], diag[:, c * 31 + j, :], U[:, c, tq * 512 + j:tq * 512 + j + 512], j == 0, j == 30,
                            [dgb, Ub[c]], [self.PB[c]])
                y_ = ysb[n2 % 2]; yb_ = ysbb[n2 % 2]; q_ = ysq[n2 % 2]; qb_ = ysqb[n2 % 2]; n2 += 1
                self.act(y_, self.PS[c][:], AF.Identity, [self.PB[c], pb], [yb_], bias=cdb[:, c:c + 1], scale=1.0)
                P.op("pool", lambda e, y_=y_, q_=q_: e.tensor_tensor(q_, y_, y_, ALU.mult), [yb_], [qb_])
                self.mm(self.PS[6][:], self.onesF, y_, c == 0, c == 5, [yb_, self.cB], [self.PB[6]])
                self.mm(self.PS[7][:], self.onesF, q_, c == 0, c == 5, [qb_, self.cB], [self.PB[7]])
            P.op("dve", lambda e: e.tensor_scalar(mean, self.PS[6][:], 1.0 / 768, None, ALU.mult), [self.PB[6]], [stb])
            P.op("dve", lambda e: e.tensor_tensor(msq, mean, mean, ALU.mult), [stb], [stb])
            P.op("dve", lambda e: e.scalar_tensor_tensor(rstd, self.PS[7][:], 1.0 / 768, msq, ALU.mult, ALU.subtract), [self.PB[7], stb], [stb])
            P.op("act", lambda e: e.activation(rstd, rstd, AF.Sqrt, bias=self.eps_t[:, 0:1], scale=1.0), [stb, self.cB], [stb])
            P.op("dve", lambda e: e.reciprocal(rstd, rstd), [stb], [stb])
            for c in range(6):
                t_ = tmp[c % 2]; tb_ = tmpb[c % 2]
                P.op("dve", lambda e, t_=t_, c=c: e.scalar_tensor_tensor(t_, self.PS[c][:], cdb[:, c:c + 1], mean, ALU.add, ALU.subtract),
                     [self.PB[c], stb, pb], [tb_])
                P.op("dve", lambda e, t_=t_: e.tensor_tensor(t_, t_, rstd, ALU.mult), [tb_, stb], [tb_])
                dst = self.CT[:, c, tq * 512:(tq + 1) * 512]
                self.act(dst, t_, AF.Silu, [tb_, pb], [self.CTb[c][tq]], bias=clb[:, c:c + 1], scale=clg[:, c:c + 1])
        P.barrier()
        self.xattn(l)


def _consts(S):
    c = {}
    c["c_ident"] = np.eye(128, dtype=np.float32)
    cr = np.zeros((128, 4 * 128 + 4), np.float32)
    inv_n = (np.float32(500000.0) ** (-np.arange(0, 16, 2, dtype=np.float32) / np.float32(16))).astype(np.float32)
    inv_m = (np.float32(10000.0) ** (-np.arange(0, 32, 2, dtype=np.float32) / np.float32(32))).astype(np.float32)
    for base in (0, 64):
        for i in range(8):
            cr[base + i + 8, base + i] = -1.0
            cr[base + i, base + i + 8] = 1.0
            cr[base + i, 512] = inv_n[i]
            cr[base + i + 8, 512] = inv_n[i]
    for i in range(64, 80):
        cr[i + 16, 128 + i] = -1.0
        cr[i, 128 + i + 16] = 1.0
        cr[i, 513] = inv_m[i - 64]
        cr[i + 16, 513] = inv_m[i - 64]
    c["c_rope"] = cr
    eb = np.zeros((64, S), np.float32)
    for key in range(S):
        eb[(key // 64) % 64, key] = 1.0
    c["c_ebig"] = eb
    ov = np.zeros((256, 64), np.float32)
    ncmp = (S - 32) // 16 + 1
    for n in range(ncmp):
        for j in range(S // 64):
            if 16 * n < 64 * j + 64 and 16 * n + 32 > 64 * j:
                ov[n, j] = 1.0
    c["c_ov"] = ov
    fb = np.zeros((S, 64), np.float32)
    for t in range(S):
        cur = t // 64
        fb[t, cur + 1:] = -1.0e9
        fb[t, cur] = 1.0e9
        if cur >= 1:
            fb[t, cur - 1] = 2.0e9
        fb[t, 0] = 3.0e9
    c["c_fb"] = fb
    sel = np.zeros((3, 3 * 64), np.float32)
    for r in range(3):
        sel[r, r * 64:(r + 1) * 64] = 1.0
    c["c_sel"] = sel
    return c


def layer_inputs(l, kind, j, w):
    f = lambda a: np.ascontiguousarray(a, dtype=np.float32)
    d = {}
    d[f"wkv{l}"] = f(w["mem_w_kv"][l]); d[f"wo{l}"] = f(w["w_out"][l])
    d[f"lng{l}"] = f(w["ln_g"][l]); d[f"lnb{l}"] = f(w["ln_b"][l])
    d[f"wr{l}"] = f(np.concatenate([w["moe_w_grp"][l], w["moe_w_exp"][l]], axis=1))
    d[f"br{l}"] = f(np.concatenate([w["moe_b_grp"][l], w["moe_b_exp"][l]])[None, :])
    d[f"wg{l}"] = f(w["moe_w_gate"][l]); d[f"wu{l}"] = f(w["moe_w_up"][l]); d[f"wd{l}"] = f(w["moe_w_down"][l])
    if kind == 2:
        d[f"win{l}"] = f(w["conv_w_in"][j])
        d[f"cbin{l}"] = f(w["conv_b_in"][j].reshape(12, 128).T)
        d[f"cdw{l}"] = f(w["conv_dw_w"][j].T.reshape(6, 128, 31).transpose(1, 0, 2).reshape(128, 186))
        d[f"cdb{l}"] = f(w["conv_dw_b"][j].reshape(6, 128).T)
        d[f"clg{l}"] = f(w["conv_ln_g"][j].reshape(6, 128).T)
        d[f"clb{l}"] = f(w["conv_ln_b"][j].reshape(6, 128).T)
    elif kind == 1:
        d[f"win{l}"] = f(w["mla_w_in"][j])
        d[f"qn{l}"] = f(w["mla_q_norm"][j].reshape(2, 128).T); d[f"kvn{l}"] = f(w["mla_kv_norm"][j][:, None])
        d[f"wuq{l}"] = f(w["mla_w_uq"][j]); d[f"wukv{l}"] = f(w["mla_w_ukv"][j])
    else:
        d[f"win{l}"] = f(w["nsa_w_in"][j])
        d[f"cpe{l}"] = f(np.transpose(w["nsa_cmp_pe"][j], (0, 2, 1)))
        d[f"cw1{l}"] = f(w["nsa_cmp_w1"][j].reshape(2, 32, 64, 128).transpose(0, 2, 1, 3))
        d[f"cw2{l}"] = f(w["nsa_cmp_w2"][j])
    return d


_NC_CACHE = {}


def run_model(S, kinds, js, x, mem, positions, w, n_cores=NCORES):
    key = (S, tuple(kinds))
    if key not in _NC_CACHE:
        _NC_CACHE[key] = Builder(S, kinds).build()
    nc = _NC_CACHE[key]
    shared = _consts(S)
    for l, (kind, j) in enumerate(zip(kinds, js)):
        shared.update(layer_inputs(l, kind, j, w))
    in_maps = []
    for b in range(n_cores):
        m = dict(shared)
        m["x"] = np.ascontiguousarray(x[b], dtype=np.float32)
        m["mem"] = np.ascontiguousarray(mem[b], dtype=np.float32)
        m["pos"] = np.ascontiguousarray(positions[b][None, :], dtype=np.int32)
        pe = np.asarray(positions[b])[31::16]
        pe = np.concatenate([pe, np.repeat(pe[-1:], 256 - len(pe))])[:256]
        m["pos_end"] = np.ascontiguousarray(pe[None, :], dtype=np.int32)
        in_maps.append(m)
    res = run_bass_kernel_spmd(nc, in_maps, core_ids=list(range(n_cores)))
    import os
    if os.environ.get("DBG_CT"):
        np.save("dbg_ct.npy", np.asarray(res.results[0]["dbg"]).astype(np.float32))
    if os.environ.get("DBG_GL"):
        a = np.asarray(res.results[0]["glT"]); print("glT", a.dtype, a.shape); np.save("d_glT.npy", a)
    if os.environ.get("DBG_NSA"):
        for nme in res.results[0]:
            if nme.startswith("d_"):
                a = np.asarray(res.results[0][nme])
                print(nme, a.dtype, a.shape)
                np.save(nme + ".npy", a.astype(np.float32))
    return np.stack([np.asarray(r["out"]) for r in res.results], axis=0)


def kernel(**inputs):
    x = np.asarray(inputs["x"]); mem = np.asarray(inputs["mem"]); positions = np.asarray(inputs["positions"])
    w = {k: np.asarray(v) for k, v in inputs.items() if k not in ("x", "mem", "positions")}
    kinds = [0, 1, 2, 0]
    js = [0, 0, 0, 1]
    out = run_model(x.shape[1], kinds, js, x, mem, positions, w)
    return out.astype(np.float32)


def _mixer_mla(self, l):
    P, C, I, S = self.P, self.C, self.I, self.S
    NT, NQ = self.NT, self.NQ
    win = I[f"win{l}"].rearrange("(k p) n -> p k n", p=128)
    self.proj_xq(win, 416)
    stg = [C.bf(128, 512) for _ in range(2)]; stgb = [Buf() for _ in range(2)]
    qn = C.f32(128, 2); kvn = C.f32(128, 2); nb = Buf()
    P.dma("sp", qn, I[f"qn{l}"], writes=[nb]); P.dma("sp", kvn[:, 0:1], I[f"kvn{l}"], writes=[nb])
    CQ = C.bf(128, 2, S); CKV = C.bf(128, S); KRr = C.bf(96, S)
    cqb = [Buf() for _ in range(NQ)]; krb = Buf()
    P.op("pool", lambda e: e.memset(KRr[0:64, :], 0.0), [], [krb])
    mark = C.off
    Win = C.bf(128, 8, 416); winb = Buf()
    P.dma("pool", Win, win[:, :, 0:416], writes=[winb])
    tk = [C.f32(128, 416) for _ in range(2)]; tkb = [Buf() for _ in range(2)]
    junk = C.f32(128, 256); ssq = [C.f32(128, 4) for _ in range(2)]
    for t in range(NT):
        pi = t % 2
        j = t % 2
        for k in range(8):
            self.mm(self.PS[pi][:, 0:416], self.XT[:, k, t * 128:(t + 1) * 128], Win[:, k, :], k == 0, k == 7,
                    [winb, self.XTb[t // 4]], [self.PB[pi]])
        sq = ssq[j]
        self.act(junk, self.PS[pi][:, 0:256], AF.Square, [self.PB[pi]], [tkb[j]], accum_out=sq[:, 0:1])
        self.act(junk[:, 0:128], self.PS[pi][:, 256:384], AF.Square, [self.PB[pi]], [tkb[j]], accum_out=sq[:, 1:2])
        self.act(sq[:, 0:1], sq[:, 0:1], AF.Sqrt, [tkb[j], self.cB], [tkb[j]], bias=self.eps_t[:, 1:2], scale=1.0 / 256)
        self.act(sq[:, 1:2], sq[:, 1:2], AF.Sqrt, [tkb[j], self.cB], [tkb[j]], bias=self.eps_t[:, 1:2], scale=1.0 / 128)
        P.op("dve", lambda e, sq=sq: e.reciprocal(sq[:, 0:2], sq[:, 0:2]), [tkb[j]], [tkb[j]])
        P.op("dve", lambda e, j=j, pi=pi, sq=sq: e.tensor_scalar(tk[j][:, 0:256], self.PS[pi][:, 0:256], sq[:, 0:1], None, ALU.mult), [self.PB[pi], tkb[j]], [tkb[j]])
        P.op("dve", lambda e, j=j, pi=pi, sq=sq: e.tensor_scalar(tk[j][:, 256:384], self.PS[pi][:, 256:384], sq[:, 1:2], None, ALU.mult), [self.PB[pi], tkb[j]], [tkb[j]])
        P.op("dve", lambda e, j=j, pi=pi: e.tensor_copy(tk[j][:, 384:416], self.PS[pi][:, 384:416]), [self.PB[pi], tkb[j]], [tkb[j]])
        pt = 2 + (t % 2)
        for bi, (c0, c1) in enumerate(((0, 128), (128, 256), (256, 384), (320, 416))):
            self.tr(self.PS[pt][0:c1 - c0, bi * 128:(bi + 1) * 128], tk[j][:, c0:c1], self.identF, [tkb[j], self.cB], [self.PB[pt]])
        tsl = slice(t * 128, (t + 1) * 128)
        q = t // 4
        self.act(CQ[:, 0, tsl], self.PS[pt][:, 0:128], AF.Copy, [self.PB[pt], nb], [cqb[q]], scale=qn[:, 0:1])
        self.act(CQ[:, 1, tsl], self.PS[pt][:, 128:256], AF.Copy, [self.PB[pt], nb], [cqb[q]], scale=qn[:, 1:2])
        self.act(CKV[:, tsl], self.PS[pt][:, 256:384], AF.Copy, [self.PB[pt], nb], [cqb[q]], scale=kvn[:, 0:1])
        P.op("dve", lambda e, pt=pt, tsl=tsl: e.tensor_copy(KRr[64:96, tsl], self.PS[pt][64:96, 384:512]), [self.PB[pt]], [cqb[q], krb])
    P.barrier()
    import os
    STOP = int(os.environ.get("MLA_STOP", "9"))
    if STOP == 1:
        self.xattn(l); return
    C.off = mark
    Wuq = C.bf(128, 2, 1152); Wukv = C.bf(128, 1536); ub = Buf()
    P.dma("pool", Wuq, I[f"wuq{l}"].rearrange("(k p) n -> p k n", p=128), writes=[ub])
    P.dma("pool", Wukv, I[f"wukv{l}"], writes=[ub])
    posb_t = C.f32(96, 512, dt=I32); posf = C.f32(96, 512); kf = C.f32(96, 512); ki = posb_t
    Ct = C.f32(96, 512); St = C.f32(96, 512); tb = Buf()
    qraw = [C.bf(96, 512) for _ in range(2)]; qrb = [Buf() for _ in range(2)]
    t1 = [C.f32(96, 512) for _ in range(2)]; t2 = [C.f32(96, 512) for _ in range(2)]; t12b = [Buf() for _ in range(2)]
    qo = [C.bf(96, 512) for _ in range(2)]; qob = [Buf() for _ in range(2)]
    vst = [C.bf(128, 768) for _ in range(2)]; vstb = [Buf() for _ in range(2)]
    Rm = self.RmB[0:96, 0:96]
    n = 0
    for tq in range(NQ):
        csl = slice(tq * 512, (tq + 1) * 512)
        SK = os.environ.get("MLA_SKIP", "")
        if "r" not in SK:
            self.rope_tables(tq, 96, self.invF[0:96, 1:2], posb_t, posf, kf, ki, Ct, St, tb)
        j = n % 2; n += 1
        if "k" not in SK:
            self.mm(self.PS[0][0:96, :], Rm, KRr[:, csl], True, True, [self.cB, cqb[tq], krb], [self.PB[0]])
            P.op("dve", lambda e, j=j, csl=csl: e.tensor_tensor(t1[j], KRr[:, csl], Ct, ALU.mult), [cqb[tq], krb, tb], [t12b[j]])
            P.op("dve", lambda e, j=j: e.tensor_tensor(t2[j], self.PS[0][0:96, :], St, ALU.mult), [self.PB[0], tb], [t12b[j]])
            P.op("pool", lambda e, j=j: e.tensor_tensor(qo[j], t1[j], t2[j], ALU.add), [t12b[j]], [qob[j]])
            P.dma("sp", self.pT[1920:1952, csl], qo[j][64:96, :], reads=[qob[j]], writes=[self.pT_b])
        for h in range(int(os.environ.get("MLA_NH", "12")) if "q" not in SK else 0):
            pq, pr = 1 + (h % 2) * 2, 2 + (h % 2) * 2
            for rc in range(2):
                self.mm(self.PS[pq][0:96, :], Wuq[:, rc, h * 96:(h + 1) * 96], CQ[:, rc, csl], rc == 0, rc == 1, [ub, cqb[tq]], [self.PB[pq]])
            j = n % 2; n += 1
            if "a" in SK:
                continue
            self.act(qraw[j], self.PS[pq][0:96, :], AF.Copy, [self.PB[pq]], [qrb[j]])
            if "b" in SK:
                continue
            self.mm(self.PS[pr][0:96, :], Rm, qraw[j], True, True, [self.cB, qrb[j]], [self.PB[pr]])
            if "c" in SK:
                continue
            VAR = os.environ.get("MLA_VAR", "0")
            if VAR == "0":
                P.op("dve", lambda e, j=j, pq=pq: e.tensor_tensor(t1[j], self.PS[pq][0:96, :], Ct, ALU.mult), [self.PB[pq], tb], [t12b[j]])
            elif VAR == "1":
                P.op("dve", lambda e, j=j, pq=pq: e.tensor_tensor(t1[j], self.PS[pq][0:96, :], St, ALU.mult), [self.PB[pq], tb], [t12b[j]])
            elif VAR == "2":
                P.op("dve", lambda e, j=j, pq=pq: e.tensor_tensor(t1[j], self.PS[pq][0:96, :], Ct, ALU.mult), [self.PB[pq], tb, qrb[j]], [t12b[j]])
            elif VAR == "3":
                P.op("dve", lambda e, j=j, pq=pq: e.tensor_tensor(t1[j], qraw[j], Ct, ALU.mult), [qrb[j], tb], [t12b[j]])
            if "e" in SK:
                continue
            P.op("dve", lambda e, j=j, pr=pr: e.tensor_tensor(t2[j], self.PS[pr][0:96, :], St, ALU.mult), [self.PB[pr], tb], [t12b[j]])
            if "f" in SK:
                continue
            P.op("dve" if "p" in SK else "pool", lambda e, j=j: e.tensor_tensor(qo[j], t1[j], t2[j], ALU.add), [t12b[j]], [qob[j]])
            if "d" not in SK:
                P.dma("sp", self.pT[h * 96:(h + 1) * 96, csl], qo[j], reads=[qob[j]], writes=[self.pT_b])
            if "n" in SK:
                continue
            pk = 5 + (h % 2)
            self.mm(self.PS[pk][0:64, :], Wukv[:, h * 128:h * 128 + 64], CKV[:, csl], True, True, [ub, cqb[tq]], [self.PB[pk]])
            sj = stg[h % 2]; sjb = stgb[h % 2]
            self.act(sj[0:64, :], self.PS[pk][0:64, :], AF.Copy, [self.PB[pk]], [sjb])
            P.dma("sp", self.pT[1152 + h * 64:1152 + (h + 1) * 64, csl], sj[0:64, :], reads=[sjb], writes=[self.pT_b])
        Wv = Wukv[:].rearrange("p (h c) -> p h c", c=128)
        for ts in range(4 if "v" not in SK else 0):
            t = tq * 4 + ts
            j = t % 2
            for hv in range(2):
                pv = 6 + hv
                self.mm(self.PS[pv][:, 0:384], CKV[:, t * 128:(t + 1) * 128], Wv[:, hv * 6:(hv + 1) * 6, 64:128], True, True, [ub, cqb[tq]], [self.PB[pv]])
                P.op("dve", lambda e, j=j, hv=hv, pv=pv: e.tensor_copy(vst[j][:, hv * 384:(hv + 1) * 384], self.PS[pv][:, 0:384]), [self.PB[pv]], [vstb[j]])
            P.dma("sp", self.vtok[t * 128:(t + 1) * 128, :], vst[j], reads=[vstb[j]], writes=[self.vtok_b])
    P.barrier()
    if STOP == 2:
        self.xattn(l); return
    C.reset()
    kT = [C.bf(96, S) for _ in range(2)]; qT = [C.bf(96, S) for _ in range(2)]
    vA = [C.bf(128, NT, 128) for _ in range(2)]
    hb = [Buf() for _ in range(2)]
    E = [C.bf(128, 512) for _ in range(4)]; Eb = [Buf() for _ in range(4)]
    rd = [C.f32(64, 512) for _ in range(2)]; rdb = [Buf() for _ in range(2)]
    for j in range(2):
        P.op("pool", lambda e, j=j: e.memset(vA[j][:, :, 64:128], 1.0), [], [hb[j]])
    scale = 1.0 / math.sqrt(96.0)
    ne = 0
    vt = self.vtok.rearrange("(t p) c -> p t c", p=128)

    def load_head(h):
        j = h % 2
        P.dma("sp", qT[j], self.pT[h * 96:(h + 1) * 96, :], reads=[self.pT_b], writes=[hb[j]])
        P.dma("sp", kT[j][0:64, :], self.pT[1152 + h * 64:1152 + (h + 1) * 64, :], reads=[self.pT_b], writes=[hb[j]])
        P.dma("sp", kT[j][64:96, :], self.pT[1920:1952, :], reads=[self.pT_b], writes=[hb[j]])
        with self.nc.allow_non_contiguous_dma(reason="per-head V gather (128B runs)"):
            for t0 in range(0, NT, 4):
                P.dma("sp", vA[j][:, t0:t0 + 4, 0:64], vt[:, t0:t0 + 4, h * 64:(h + 1) * 64], reads=[self.vtok_b], writes=[hb[j]])
    load_head(0)
    for h in range(12):
        if h + 1 < 12:
            load_head(h + 1)
        j = h % 2
        for tq in range(NQ):
            po = 4 + (tq % 2)
            nk = 4 * tq + 4
            for kt in range(nk):
                ps = ne % 4
                Ei = E[ne % 4]; Ebi = Eb[ne % 4]; ne += 1
                self.mm(self.PS[ps][:], kT[j][:, kt * 128:(kt + 1) * 128], qT[j][:, tq * 512:(tq + 1) * 512], True, True, [hb[j]], [self.PB[ps]])
                self.act(Ei, self.PS[ps][:], AF.Exp, [self.PB[ps]], [Ebi], scale=scale)
                if kt >= 4 * tq:
                    base = tq * 512 - kt * 128
                    P.op("pool", lambda e, Ei=Ei, base=base: e.affine_select(Ei, Ei, [[1, 512]], ALU.is_ge, 0.0, base=base, channel_multiplier=-1), [Ebi], [Ebi])
                self.mm(self.PS[po][:], vA[j][:, kt, :], Ei, kt == 0, kt == nk - 1, [hb[j], Ebi], [self.PB[po]])
            r = rd[tq % 2]; rb = rdb[tq % 2]
            P.op("dve", lambda e, r=r, po=po: e.reciprocal(r, self.PS[po][64:128, :]), [self.PB[po]], [rb])
            dst = self.CT[64 * (h % 2):64 * (h % 2) + 64, h // 2, tq * 512:(tq + 1) * 512]
            P.op("dve", lambda e, r=r, po=po, dst=dst: e.tensor_tensor(dst, self.PS[po][0:64, :], r, ALU.mult),
                 [self.PB[po], rb], [self.CTb[h // 2][tq]])
    P.barrier()
    self.xattn(l)


Builder.mixer_mla = _mixer_mla


def _mixer_nsa(self, l):
    P, C, I, S = self.P, self.C, self.I, self.S
    NT, NQ = self.NT, self.NQ
    NCMP = (S - 32) // 16 + 1
    NCT = (NCMP + 127) // 128
    NCP = NCT * 128
    win = I[f"win{l}"].rearrange("(k p) n -> p k n", p=128)
    self.proj_xq(win, 2340)
    stg = [C.bf(128, 512) for _ in range(2)]; stgb = [Buf() for _ in range(2)]
    Wq = C.bf(128, 8, 768); Wk = C.bf(128, 8, 1024); Wg = C.bf(128, 8, 36); Wv = C.bf(128, 8, 512); wb = Buf()
    P.dma("pool", Wq, win[:, :, 0:768], writes=[wb])
    P.dma("pool", Wk[:, :, 0:256], win[:, :, 1280:1536], writes=[wb])
    P.dma("pool", Wk[:, :, 256:512], win[:, :, 1792:2048], writes=[wb])
    P.dma("pool", Wk[:, :, 512:1024], win[:, :, 768:1280], writes=[wb])
    P.dma("pool", Wg, win[:, :, 2304:2340], writes=[wb])
    P.dma("pool", Wv[:, :, 0:256], win[:, :, 1536:1792], writes=[wb])
    P.dma("pool", Wv[:, :, 256:512], win[:, :, 2048:2304], writes=[wb])
    posb_t = C.f32(128, 512, dt=I32); posf = C.f32(128, 512); kf = C.f32(128, 512); ki = posb_t
    Ct = C.f32(128, 512); St = C.f32(128, 512); tb = Buf()
    qraw = [C.bf(128, 512) for _ in range(2)]; qrb = [Buf() for _ in range(2)]
    t1 = [C.f32(128, 512) for _ in range(2)]; t2 = [C.f32(128, 512) for _ in range(2)]; t12b = [Buf() for _ in range(2)]
    qo = [C.bf(128, 512) for _ in range(2)]; qob = [Buf() for _ in range(2)]
    vst = [C.bf(128, 512) for _ in range(2)]; vstb = [Buf() for _ in range(2)]
    gst = [C.f32(36, 512) for _ in range(2)]; gstb = [Buf() for _ in range(2)]
    n = 0
    for tq in range(NQ):
        csl = slice(tq * 512, (tq + 1) * 512)
        self.rope_tables(tq, 128, self.invF[:, 0:1], posb_t, posf, kf, ki, Ct, St, tb)
        for ci in range(10):
            Wsrc, col0 = (Wq, ci * 128) if ci < 6 else (Wk, (ci - 6) * 128)
            row0 = ci * 128
            pq, pr = 0 + (ci % 2) * 2, 1 + (ci % 2) * 2
            self.proj_chunk(pq, 128, Wsrc, wb, col0, tq)
            j = n % 2; n += 1
            self.act(qraw[j], self.PS[pq][:], AF.Copy, [self.PB[pq]], [qrb[j]])
            self.mm(self.PS[pr][:], self.RnB, qraw[j], True, True, [self.cB, qrb[j]], [self.PB[pr]])
            P.op("dve", lambda e, j=j, pq=pq: e.tensor_tensor(t1[j], self.PS[pq][:], Ct, ALU.mult), [self.PB[pq], tb], [t12b[j]])
            P.op("dve", lambda e, j=j, pr=pr: e.tensor_tensor(t2[j], self.PS[pr][:], St, ALU.mult), [self.PB[pr], tb], [t12b[j]])
            P.op("pool", lambda e, j=j: e.tensor_tensor(qo[j], t1[j], t2[j], ALU.add), [t12b[j]], [qob[j]])
            P.dma("sp", self.pT[row0:row0 + 128, csl], qo[j], reads=[qob[j]], writes=[self.pT_b])
        for ci in range(4):
            pi = 4 + (ci % 2)
            self.proj_chunk(pi, 128, Wk, wb, 512 + ci * 128, tq)
            sj = stg[ci % 2]; sjb = stgb[ci % 2]
            self.act(sj, self.PS[pi][:], AF.Copy, [self.PB[pi]], [sjb])
            P.dma("sp", self.pT[1280 + ci * 128:1280 + (ci + 1) * 128, csl], sj, reads=[sjb], writes=[self.pT_b])
        self.proj_chunk(6, 36, Wg, wb, 0, tq)
        gj = gst[tq % 2]; gjb = gstb[tq % 2]
        self.act(gj, self.PS[6][0:36, :], AF.Copy, [self.PB[6]], [gjb])
        P.dma("sp", self.glT[:, csl], gj, reads=[gjb], writes=[self.glT_b])
        for ts in range(4):
            t = tq * 4 + ts
            j = t % 2
            pv = 5 if ts % 2 == 0 else 7
            for k in range(8):
                self.mm(self.PS[pv][:], self.XT[:, k, t * 128:(t + 1) * 128], Wv[:, k, :], k == 0, k == 7,
                        [wb, self.XTb[tq]], [self.PB[pv]])
            P.op("dve", lambda e, j=j, pv=pv: e.tensor_copy(vst[j], self.PS[pv][:]), [self.PB[pv]], [vstb[j]])
            P.dma("sp", self.vtok[t * 128:(t + 1) * 128, 0:512], vst[j], reads=[vstb[j]], writes=[self.vtok_b])
    P.barrier()
    C.reset()
    kcmpT = C.bf(64, 4, NCP); vcA = C.bf(128, 4, NCT, 128); cmb = Buf()
    Ebig = self.A_t[0:64, 4 * S:5 * S]
    Ov = C.bf(128, NCT, 64); Sel = C.bf(3, 3, 64); kb = Buf()
    P.dma("pool", Ebig, I["c_ebig"], writes=[kb])
    P.dma("pool", Ov, I["c_ov"][0:NCP, :].rearrange("(t p) j -> p t j", p=128), writes=[kb])
    P.dma("pool", Sel, I["c_sel"].rearrange("r (a m) -> r a m", m=64), writes=[kb])
    P.op("pool", lambda e: e.memset(vcA[:, :, :, 0:64], 0.0), [], [cmb])
    P.op("pool", lambda e: e.memset(vcA[:, :, :, 64:128], 1.0), [], [cmb])
    P.op("pool", lambda e: e.memset(kcmpT, 0.0), [], [cmb])
    mark = C.off
    cw1 = C.bf(64, 2, 32, 128); cpe = C.bf(64, 2, 32); cw2 = C.bf(128, 2, 64); cwb = Buf()
    P.dma("pool", cw1, I[f"cw1{l}"].rearrange("a d l f -> d a l f"), writes=[cwb])
    P.dma("pool", cpe, I[f"cpe{l}"].rearrange("a d l -> d a l"), writes=[cwb])
    P.dma("pool", cw2, I[f"cw2{l}"].rearrange("a f d -> f a d"), writes=[cwb])
    kcT = [C.bf(64, S) for _ in range(2)]; kcb = [Buf() for _ in range(2)]
    hbias = C.f32(128, 2); hbb = Buf()
    hT = [C.bf(128, NCP) for _ in range(2)]; hTb = [Buf() for _ in range(2)]
    posb2 = C.f32(64, NCP, dt=I32); posf2 = C.f32(64, NCP); kf2 = C.f32(64, NCP); ki2 = posb2
    Ct2 = C.f32(64, NCP); St2 = C.f32(64, NCP); tb2 = Buf()
    kraw = C.bf(64, NCP); krb_ = Buf(); u1 = C.f32(64, NCP); u2 = C.f32(64, NCP)
    self.rope_tables(0, 64, self.invF[0:64, 0:1], posb2, posf2, kf2, ki2, Ct2, St2, tb2,
                     pos_src=I["pos_end"][0:1, 0:NCP], width=NCP)
    for a in range(2):
        for ll in range(32):
            self.mm(self.PS[6][:, a:a + 1], cw1[:, a, ll, :], cpe[:, a, ll:ll + 1], ll == 0, ll == 31, [cwb], [self.PB[6]])
    P.op("dve", lambda e: e.tensor_copy(hbias, self.PS[6][:, 0:2]), [self.PB[6]], [hbb])
    nn = 0
    for a in range(2):
        for k in range(4):
            jj = nn % 2; nn += 1
            if a == 0:
                P.dma("sp", kcT[jj], self.pT[1280 + 64 * k:1280 + 64 * (k + 1), :], reads=[self.pT_b], writes=[kcb[jj]])
            else:
                P.dma("sp", kcT[jj], self.pT[1536 + 64 * k:1536 + 64 * (k + 1), :], reads=[self.pT_b], writes=[kcb[jj]])
            ph = jj
            src = kcT[jj]
            for ll in range(32):
                rhs = src[:, ll:ll + 16 * (NCMP - 1) + 1:16]
                self.mm(self.PS[ph][:, 0:NCMP], cw1[:, a, ll, :], rhs, ll == 0, ll == 31, [cwb, kcb[jj]], [self.PB[ph]])
            h_ = hT[jj]; hb_ = hTb[jj]
            if NCMP < NCP:
                P.op("pool", lambda e, h_=h_: e.memset(h_[:, NCMP:NCP], 0.0), [], [hb_])
            self.act(h_[:, 0:NCMP], self.PS[ph][:, 0:NCMP], AF.Silu, [self.PB[ph], hbb], [hb_], bias=hbias[:, a:a + 1], scale=1.0)
            if a == 0:
                self.mm(self.PS[2][0:64, 0:NCP], cw2[:, 0, :], h_, True, True, [cwb, hb_], [self.PB[2]])
                self.act(kraw, self.PS[2][0:64, 0:NCP], AF.Copy, [self.PB[2]], [krb_])
                self.mm(self.PS[3][0:64, 0:NCP], self.RnB[0:64, 0:64], kraw, True, True, [self.cB, krb_], [self.PB[3]])
                P.op("dve", lambda e: e.tensor_tensor(u1, self.PS[2][0:64, 0:NCP], Ct2, ALU.mult), [self.PB[2], tb2], [krb_])
                P.op("dve", lambda e: e.tensor_tensor(u2, self.PS[3][0:64, 0:NCP], St2, ALU.mult), [self.PB[3], tb2], [krb_])
                P.op("dve", lambda e, k=k: e.tensor_tensor(kcmpT[:, k, 0:NCMP], u1[:, 0:NCMP], u2[:, 0:NCMP], ALU.add), [krb_], [cmb])
            else:
                for nt in range(NCT):
                    m_ = min(128, NCMP - nt * 128)
                    self.mm(self.PS[4 + nt][0:m_, 0:64], h_[:, nt * 128:nt * 128 + m_], cw2[:, 1, :], True, True, [cwb, hb_], [self.PB[4 + nt]])
                    P.op("dve", lambda e, k=k, nt=nt, m_=m_: e.tensor_copy(vcA[0:m_, k, nt, 0:64], self.PS[4 + nt][0:m_, 0:64]), [self.PB[4 + nt]], [cmb])
    P.barrier()
    self.nsa_attention(l, kcmpT, vcA, cmb, Ebig, Ov, Sel, kb, mark, NCMP, NCT)


Builder.mixer_nsa = _mixer_nsa


def _nsa_attention(self, l, kcmpT, vcA, cmb, Ebig, Ov, Sel, kb, mark, NCMP, NCT):
    P, C, I, S = self.P, self.C, self.I, self.S
    NT, NQ = self.NT, self.NQ
    C.off = mark
    A = self.A_t
    ksT = A[0:64, 0:S]; kwT = A[0:64, S:2 * S]
    vsA = A[:, 2 * S:3 * S].rearrange("p (t c) -> p t c", c=128)
    vwA = A[:, 3 * S:4 * S].rearrange("p (t c) -> p t c", c=128)
    kvb = Buf()
    P.op("pool", lambda e: e.memset(vsA[:, :, 64:128], 1.0), [], [kvb])
    P.op("pool", lambda e: e.memset(vwA[:, :, 64:128], 1.0), [], [kvb])
    qc = [[C.bf(64, 512) for _ in range(3)] for _ in range(2)]; qcb = [[Buf() for _ in range(3)] for _ in range(2)]
    Gl = [[C.f32(3, 512) for _ in range(3)] for _ in range(2)]
    Glh = [[C.bf(3, 512) for _ in range(3)] for _ in range(2)]
    Gll = [[C.bf(3, 512) for _ in range(3)] for _ in range(2)]
    Gtmp = C.f32(3, 512); gtb = Buf()
    glb = [[Buf() for _ in range(3)] for _ in range(2)]
    FBt = [C.f32(128, 4, 64) for _ in range(2)]; CBt = [C.f32(128, 4, 64) for _ in range(2)]; fbb = [Buf() for _ in range(2)]
    E = [C.bf(128, 512) for _ in range(4)]; Eb = [Buf() for _ in range(4)]
    rd = [C.f32(64, 512) for _ in range(2)]; rdb = [Buf() for _ in range(2)]
    gs = [C.f32(64, 512) for _ in range(2)]; gsb = [Buf() for _ in range(2)]
    acc = [C.f32(64, 512) for _ in range(3)]; accb = [Buf() for _ in range(3)]
    impacc = C.f32(64, 512); impb = Buf()
    selbT = C.bf(64, 512); selTb = Buf()
    impm = [C.f32(128, 64) for _ in range(2)]; work = [C.f32(128, 64) for _ in range(2)]
    m8a = [C.f32(128, 8) for _ in range(2)]; m8b = [C.f32(128, 8) for _ in range(2)]
    selt = [C.f32(128, 64) for _ in range(2)]; selbf = [C.bf(128, 64) for _ in range(2)]; slb = [Buf() for _ in range(2)]
    PS, PB = self.PS, self.PB
    self.dbg_off = {"selbT": selbT.offset, "qc00": qc[0][0].offset, "impacc": impacc.offset, "selbf0": selbf[0].offset,
                    "CBt0": CBt[0].offset, "Glh00": Glh[0][0].offset, "E0": E[0].offset}
    PS7b = PS[7][:].bitcast(BF16)
    vt = self.vtok.rearrange("(t p) c -> p t c", p=128)
    fbv = I["c_fb"].rearrange("(q ts p) j -> q p ts j", p=128, ts=4)
    st = {"ne": 0, "nr": 0, "ns": 0}

    def new_E():
        i = st["ne"] % 4; st["ne"] += 1
        return E[i], Eb[i]

    def new_S():
        i = st["ns"] % 4; st["ns"] += 1
        return i

    def finish(k, g, tq, po, b, first, last, par, with_imp=False):
        h = 3 * k + g
        i = st["nr"] % 2; st["nr"] += 1
        r_, rb_ = rd[i], rdb[i]
        g_, gb_ = gs[i], gsb[i]
        P.op("dve", lambda e: e.tensor_scalar(r_, PS[po][64:128, :], 1.0e-30, None, ALU.max), [PB[po]], [rb_])
        P.op("dve", lambda e: e.reciprocal(r_, r_), [rb_], [rb_])
        if with_imp:
            if g == 0:
                P.op("dve", lambda e: e.tensor_tensor(impacc, PS[7][0:64, :], r_, ALU.mult), [PB[7], rb_], [impb])
            else:
                P.op("dve", lambda e: e.tensor_tensor(g_, PS[7][0:64, :], r_, ALU.mult), [PB[7], rb_], [gb_])
                P.op("dve", lambda e: e.tensor_tensor(impacc, impacc, g_, ALU.add), [gb_, impb], [impb])
        self.mm(PS[6][0:64, :], Sel[:, b, :], Glh[par][g], True, False, [kb, glb[par][g]], [PB[6]])
        self.mm(PS[6][0:64, :], Sel[:, b, :], Gll[par][g], False, True, [kb, glb[par][g]], [PB[6]])
        self.act(g_, PS[6][0:64, :], AF.Sigmoid, [PB[6]], [gb_])
        P.op("dve", lambda e: e.tensor_tensor(g_, g_, r_, ALU.mult), [gb_, rb_], [gb_])
        import os
        SKB = os.environ.get("NSA_SKIPB", "")
        if str(b) in SKB:
            P.op("dve", lambda e: e.memset(g_, 0.0), [], [gb_])
        if first:
            P.op("dve", lambda e: e.tensor_tensor(acc[g], PS[po][0:64, :], g_, ALU.mult), [PB[po], gb_], [accb[g]])
        else:
            P.op("dve", lambda e: e.tensor_tensor(r_, PS[po][0:64, :], g_, ALU.mult), [PB[po], gb_], [rb_])
            if not last:
                P.op("dve", lambda e: e.tensor_tensor(acc[g], acc[g], r_, ALU.add), [rb_, accb[g]], [accb[g]])
            else:
                dst = self.CT[64 * (h % 2):64 * (h % 2) + 64, h // 2, tq * 512:(tq + 1) * 512]
                P.op("dve", lambda e: e.tensor_tensor(dst, acc[g], r_, ALU.add), [rb_, accb[g]], [self.CTb[h // 2][tq]])

    for k in range(4):
        P.dma("sp", ksT, self.pT[768 + 64 * k:768 + 64 * (k + 1), :], reads=[self.pT_b], writes=[kvb])
        P.dma("sp", kwT, self.pT[1024 + 64 * k:1024 + 64 * (k + 1), :], reads=[self.pT_b], writes=[kvb])
        with self.nc.allow_non_contiguous_dma(reason="per-head V gather (128B runs)"):
            for t0 in range(0, NT, 4):
                P.dma("sp", vsA[:, t0:t0 + 4, 0:64], vt[:, t0:t0 + 4, 64 * k:64 * (k + 1)], reads=[self.vtok_b], writes=[kvb])
                P.dma("sp", vwA[:, t0:t0 + 4, 0:64], vt[:, t0:t0 + 4, 256 + 64 * k:256 + 64 * (k + 1)], reads=[self.vtok_b], writes=[kvb])
        for tq in range(NQ):
            par = tq % 2
            q0 = tq * 512
            csl = slice(q0, q0 + 512)
            for g in range(3):
                h = 3 * k + g
                P.dma("sp", qc[par][g], self.pT[64 * h:64 * (h + 1), csl], reads=[self.pT_b], writes=[qcb[par][g]])
                P.dma("sp", Gl[par][g], self.glT[3 * h:3 * h + 3, csl], reads=[self.glT_b], writes=[glb[par][g]])
                P.op("act", lambda e, par=par, g=g: e.copy(Glh[par][g], Gl[par][g]), [glb[par][g]], [glb[par][g]])
                P.op("dve", lambda e, par=par, g=g: e.tensor_tensor(Gtmp, Gl[par][g], Glh[par][g], ALU.subtract), [glb[par][g]], [glb[par][g], gtb])
                P.op("dve", lambda e, par=par, g=g: e.tensor_copy(Gll[par][g], Gtmp), [glb[par][g], gtb], [glb[par][g]])
            P.dma("sp", FBt[par], fbv[tq], writes=[fbb[par]])
            P.op("dve", lambda e, par=par: e.tensor_scalar(CBt[par], FBt[par], 0.0, 3.0e-5, ALU.min, ALU.mult), [fbb[par]], [fbb[par]])
            tiles = [nt for nt in range(NCT) if nt * 2048 + 31 <= q0 + 511]
            for g in range(3):
                po = 4 + (g % 2)
                for ii, nt in enumerate(tiles):
                    ps = new_S()
                    self.mm(PS[ps][:], kcmpT[:, k, nt * 128:(nt + 1) * 128], qc[par][g], True, True, [cmb, qcb[par][g]], [PB[ps]])
                    Ei, Ebi = new_E()
                    self.act(Ei, PS[ps][:], AF.Exp, [PB[ps]], [Ebi], scale=0.125)
                    base = q0 - 16 * nt * 128 - 31
                    P.op("pool", lambda e, Ei=Ei, base=base: e.affine_select(Ei, Ei, [[1, 512]], ALU.is_ge, 0.0, base=base, channel_multiplier=-16), [Ebi], [Ebi])
                    self.mm(PS[po][:], vcA[:, k, nt, :], Ei, ii == 0, ii == len(tiles) - 1, [cmb, Ebi], [PB[po]])
                    self.mm(PS[7][0:64, :], Ov[:, nt, :], Ei, ii == 0, ii == len(tiles) - 1, [kb, Ebi], [PB[7]])
                finish(k, g, tq, po, 0, True, False, par, with_imp=True)
            trivial = (q0 + 511 < 16 * 64)
            for ts in range(4):
                i = ts % 2
                if trivial:
                    P.op("dve", lambda e, i=i, ts=ts, par=par: e.tensor_copy(selbf[i], CBt[par][:, ts, :]), [fbb[par]], [slb[i]])
                    P.op("dve", lambda e, i=i: e.memset(m8a[i], 0.0), [], [slb[i]])
                    self.tr(PS7b[0:64, ts * 128:(ts + 1) * 128], selbf[i], self.identB, [slb[i], self.cB], [PB[7]])
                    continue
                self.tr(PS[6][:, 0:64], impacc[:, ts * 128:(ts + 1) * 128], self.identF[0:64, 0:64], [impb, self.cB], [PB[6]])
                P.op("dve", lambda e, i=i, ts=ts, par=par: e.tensor_tensor(impm[i], PS[6][:, 0:64], FBt[par][:, ts, :], ALU.add), [PB[6], fbb[par]], [slb[i]])
                P.op("dve", lambda e, i=i: e.max(out=m8a[i], in_=impm[i]), [slb[i]], [slb[i]])
                P.op("dve", lambda e, i=i: e.match_replace(out=work[i], in_to_replace=m8a[i], in_values=impm[i], imm_value=-3.0e9), [slb[i]], [slb[i]])
                P.op("dve", lambda e, i=i: e.max(out=m8b[i], in_=work[i]), [slb[i]], [slb[i]])
                P.op("dve", lambda e, i=i: e.tensor_scalar(selt[i], impm[i], m8b[i][:, 7:8], None, ALU.is_ge), [slb[i]], [slb[i]])
                P.op("dve", lambda e, i=i: e.tensor_scalar(selt[i], selt[i], 1.0, 30000.0, ALU.subtract, ALU.mult), [slb[i]], [slb[i]])
                P.op("dve", lambda e, i=i, ts=ts, par=par: e.tensor_tensor(selbf[i], selt[i], CBt[par][:, ts, :], ALU.add), [slb[i], fbb[par]], [slb[i]])
                if k == 0 and tq == 0 and ts < 2:
                    self.dbg_dump(f"d_impm{ts}", impm[i], [128, 64], F32, [slb[i]])
                    self.dbg_dump(f"d_selt{ts}", selt[i], [128, 64], F32, [slb[i]])
                    self.dbg_dump(f"d_m8b{ts}", m8b[i], [128, 8], F32, [slb[i]])
                    self.dbg_dump(f"d_m8a{ts}", m8a[i], [128, 8], F32, [slb[i]])
                    self.dbg_dump(f"d_work{ts}", work[i], [128, 64], F32, [slb[i]])
                P.op("dve", lambda e, i=i: e.memset(m8a[i], 0.0), [], [slb[i]])
                self.tr(PS7b[0:64, ts * 128:(ts + 1) * 128], selbf[i], self.identB, [slb[i], self.cB], [PB[7]])
            self.act(selbT, PS7b[0:64, 0:512], AF.Copy, [PB[7]], [selTb])
            if k == 0 and tq == 0:
                self.dbg_dump("d_selbT", selbT, [64, 512], BF16, [selTb])
                self.dbg_dump("d_impacc", impacc, [64, 512], F32, [impb])
                self.dbg_dump("d_fbt", FBt[par], [128, 4, 64], F32, [fbb[par]])
                self.dbg_dump("d_cbt", CBt[par], [128, 4, 64], F32, [fbb[par]])
            for g in range(3):
                po = 4 + ((g + 1) % 2)
                nk = 4 * tq + 4
                for kt in range(nk):
                    ps = new_S()
                    self.mm(PS[ps][:], ksT[:, kt * 128:(kt + 1) * 128], qc[par][g], True, False, [kvb, qcb[par][g]], [PB[ps]])
                    self.mm(PS[ps][:], Ebig[:, kt * 128:(kt + 1) * 128], selbT, False, True, [kb, selTb], [PB[ps]])
                    Ei, Ebi = new_E()
                    self.act(Ei, PS[ps][:], AF.Exp, [PB[ps]], [Ebi], scale=0.125)
                    if kt >= 4 * tq:
                        base = q0 - kt * 128
                        P.op("pool", lambda e, Ei=Ei, base=base: e.affine_select(Ei, Ei, [[1, 512]], ALU.is_ge, 0.0, base=base, channel_multiplier=-1), [Ebi], [Ebi])
                    self.mm(PS[po][:], vsA[:, kt, :], Ei, kt == 0, kt == nk - 1, [kvb, Ebi], [PB[po]])
                finish(k, g, tq, po, 1, False, False, par)
            for g in range(3):
                po = 4 + (g % 2)
                kts = list(range(max(0, 4 * tq - 4), 4 * tq + 4))
                for ii, kt in enumerate(kts):
                    ps = new_S()
                    self.mm(PS[ps][:], kwT[:, kt * 128:(kt + 1) * 128], qc[par][g], True, True, [kvb, qcb[par][g]], [PB[ps]])
                    Ei, Ebi = new_E()
                    self.act(Ei, PS[ps][:], AF.Exp, [PB[ps]], [Ebi], scale=0.125)
                    if kt >= 4 * tq:
                        base = q0 - kt * 128
                        P.op("pool", lambda e, Ei=Ei, base=base: e.affine_select(Ei, Ei, [[1, 512]], ALU.is_ge, 0.0, base=base, channel_multiplier=-1), [Ebi], [Ebi])
                    else:
                        base = 511 - q0 + kt * 128
                        P.op("pool", lambda e, Ei=Ei, base=base: e.affine_select(Ei, Ei, [[-1, 512]], ALU.is_ge, 0.0, base=base, channel_multiplier=1), [Ebi], [Ebi])
                    self.mm(PS[po][:], vwA[:, kt, :], Ei, ii == 0, ii == len(kts) - 1, [kvb, Ebi], [PB[po]])
                finish(k, g, tq, po, 2, False, True, par)
    P.barrier()
    self.xattn(l)


Builder.nsa_attention = _nsa_attention
```

```python
import contextlib
import math
import numpy as np
import concourse.bass as bass
import concourse.mybir as mybir
from concourse.bass_utils import run_bass_kernel_spmd

F32 = mybir.dt.float32
BF16 = mybir.dt.bfloat16
I32 = mybir.dt.int32
AF = mybir.ActivationFunctionType
ALU = mybir.AluOpType
AX = mybir.AxisListType

D = 1024
NCORES = 8
N_MEM = 256
ALPHA = (2.0 * 4) ** 0.25
LN_EPS = 1e-5
RMS_EPS = 1e-6
NEG = -30000.0
EPOCH = 16000
ENGS = ("pe", "act", "dve", "pool", "sp")


class Buf:
    __slots__ = ("name", "w", "r", "excl")

    def __init__(self, name="", excl=False):
        self.name = name
        self.w = None
        self.r = {}
        self.excl = excl


class Prog:
    def __init__(self, nc, stack, n_dma_slots=(("sp", 16), ("pool", 16), ("act", 4))):
        self.nc = nc
        self.stack = stack
        self.streams = {e: [] for e in ENGS}
        self.cnt = {e: 0 for e in ENGS}
        self.sems = []
        self.eng_sems = {e: [] for e in ENGS}
        self.waited = {e: {} for e in ENGS}
        self.dma_slots = {}
        for q, n in n_dma_slots:
            self.dma_slots[q] = [[self._new_sem(f"d{q}{i}"), 0] for i in range(n)]
        self.dma_rr = {q: 0 for q, _ in n_dma_slots}
        self.ninstr = 0

    def _new_sem(self, name):
        h = self.stack.enter_context(self.nc.semaphore(name))
        self.sems.append(h)
        return len(self.sems) - 1

    def _eng_sem(self, eng, epoch):
        lst = self.eng_sems[eng]
        while len(lst) <= epoch:
            lst.append(self._new_sem(f"s{eng}{len(lst)}"))
        return lst[epoch]

    def _need(self, eng, deps):
        w = self.waited[eng]
        for si, val in deps.items():
            if w.get(si, 0) >= val:
                continue
            w[si] = val
            h = self.sems[si]
            self.streams[eng].append(lambda e, h=h, val=val: e.wait_ge(h, val))

    def _collect(self, reads, writes, eng=None):
        deps = {}
        own = self.eng_sems.get(eng, ()) if eng else ()
        for b in reads:
            ev = b.w
            if ev is not None and deps.get(ev[0], 0) < ev[1]:
                deps[ev[0]] = ev[1]
            if b.excl:
                for si, v in b.r.items():
                    if si not in own and deps.get(si, 0) < v:
                        deps[si] = v
        for b in writes:
            ev = b.w
            if ev is not None and deps.get(ev[0], 0) < ev[1]:
                deps[ev[0]] = ev[1]
            for si, v in b.r.items():
                if deps.get(si, 0) < v:
                    deps[si] = v
        return deps

    @staticmethod
    def _mark(ev, reads, writes):
        si, v = ev
        for b in writes:
            b.w = ev
            b.r = {}
        for b in reads:
            if b.r.get(si, 0) < v:
                b.r[si] = v

    def op(self, eng, fn, reads=(), writes=()):
        deps = self._collect(reads, writes, eng)
        n = self.cnt[eng]
        epoch, within = divmod(n, EPOCH)
        si = self._eng_sem(eng, epoch)
        if eng == "pe":
            for s in self.eng_sems["pe"]:
                deps.pop(s, None)
        self._need(eng, deps)
        h = self.sems[si]
        self.streams[eng].append(lambda e, fn=fn, h=h: fn(e).then_inc(h, 1))
        self.cnt[eng] = n + 1
        ev = (si, within + 1)
        self._mark(ev, reads, writes)
        self.ninstr += 1
        return ev

    def dma(self, q, out, in_, reads=(), writes=(), **kw):
        deps = self._collect(reads, writes)
        slots = self.dma_slots[q]
        k = self.dma_rr[q]
        self.dma_rr[q] = (k + 1) % len(slots)
        slot = slots[k]
        si = slot[0]
        if slot[1] > 0:
            deps[si] = max(deps.get(si, 0), slot[1])
        self._need(q, deps)
        slot[1] += 16
        val = slot[1]
        h = self.sems[si]
        self.streams[q].append(
            lambda e, out=out, in_=in_, h=h, kw=kw: e.dma_start(out=out, in_=in_, **kw).then_inc(h, 16))
        ev = (si, val)
        self._mark(ev, reads, writes)
        self.ninstr += 1
        return ev

    def barrier(self):
        deps = {}
        for e in ENGS:
            n = self.cnt[e]
            if n == 0:
                continue
            epoch, within = divmod(n - 1, EPOCH)
            deps[self.eng_sems[e][epoch]] = within + 1
            for ep in range(epoch):
                deps[self.eng_sems[e][ep]] = EPOCH
        for q in self.dma_slots:
            for si, v in self.dma_slots[q]:
                if v:
                    deps[si] = v
        for e in ENGS:
            self._need(e, dict(deps))

    def emit(self):
        nc = self.nc
        with nc.Block() as block:
            @block.tensor
            def _(e):
                for f in self.streams["pe"]:
                    f(e)

            @block.scalar
            def _(e):
                for f in self.streams["act"]:
                    f(e)

            @block.vector
            def _(e):
                for f in self.streams["dve"]:
                    f(e)

            @block.gpsimd
            def _(e):
                for f in self.streams["pool"]:
                    f(e)

            @block.sync
            def _(e):
                for f in self.streams["sp"]:
                    f(e)


class Arena:
    def __init__(self, ap, nbf16):
        self.ap = ap
        self.n = nbf16
        self.off = 0

    def reset(self):
        self.off = 0

    def bf(self, parts, *shape):
        self.off = (self.off + 31) // 32 * 32
        n = int(np.prod(shape))
        n2 = (n + 1) // 2 * 2
        assert self.off + n2 <= self.n, ("arena overflow", self.off, n2, self.n)
        v = self.ap[0:parts, self.off:self.off + n]
        self.off += n2
        if len(shape) == 2:
            v = v.rearrange("p (a b) -> p a b", b=shape[1])
        elif len(shape) == 3:
            v = v.rearrange("p (a b c) -> p a b c", b=shape[1], c=shape[2])
        return v

    def f32(self, parts, *shape, dt=F32):
        self.off = (self.off + 31) // 32 * 32
        n = int(np.prod(shape))
        assert self.off + 2 * n <= self.n, ("arena overflow", self.off, 2 * n, self.n)
        v = self.ap[0:parts, self.off:self.off + 2 * n].bitcast(dt)
        self.off += 2 * n
        if len(shape) == 2:
            v = v.rearrange("p (a b) -> p a b", b=shape[1])
        elif len(shape) == 3:
            v = v.rearrange("p (a b c) -> p a b c", b=shape[1], c=shape[2])
        return v


NSA_IN = 2596
MLA_IN = 672
CONV_IN = 1792


class Builder:
    def __init__(self, S, kinds, last_only_out=True):
        self.S = S
        self.kinds = list(kinds)
        self.NT = S // 128
        self.NQ = S // 512
        self.SC = min(2048, S // 2)

    def mm(self, out, lhsT, rhs, start, stop, reads, writes):
        return self.P.op("pe", lambda e: e.matmul(out, lhsT, rhs, start=start, stop=stop), reads, writes)

    def tr(self, out, in_, ident, reads, writes):
        return self.P.op("pe", lambda e: e.transpose(out, in_, ident), reads, writes)

    def act(self, out, in_, func, reads, writes, **kw):
        return self.P.op("act", lambda e: e.activation(out, in_, func, **kw), reads, writes)

    def dbg_dump(self, name, ap, shape, dt, reads):
        import os
        if not os.environ.get("DBG_NSA"):
            return
        self.dbg_names = getattr(self, "dbg_names", [])
        d = self.dram(name, shape, dt, kind="ExternalOutput")
        self.P.dma("sp", d, ap, reads=reads)
        self.dbg_names.append(name)

    def dram(self, name, shape, dt, kind="Internal"):
        return self.nc.dram_tensor(name, list(shape), dt, kind=kind).ap()

    def build(self):
        S = self.S
        nc = self.nc = bass.Bass("TRN2", target_bir_lowering=False)
        I = self.I = {}

        def inp(name, shape, dt=F32):
            I[name] = self.dram(name, shape, dt, kind="ExternalInput")
            return I[name]

        inp("x", [S, D]); inp("mem", [N_MEM, D]); inp("pos", [1, S], I32); inp("pos_end", [1, 256], I32)
        inp("c_ident", [128, 128]); inp("c_rope", [128, 4 * 128 + 4])
        inp("c_ebig", [64, S]); inp("c_ov", [256, 64]); inp("c_fb", [S, 64]); inp("c_sel", [3, 3 * 64])
        for l, kind in enumerate(self.kinds):
            nin = (NSA_IN, MLA_IN, CONV_IN)[kind]
            inp(f"win{l}", [D, nin]); inp(f"wkv{l}", [D, 512]); inp(f"wo{l}", [D, D])
            inp(f"lng{l}", [2, D]); inp(f"lnb{l}", [2, D])
            inp(f"wr{l}", [D, 20]); inp(f"br{l}", [1, 20])
            inp(f"wg{l}", [16, D, 512]); inp(f"wu{l}", [16, D, 512]); inp(f"wd{l}", [16, 512, D])
            if kind == 0:
                inp(f"cpe{l}", [2, 64, 32]); inp(f"cw1{l}", [2, 64, 32, 128]); inp(f"cw2{l}", [2, 128, 64])
            elif kind == 1:
                inp(f"qn{l}", [128, 2]); inp(f"kvn{l}", [128, 1]); inp(f"wuq{l}", [256, 1152]); inp(f"wukv{l}", [128, 1536])
            else:
                inp(f"cbin{l}", [128, 12]); inp(f"cdw{l}", [128, 6 * 31]); inp(f"cdb{l}", [128, 6])
                inp(f"clg{l}", [128, 6]); inp(f"clb{l}", [128, 6])
        self.out = self.dram("out", [S, D], F32, kind="ExternalOutput")
        self.xres = self.dram("xres", [S, D], F32)
        self.pT = self.dram("pT", [2304, S], BF16)
        self.vtok = self.dram("vtok", [S, 768], BF16)
        import os
        self.glT = self.dram("glT", [36, S], F32, kind=("ExternalOutput" if os.environ.get("DBG_GL") else "Internal"))

        with contextlib.ExitStack() as st:
            P = self.P = Prog(nc, st)
            sb = lambda name, shape, dt: st.enter_context(nc.sbuf_tensor(name, shape, dt))
            self.A_t = sb("arenaA", [128, max(8 * S, 186 * 128)], BF16)
            self.B_t = sb("arenaB", [128, 8 * S], BF16)
            ZN = 4608
            self.Z = Arena(sb("arenaZ", [128, ZN], BF16)[:], ZN)
            rem = int(nc.sbuf_bytes_remaining) - 1024
            CN = min(rem // 2, 37000)
            self.C = Arena(sb("arenaC", [128, CN], BF16)[:], CN)
            self.PS = [st.enter_context(nc.psum_tensor(f"ps{i}", [128, 512], F32)) for i in range(8)]
            self.PB = [Buf(f"ps{i}", excl=True) for i in range(8)]
            self.XT = self.A_t[:, 0:8 * S].rearrange("p (k s) -> p k s", s=S)
            self.CT = self.B_t[:].rearrange("p (k s) -> p k s", s=S)
            self.XTb = [Buf(f"xt{q}") for q in range(self.NQ)]
            self.CTb = [[Buf(f"ct{k}_{q}") for q in range(self.NQ)] for k in range(8)]
            self.xres_b = [Buf(f"xr{t}") for t in range(self.NT)]
            self.pT_b = Buf("pT"); self.vtok_b = Buf("vtok"); self.glT_b = Buf("glT")
            self.setup_consts()
            for l, kind in enumerate(self.kinds):
                self.layer(l, kind)
            P.barrier()
            P.emit()
        return nc

    def setup_consts(self):
        P, Z, I = self.P, self.Z, self.I
        self.identF = Z.f32(128, 128); self.identB = Z.bf(128, 128)
        self.onesB = Z.bf(128, 128)
        self.onesF = Z.f32(128, 128)
        self.eps_t = Z.f32(128, 2)
        self.RnB = Z.bf(128, 128); self.RmB = Z.bf(128, 128); self.invF = Z.f32(128, 4)
        self.memT = Z.bf(128, 8, 256)
        self.gates = Z.f32(128, self.NT, 16)
        self.cB = Buf("consts"); self.memT_b = Buf("memT"); self.gates_b = [Buf() for _ in range(self.NT)]
        P.dma("sp", self.identF, I["c_ident"], writes=[self.cB])
        P.dma("pool", self.identB, I["c_ident"], writes=[self.cB])
        P.op("dve", lambda e: e.memset(self.onesB, 1.0), writes=[self.cB])
        P.op("dve", lambda e: e.memset(self.onesF, 1.0), writes=[self.cB])
        P.op("dve", lambda e: e.memset(self.eps_t[:, 0:1], LN_EPS), writes=[self.cB])
        P.op("dve", lambda e: e.memset(self.eps_t[:, 1:2], RMS_EPS), writes=[self.cB])
        P.dma("pool", self.RnB, I["c_rope"][:, 0:128], writes=[self.cB])
        P.dma("pool", self.RmB, I["c_rope"][:, 128:256], writes=[self.cB])
        P.dma("sp", self.invF, I["c_rope"][:, 512:516], writes=[self.cB])
        C = self.C
        C.reset()
        m = C.f32(128, 2, D)
        mb = Buf()
        P.dma("sp", m, I["mem"].rearrange("(t p) d -> p t d", p=128), writes=[mb])
        for t in range(2):
            for half in range(2):
                pi = t * 2 + half
                for k4 in range(4):
                    k = half * 4 + k4
                    self.tr(self.PS[pi][:, k4 * 128:(k4 + 1) * 128], m[:, t, k * 128:(k + 1) * 128], self.identF,
                            [mb, self.cB], [self.PB[pi]])
                src = self.PS[pi][:].rearrange("p (k s) -> p k s", s=128)
                dst = self.memT[:, half * 4:half * 4 + 4, t * 128:(t + 1) * 128]
                P.op("act", lambda e, dst=dst, src=src: e.copy(dst, src), [self.PB[pi]], [self.memT_b])
        P.barrier()

    def layer(self, l, kind):
        first = (l == 0)
        last = (l == len(self.kinds) - 1)
        self.xsrc = self.I["x"] if first else self.xres
        if first:
            self.load_xt_from_x()
        if kind == 2:
            self.mixer_conv(l)
        elif kind == 1:
            self.mixer_mla(l)
        else:
            self.mixer_nsa(l)
        import os
        if os.environ.get("DBG_CT") and l == 0:
            dbg = self.dram("dbg", [128, 8 * self.S], BF16, kind="ExternalOutput")
            self.P.dma("sp", dbg, self.B_t[:, 0:8 * self.S])
            self.P.barrier()
        self.phase_out(l)
        self.phase_moe(l, last)

    def load_xt_from_x(self):
        P, C = self.P, self.C
        C.reset()
        xt = [C.f32(128, D) for _ in range(3)]
        xb = [Buf() for _ in range(3)]
        x = self.I["x"]
        for t in range(self.NT):
            j = t % 3
            P.dma("sp", xt[j], x[t * 128:(t + 1) * 128, :], writes=[xb[j]])
            self.transpose_to_xt(xt[j], xb[j], t, bank0=(t % 2) * 2)
        P.barrier()

    def transpose_to_xt(self, src, srcb, t, bank0, scale=None):
        P = self.P
        q = t // 4
        for half in range(2):
            pi = bank0 + half
            for k4 in range(4):
                k = half * 4 + k4
                self.tr(self.PS[pi][:, k4 * 128:(k4 + 1) * 128], src[:, k * 128:(k + 1) * 128], self.identF,
                        [srcb, self.cB], [self.PB[pi]])
            s_ = self.PS[pi][:].rearrange("p (k s) -> p k s", s=128)
            d_ = self.XT[:, half * 4:half * 4 + 4, t * 128:(t + 1) * 128]
            P.op("act", lambda e, d_=d_, s_=s_: e.copy(d_, s_), [self.PB[pi]], [self.XTb[q]])

    def ln_tile(self, v, vb, gbc, bbc, gbb, scr, j):
        P = self.P
        st_, mv, rs = scr["st"][j], scr["mv"][j], scr["rs"][j][:, 0:1]
        sb_ = scr["b"][j]
        P.op("dve", lambda e: e.bn_stats(st_[:, 0, :], v[:, 0:512]), [vb], [sb_])
        P.op("dve", lambda e: e.bn_stats(st_[:, 1, :], v[:, 512:1024]), [vb], [sb_])
        P.op("dve", lambda e: e.bn_aggr(mv, st_[:].rearrange("p a b -> p (a b)")), [sb_], [sb_])
        P.op("act", lambda e: e.activation(rs, mv[:, 1:2], AF.Sqrt, bias=self.eps_t[:, 0:1], scale=1.0), [sb_, self.cB], [sb_])
        P.op("dve", lambda e: e.reciprocal(rs, rs), [sb_], [sb_])
        P.op("dve", lambda e: e.tensor_scalar(v, v, mv[:, 0:1], rs, ALU.subtract, ALU.mult), [vb, sb_], [vb])
        P.op("pool", lambda e: e.tensor_tensor(v, v, gbc, ALU.mult), [vb, gbb], [vb])
        P.op("pool", lambda e: e.tensor_tensor(v, v, bbc, ALU.add), [vb, gbb], [vb])

    def ln_scratch(self, n=2):
        C = self.C
        return {"st": [C.f32(128, 2, 6) for _ in range(n)], "mv": [C.f32(128, 2) for _ in range(n)],
                "rs": [C.f32(128, 2) for _ in range(n)], "b": [Buf() for _ in range(n)]}

    def phase_out(self, l):
        P, C, I = self.P, self.C, self.I
        C.reset()
        Wo = C.bf(128, 8, D); wob = Buf()
        P.dma("pool", Wo, I[f"wo{l}"].rearrange("(k p) n -> p k n", p=128), writes=[wob])
        gbc = C.f32(128, D); bbc = C.f32(128, D); gbb = Buf()
        P.dma("sp", gbc, I[f"lng{l}"][0:1, :].broadcast_to([128, D]), writes=[gbb])
        P.dma("sp", bbc, I[f"lnb{l}"][0:1, :].broadcast_to([128, D]), writes=[gbb])
        scr = self.ln_scratch()
        NB = 3
        xo = [C.f32(128, D) for _ in range(NB)]
        xb = [Buf() for _ in range(NB)]

        def load(t):
            P.dma("sp", xo[t % NB], self.xsrc[t * 128:(t + 1) * 128, :], reads=[self.xres_b[t]], writes=[xb[t % NB]])
        load(0)
        for t in range(self.NT):
            if t + 1 < self.NT:
                load(t + 1)
            j = t % NB
            q = t // 4
            for half in range(2):
                pi = (t % 2) * 2 + half
                for k in range(8):
                    self.mm(self.PS[pi][:], self.CT[:, k, t * 128:(t + 1) * 128], Wo[:, k, half * 512:(half + 1) * 512],
                            k == 0, k == 7, [self.CTb[k][q], wob], [self.PB[pi]])
                xs = xo[j][:, half * 512:(half + 1) * 512]
                P.op("dve", lambda e, xs=xs, pi=pi: e.scalar_tensor_tensor(xs, xs, ALPHA, self.PS[pi][:], ALU.mult, ALU.add),
                     [xb[j], self.PB[pi]], [xb[j]])
            self.ln_tile(xo[j], xb[j], gbc, bbc, gbb, scr, t % 2)
            P.dma("pool", self.xres[t * 128:(t + 1) * 128, :], xo[j], reads=[xb[j]], writes=[self.xres_b[t]])
            self.transpose_to_xt(xo[j], xb[j], t, bank0=4 + (t % 2) * 2)
        P.barrier()

    def phase_moe(self, l, last):
        P, C, I, S = self.P, self.C, self.I, self.S
        C.reset()
        NT, SC = self.NT, self.SC
        nsc = S // SC
        tps = SC // 128
        cps = SC // 512
        self.router(l)
        C.reset()
        gbc = C.f32(128, D); bbc = C.f32(128, D); gbb = Buf()
        P.dma("sp", gbc, I[f"lng{l}"][1:2, :].broadcast_to([128, D]), writes=[gbb])
        P.dma("sp", bbc, I[f"lnb{l}"][1:2, :].broadcast_to([128, D]), writes=[gbb])
        scr = self.ln_scratch()
        wg = [C.bf(128, 8, 512) for _ in range(2)]; wgb = [Buf() for _ in range(2)]
        wu = [C.bf(128, 8, 512) for _ in range(2)]; wub = [Buf() for _ in range(2)]
        wd = [C.bf(128, 4, D) for _ in range(1)]; wdb = [Buf() for _ in range(1)]
        hd = [C.bf(128, 4, 512) for _ in range(2)]; hdb = [Buf() for _ in range(2)]
        sg = [C.bf(128, 512) for _ in range(2)]; sgb = [Buf() for _ in range(2)]
        xo = [C.f32(128, D) for _ in range(2)]; xb = [Buf() for _ in range(2)]
        facc = self.B_t[:].bitcast(F32).rearrange("p (t d) -> p t d", d=D)
        fab = [Buf() for _ in range(tps)]

        def fa_bufs(tl):
            lo, hi = 2048 * tl, 2048 * tl + 2048
            res = [fab[tl]]
            for k in range(lo // S, (hi - 1) // S + 1):
                s0 = max(lo, k * S) - k * S
                s1 = min(hi, (k + 1) * S) - k * S
                for q in range(s0 // 512, (s1 - 1) // 512 + 1):
                    res.append(self.CTb[k][q])
            return res

        wgv = I[f"wg{l}"]; wuv = I[f"wu{l}"]; wdv = I[f"wd{l}"]

        def load_w(e):
            P.dma("pool", wg[e % 2], wgv[e].rearrange("(k p) n -> p k n", p=128), writes=[wgb[e % 2]])
            P.dma("pool", wu[e % 2], wuv[e].rearrange("(k p) n -> p k n", p=128), writes=[wub[e % 2]])

        def load_wd(e):
            P.dma("pool", wd[0], wdv[e].rearrange("(k p) n -> p k n", p=128), writes=[wdb[0]])

        for sc in range(nsc):
            tok0 = sc * SC
            load_w(0)
            load_wd(0)
            for e in range(16):
                if e + 1 < 16:
                    load_w(e + 1)
                for c in range(cps):
                    q = (tok0 // 512) + c
                    hb = hd[c % 2]
                    for f in range(4):
                        pg, pu = (f % 2) * 2, (f % 2) * 2 + 1
                        for k in range(8):
                            self.mm(self.PS[pg][:], wg[e % 2][:, k, f * 128:(f + 1) * 128], self.XT[:, k, q * 512:(q + 1) * 512],
                                    k == 0, k == 7, [wgb[e % 2], self.XTb[q]], [self.PB[pg]])
                        for k in range(8):
                            self.mm(self.PS[pu][:], wu[e % 2][:, k, f * 128:(f + 1) * 128], self.XT[:, k, q * 512:(q + 1) * 512],
                                    k == 0, k == 7, [wub[e % 2], self.XTb[q]], [self.PB[pu]])
                        s_ = sg[f % 2]
                        self.act(s_, self.PS[pg][:], AF.Silu, [self.PB[pg]], [sgb[f % 2]])
                        P.op("dve", lambda e_, f=f, s_=s_, pu=pu, hb=hb: e_.tensor_tensor(hb[:, f, :], s_, self.PS[pu][:], ALU.mult),
                             [sgb[f % 2], self.PB[pu]], [hdb[c % 2]])
                    for ts in range(4):
                        tl = c * 4 + ts
                        tg = tok0 // 128 + tl
                        for half in range(2):
                            po = 4 + ((ts * 2 + half) % 4)
                            for f in range(4):
                                self.mm(self.PS[po][:], hb[:, f, ts * 128:(ts + 1) * 128], wd[0][:, f, half * 512:(half + 1) * 512],
                                        f == 0, f == 3, [hdb[c % 2], wdb[0]], [self.PB[po]])
                            fa = facc[:, tl, half * 512:(half + 1) * 512]
                            gsc = self.gates[:, tg, e:e + 1]
                            if e == 0:
                                P.op("dve", lambda e_, fa=fa, po=po, gsc=gsc: e_.tensor_scalar(fa, self.PS[po][:], gsc, None, ALU.mult),
                                     [self.PB[po], self.gates_b[tg]], fa_bufs(tl))
                            else:
                                P.op("dve", lambda e_, fa=fa, po=po, gsc=gsc: e_.scalar_tensor_tensor(fa, self.PS[po][:], gsc, fa, ALU.mult, ALU.add),
                                     [self.PB[po], self.gates_b[tg]], fa_bufs(tl))
                if e + 1 < 16:
                    load_wd(e + 1)
            dst = self.out if last else self.xres

            def load(tl):
                tg = tok0 // 128 + tl
                P.dma("sp", xo[tl % 2], self.xres[tg * 128:(tg + 1) * 128, :], reads=[self.xres_b[tg]], writes=[xb[tl % 2]])
            load(0)
            for tl in range(tps):
                if tl + 1 < tps:
                    load(tl + 1)
                tg = tok0 // 128 + tl
                j = tl % 2
                P.op("dve", lambda e_, j=j, tl=tl: e_.scalar_tensor_tensor(xo[j], xo[j], ALPHA, facc[:, tl, :], ALU.mult, ALU.add),
                     [xb[j]] + fa_bufs(tl), [xb[j]])
                self.ln_tile(xo[j], xb[j], gbc, bbc, gbb, scr, j)
                P.dma("pool", dst[tg * 128:(tg + 1) * 128, :], xo[j], reads=[xb[j]], writes=[self.xres_b[tg]])
                if not last:
                    self.transpose_to_xt(xo[j], xb[j], tg, bank0=(tl % 2) * 2)
        P.barrier()

    def router(self, l):
        P, C, I = self.P, self.C, self.I
        NT = self.NT
        C.reset()
        Wr = C.bf(128, 8, 20); wrb = Buf()
        P.dma("pool", Wr, I[f"wr{l}"].rearrange("(k p) n -> p k n", p=128), writes=[wrb])
        brc = C.f32(128, 20)
        P.dma("sp", brc, I[f"br{l}"][0:1, :].broadcast_to([128, 20]), writes=[wrb])
        lg = C.f32(128, NT, 20); rb = Buf()
        for t in range(NT):
            q = t // 4
            pi = t % 4
            lgp = self.PS[pi][:, 0:20]
            for k in range(8):
                self.mm(lgp, self.XT[:, k, t * 128:(t + 1) * 128], Wr[:, k, :], k == 0, k == 7,
                        [self.XTb[q], wrb], [self.PB[pi]])
            P.op("dve", lambda e, t=t, lgp=lgp: e.tensor_tensor(lg[:, t, :], lgp, brc, ALU.add), [self.PB[pi], wrb], [rb])
        lgg = lg[:, :, 0:4]
        le = lg[:, :, 4:20].rearrange("p t (g j) -> p t g j", j=4)
        m = C.f32(128, NT); oh = C.f32(128, NT, 4); eg = C.f32(128, NT, 4); gs = C.f32(128, NT)
        m1 = C.f32(128, NT, 4); is1 = C.f32(128, NT, 4, 4); le2 = C.f32(128, NT, 4, 4); m2 = C.f32(128, NT, 4)
        sel2 = C.f32(128, NT, 4, 4); ee = C.f32(128, NT, 4, 4); ss = C.f32(128, NT, 4); w = C.f32(128, NT, 4)
        bc3 = lambda a: a.unsqueeze(2).broadcast_to([128, NT, 4])
        bc4 = lambda a: a.unsqueeze(3).broadcast_to([128, NT, 4, 4])
        R = [rb]
        dv = lambda fn: P.op("dve", fn, R, R)
        dv(lambda e: e.tensor_reduce(m, lgg, AX.X, ALU.max))
        dv(lambda e: e.tensor_tensor(oh, lgg, bc3(m), ALU.is_equal))
        dv(lambda e: e.tensor_tensor(eg, lgg, bc3(m), ALU.subtract))
        P.op("act", lambda e: e.activation(eg, eg, AF.Exp), R, R)
        dv(lambda e: e.tensor_reduce(gs, eg, AX.X, ALU.add))
        dv(lambda e: e.reciprocal(gs, gs))
        dv(lambda e: e.tensor_reduce(m1, le, AX.X, ALU.max))
        dv(lambda e: e.tensor_tensor(is1, le, bc4(m1), ALU.is_equal))
        dv(lambda e: e.scalar_tensor_tensor(le2, is1, -1.0e9, le, ALU.mult, ALU.add))
        dv(lambda e: e.tensor_reduce(m2, le2, AX.X, ALU.max))
        dv(lambda e: e.tensor_tensor(sel2, le, bc4(m2), ALU.is_ge))
        dv(lambda e: e.tensor_tensor(ee, le, bc4(m1), ALU.subtract))
        P.op("act", lambda e: e.activation(ee, ee, AF.Exp), R, R)
        dv(lambda e: e.tensor_tensor(ee, ee, sel2, ALU.mult))
        dv(lambda e: e.tensor_reduce(ss, ee, AX.X, ALU.add))
        dv(lambda e: e.reciprocal(ss, ss))
        dv(lambda e: e.tensor_tensor(w, ss, oh, ALU.mult))
        dv(lambda e: e.tensor_tensor(w, w, bc3(gs), ALU.mult))
        gv = self.gates[:].rearrange("p t (g j) -> p t g j", j=4)
        P.op("dve", lambda e: e.tensor_tensor(gv, ee, bc4(w), ALU.mult), R, R + self.gates_b)
        P.barrier()

    def run_pipeline(self, pairs, new_S, new_E, depth=2):
        n = len(pairs)
        slots = [None] * n

        def do_score(i):
            slots[i] = new_S()
            pairs[i]["score"](slots[i])
        for i in range(min(depth, n)):
            do_score(i)
        for i in range(n):
            Ei, Ebi = new_E()
            pairs[i]["post"](slots[i], Ei, Ebi)
            if i + depth < n:
                do_score(i + depth)
            pairs[i]["pv"](Ei, Ebi)
            if pairs[i].get("after"):
                pairs[i]["after"]()

    def proj_chunk(self, pi, M, Wb, wbuf, col0, tq):
        for k in range(8):
            self.mm(self.PS[pi][0:M, :], Wb[:, k, col0:col0 + M], self.XT[:, k, tq * 512:(tq + 1) * 512],
                    k == 0, k == 7, [wbuf, self.XTb[tq]], [self.PB[pi]])

    def proj_xq(self, win, col0):
        P, C = self.P, self.C
        C.reset()
        stg = [C.bf(128, 512) for _ in range(2)]; stgb = [Buf() for _ in range(2)]
        Wx = C.bf(128, 8, 256); Wxb = Buf()
        P.dma("pool", Wx, win[:, :, col0:col0 + 256], writes=[Wxb])
        ns = 0
        for hh in range(2):
            for tq in range(self.NQ):
                pi = ns % 2
                self.proj_chunk(pi, 128, Wx, Wxb, hh * 128, tq)
                sg_ = stg[ns % 2]; sb_ = stgb[ns % 2]; ns += 1
                P.op("act", lambda e, sg_=sg_, pi=pi: e.copy(sg_, self.PS[pi][:]), [self.PB[pi]], [sb_])
                P.dma("sp", self.pT[2048 + hh * 128:2048 + (hh + 1) * 128, tq * 512:(tq + 1) * 512], sg_, reads=[sb_], writes=[self.pT_b])
        P.barrier()
        C.reset()

    def rope_tables(self, tq, npart, inv, posb_t, posf, kf, ki, Ct, St, tb, pos_src=None, width=512):
        P = self.P
        src = pos_src if pos_src is not None else self.I["pos"][0:1, tq * 512:(tq + 1) * 512]
        P.dma("sp", posb_t, src.broadcast_to([npart, width]), writes=[tb])
        P.op("dve", lambda e: e.tensor_copy(posf, posb_t), [tb], [tb])
        P.op("dve", lambda e: e.tensor_scalar(posf, posf, inv, None, ALU.mult), [tb, self.cB], [tb])
        TWO_PI = 2.0 * math.pi
        C1 = 6.28125
        C2 = TWO_PI - C1
        for out_t, shift in ((St, 0.0), (Ct, math.pi / 2)):
            P.op("dve", lambda e, shift=shift: e.tensor_scalar(kf, posf, shift, 1.0 / TWO_PI, ALU.add, ALU.mult), [tb], [tb])
            P.op("dve", lambda e: e.tensor_copy(ki, kf), [tb], [tb])
            P.op("dve", lambda e: e.tensor_copy(kf, ki), [tb], [tb])
            P.op("dve", lambda e, out_t=out_t, shift=shift: e.tensor_scalar(out_t, posf, shift, None, ALU.add), [tb], [tb])
            P.op("dve", lambda e, out_t=out_t: e.scalar_tensor_tensor(out_t, kf, -C1, out_t, ALU.mult, ALU.add), [tb], [tb])
            P.op("dve", lambda e, out_t=out_t: e.scalar_tensor_tensor(out_t, kf, -C2, out_t, ALU.mult, ALU.add), [tb], [tb])
            P.op("dve", lambda e, out_t=out_t: e.tensor_scalar(kf, out_t, math.pi, -TWO_PI, ALU.is_gt, ALU.mult), [tb], [tb])
            P.op("dve", lambda e, out_t=out_t: e.tensor_tensor(out_t, out_t, kf, ALU.add), [tb], [tb])
            P.op("dve", lambda e, out_t=out_t: e.tensor_scalar(kf, out_t, -math.pi, TWO_PI, ALU.is_lt, ALU.mult), [tb], [tb])
            P.op("dve", lambda e, out_t=out_t: e.tensor_tensor(out_t, out_t, kf, ALU.add), [tb], [tb])
            P.op("dve", lambda e, out_t=out_t: e.tensor_scalar(out_t, out_t, 3.1415925, -3.1415925, ALU.min, ALU.max), [tb], [tb])
            P.op("act", lambda e, out_t=out_t: e.activation(out_t, out_t, AF.Sin), [tb], [tb])

    def xattn(self, l):
        P, C, I, S = self.P, self.C, self.I, self.S
        C.reset()
        Wkv = C.bf(128, 8, 512); wb = Buf()
        P.dma("pool", Wkv, I[f"wkv{l}"].rearrange("(k p) n -> p k n", p=128), writes=[wb])
        mkT = C.bf(64, 4, 256); mkb = Buf()
        mv = C.bf(128, 2, 4, 128); mvb = Buf()
        xq = [C.bf(64, S) for _ in range(2)]; xqb = [Buf() for _ in range(2)]
        E = [C.bf(128, 512) for _ in range(3)]; Eb = [Buf() for _ in range(3)]
        rd = [C.f32(64, 512) for _ in range(2)]; rdb = [Buf() for _ in range(2)]
        P.op("pool", lambda e: e.memset(mv, 1.0), [], [mvb])
        for h in range(4):
            pi = h % 2
            for k in range(8):
                self.mm(self.PS[pi][0:64, 0:256], Wkv[:, k, h * 64:(h + 1) * 64], self.memT[:, k, :], k == 0, k == 7,
                        [wb, self.memT_b], [self.PB[pi]])
            P.op("act", lambda e, h=h, pi=pi: e.copy(mkT[:, h, :], self.PS[pi][0:64, 0:256]), [self.PB[pi]], [mkb])
        for mt in range(2):
            pi = 2 + mt
            for k in range(8):
                self.mm(self.PS[pi][:, 0:256], self.memT[:, k, mt * 128:(mt + 1) * 128], Wkv[:, k, 256:512], k == 0, k == 7,
                        [wb, self.memT_b], [self.PB[pi]])
            src = self.PS[pi][:, 0:256].rearrange("p (h d) -> p h d", d=64)
            P.op("act", lambda e, mt=mt, src=src: e.copy(mv[:, mt, :, 0:64], src), [self.PB[pi]], [mvb])
        ne = 0
        for h in range(4):
            xh = xq[h % 2]; xhb = xqb[h % 2]
            P.dma("sp", xh, self.pT[2048 + 64 * h:2048 + 64 * (h + 1), :], reads=[self.pT_b], writes=[xhb])
            for tq in range(self.NQ):
                po = 4 + (tq % 2)
                for mt in range(2):
                    ps = mt + 2 * (tq % 2)
                    self.mm(self.PS[ps][:], mkT[:, h, mt * 128:(mt + 1) * 128], xh[:, tq * 512:(tq + 1) * 512], True, True,
                            [mkb, xhb], [self.PB[ps]])
                    Ei = E[ne % 3]; Ebi = Eb[ne % 3]; ne += 1
                    self.act(Ei, self.PS[ps][:], AF.Exp, [self.PB[ps]], [Ebi], scale=0.125)
                    self.mm(self.PS[po][:], mv[:, mt, h, :], Ei, mt == 0, mt == 1, [mvb, Ebi], [self.PB[po]])
                r = rd[tq % 2]; rb = rdb[tq % 2]
                P.op("dve", lambda e, r=r, po=po: e.reciprocal(r, self.PS[po][64:128, :]), [self.PB[po]], [rb])
                dst = self.CT[64 * (h % 2):64 * (h % 2) + 64, 6 + h // 2, tq * 512:(tq + 1) * 512]
                P.op("dve", lambda e, r=r, po=po, dst=dst: e.tensor_tensor(dst, self.PS[po][0:64, :], r, ALU.mult),
                     [self.PB[po], rb], [self.CTb[6 + h // 2][tq]])
        P.barrier()

    def mixer_conv(self, l):
        P, C, I, S = self.P, self.C, self.I, self.S
        NQ = self.NQ
        win = I[f"win{l}"].rearrange("(k p) n -> p k n", p=128)
        self.proj_xq(win, 1536)
        UW = 30 + S
        U = C.bf(128, 6, UW); Ub = [Buf() for _ in range(6)]
        cb = C.f32(128, 12); cdw = C.f32(128, 186); cdb = C.f32(128, 6); clg = C.f32(128, 6); clb = C.f32(128, 6)
        pb = Buf()
        for t_, n_ in ((cb, "cbin"), (cdw, "cdw"), (cdb, "cdb"), (clg, "clg"), (clb, "clb")):
            P.dma("sp", t_, I[f"{n_}{l}"], writes=[pb])
        mark = C.off
        Wc = [C.bf(128, 8, 256) for _ in range(2)]; Wcb = [Buf() for _ in range(2)]
        sig = [C.f32(128, 512) for _ in range(2)]; sigb = [Buf() for _ in range(2)]
        for c in range(6):
            P.op("pool", lambda e, c=c: e.memset(U[:, c, 0:30], 0.0), [], [Ub[c]])
        for c in range(6):
            W_ = Wc[c % 2]; Wb_ = Wcb[c % 2]
            P.dma("pool", W_[:, :, 0:128], win[:, :, c * 128:(c + 1) * 128], writes=[Wb_])
            P.dma("pool", W_[:, :, 128:256], win[:, :, 768 + c * 128:768 + (c + 1) * 128], writes=[Wb_])
            for tq in range(NQ):
                p1, p2 = 2 + (tq % 2) * 2, 3 + (tq % 2) * 2
                self.proj_chunk(p1, 128, W_, Wb_, 0, tq)
                self.proj_chunk(p2, 128, W_, Wb_, 128, tq)
                sg_ = sig[tq % 2]; sb_ = sigb[tq % 2]
                self.act(sg_, self.PS[p2][:], AF.Sigmoid, [self.PB[p2], pb], [sb_], bias=cb[:, 6 + c:7 + c], scale=1.0)
                dst = U[:, c, 30 + tq * 512:30 + (tq + 1) * 512]
                P.op("dve", lambda e, dst=dst, p1=p1, c=c, sg_=sg_: e.scalar_tensor_tensor(dst, self.PS[p1][:], cb[:, c:c + 1], sg_, ALU.add, ALU.mult),
                     [self.PB[p1], sb_, pb], [Ub[c]])
        P.barrier()
        C.off = mark
        diag = self.A_t[:, 0:186 * 128].rearrange("p (j m) -> p j m", m=128)
        dgb = Buf()
        for idx in range(186):
            eng = "dve" if idx % 2 == 0 else "pool"
            P.op(eng, lambda e, idx=idx: e.tensor_scalar(diag[:, idx, :], self.identB, cdw[:, idx:idx + 1], None, ALU.mult),
                 [self.cB, pb] , [dgb] + self.XTb)
        ysb = [C.f32(128, 512) for _ in range(2)]; ysbb = [Buf() for _ in range(2)]
        ysq = [C.f32(128, 512) for _ in range(2)]; ysqb = [Buf() for _ in range(2)]
        mean = C.f32(128, 512); rstd = C.f32(128, 512); msq = C.f32(128, 512); stb = Buf()
        tmp = [C.f32(128, 512) for _ in range(2)]; tmpb = [Buf() for _ in range(2)]
        n2 = 0
        for tq in range(NQ):
            for c in range(6):
                for j in range(31):
                    self.mm(self.PS[c][:], diag[:, c * 31 + j, :], U[:, c, tq * 512 + j:tq * 512 + j + 512], j == 0, j == 30,
                            [dgb, Ub[c]], [self.PB[c]])
                y_ = ysb[n2 % 2]; yb_ = ysbb[n2 % 2]; q_ = ysq[n2 % 2]; qb_ = ysqb[n2 % 2]; n2 += 1
                self.act(y_, self.PS[c][:], AF.Identity, [self.PB[c], pb], [yb_], bias=cdb[:, c:c + 1], scale=1.0)
                P.op("pool", lambda e, y_=y_, q_=q_: e.tensor_tensor(q_, y_, y_, ALU.mult), [yb_], [qb_])
                self.mm(self.PS[6][:], self.onesF, y_, c == 0, c == 5, [yb_, self.cB], [self.PB[6]])
                self.mm(self.PS[7][:], self.onesF, q_, c == 0, c == 5, [qb_, self.cB], [self.PB[7]])
            P.op("dve", lambda e: e.tensor_scalar(mean, self.PS[6][:], 1.0 / 768, None, ALU.mult), [self.PB[6]], [stb])
            P.op("dve", lambda e: e.tensor_tensor(msq, mean, mean, ALU.mult), [stb], [stb])
            P.op("dve", lambda e: e.scalar_tensor_tensor(rstd, self.PS[7][:], 1.0 / 768, msq, ALU.mult, ALU.subtract), [self.PB[7], stb], [stb])
            P.op("act", lambda e: e.activation(rstd, rstd, AF.Sqrt, bias=self.eps_t[:, 0:1], scale=1.0), [stb, self.cB], [stb])
            P.op("dve", lambda e: e.reciprocal(rstd, rstd), [stb], [stb])
            for c in range(6):
                t_ = tmp[c % 2]; tb_ = tmpb[c % 2]
                P.op("dve", lambda e, t_=t_, c=c: e.scalar_tensor_tensor(t_, self.PS[c][:], cdb[:, c:c + 1], mean, ALU.add, ALU.subtract),
                     [self.PB[c], stb, pb], [tb_])
                P.op("dve", lambda e, t_=t_: e.tensor_tensor(t_, t_, rstd, ALU.mult), [tb_, stb], [tb_])
                dst = self.CT[:, c, tq * 512:(tq + 1) * 512]
                self.act(dst, t_, AF.Silu, [tb_, pb], [self.CTb[c][tq]], bias=clb[:, c:c + 1], scale=clg[:, c:c + 1])
        P.barrier()
        self.xattn(l)


def _consts(S):
    c = {}
    c["c_ident"] = np.eye(128, dtype=np.float32)
    cr = np.zeros((128, 4 * 128 + 4), np.float32)
    inv_n = (np.float32(500000.0) ** (-np.arange(0, 16, 2, dtype=np.float32) / np.float32(16))).astype(np.float32)
    inv_m = (np.float32(10000.0) ** (-np.arange(0, 32, 2, dtype=np.float32) / np.float32(32))).astype(np.float32)
    for base in (0, 64):
        for i in range(8):
            cr[base + i + 8, base + i] = -1.0
            cr[base + i, base + i + 8] = 1.0
            cr[base + i, 512] = inv_n[i]
            cr[base + i + 8, 512] = inv_n[i]
    for i in range(64, 80):
        cr[i + 16, 128 + i] = -1.0
        cr[i, 128 + i + 16] = 1.0
        cr[i, 513] = inv_m[i - 64]
        cr[i + 16, 513] = inv_m[i - 64]
    c["c_rope"] = cr
    eb = np.zeros((64, S), np.float32)
    for key in range(S):
        eb[(key // 64) % 64, key] = 1.0
    c["c_ebig"] = eb
    ov = np.zeros((256, 64), np.float32)
    ncmp = (S - 32) // 16 + 1
    for n in range(ncmp):
        for j in range(S // 64):
            if 16 * n < 64 * j + 64 and 16 * n + 32 > 64 * j:
                ov[n, j] = 1.0
    c["c_ov"] = ov
    fb = np.zeros((S, 64), np.float32)
    for t in range(S):
        cur = t // 64
        fb[t, cur + 1:] = -1.0e9
        fb[t, cur] = 1.0e9
        if cur >= 1:
            fb[t, cur - 1] = 2.0e9
        fb[t, 0] = 3.0e9
    c["c_fb"] = fb
    sel = np.zeros((3, 3 * 64), np.float32)
    for r in range(3):
        sel[r, r * 64:(r + 1) * 64] = 1.0
    c["c_sel"] = sel
    return c


def layer_inputs(l, kind, j, w):
    f = lambda a: np.ascontiguousarray(a, dtype=np.float32)
    d = {}
    d[f"wkv{l}"] = f(w["mem_w_kv"][l]); d[f"wo{l}"] = f(w["w_out"][l])
    d[f"lng{l}"] = f(w["ln_g"][l]); d[f"lnb{l}"] = f(w["ln_b"][l])
    d[f"wr{l}"] = f(np.concatenate([w["moe_w_grp"][l], w["moe_w_exp"][l]], axis=1))
    d[f"br{l}"] = f(np.concatenate([w["moe_b_grp"][l], w["moe_b_exp"][l]])[None, :])
    d[f"wg{l}"] = f(w["moe_w_gate"][l]); d[f"wu{l}"] = f(w["moe_w_up"][l]); d[f"wd{l}"] = f(w["moe_w_down"][l])
    if kind == 2:
        d[f"win{l}"] = f(w["conv_w_in"][j])
        d[f"cbin{l}"] = f(w["conv_b_in"][j].reshape(12, 128).T)
        d[f"cdw{l}"] = f(w["conv_dw_w"][j].T.reshape(6, 128, 31).transpose(1, 0, 2).reshape(128, 186))
        d[f"cdb{l}"] = f(w["conv_dw_b"][j].reshape(6, 128).T)
        d[f"clg{l}"] = f(w["conv_ln_g"][j].reshape(6, 128).T)
        d[f"clb{l}"] = f(w["conv_ln_b"][j].reshape(6, 128).T)
    elif kind == 1:
        d[f"win{l}"] = f(w["mla_w_in"][j])
        d[f"qn{l}"] = f(w["mla_q_norm"][j].reshape(2, 128).T); d[f"kvn{l}"] = f(w["mla_kv_norm"][j][:, None])
        d[f"wuq{l}"] = f(w["mla_w_uq"][j]); d[f"wukv{l}"] = f(w["mla_w_ukv"][j])
    else:
        d[f"win{l}"] = f(w["nsa_w_in"][j])
        d[f"cpe{l}"] = f(np.transpose(w["nsa_cmp_pe"][j], (0, 2, 1)))
        d[f"cw1{l}"] = f(w["nsa_cmp_w1"][j].reshape(2, 32, 64, 128).transpose(0, 2, 1, 3))
        d[f"cw2{l}"] = f(w["nsa_cmp_w2"][j])
    return d


_NC_CACHE = {}


def run_model(S, kinds, js, x, mem, positions, w, n_cores=NCORES):
    key = (S, tuple(kinds))
    if key not in _NC_CACHE:
        _NC_CACHE[key] = Builder(S, kinds).build()
    nc = _NC_CACHE[key]
    shared = _consts(S)
    for l, (kind, j) in enumerate(zip(kinds, js)):
        shared.update(layer_inputs(l, kind, j, w))
    in_maps = []
    for b in range(n_cores):
        m = dict(shared)
        m["x"] = np.ascontiguousarray(x[b], dtype=np.float32)
        m["mem"] = np.ascontiguousarray(mem[b], dtype=np.float32)
        m["pos"] = np.ascontiguousarray(positions[b][None, :], dtype=np.int32)
        pe = np.asarray(positions[b])[31::16]
        pe = np.concatenate([pe, np.repeat(pe[-1:], 256 - len(pe))])[:256]
        m["pos_end"] = np.ascontiguousarray(pe[None, :], dtype=np.int32)
        in_maps.append(m)
    import os
    if os.environ.get("K_TRACE"):
        res = run_bass_kernel_spmd(nc, in_maps, core_ids=list(range(n_cores)), trace=True)
        print("EXEC_TIME_NS", res.exec_time_ns)
    else:
        res = run_bass_kernel_spmd(nc, in_maps, core_ids=list(range(n_cores)))
    import os
    if os.environ.get("DBG_CT"):
        np.save("dbg_ct.npy", np.asarray(res.results[0]["dbg"]).astype(np.float32))
    if os.environ.get("DBG_GL"):
        a = np.asarray(res.results[0]["glT"]); print("glT", a.dtype, a.shape); np.save("d_glT.npy", a)
    if os.environ.get("DBG_NSA"):
        for nme in res.results[0]:
            if nme.startswith("d_"):
                a = np.asarray(res.results[0][nme])
                print(nme, a.dtype, a.shape)
                np.save(nme + ".npy", a.astype(np.float32))
    return np.stack([np.asarray(r["out"]) for r in res.results], axis=0)


def kernel(**inputs):
    x = np.asarray(inputs["x"]); mem = np.asarray(inputs["mem"]); positions = np.asarray(inputs["positions"])
    w = {k: np.asarray(v) for k, v in inputs.items() if k not in ("x", "mem", "positions")}
    kinds = [0, 1, 2, 0]
    js = [0, 0, 0, 1]
    out = run_model(x.shape[1], kinds, js, x, mem, positions, w)
    return out.astype(np.float32)


def _mixer_mla(self, l):
    P, C, I, S = self.P, self.C, self.I, self.S
    NT, NQ = self.NT, self.NQ
    win = I[f"win{l}"].rearrange("(k p) n -> p k n", p=128)
    self.proj_xq(win, 416)
    stg = [C.bf(128, 512) for _ in range(2)]; stgb = [Buf() for _ in range(2)]
    qn = C.f32(128, 2); kvn = C.f32(128, 2); nb = Buf()
    P.dma("sp", qn, I[f"qn{l}"], writes=[nb]); P.dma("sp", kvn[:, 0:1], I[f"kvn{l}"], writes=[nb])
    CQ = C.bf(128, 2, S); CKV = C.bf(128, S); KRr = C.bf(96, S)
    cqb = [Buf() for _ in range(NQ)]; krb = Buf()
    P.op("pool", lambda e: e.memset(KRr[0:64, :], 0.0), [], [krb])
    mark = C.off
    Win = C.bf(128, 8, 416); winb = Buf()
    P.dma("pool", Win, win[:, :, 0:416], writes=[winb])
    tk = [C.f32(128, 416) for _ in range(2)]; tkb = [Buf() for _ in range(2)]
    junk = C.f32(128, 256); ssq = [C.f32(128, 4) for _ in range(2)]
    for t in range(NT):
        pi = t % 2
        j = t % 2
        for k in range(8):
            self.mm(self.PS[pi][:, 0:416], self.XT[:, k, t * 128:(t + 1) * 128], Win[:, k, :], k == 0, k == 7,
                    [winb, self.XTb[t // 4]], [self.PB[pi]])
        sq = ssq[j]
        self.act(junk, self.PS[pi][:, 0:256], AF.Square, [self.PB[pi]], [tkb[j]], accum_out=sq[:, 0:1])
        self.act(junk[:, 0:128], self.PS[pi][:, 256:384], AF.Square, [self.PB[pi]], [tkb[j]], accum_out=sq[:, 1:2])
        self.act(sq[:, 0:1], sq[:, 0:1], AF.Sqrt, [tkb[j], self.cB], [tkb[j]], bias=self.eps_t[:, 1:2], scale=1.0 / 256)
        self.act(sq[:, 1:2], sq[:, 1:2], AF.Sqrt, [tkb[j], self.cB], [tkb[j]], bias=self.eps_t[:, 1:2], scale=1.0 / 128)
        P.op("dve", lambda e, sq=sq: e.reciprocal(sq[:, 0:2], sq[:, 0:2]), [tkb[j]], [tkb[j]])
        P.op("dve", lambda e, j=j, pi=pi, sq=sq: e.tensor_scalar(tk[j][:, 0:256], self.PS[pi][:, 0:256], sq[:, 0:1], None, ALU.mult), [self.PB[pi], tkb[j]], [tkb[j]])
        P.op("dve", lambda e, j=j, pi=pi, sq=sq: e.tensor_scalar(tk[j][:, 256:384], self.PS[pi][:, 256:384], sq[:, 1:2], None, ALU.mult), [self.PB[pi], tkb[j]], [tkb[j]])
        P.op("dve", lambda e, j=j, pi=pi: e.tensor_copy(tk[j][:, 384:416], self.PS[pi][:, 384:416]), [self.PB[pi], tkb[j]], [tkb[j]])
        pt = 2 + (t % 2)
        for bi, (c0, c1) in enumerate(((0, 128), (128, 256), (256, 384), (320, 416))):
            self.tr(self.PS[pt][0:c1 - c0, bi * 128:(bi + 1) * 128], tk[j][:, c0:c1], self.identF, [tkb[j], self.cB], [self.PB[pt]])
        tsl = slice(t * 128, (t + 1) * 128)
        q = t // 4
        self.act(CQ[:, 0, tsl], self.PS[pt][:, 0:128], AF.Copy, [self.PB[pt], nb], [cqb[q]], scale=qn[:, 0:1])
        self.act(CQ[:, 1, tsl], self.PS[pt][:, 128:256], AF.Copy, [self.PB[pt], nb], [cqb[q]], scale=qn[:, 1:2])
        self.act(CKV[:, tsl], self.PS[pt][:, 256:384], AF.Copy, [self.PB[pt], nb], [cqb[q]], scale=kvn[:, 0:1])
        P.op("dve", lambda e, pt=pt, tsl=tsl: e.tensor_copy(KRr[64:96, tsl], self.PS[pt][64:96, 384:512]), [self.PB[pt]], [cqb[q], krb])
    P.barrier()
    import os
    STOP = int(os.environ.get("MLA_STOP", "9"))
    if STOP == 1:
        self.xattn(l); return
    C.off = mark
    Wuq = C.bf(128, 2, 1152); Wukv = C.bf(128, 1536); ub = Buf()
    P.dma("pool", Wuq, I[f"wuq{l}"].rearrange("(k p) n -> p k n", p=128), writes=[ub])
    P.dma("pool", Wukv, I[f"wukv{l}"], writes=[ub])
    posb_t = C.f32(96, 512, dt=I32); posf = C.f32(96, 512); kf = C.f32(96, 512); ki = posb_t
    Ct = C.f32(96, 512); St = C.f32(96, 512); tb = Buf()
    qraw = [C.bf(96, 512) for _ in range(2)]; qrb = [Buf() for _ in range(2)]
    t1 = [C.f32(96, 512) for _ in range(2)]; t2 = [C.f32(96, 512) for _ in range(2)]; t12b = [Buf() for _ in range(2)]
    qo = [C.bf(96, 512) for _ in range(2)]; qob = [Buf() for _ in range(2)]
    vst = [C.bf(128, 768) for _ in range(2)]; vstb = [Buf() for _ in range(2)]
    Rm = self.RmB[0:96, 0:96]
    n = 0
    for tq in range(NQ):
        csl = slice(tq * 512, (tq + 1) * 512)
        SK = os.environ.get("MLA_SKIP", "")
        if "r" not in SK:
            self.rope_tables(tq, 96, self.invF[0:96, 1:2], posb_t, posf, kf, ki, Ct, St, tb)
        j = n % 2; n += 1
        if "k" not in SK:
            self.mm(self.PS[0][0:96, :], Rm, KRr[:, csl], True, True, [self.cB, cqb[tq], krb], [self.PB[0]])
            P.op("dve", lambda e, j=j, csl=csl: e.tensor_tensor(t1[j], KRr[:, csl], Ct, ALU.mult), [cqb[tq], krb, tb], [t12b[j]])
            P.op("dve", lambda e, j=j: e.tensor_tensor(t2[j], self.PS[0][0:96, :], St, ALU.mult), [self.PB[0], tb], [t12b[j]])
            P.op("pool", lambda e, j=j: e.tensor_tensor(qo[j], t1[j], t2[j], ALU.add), [t12b[j]], [qob[j]])
            P.dma("sp", self.pT[1920:1952, csl], qo[j][64:96, :], reads=[qob[j]], writes=[self.pT_b])
        for h in range(int(os.environ.get("MLA_NH", "12")) if "q" not in SK else 0):
            pq, pr = 1 + (h % 2) * 2, 2 + (h % 2) * 2
            for rc in range(2):
                self.mm(self.PS[pq][0:96, :], Wuq[:, rc, h * 96:(h + 1) * 96], CQ[:, rc, csl], rc == 0, rc == 1, [ub, cqb[tq]], [self.PB[pq]])
            j = n % 2; n += 1
            if "a" in SK:
                continue
            self.act(qraw[j], self.PS[pq][0:96, :], AF.Copy, [self.PB[pq]], [qrb[j]])
            if "b" in SK:
                continue
            self.mm(self.PS[pr][0:96, :], Rm, qraw[j], True, True, [self.cB, qrb[j]], [self.PB[pr]])
            if "c" in SK:
                continue
            VAR = os.environ.get("MLA_VAR", "0")
            if VAR == "0":
                P.op("dve", lambda e, j=j, pq=pq: e.tensor_tensor(t1[j], self.PS[pq][0:96, :], Ct, ALU.mult), [self.PB[pq], tb], [t12b[j]])
            elif VAR == "1":
                P.op("dve", lambda e, j=j, pq=pq: e.tensor_tensor(t1[j], self.PS[pq][0:96, :], St, ALU.mult), [self.PB[pq], tb], [t12b[j]])
            elif VAR == "2":
                P.op("dve", lambda e, j=j, pq=pq: e.tensor_tensor(t1[j], self.PS[pq][0:96, :], Ct, ALU.mult), [self.PB[pq], tb, qrb[j]], [t12b[j]])
            elif VAR == "3":
                P.op("dve", lambda e, j=j, pq=pq: e.tensor_tensor(t1[j], qraw[j], Ct, ALU.mult), [qrb[j], tb], [t12b[j]])
            if "e" in SK:
                continue
            P.op("dve", lambda e, j=j, pr=pr: e.tensor_tensor(t2[j], self.PS[pr][0:96, :], St, ALU.mult), [self.PB[pr], tb], [t12b[j]])
            if "f" in SK:
                continue
            P.op("dve" if "p" in SK else "pool", lambda e, j=j: e.tensor_tensor(qo[j], t1[j], t2[j], ALU.add), [t12b[j]], [qob[j]])
            if "d" not in SK:
                P.dma("sp", self.pT[h * 96:(h + 1) * 96, csl], qo[j], reads=[qob[j]], writes=[self.pT_b])
            if "n" in SK:
                continue
            pk = 5 + (h % 2)
            self.mm(self.PS[pk][0:64, :], Wukv[:, h * 128:h * 128 + 64], CKV[:, csl], True, True, [ub, cqb[tq]], [self.PB[pk]])
            sj = stg[h % 2]; sjb = stgb[h % 2]
            self.act(sj[0:64, :], self.PS[pk][0:64, :], AF.Copy, [self.PB[pk]], [sjb])
            P.dma("sp", self.pT[1152 + h * 64:1152 + (h + 1) * 64, csl], sj[0:64, :], reads=[sjb], writes=[self.pT_b])
        Wv = Wukv[:].rearrange("p (h c) -> p h c", c=128)
        for ts in range(4 if "v" not in SK else 0):
            t = tq * 4 + ts
            j = t % 2
            for hv in range(2):
                pv = 6 + hv
                self.mm(self.PS[pv][:, 0:384], CKV[:, t * 128:(t + 1) * 128], Wv[:, hv * 6:(hv + 1) * 6, 64:128], True, True, [ub, cqb[tq]], [self.PB[pv]])
                P.op("dve", lambda e, j=j, hv=hv, pv=pv: e.tensor_copy(vst[j][:, hv * 384:(hv + 1) * 384], self.PS[pv][:, 0:384]), [self.PB[pv]], [vstb[j]])
            P.dma("sp", self.vtok[t * 128:(t + 1) * 128, :], vst[j], reads=[vstb[j]], writes=[self.vtok_b])
    P.barrier()
    if STOP == 2:
        self.xattn(l); return
    C.reset()
    kT = [C.bf(96, S) for _ in range(2)]; qT = [C.bf(96, S) for _ in range(2)]
    vA = [C.bf(128, NT, 128) for _ in range(2)]
    hb = [Buf() for _ in range(2)]
    E = [C.bf(128, 512) for _ in range(4)]; Eb = [Buf() for _ in range(4)]
    rd = [C.f32(64, 512) for _ in range(2)]; rdb = [Buf() for _ in range(2)]
    for j in range(2):
        P.op("pool", lambda e, j=j: e.memset(vA[j][:, :, 64:128], 1.0), [], [hb[j]])
    scale = 1.0 / math.sqrt(96.0)
    ne = 0
    vt = self.vtok.rearrange("(t p) c -> p t c", p=128)

    def load_head(h):
        j = h % 2
        P.dma("sp", qT[j], self.pT[h * 96:(h + 1) * 96, :], reads=[self.pT_b], writes=[hb[j]])
        P.dma("sp", kT[j][0:64, :], self.pT[1152 + h * 64:1152 + (h + 1) * 64, :], reads=[self.pT_b], writes=[hb[j]])
        P.dma("sp", kT[j][64:96, :], self.pT[1920:1952, :], reads=[self.pT_b], writes=[hb[j]])
        with self.nc.allow_non_contiguous_dma(reason="per-head V gather (128B runs)"):
            for t0 in range(0, NT, 4):
                P.dma("sp", vA[j][:, t0:t0 + 4, 0:64], vt[:, t0:t0 + 4, h * 64:(h + 1) * 64], reads=[self.vtok_b], writes=[hb[j]])
    st_ = {"ne": 0, "ns": 0}

    def new_E():
        i = st_["ne"] % 4; st_["ne"] += 1
        return E[i], Eb[i]

    def new_S():
        i = st_["ns"] % 4; st_["ns"] += 1
        return i
    load_head(0)
    for h in range(12):
        if h + 1 < 12:
            load_head(h + 1)
        j = h % 2
        pairs = []
        for tq in range(NQ):
            po = 4 + (tq % 2)
            nk = 4 * tq + 4
            for kt in range(nk):
                def score(ps, kt=kt, tq=tq, j=j):
                    self.mm(self.PS[ps][:], kT[j][:, kt * 128:(kt + 1) * 128], qT[j][:, tq * 512:(tq + 1) * 512], True, True, [hb[j]], [self.PB[ps]])

                def post(ps, Ei, Ebi, kt=kt, tq=tq):
                    self.act(Ei, self.PS[ps][:], AF.Exp, [self.PB[ps]], [Ebi], scale=scale)
                    if kt >= 4 * tq:
                        base = tq * 512 - kt * 128
                        P.op("pool", lambda e: e.affine_select(Ei, Ei, [[1, 512]], ALU.is_ge, 0.0, base=base, channel_multiplier=-1), [Ebi], [Ebi])

                def pv(Ei, Ebi, kt=kt, nk=nk, po=po, j=j):
                    self.mm(self.PS[po][:], vA[j][:, kt, :], Ei, kt == 0, kt == nk - 1, [hb[j], Ebi], [self.PB[po]])
                d = {"score": score, "post": post, "pv": pv}
                if kt == nk - 1:
                    def after(tq=tq, po=po, h=h):
                        r = rd[tq % 2]; rb = rdb[tq % 2]
                        P.op("dve", lambda e: e.reciprocal(r, self.PS[po][64:128, :]), [self.PB[po]], [rb])
                        dst = self.CT[64 * (h % 2):64 * (h % 2) + 64, h // 2, tq * 512:(tq + 1) * 512]
                        P.op("dve", lambda e: e.tensor_tensor(dst, self.PS[po][0:64, :], r, ALU.mult),
                             [self.PB[po], rb], [self.CTb[h // 2][tq]])
                    d["after"] = after
                pairs.append(d)
        self.run_pipeline(pairs, new_S, new_E)
    P.barrier()
    self.xattn(l)


Builder.mixer_mla = _mixer_mla


def _mixer_nsa(self, l):
    P, C, I, S = self.P, self.C, self.I, self.S
    NT, NQ = self.NT, self.NQ
    NCMP = (S - 32) // 16 + 1
    NCT = (NCMP + 127) // 128
    NCP = NCT * 128
    win = I[f"win{l}"].rearrange("(k p) n -> p k n", p=128)
    self.proj_xq(win, 2340)
    stg = [C.bf(128, 512) for _ in range(2)]; stgb = [Buf() for _ in range(2)]
    Wq = C.bf(128, 8, 768); Wk = C.bf(128, 8, 1024); Wg = C.bf(128, 8, 36); Wv = C.bf(128, 8, 512); wb = Buf()
    P.dma("pool", Wq, win[:, :, 0:768], writes=[wb])
    P.dma("pool", Wk[:, :, 0:256], win[:, :, 1280:1536], writes=[wb])
    P.dma("pool", Wk[:, :, 256:512], win[:, :, 1792:2048], writes=[wb])
    P.dma("pool", Wk[:, :, 512:1024], win[:, :, 768:1280], writes=[wb])
    P.dma("pool", Wg, win[:, :, 2304:2340], writes=[wb])
    P.dma("pool", Wv[:, :, 0:256], win[:, :, 1536:1792], writes=[wb])
    P.dma("pool", Wv[:, :, 256:512], win[:, :, 2048:2304], writes=[wb])
    posb_t = C.f32(128, 512, dt=I32); posf = C.f32(128, 512); kf = C.f32(128, 512); ki = posb_t
    Ct = C.f32(128, 512); St = C.f32(128, 512); tb = Buf()
    qraw = [C.bf(128, 512) for _ in range(2)]; qrb = [Buf() for _ in range(2)]
    t1 = [C.f32(128, 512) for _ in range(2)]; t2 = [C.f32(128, 512) for _ in range(2)]; t12b = [Buf() for _ in range(2)]
    qo = [C.bf(128, 512) for _ in range(2)]; qob = [Buf() for _ in range(2)]
    vst = [C.bf(128, 512) for _ in range(2)]; vstb = [Buf() for _ in range(2)]
    gst = [C.f32(36, 512) for _ in range(2)]; gstb = [Buf() for _ in range(2)]
    n = 0
    for tq in range(NQ):
        csl = slice(tq * 512, (tq + 1) * 512)
        self.rope_tables(tq, 128, self.invF[:, 0:1], posb_t, posf, kf, ki, Ct, St, tb)
        for ci in range(10):
            Wsrc, col0 = (Wq, ci * 128) if ci < 6 else (Wk, (ci - 6) * 128)
            row0 = ci * 128
            pq, pr = 0 + (ci % 2) * 2, 1 + (ci % 2) * 2
            self.proj_chunk(pq, 128, Wsrc, wb, col0, tq)
            j = n % 2; n += 1
            self.act(qraw[j], self.PS[pq][:], AF.Copy, [self.PB[pq]], [qrb[j]])
            self.mm(self.PS[pr][:], self.RnB, qraw[j], True, True, [self.cB, qrb[j]], [self.PB[pr]])
            P.op("dve", lambda e, j=j, pq=pq: e.tensor_tensor(t1[j], self.PS[pq][:], Ct, ALU.mult), [self.PB[pq], tb], [t12b[j]])
            P.op("dve", lambda e, j=j, pr=pr: e.tensor_tensor(t2[j], self.PS[pr][:], St, ALU.mult), [self.PB[pr], tb], [t12b[j]])
            P.op("pool", lambda e, j=j: e.tensor_tensor(qo[j], t1[j], t2[j], ALU.add), [t12b[j]], [qob[j]])
            P.dma("sp", self.pT[row0:row0 + 128, csl], qo[j], reads=[qob[j]], writes=[self.pT_b])
        for ci in range(4):
            pi = 4 + (ci % 2)
            self.proj_chunk(pi, 128, Wk, wb, 512 + ci * 128, tq)
            sj = stg[ci % 2]; sjb = stgb[ci % 2]
            self.act(sj, self.PS[pi][:], AF.Copy, [self.PB[pi]], [sjb])
            P.dma("sp", self.pT[1280 + ci * 128:1280 + (ci + 1) * 128, csl], sj, reads=[sjb], writes=[self.pT_b])
        self.proj_chunk(6, 36, Wg, wb, 0, tq)
        gj = gst[tq % 2]; gjb = gstb[tq % 2]
        self.act(gj, self.PS[6][0:36, :], AF.Copy, [self.PB[6]], [gjb])
        P.dma("sp", self.glT[:, csl], gj, reads=[gjb], writes=[self.glT_b])
        for ts in range(4):
            t = tq * 4 + ts
            j = t % 2
            pv = 5 if ts % 2 == 0 else 7
            for k in range(8):
                self.mm(self.PS[pv][:], self.XT[:, k, t * 128:(t + 1) * 128], Wv[:, k, :], k == 0, k == 7,
                        [wb, self.XTb[tq]], [self.PB[pv]])
            P.op("dve", lambda e, j=j, pv=pv: e.tensor_copy(vst[j], self.PS[pv][:]), [self.PB[pv]], [vstb[j]])
            P.dma("sp", self.vtok[t * 128:(t + 1) * 128, 0:512], vst[j], reads=[vstb[j]], writes=[self.vtok_b])
    P.barrier()
    C.reset()
    kcmpT = C.bf(64, 4, NCP); vcA = C.bf(128, 4, NCT, 128); cmb = Buf()
    Ebig = self.A_t[0:64, 4 * S:5 * S]
    Ov = C.bf(128, NCT, 64); Sel = C.bf(3, 3, 64); kb = Buf()
    P.dma("pool", Ebig, I["c_ebig"], writes=[kb])
    P.dma("pool", Ov, I["c_ov"][0:NCP, :].rearrange("(t p) j -> p t j", p=128), writes=[kb])
    P.dma("pool", Sel, I["c_sel"].rearrange("r (a m) -> r a m", m=64), writes=[kb])
    P.op("pool", lambda e: e.memset(vcA[:, :, :, 0:64], 0.0), [], [cmb])
    P.op("pool", lambda e: e.memset(vcA[:, :, :, 64:128], 1.0), [], [cmb])
    P.op("pool", lambda e: e.memset(kcmpT, 0.0), [], [cmb])
    mark = C.off
    cw1 = C.bf(64, 2, 32, 128); cpe = C.bf(64, 2, 32); cw2 = C.bf(128, 2, 64); cwb = Buf()
    P.dma("pool", cw1, I[f"cw1{l}"].rearrange("a d l f -> d a l f"), writes=[cwb])
    P.dma("pool", cpe, I[f"cpe{l}"].rearrange("a d l -> d a l"), writes=[cwb])
    P.dma("pool", cw2, I[f"cw2{l}"].rearrange("a f d -> f a d"), writes=[cwb])
    kcT = [C.bf(64, S) for _ in range(2)]; kcb = [Buf() for _ in range(2)]
    hbias = C.f32(128, 2); hbb = Buf()
    hT = [C.bf(128, NCP) for _ in range(2)]; hTb = [Buf() for _ in range(2)]
    posb2 = C.f32(64, NCP, dt=I32); posf2 = C.f32(64, NCP); kf2 = C.f32(64, NCP); ki2 = posb2
    Ct2 = C.f32(64, NCP); St2 = C.f32(64, NCP); tb2 = Buf()
    kraw = C.bf(64, NCP); krb_ = Buf(); u1 = C.f32(64, NCP); u2 = C.f32(64, NCP)
    self.rope_tables(0, 64, self.invF[0:64, 0:1], posb2, posf2, kf2, ki2, Ct2, St2, tb2,
                     pos_src=I["pos_end"][0:1, 0:NCP], width=NCP)
    for a in range(2):
        for ll in range(32):
            self.mm(self.PS[6][:, a:a + 1], cw1[:, a, ll, :], cpe[:, a, ll:ll + 1], ll == 0, ll == 31, [cwb], [self.PB[6]])
    P.op("dve", lambda e: e.tensor_copy(hbias, self.PS[6][:, 0:2]), [self.PB[6]], [hbb])
    nn = 0
    for a in range(2):
        for k in range(4):
            jj = nn % 2; nn += 1
            if a == 0:
                P.dma("sp", kcT[jj], self.pT[1280 + 64 * k:1280 + 64 * (k + 1), :], reads=[self.pT_b], writes=[kcb[jj]])
            else:
                P.dma("sp", kcT[jj], self.pT[1536 + 64 * k:1536 + 64 * (k + 1), :], reads=[self.pT_b], writes=[kcb[jj]])
            ph = jj
            src = kcT[jj]
            for ll in range(32):
                rhs = src[:, ll:ll + 16 * (NCMP - 1) + 1:16]
                self.mm(self.PS[ph][:, 0:NCMP], cw1[:, a, ll, :], rhs, ll == 0, ll == 31, [cwb, kcb[jj]], [self.PB[ph]])
            h_ = hT[jj]; hb_ = hTb[jj]
            if NCMP < NCP:
                P.op("pool", lambda e, h_=h_: e.memset(h_[:, NCMP:NCP], 0.0), [], [hb_])
            self.act(h_[:, 0:NCMP], self.PS[ph][:, 0:NCMP], AF.Silu, [self.PB[ph], hbb], [hb_], bias=hbias[:, a:a + 1], scale=1.0)
            if a == 0:
                self.mm(self.PS[2][0:64, 0:NCP], cw2[:, 0, :], h_, True, True, [cwb, hb_], [self.PB[2]])
                self.act(kraw, self.PS[2][0:64, 0:NCP], AF.Copy, [self.PB[2]], [krb_])
                self.mm(self.PS[3][0:64, 0:NCP], self.RnB[0:64, 0:64], kraw, True, True, [self.cB, krb_], [self.PB[3]])
                P.op("dve", lambda e: e.tensor_tensor(u1, self.PS[2][0:64, 0:NCP], Ct2, ALU.mult), [self.PB[2], tb2], [krb_])
                P.op("dve", lambda e: e.tensor_tensor(u2, self.PS[3][0:64, 0:NCP], St2, ALU.mult), [self.PB[3], tb2], [krb_])
                P.op("dve", lambda e, k=k: e.tensor_tensor(kcmpT[:, k, 0:NCMP], u1[:, 0:NCMP], u2[:, 0:NCMP], ALU.add), [krb_], [cmb])
            else:
                for nt in range(NCT):
                    m_ = min(128, NCMP - nt * 128)
                    self.mm(self.PS[4 + nt][0:m_, 0:64], h_[:, nt * 128:nt * 128 + m_], cw2[:, 1, :], True, True, [cwb, hb_], [self.PB[4 + nt]])
                    P.op("dve", lambda e, k=k, nt=nt, m_=m_: e.tensor_copy(vcA[0:m_, k, nt, 0:64], self.PS[4 + nt][0:m_, 0:64]), [self.PB[4 + nt]], [cmb])
    P.barrier()
    self.nsa_attention(l, kcmpT, vcA, cmb, Ebig, Ov, Sel, kb, mark, NCMP, NCT)


Builder.mixer_nsa = _mixer_nsa


def _nsa_attention(self, l, kcmpT, vcA, cmb, Ebig, Ov, Sel, kb, mark, NCMP, NCT):
    P, C, I, S = self.P, self.C, self.I, self.S
    NT, NQ = self.NT, self.NQ
    C.off = mark
    A = self.A_t
    ksT = A[0:64, 0:S]; kwT = A[0:64, S:2 * S]
    vsA = A[:, 2 * S:3 * S].rearrange("p (t c) -> p t c", c=128)
    vwA = A[:, 3 * S:4 * S].rearrange("p (t c) -> p t c", c=128)
    kvb = Buf()
    P.op("pool", lambda e: e.memset(vsA[:, :, 64:128], 1.0), [], [kvb])
    P.op("pool", lambda e: e.memset(vwA[:, :, 64:128], 1.0), [], [kvb])
    qc = [[C.bf(64, 512) for _ in range(3)] for _ in range(2)]; qcb = [[Buf() for _ in range(3)] for _ in range(2)]
    Gl = [[C.f32(3, 512) for _ in range(3)] for _ in range(2)]
    Glh = [[C.bf(3, 512) for _ in range(3)] for _ in range(2)]
    Gll = [[C.bf(3, 512) for _ in range(3)] for _ in range(2)]
    Gtmp = C.f32(3, 512); gtb = Buf()
    glb = [[Buf() for _ in range(3)] for _ in range(2)]
    FBt = [C.f32(128, 4, 64) for _ in range(2)]; CBt = [C.f32(128, 4, 64) for _ in range(2)]; fbb = [Buf() for _ in range(2)]
    E = [C.bf(128, 512) for _ in range(4)]; Eb = [Buf() for _ in range(4)]
    rd = [C.f32(64, 512) for _ in range(2)]; rdb = [Buf() for _ in range(2)]
    gs = [C.f32(64, 512) for _ in range(2)]; gsb = [Buf() for _ in range(2)]
    acc = [C.f32(64, 512) for _ in range(3)]; accb = [Buf() for _ in range(3)]
    impacc = C.f32(64, 512); impb = Buf()
    selbT = C.bf(64, 512); selTb = Buf()
    impm = [C.f32(128, 64) for _ in range(2)]; work = [C.f32(128, 64) for _ in range(2)]
    m8a = [C.f32(128, 8) for _ in range(2)]; m8b = [C.f32(128, 8) for _ in range(2)]
    selt = [C.f32(128, 64) for _ in range(2)]; selbf = [C.bf(128, 64) for _ in range(2)]; slb = [Buf() for _ in range(2)]
    PS, PB = self.PS, self.PB
    self.dbg_off = {"selbT": selbT.offset, "qc00": qc[0][0].offset, "impacc": impacc.offset, "selbf0": selbf[0].offset,
                    "CBt0": CBt[0].offset, "Glh00": Glh[0][0].offset, "E0": E[0].offset}
    PS7b = PS[7][:].bitcast(BF16)
    vt = self.vtok.rearrange("(t p) c -> p t c", p=128)
    fbv = I["c_fb"].rearrange("(q ts p) j -> q p ts j", p=128, ts=4)
    st = {"ne": 0, "nr": 0, "ns": 0}

    def new_E():
        i = st["ne"] % 4; st["ne"] += 1
        return E[i], Eb[i]

    def new_S():
        i = st["ns"] % 4; st["ns"] += 1
        return i

    def finish(k, g, tq, po, b, first, last, par, with_imp=False):
        h = 3 * k + g
        i = st["nr"] % 2; st["nr"] += 1
        r_, rb_ = rd[i], rdb[i]
        g_, gb_ = gs[i], gsb[i]
        P.op("dve", lambda e: e.tensor_scalar(r_, PS[po][64:128, :], 1.0e-30, None, ALU.max), [PB[po]], [rb_])
        P.op("dve", lambda e: e.reciprocal(r_, r_), [rb_], [rb_])
        if with_imp:
            if g == 0:
                P.op("dve", lambda e: e.tensor_tensor(impacc, PS[7][0:64, :], r_, ALU.mult), [PB[7], rb_], [impb])
            else:
                P.op("dve", lambda e: e.tensor_tensor(g_, PS[7][0:64, :], r_, ALU.mult), [PB[7], rb_], [gb_])
                P.op("dve", lambda e: e.tensor_tensor(impacc, impacc, g_, ALU.add), [gb_, impb], [impb])
        self.mm(PS[6][0:64, :], Sel[:, b, :], Glh[par][g], True, False, [kb, glb[par][g]], [PB[6]])
        self.mm(PS[6][0:64, :], Sel[:, b, :], Gll[par][g], False, True, [kb, glb[par][g]], [PB[6]])
        self.act(g_, PS[6][0:64, :], AF.Sigmoid, [PB[6]], [gb_])
        P.op("dve", lambda e: e.tensor_tensor(g_, g_, r_, ALU.mult), [gb_, rb_], [gb_])
        import os
        SKB = os.environ.get("NSA_SKIPB", "")
        if str(b) in SKB:
            P.op("dve", lambda e: e.memset(g_, 0.0), [], [gb_])
        if first:
            P.op("dve", lambda e: e.tensor_tensor(acc[g], PS[po][0:64, :], g_, ALU.mult), [PB[po], gb_], [accb[g]])
        else:
            P.op("dve", lambda e: e.tensor_tensor(r_, PS[po][0:64, :], g_, ALU.mult), [PB[po], gb_], [rb_])
            if not last:
                P.op("dve", lambda e: e.tensor_tensor(acc[g], acc[g], r_, ALU.add), [rb_, accb[g]], [accb[g]])
            else:
                dst = self.CT[64 * (h % 2):64 * (h % 2) + 64, h // 2, tq * 512:(tq + 1) * 512]
                P.op("dve", lambda e: e.tensor_tensor(dst, acc[g], r_, ALU.add), [rb_, accb[g]], [self.CTb[h // 2][tq]])

    for k in range(4):
        P.dma("sp", ksT, self.pT[768 + 64 * k:768 + 64 * (k + 1), :], reads=[self.pT_b], writes=[kvb])
        P.dma("sp", kwT, self.pT[1024 + 64 * k:1024 + 64 * (k + 1), :], reads=[self.pT_b], writes=[kvb])
        with self.nc.allow_non_contiguous_dma(reason="per-head V gather (128B runs)"):
            for t0 in range(0, NT, 4):
                P.dma("sp", vsA[:, t0:t0 + 4, 0:64], vt[:, t0:t0 + 4, 64 * k:64 * (k + 1)], reads=[self.vtok_b], writes=[kvb])
                P.dma("sp", vwA[:, t0:t0 + 4, 0:64], vt[:, t0:t0 + 4, 256 + 64 * k:256 + 64 * (k + 1)], reads=[self.vtok_b], writes=[kvb])
        for tq in range(NQ):
            par = tq % 2
            q0 = tq * 512
            csl = slice(q0, q0 + 512)
            for g in range(3):
                h = 3 * k + g
                P.dma("sp", qc[par][g], self.pT[64 * h:64 * (h + 1), csl], reads=[self.pT_b], writes=[qcb[par][g]])
                P.dma("sp", Gl[par][g], self.glT[3 * h:3 * h + 3, csl], reads=[self.glT_b], writes=[glb[par][g]])
                P.op("act", lambda e, par=par, g=g: e.copy(Glh[par][g], Gl[par][g]), [glb[par][g]], [glb[par][g]])
                P.op("dve", lambda e, par=par, g=g: e.tensor_tensor(Gtmp, Gl[par][g], Glh[par][g], ALU.subtract), [glb[par][g]], [glb[par][g], gtb])
                P.op("dve", lambda e, par=par, g=g: e.tensor_copy(Gll[par][g], Gtmp), [glb[par][g], gtb], [glb[par][g]])
            P.dma("sp", FBt[par], fbv[tq], writes=[fbb[par]])
            P.op("dve", lambda e, par=par: e.tensor_scalar(CBt[par], FBt[par], 0.0, 3.0e-5, ALU.min, ALU.mult), [fbb[par]], [fbb[par]])
            tiles = [nt for nt in range(NCT) if nt * 2048 + 31 <= q0 + 511]
            kts_w = list(range(max(0, 4 * tq - 4), 4 * tq + 4))
            pairsA = []
            grp = 0
            for g in range(3):
                po = 4 + (grp % 2); grp += 1
                for ii, nt in enumerate(tiles):
                    def score(ps, nt=nt, g=g):
                        self.mm(PS[ps][:], kcmpT[:, k, nt * 128:(nt + 1) * 128], qc[par][g], True, True, [cmb, qcb[par][g]], [PB[ps]])

                    def post(ps, Ei, Ebi, nt=nt):
                        self.act(Ei, PS[ps][:], AF.Exp, [PB[ps]], [Ebi], scale=0.125)
                        base = q0 - 16 * nt * 128 - 31
                        P.op("pool", lambda e: e.affine_select(Ei, Ei, [[1, 512]], ALU.is_ge, 0.0, base=base, channel_multiplier=-16), [Ebi], [Ebi])

                    def pv(Ei, Ebi, nt=nt, ii=ii, po=po):
                        self.mm(PS[po][:], vcA[:, k, nt, :], Ei, ii == 0, ii == len(tiles) - 1, [cmb, Ebi], [PB[po]])
                        self.mm(PS[7][0:64, :], Ov[:, nt, :], Ei, ii == 0, ii == len(tiles) - 1, [kb, Ebi], [PB[7]])
                    d = {"score": score, "post": post, "pv": pv}
                    if ii == len(tiles) - 1:
                        d["after"] = (lambda g=g, po=po: finish(k, g, tq, po, 0, True, False, par, with_imp=True))
                    pairsA.append(d)
            for g in range(3):
                po = 4 + (grp % 2); grp += 1
                for ii, kt in enumerate(kts_w):
                    def score(ps, kt=kt, g=g):
                        self.mm(PS[ps][:], kwT[:, kt * 128:(kt + 1) * 128], qc[par][g], True, True, [kvb, qcb[par][g]], [PB[ps]])

                    def post(ps, Ei, Ebi, kt=kt):
                        self.act(Ei, PS[ps][:], AF.Exp, [PB[ps]], [Ebi], scale=0.125)
                        if kt >= 4 * tq:
                            base = q0 - kt * 128
                            P.op("pool", lambda e: e.affine_select(Ei, Ei, [[1, 512]], ALU.is_ge, 0.0, base=base, channel_multiplier=-1), [Ebi], [Ebi])
                        else:
                            base = 511 - q0 + kt * 128
                            P.op("pool", lambda e: e.affine_select(Ei, Ei, [[-1, 512]], ALU.is_ge, 0.0, base=base, channel_multiplier=1), [Ebi], [Ebi])

                    def pv(Ei, Ebi, kt=kt, ii=ii, po=po):
                        self.mm(PS[po][:], vwA[:, kt, :], Ei, ii == 0, ii == len(kts_w) - 1, [kvb, Ebi], [PB[po]])
                    d = {"score": score, "post": post, "pv": pv}
                    if ii == len(kts_w) - 1:
                        d["after"] = (lambda g=g, po=po: finish(k, g, tq, po, 2, False, False, par))
                    pairsA.append(d)
            self.run_pipeline(pairsA, new_S, new_E)
            trivial = (q0 + 511 < 16 * 64)
            for ts in range(4):
                i = ts % 2
                if trivial:
                    P.op("dve", lambda e, i=i, ts=ts, par=par: e.tensor_copy(selbf[i], CBt[par][:, ts, :]), [fbb[par]], [slb[i]])
                    self.tr(PS7b[0:64, ts * 128:(ts + 1) * 128], selbf[i], self.identB, [slb[i], self.cB], [PB[7]])
                    continue
                self.tr(PS[6][:, 0:64], impacc[:, ts * 128:(ts + 1) * 128], self.identF[0:64, 0:64], [impb, self.cB], [PB[6]])
                P.op("dve", lambda e, i=i, ts=ts, par=par: e.tensor_tensor(impm[i], PS[6][:, 0:64], FBt[par][:, ts, :], ALU.add), [PB[6], fbb[par]], [slb[i]])
                P.op("dve", lambda e, i=i: e.max(out=m8a[i], in_=impm[i]), [slb[i]], [slb[i]])
                P.op("dve", lambda e, i=i: e.match_replace(out=work[i], in_to_replace=m8a[i], in_values=impm[i], imm_value=-3.0e9), [slb[i]], [slb[i]])
                P.op("dve", lambda e, i=i: e.max(out=m8b[i], in_=work[i]), [slb[i]], [slb[i]])
                P.op("dve", lambda e, i=i: e.tensor_scalar(selt[i], impm[i], m8b[i][:, 7:8], None, ALU.is_ge), [slb[i]], [slb[i]])
                P.op("dve", lambda e, i=i: e.tensor_scalar(selt[i], selt[i], 1.0, 30000.0, ALU.subtract, ALU.mult), [slb[i]], [slb[i]])
                P.op("dve", lambda e, i=i, ts=ts, par=par: e.tensor_tensor(selbf[i], selt[i], CBt[par][:, ts, :], ALU.add), [slb[i], fbb[par]], [slb[i]])
                self.tr(PS7b[0:64, ts * 128:(ts + 1) * 128], selbf[i], self.identB, [slb[i], self.cB], [PB[7]])
            self.act(selbT, PS7b[0:64, 0:512], AF.Copy, [PB[7]], [selTb])
            pairsB = []
            nk = 4 * tq + 4
            for g in range(3):
                po = 4 + (grp % 2); grp += 1
                for kt in range(nk):
                    def score(ps, kt=kt, g=g):
                        self.mm(PS[ps][:], ksT[:, kt * 128:(kt + 1) * 128], qc[par][g], True, False, [kvb, qcb[par][g]], [PB[ps]])
                        self.mm(PS[ps][:], Ebig[:, kt * 128:(kt + 1) * 128], selbT, False, True, [kb, selTb], [PB[ps]])

                    def post(ps, Ei, Ebi, kt=kt):
                        self.act(Ei, PS[ps][:], AF.Exp, [PB[ps]], [Ebi], scale=0.125)
                        if kt >= 4 * tq:
                            base = q0 - kt * 128
                            P.op("pool", lambda e: e.affine_select(Ei, Ei, [[1, 512]], ALU.is_ge, 0.0, base=base, channel_multiplier=-1), [Ebi], [Ebi])

                    def pv(Ei, Ebi, kt=kt, po=po):
                        self.mm(PS[po][:], vsA[:, kt, :], Ei, kt == 0, kt == nk - 1, [kvb, Ebi], [PB[po]])
                    d = {"score": score, "post": post, "pv": pv}
                    if kt == nk - 1:
                        d["after"] = (lambda g=g, po=po: finish(k, g, tq, po, 1, False, True, par))
                    pairsB.append(d)
            self.run_pipeline(pairsB, new_S, new_E)
    P.barrier()
    self.xattn(l)


Builder.nsa_attention = _nsa_attention
```

```python
import contextlib
import math
import numpy as np
import concourse.bass as bass
import concourse.mybir as mybir
from concourse.bass_utils import run_bass_kernel_spmd

F32 = mybir.dt.float32
BF16 = mybir.dt.bfloat16
I32 = mybir.dt.int32
AF = mybir.ActivationFunctionType
ALU = mybir.AluOpType
AX = mybir.AxisListType

D = 1024
NCORES = 8
N_MEM = 256
ALPHA = (2.0 * 4) ** 0.25
LN_EPS = 1e-5
RMS_EPS = 1e-6
NEG = -30000.0
EPOCH = 16000
ENGS = ("pe", "act", "dve", "pool", "sp")


class Buf:
    __slots__ = ("name", "w", "r", "excl")

    def __init__(self, name="", excl=False):
        self.name = name
        self.w = None
        self.r = {}
        self.excl = excl


class Prog:
    def __init__(self, nc, stack, n_dma_slots=(("sp", 16), ("pool", 16), ("act", 4))):
        self.nc = nc
        self.stack = stack
        self.streams = {e: [] for e in ENGS}
        self.cnt = {e: 0 for e in ENGS}
        self.sems = []
        self.eng_sems = {e: [] for e in ENGS}
        self.waited = {e: {} for e in ENGS}
        self.dma_slots = {}
        for q, n in n_dma_slots:
            self.dma_slots[q] = [[self._new_sem(f"d{q}{i}"), 0] for i in range(n)]
        self.dma_rr = {q: 0 for q, _ in n_dma_slots}
        self.ninstr = 0

    def _new_sem(self, name):
        h = self.stack.enter_context(self.nc.semaphore(name))
        self.sems.append(h)
        return len(self.sems) - 1

    def _eng_sem(self, eng, epoch):
        lst = self.eng_sems[eng]
        while len(lst) <= epoch:
            lst.append(self._new_sem(f"s{eng}{len(lst)}"))
        return lst[epoch]

    def _need(self, eng, deps):
        w = self.waited[eng]
        for si, val in deps.items():
            if w.get(si, 0) >= val:
                continue
            w[si] = val
            h = self.sems[si]
            self.streams[eng].append(lambda e, h=h, val=val: e.wait_ge(h, val))

    def _collect(self, reads, writes, eng=None):
        deps = {}
        own = self.eng_sems.get(eng, ()) if eng else ()
        for b in reads:
            ev = b.w
            if ev is not None and deps.get(ev[0], 0) < ev[1]:
                deps[ev[0]] = ev[1]
            if b.excl:
                for si, v in b.r.items():
                    if si not in own and deps.get(si, 0) < v:
                        deps[si] = v
        for b in writes:
            ev = b.w
            if ev is not None and deps.get(ev[0], 0) < ev[1]:
                deps[ev[0]] = ev[1]
            for si, v in b.r.items():
                if deps.get(si, 0) < v:
                    deps[si] = v
        return deps

    @staticmethod
    def _mark(ev, reads, writes):
        si, v = ev
        for b in writes:
            b.w = ev
            b.r = {}
        for b in reads:
            if b.r.get(si, 0) < v:
                b.r[si] = v

    def op(self, eng, fn, reads=(), writes=()):
        deps = self._collect(reads, writes, eng)
        n = self.cnt[eng]
        epoch, within = divmod(n, EPOCH)
        si = self._eng_sem(eng, epoch)
        if eng == "pe":
            for s in self.eng_sems["pe"]:
                deps.pop(s, None)
        self._need(eng, deps)
        h = self.sems[si]
        self.streams[eng].append(lambda e, fn=fn, h=h: fn(e).then_inc(h, 1))
        self.cnt[eng] = n + 1
        ev = (si, within + 1)
        self._mark(ev, reads, writes)
        self.ninstr += 1
        return ev

    def dma(self, q, out, in_, reads=(), writes=(), **kw):
        deps = self._collect(reads, writes)
        slots = self.dma_slots[q]
        k = self.dma_rr[q]
        self.dma_rr[q] = (k + 1) % len(slots)
        slot = slots[k]
        si = slot[0]
        if slot[1] > 0:
            deps[si] = max(deps.get(si, 0), slot[1])
        self._need(q, deps)
        slot[1] += 16
        val = slot[1]
        h = self.sems[si]
        self.streams[q].append(
            lambda e, out=out, in_=in_, h=h, kw=kw: e.dma_start(out=out, in_=in_, **kw).then_inc(h, 16))
        ev = (si, val)
        self._mark(ev, reads, writes)
        self.ninstr += 1
        return ev

    def barrier(self):
        deps = {}
        for e in ENGS:
            n = self.cnt[e]
            if n == 0:
                continue
            epoch, within = divmod(n - 1, EPOCH)
            deps[self.eng_sems[e][epoch]] = within + 1
            for ep in range(epoch):
                deps[self.eng_sems[e][ep]] = EPOCH
        for q in self.dma_slots:
            for si, v in self.dma_slots[q]:
                if v:
                    deps[si] = v
        for e in ENGS:
            self._need(e, dict(deps))

    def emit(self):
        nc = self.nc
        with nc.Block() as block:
            @block.tensor
            def _(e):
                for f in self.streams["pe"]:
                    f(e)

            @block.scalar
            def _(e):
                for f in self.streams["act"]:
                    f(e)

            @block.vector
            def _(e):
                for f in self.streams["dve"]:
                    f(e)

            @block.gpsimd
            def _(e):
                for f in self.streams["pool"]:
                    f(e)

            @block.sync
            def _(e):
                for f in self.streams["sp"]:
                    f(e)


class Arena:
    def __init__(self, ap, nbf16):
        self.ap = ap
        self.n = nbf16
        self.off = 0

    def reset(self):
        self.off = 0

    def bf(self, parts, *shape):
        self.off = (self.off + 31) // 32 * 32
        n = int(np.prod(shape))
        n2 = (n + 1) // 2 * 2
        assert self.off + n2 <= self.n, ("arena overflow", self.off, n2, self.n)
        v = self.ap[0:parts, self.off:self.off + n]
        self.off += n2
        if len(shape) == 2:
            v = v.rearrange("p (a b) -> p a b", b=shape[1])
        elif len(shape) == 3:
            v = v.rearrange("p (a b c) -> p a b c", b=shape[1], c=shape[2])
        return v

    def f32(self, parts, *shape, dt=F32):
        self.off = (self.off + 31) // 32 * 32
        n = int(np.prod(shape))
        assert self.off + 2 * n <= self.n, ("arena overflow", self.off, 2 * n, self.n)
        v = self.ap[0:parts, self.off:self.off + 2 * n].bitcast(dt)
        self.off += 2 * n
        if len(shape) == 2:
            v = v.rearrange("p (a b) -> p a b", b=shape[1])
        elif len(shape) == 3:
            v = v.rearrange("p (a b c) -> p a b c", b=shape[1], c=shape[2])
        return v


NSA_IN = 2596
MLA_IN = 672
CONV_IN = 1792


class Builder:
    def __init__(self, S, kinds, last_only_out=True):
        self.S = S
        self.kinds = list(kinds)
        self.NT = S // 128
        self.NQ = S // 512
        self.SC = min(2048, S // 2)

    def mm(self, out, lhsT, rhs, start, stop, reads, writes):
        return self.P.op("pe", lambda e: e.matmul(out, lhsT, rhs, start=start, stop=stop), reads, writes)

    def tr(self, out, in_, ident, reads, writes):
        return self.P.op("pe", lambda e: e.transpose(out, in_, ident), reads, writes)

    def act(self, out, in_, func, reads, writes, **kw):
        return self.P.op("act", lambda e: e.activation(out, in_, func, **kw), reads, writes)

    def dbg_dump(self, name, ap, shape, dt, reads):
        import os
        if not os.environ.get("DBG_NSA"):
            return
        self.dbg_names = getattr(self, "dbg_names", [])
        d = self.dram(name, shape, dt, kind="ExternalOutput")
        self.P.dma("sp", d, ap, reads=reads)
        self.dbg_names.append(name)

    def dram(self, name, shape, dt, kind="Internal"):
        return self.nc.dram_tensor(name, list(shape), dt, kind=kind).ap()

    def build(self):
        S = self.S
        nc = self.nc = bass.Bass("TRN2", target_bir_lowering=False)
        I = self.I = {}

        def inp(name, shape, dt=F32):
            I[name] = self.dram(name, shape, dt, kind="ExternalInput")
            return I[name]

        inp("x", [S, D]); inp("mem", [N_MEM, D]); inp("pos", [1, S], I32); inp("pos_end", [1, 256], I32)
        inp("c_ident", [128, 128]); inp("c_rope", [128, 4 * 128 + 4])
        inp("c_ebig", [64, S]); inp("c_ov", [256, 64]); inp("c_fb", [S, 64]); inp("c_sel", [3, 3 * 64])
        for l, kind in enumerate(self.kinds):
            nin = (NSA_IN, MLA_IN, CONV_IN)[kind]
            inp(f"win{l}", [D, nin]); inp(f"wkv{l}", [D, 512]); inp(f"wo{l}", [D, D])
            inp(f"lng{l}", [2, D]); inp(f"lnb{l}", [2, D])
            inp(f"wr{l}", [D, 20]); inp(f"br{l}", [1, 20])
            inp(f"wg{l}", [16, D, 512]); inp(f"wu{l}", [16, D, 512]); inp(f"wd{l}", [16, 512, D])
            if kind == 0:
                inp(f"cpe{l}", [2, 64, 32]); inp(f"cw1{l}", [2, 64, 32, 128]); inp(f"cw2{l}", [2, 128, 64])
            elif kind == 1:
                inp(f"qn{l}", [128, 2]); inp(f"kvn{l}", [128, 1]); inp(f"wuq{l}", [256, 1152]); inp(f"wukv{l}", [128, 1536])
            else:
                inp(f"cbin{l}", [128, 12]); inp(f"cdw{l}", [128, 6 * 31]); inp(f"cdb{l}", [128, 6])
                inp(f"clg{l}", [128, 6]); inp(f"clb{l}", [128, 6])
        self.out = self.dram("out", [S, D], F32, kind="ExternalOutput")
        self.xres = self.dram("xres", [S, D], F32)
        self.pT = self.dram("pT", [2304, S], BF16)
        self.vtok = self.dram("vtok", [S, 768], BF16)
        import os
        self.glT = self.dram("glT", [36, S], F32, kind=("ExternalOutput" if os.environ.get("DBG_GL") else "Internal"))

        with contextlib.ExitStack() as st:
            P = self.P = Prog(nc, st)
            sb = lambda name, shape, dt: st.enter_context(nc.sbuf_tensor(name, shape, dt))
            self.A_t = sb("arenaA", [128, max(8 * S, 186 * 128)], BF16)
            self.B_t = sb("arenaB", [128, 8 * S], BF16)
            ZN = 4608
            self.Z = Arena(sb("arenaZ", [128, ZN], BF16)[:], ZN)
            rem = int(nc.sbuf_bytes_remaining) - 1024
            CN = min(rem // 2, 37000)
            self.C = Arena(sb("arenaC", [128, CN], BF16)[:], CN)
            self.PS = [st.enter_context(nc.psum_tensor(f"ps{i}", [128, 512], F32)) for i in range(8)]
            self.PB = [Buf(f"ps{i}", excl=True) for i in range(8)]
            self.XT = self.A_t[:, 0:8 * S].rearrange("p (k s) -> p k s", s=S)
            self.CT = self.B_t[:].rearrange("p (k s) -> p k s", s=S)
            self.XTb = [Buf(f"xt{q}") for q in range(self.NQ)]
            self.CTb = [[Buf(f"ct{k}_{q}") for q in range(self.NQ)] for k in range(8)]
            self.xres_b = [Buf(f"xr{t}") for t in range(self.NT)]
            self.pT_b = Buf("pT"); self.vtok_b = Buf("vtok"); self.glT_b = Buf("glT")
            self.setup_consts()
            for l, kind in enumerate(self.kinds):
                self.layer(l, kind)
            P.barrier()
            P.emit()
        return nc

    def setup_consts(self):
        P, Z, I = self.P, self.Z, self.I
        self.identF = Z.f32(128, 128); self.identB = Z.bf(128, 128)
        self.onesB = Z.bf(128, 128)
        self.onesF = Z.f32(128, 128)
        self.eps_t = Z.f32(128, 2)
        self.RnB = Z.bf(128, 128); self.RmB = Z.bf(128, 128); self.invF = Z.f32(128, 4)
        self.memT = Z.bf(128, 8, 256)
        self.gates = Z.f32(128, self.NT, 16)
        self.cB = Buf("consts"); self.memT_b = Buf("memT"); self.gates_b = [Buf() for _ in range(self.NT)]
        P.dma("sp", self.identF, I["c_ident"], writes=[self.cB])
        P.dma("pool", self.identB, I["c_ident"], writes=[self.cB])
        P.op("dve", lambda e: e.memset(self.onesB, 1.0), writes=[self.cB])
        P.op("dve", lambda e: e.memset(self.onesF, 1.0), writes=[self.cB])
        P.op("dve", lambda e: e.memset(self.eps_t[:, 0:1], LN_EPS), writes=[self.cB])
        P.op("dve", lambda e: e.memset(self.eps_t[:, 1:2], RMS_EPS), writes=[self.cB])
        P.dma("pool", self.RnB, I["c_rope"][:, 0:128], writes=[self.cB])
        P.dma("pool", self.RmB, I["c_rope"][:, 128:256], writes=[self.cB])
        P.dma("sp", self.invF, I["c_rope"][:, 512:516], writes=[self.cB])
        C = self.C
        C.reset()
        m = C.f32(128, 2, D)
        mb = Buf()
        P.dma("sp", m, I["mem"].rearrange("(t p) d -> p t d", p=128), writes=[mb])
        for t in range(2):
            for half in range(2):
                pi = t * 2 + half
                for k4 in range(4):
                    k = half * 4 + k4
                    self.tr(self.PS[pi][:, k4 * 128:(k4 + 1) * 128], m[:, t, k * 128:(k + 1) * 128], self.identF,
                            [mb, self.cB], [self.PB[pi]])
                src = self.PS[pi][:].rearrange("p (k s) -> p k s", s=128)
                dst = self.memT[:, half * 4:half * 4 + 4, t * 128:(t + 1) * 128]
                P.op("act", lambda e, dst=dst, src=src: e.copy(dst, src), [self.PB[pi]], [self.memT_b])
        P.barrier()

    def layer(self, l, kind):
        first = (l == 0)
        last = (l == len(self.kinds) - 1)
        self.xsrc = self.I["x"] if first else self.xres
        if first:
            self.load_xt_from_x()
        if kind == 2:
            self.mixer_conv(l)
        elif kind == 1:
            self.mixer_mla(l)
        else:
            self.mixer_nsa(l)
        import os
        if os.environ.get("DBG_CT") and l == 0:
            dbg = self.dram("dbg", [128, 8 * self.S], BF16, kind="ExternalOutput")
            self.P.dma("sp", dbg, self.B_t[:, 0:8 * self.S])
            self.P.barrier()
        self.phase_out(l)
        self.phase_moe(l, last)

    def load_xt_from_x(self):
        P, C = self.P, self.C
        C.reset()
        xt = [C.f32(128, D) for _ in range(3)]
        xb = [Buf() for _ in range(3)]
        x = self.I["x"]
        for t in range(self.NT):
            j = t % 3
            P.dma("sp", xt[j], x[t * 128:(t + 1) * 128, :], writes=[xb[j]])
            self.transpose_to_xt(xt[j], xb[j], t, bank0=(t % 2) * 2)
        P.barrier()

    def transpose_to_xt(self, src, srcb, t, bank0, scale=None):
        P = self.P
        q = t // 4
        for half in range(2):
            pi = bank0 + half
            for k4 in range(4):
                k = half * 4 + k4
                self.tr(self.PS[pi][:, k4 * 128:(k4 + 1) * 128], src[:, k * 128:(k + 1) * 128], self.identF,
                        [srcb, self.cB], [self.PB[pi]])
            s_ = self.PS[pi][:].rearrange("p (k s) -> p k s", s=128)
            d_ = self.XT[:, half * 4:half * 4 + 4, t * 128:(t + 1) * 128]
            P.op("act", lambda e, d_=d_, s_=s_: e.copy(d_, s_), [self.PB[pi]], [self.XTb[q]])

    def ln_tile(self, v, vb, gbc, bbc, gbb, scr, j):
        P = self.P
        st_, mv, rs = scr["st"][j], scr["mv"][j], scr["rs"][j][:, 0:1]
        sb_ = scr["b"][j]
        P.op("dve", lambda e: e.bn_stats(st_[:, 0, :], v[:, 0:512]), [vb], [sb_])
        P.op("dve", lambda e: e.bn_stats(st_[:, 1, :], v[:, 512:1024]), [vb], [sb_])
        P.op("dve", lambda e: e.bn_aggr(mv, st_[:].rearrange("p a b -> p (a b)")), [sb_], [sb_])
        P.op("act", lambda e: e.activation(rs, mv[:, 1:2], AF.Sqrt, bias=self.eps_t[:, 0:1], scale=1.0), [sb_, self.cB], [sb_])
        P.op("dve", lambda e: e.reciprocal(rs, rs), [sb_], [sb_])
        P.op("dve", lambda e: e.tensor_scalar(v, v, mv[:, 0:1], rs, ALU.subtract, ALU.mult), [vb, sb_], [vb])
        P.op("pool", lambda e: e.tensor_tensor(v, v, gbc, ALU.mult), [vb, gbb], [vb])
        P.op("pool", lambda e: e.tensor_tensor(v, v, bbc, ALU.add), [vb, gbb], [vb])

    def ln_scratch(self, n=2):
        C = self.C
        return {"st": [C.f32(128, 2, 6) for _ in range(n)], "mv": [C.f32(128, 2) for _ in range(n)],
                "rs": [C.f32(128, 2) for _ in range(n)], "b": [Buf() for _ in range(n)]}

    def phase_out(self, l):
        P, C, I = self.P, self.C, self.I
        C.reset()
        Wo = C.bf(128, 8, D); wob = Buf()
        P.dma("pool", Wo, I[f"wo{l}"].rearrange("(k p) n -> p k n", p=128), writes=[wob])
        gbc = C.f32(128, D); bbc = C.f32(128, D); gbb = Buf()
        P.dma("sp", gbc, I[f"lng{l}"][0:1, :].broadcast_to([128, D]), writes=[gbb])
        P.dma("sp", bbc, I[f"lnb{l}"][0:1, :].broadcast_to([128, D]), writes=[gbb])
        scr = self.ln_scratch()
        NB = 3
        xo = [C.f32(128, D) for _ in range(NB)]
        xb = [Buf() for _ in range(NB)]

        def load(t):
            P.dma("sp", xo[t % NB], self.xsrc[t * 128:(t + 1) * 128, :], reads=[self.xres_b[t]], writes=[xb[t % NB]])
        load(0)
        for t in range(self.NT):
            if t + 1 < self.NT:
                load(t + 1)
            j = t % NB
            q = t // 4
            for half in range(2):
                pi = (t % 2) * 2 + half
                for k in range(8):
                    self.mm(self.PS[pi][:], self.CT[:, k, t * 128:(t + 1) * 128], Wo[:, k, half * 512:(half + 1) * 512],
                            k == 0, k == 7, [self.CTb[k][q], wob], [self.PB[pi]])
                xs = xo[j][:, half * 512:(half + 1) * 512]
                P.op("dve", lambda e, xs=xs, pi=pi: e.scalar_tensor_tensor(xs, xs, ALPHA, self.PS[pi][:], ALU.mult, ALU.add),
                     [xb[j], self.PB[pi]], [xb[j]])
            self.ln_tile(xo[j], xb[j], gbc, bbc, gbb, scr, t % 2)
            P.dma("pool", self.xres[t * 128:(t + 1) * 128, :], xo[j], reads=[xb[j]], writes=[self.xres_b[t]])
            self.transpose_to_xt(xo[j], xb[j], t, bank0=4 + (t % 2) * 2)
        P.barrier()

    def phase_moe(self, l, last):
        P, C, I, S = self.P, self.C, self.I, self.S
        C.reset()
        NT, SC = self.NT, self.SC
        nsc = S // SC
        tps = SC // 128
        cps = SC // 512
        self.router(l)
        C.reset()
        gbc = C.f32(128, D); bbc = C.f32(128, D); gbb = Buf()
        P.dma("sp", gbc, I[f"lng{l}"][1:2, :].broadcast_to([128, D]), writes=[gbb])
        P.dma("sp", bbc, I[f"lnb{l}"][1:2, :].broadcast_to([128, D]), writes=[gbb])
        scr = self.ln_scratch()
        wg = [C.bf(128, 8, 512) for _ in range(2)]; wgb = [Buf() for _ in range(2)]
        wu = [C.bf(128, 8, 512) for _ in range(2)]; wub = [Buf() for _ in range(2)]
        wd = [C.bf(128, 4, D) for _ in range(1)]; wdb = [Buf() for _ in range(1)]
        hd = [C.bf(128, 4, 512) for _ in range(2)]; hdb = [Buf() for _ in range(2)]
        sg = [C.bf(128, 512) for _ in range(2)]; sgb = [Buf() for _ in range(2)]
        xo = [C.f32(128, D) for _ in range(2)]; xb = [Buf() for _ in range(2)]
        facc = self.B_t[:].bitcast(F32).rearrange("p (t d) -> p t d", d=D)
        fab = [Buf() for _ in range(tps)]

        def fa_bufs(tl):
            lo, hi = 2048 * tl, 2048 * tl + 2048
            res = [fab[tl]]
            for k in range(lo // S, (hi - 1) // S + 1):
                s0 = max(lo, k * S) - k * S
                s1 = min(hi, (k + 1) * S) - k * S
                for q in range(s0 // 512, (s1 - 1) // 512 + 1):
                    res.append(self.CTb[k][q])
            return res

        wgv = I[f"wg{l}"]; wuv = I[f"wu{l}"]; wdv = I[f"wd{l}"]

        def load_w(e):
            P.dma("pool", wg[e % 2], wgv[e].rearrange("(k p) n -> p k n", p=128), writes=[wgb[e % 2]])
            P.dma("pool", wu[e % 2], wuv[e].rearrange("(k p) n -> p k n", p=128), writes=[wub[e % 2]])

        def load_wd(e):
            P.dma("pool", wd[0], wdv[e].rearrange("(k p) n -> p k n", p=128), writes=[wdb[0]])

        for sc in range(nsc):
            tok0 = sc * SC
            load_w(0)
            load_wd(0)
            for e in range(16):
                if e + 1 < 16:
                    load_w(e + 1)
                for c in range(cps):
                    q = (tok0 // 512) + c
                    hb = hd[c % 2]
                    for f in range(4):
                        pg, pu = (f % 2) * 2, (f % 2) * 2 + 1
                        for k in range(8):
                            self.mm(self.PS[pg][:], wg[e % 2][:, k, f * 128:(f + 1) * 128], self.XT[:, k, q * 512:(q + 1) * 512],
                                    k == 0, k == 7, [wgb[e % 2], self.XTb[q]], [self.PB[pg]])
                        for k in range(8):
                            self.mm(self.PS[pu][:], wu[e % 2][:, k, f * 128:(f + 1) * 128], self.XT[:, k, q * 512:(q + 1) * 512],
                                    k == 0, k == 7, [wub[e % 2], self.XTb[q]], [self.PB[pu]])
                        s_ = sg[f % 2]
                        self.act(s_, self.PS[pg][:], AF.Silu, [self.PB[pg]], [sgb[f % 2]])
                        P.op("dve", lambda e_, f=f, s_=s_, pu=pu, hb=hb: e_.tensor_tensor(hb[:, f, :], s_, self.PS[pu][:], ALU.mult),
                             [sgb[f % 2], self.PB[pu]], [hdb[c % 2]])
                    for ts in range(4):
                        tl = c * 4 + ts
                        tg = tok0 // 128 + tl
                        for half in range(2):
                            po = 4 + ((ts * 2 + half) % 4)
                            for f in range(4):
                                self.mm(self.PS[po][:], hb[:, f, ts * 128:(ts + 1) * 128], wd[0][:, f, half * 512:(half + 1) * 512],
                                        f == 0, f == 3, [hdb[c % 2], wdb[0]], [self.PB[po]])
                            fa = facc[:, tl, half * 512:(half + 1) * 512]
                            gsc = self.gates[:, tg, e:e + 1]
                            if e == 0:
                                P.op("dve", lambda e_, fa=fa, po=po, gsc=gsc: e_.tensor_scalar(fa, self.PS[po][:], gsc, None, ALU.mult),
                                     [self.PB[po], self.gates_b[tg]], fa_bufs(tl))
                            else:
                                P.op("dve", lambda e_, fa=fa, po=po, gsc=gsc: e_.scalar_tensor_tensor(fa, self.PS[po][:], gsc, fa, ALU.mult, ALU.add),
                                     [self.PB[po], self.gates_b[tg]], fa_bufs(tl))
                if e + 1 < 16:
                    load_wd(e + 1)
            dst = self.out if last else self.xres

            def load(tl):
                tg = tok0 // 128 + tl
                P.dma("sp", xo[tl % 2], self.xres[tg * 128:(tg + 1) * 128, :], reads=[self.xres_b[tg]], writes=[xb[tl % 2]])
            load(0)
            for tl in range(tps):
                if tl + 1 < tps:
                    load(tl + 1)
                tg = tok0 // 128 + tl
                j = tl % 2
                P.op("dve", lambda e_, j=j, tl=tl: e_.scalar_tensor_tensor(xo[j], xo[j], ALPHA, facc[:, tl, :], ALU.mult, ALU.add),
                     [xb[j]] + fa_bufs(tl), [xb[j]])
                self.ln_tile(xo[j], xb[j], gbc, bbc, gbb, scr, j)
                P.dma("pool", dst[tg * 128:(tg + 1) * 128, :], xo[j], reads=[xb[j]], writes=[self.xres_b[tg]])
                if not last:
                    self.transpose_to_xt(xo[j], xb[j], tg, bank0=(tl % 2) * 2)
        P.barrier()

    def router(self, l):
        P, C, I = self.P, self.C, self.I
        NT = self.NT
        C.reset()
        Wr = C.bf(128, 8, 20); wrb = Buf()
        P.dma("pool", Wr, I[f"wr{l}"].rearrange("(k p) n -> p k n", p=128), writes=[wrb])
        brc = C.f32(128, 20)
        P.dma("sp", brc, I[f"br{l}"][0:1, :].broadcast_to([128, 20]), writes=[wrb])
        lg = C.f32(128, NT, 20); rb = Buf()
        for t in range(NT):
            q = t // 4
            pi = t % 4
            lgp = self.PS[pi][:, 0:20]
            for k in range(8):
                self.mm(lgp, self.XT[:, k, t * 128:(t + 1) * 128], Wr[:, k, :], k == 0, k == 7,
                        [self.XTb[q], wrb], [self.PB[pi]])
            P.op("dve", lambda e, t=t, lgp=lgp: e.tensor_tensor(lg[:, t, :], lgp, brc, ALU.add), [self.PB[pi], wrb], [rb])
        lgg = lg[:, :, 0:4]
        le = lg[:, :, 4:20].rearrange("p t (g j) -> p t g j", j=4)
        m = C.f32(128, NT); oh = C.f32(128, NT, 4); eg = C.f32(128, NT, 4); gs = C.f32(128, NT)
        m1 = C.f32(128, NT, 4); is1 = C.f32(128, NT, 4, 4); le2 = C.f32(128, NT, 4, 4); m2 = C.f32(128, NT, 4)
        sel2 = C.f32(128, NT, 4, 4); ee = C.f32(128, NT, 4, 4); ss = C.f32(128, NT, 4); w = C.f32(128, NT, 4)
        bc3 = lambda a: a.unsqueeze(2).broadcast_to([128, NT, 4])
        bc4 = lambda a: a.unsqueeze(3).broadcast_to([128, NT, 4, 4])
        R = [rb]
        dv = lambda fn: P.op("dve", fn, R, R)
        dv(lambda e: e.tensor_reduce(m, lgg, AX.X, ALU.max))
        dv(lambda e: e.tensor_tensor(oh, lgg, bc3(m), ALU.is_equal))
        dv(lambda e: e.tensor_tensor(eg, lgg, bc3(m), ALU.subtract))
        P.op("act", lambda e: e.activation(eg, eg, AF.Exp), R, R)
        dv(lambda e: e.tensor_reduce(gs, eg, AX.X, ALU.add))
        dv(lambda e: e.reciprocal(gs, gs))
        dv(lambda e: e.tensor_reduce(m1, le, AX.X, ALU.max))
        dv(lambda e: e.tensor_tensor(is1, le, bc4(m1), ALU.is_equal))
        dv(lambda e: e.scalar_tensor_tensor(le2, is1, -1.0e9, le, ALU.mult, ALU.add))
        dv(lambda e: e.tensor_reduce(m2, le2, AX.X, ALU.max))
        dv(lambda e: e.tensor_tensor(sel2, le, bc4(m2), ALU.is_ge))
        dv(lambda e: e.tensor_tensor(ee, le, bc4(m1), ALU.subtract))
        P.op("act", lambda e: e.activation(ee, ee, AF.Exp), R, R)
        dv(lambda e: e.tensor_tensor(ee, ee, sel2, ALU.mult))
        dv(lambda e: e.tensor_reduce(ss, ee, AX.X, ALU.add))
        dv(lambda e: e.reciprocal(ss, ss))
        dv(lambda e: e.tensor_tensor(w, ss, oh, ALU.mult))
        dv(lambda e: e.tensor_tensor(w, w, bc3(gs), ALU.mult))
        gv = self.gates[:].rearrange("p t (g j) -> p t g j", j=4)
        P.op("dve", lambda e: e.tensor_tensor(gv, ee, bc4(w), ALU.mult), R, R + self.gates_b)
        P.barrier()

    def run_pipeline(self, pairs, new_S, new_E, depth=3):
        n = len(pairs)
        slots = [None] * n

        def do_score(i):
            slots[i] = new_S()
            pairs[i]["score"](slots[i])
        for i in range(min(depth, n)):
            do_score(i)
        for i in range(n):
            Ei, Ebi = new_E()
            pairs[i]["post"](slots[i], Ei, Ebi)
            if i + depth < n:
                do_score(i + depth)
            pairs[i]["pv"](Ei, Ebi)
            if pairs[i].get("after"):
                pairs[i]["after"]()

    def proj_chunk(self, pi, M, Wb, wbuf, col0, tq):
        for k in range(8):
            self.mm(self.PS[pi][0:M, :], Wb[:, k, col0:col0 + M], self.XT[:, k, tq * 512:(tq + 1) * 512],
                    k == 0, k == 7, [wbuf, self.XTb[tq]], [self.PB[pi]])

    def proj_xq(self, win, col0):
        P, C = self.P, self.C
        C.reset()
        stg = [C.bf(128, 512) for _ in range(2)]; stgb = [Buf() for _ in range(2)]
        Wx = C.bf(128, 8, 256); Wxb = Buf()
        P.dma("pool", Wx, win[:, :, col0:col0 + 256], writes=[Wxb])
        ns = 0
        for hh in range(2):
            for tq in range(self.NQ):
                pi = ns % 2
                self.proj_chunk(pi, 128, Wx, Wxb, hh * 128, tq)
                sg_ = stg[ns % 2]; sb_ = stgb[ns % 2]; ns += 1
                P.op("act", lambda e, sg_=sg_, pi=pi: e.copy(sg_, self.PS[pi][:]), [self.PB[pi]], [sb_])
                P.dma("sp", self.pT[2048 + hh * 128:2048 + (hh + 1) * 128, tq * 512:(tq + 1) * 512], sg_, reads=[sb_], writes=[self.pT_b])
        P.barrier()
        C.reset()

    def rope_tables(self, tq, npart, inv, posb_t, posf, kf, ki, Ct, St, tb, pos_src=None, width=512):
        P = self.P
        src = pos_src if pos_src is not None else self.I["pos"][0:1, tq * 512:(tq + 1) * 512]
        P.dma("sp", posb_t, src.broadcast_to([npart, width]), writes=[tb])
        P.op("dve", lambda e: e.tensor_copy(posf, posb_t), [tb], [tb])
        P.op("dve", lambda e: e.tensor_scalar(posf, posf, inv, None, ALU.mult), [tb, self.cB], [tb])
        TWO_PI = 2.0 * math.pi
        C1 = 6.28125
        C2 = TWO_PI - C1
        for out_t, shift in ((St, 0.0), (Ct, math.pi / 2)):
            P.op("dve", lambda e, shift=shift: e.tensor_scalar(kf, posf, shift, 1.0 / TWO_PI, ALU.add, ALU.mult), [tb], [tb])
            P.op("dve", lambda e: e.tensor_copy(ki, kf), [tb], [tb])
            P.op("dve", lambda e: e.tensor_copy(kf, ki), [tb], [tb])
            P.op("dve", lambda e, out_t=out_t, shift=shift: e.tensor_scalar(out_t, posf, shift, None, ALU.add), [tb], [tb])
            P.op("dve", lambda e, out_t=out_t: e.scalar_tensor_tensor(out_t, kf, -C1, out_t, ALU.mult, ALU.add), [tb], [tb])
            P.op("dve", lambda e, out_t=out_t: e.scalar_tensor_tensor(out_t, kf, -C2, out_t, ALU.mult, ALU.add), [tb], [tb])
            P.op("dve", lambda e, out_t=out_t: e.tensor_scalar(kf, out_t, math.pi, -TWO_PI, ALU.is_gt, ALU.mult), [tb], [tb])
            P.op("dve", lambda e, out_t=out_t: e.tensor_tensor(out_t, out_t, kf, ALU.add), [tb], [tb])
            P.op("dve", lambda e, out_t=out_t: e.tensor_scalar(kf, out_t, -math.pi, TWO_PI, ALU.is_lt, ALU.mult), [tb], [tb])
            P.op("dve", lambda e, out_t=out_t: e.tensor_tensor(out_t, out_t, kf, ALU.add), [tb], [tb])
            P.op("dve", lambda e, out_t=out_t: e.tensor_scalar(out_t, out_t, 3.1415925, -3.1415925, ALU.min, ALU.max), [tb], [tb])
            P.op("act", lambda e, out_t=out_t: e.activation(out_t, out_t, AF.Sin), [tb], [tb])

    def xattn(self, l):
        P, C, I, S = self.P, self.C, self.I, self.S
        C.reset()
        Wkv = C.bf(128, 8, 512); wb = Buf()
        P.dma("pool", Wkv, I[f"wkv{l}"].rearrange("(k p) n -> p k n", p=128), writes=[wb])
        mkT = C.bf(64, 4, 256); mkb = Buf()
        mv = C.bf(128, 2, 4, 128); mvb = Buf()
        xq = [C.bf(64, S) for _ in range(2)]; xqb = [Buf() for _ in range(2)]
        E = [C.bf(128, 512) for _ in range(3)]; Eb = [Buf() for _ in range(3)]
        rd = [C.f32(64, 512) for _ in range(2)]; rdb = [Buf() for _ in range(2)]
        P.op("pool", lambda e: e.memset(mv, 1.0), [], [mvb])
        for h in range(4):
            pi = h % 2
            for k in range(8):
                self.mm(self.PS[pi][0:64, 0:256], Wkv[:, k, h * 64:(h + 1) * 64], self.memT[:, k, :], k == 0, k == 7,
                        [wb, self.memT_b], [self.PB[pi]])
            P.op("act", lambda e, h=h, pi=pi: e.copy(mkT[:, h, :], self.PS[pi][0:64, 0:256]), [self.PB[pi]], [mkb])
        for mt in range(2):
            pi = 2 + mt
            for k in range(8):
                self.mm(self.PS[pi][:, 0:256], self.memT[:, k, mt * 128:(mt + 1) * 128], Wkv[:, k, 256:512], k == 0, k == 7,
                        [wb, self.memT_b], [self.PB[pi]])
            src = self.PS[pi][:, 0:256].rearrange("p (h d) -> p h d", d=64)
            P.op("act", lambda e, mt=mt, src=src: e.copy(mv[:, mt, :, 0:64], src), [self.PB[pi]], [mvb])
        ne = 0
        for h in range(4):
            xh = xq[h % 2]; xhb = xqb[h % 2]
            P.dma("sp", xh, self.pT[2048 + 64 * h:2048 + 64 * (h + 1), :], reads=[self.pT_b], writes=[xhb])
            for tq in range(self.NQ):
                po = 4 + (tq % 2)
                for mt in range(2):
                    ps = mt + 2 * (tq % 2)
                    self.mm(self.PS[ps][:], mkT[:, h, mt * 128:(mt + 1) * 128], xh[:, tq * 512:(tq + 1) * 512], True, True,
                            [mkb, xhb], [self.PB[ps]])
                    Ei = E[ne % 3]; Ebi = Eb[ne % 3]; ne += 1
                    self.act(Ei, self.PS[ps][:], AF.Exp, [self.PB[ps]], [Ebi], scale=0.125)
                    self.mm(self.PS[po][:], mv[:, mt, h, :], Ei, mt == 0, mt == 1, [mvb, Ebi], [self.PB[po]])
                r = rd[tq % 2]; rb = rdb[tq % 2]
                self.act(r, self.PS[po][64:128, :], AF.Ln, [self.PB[po]], [rb])
                self.act(r, r, AF.Exp, [rb], [rb], scale=-1.0)
                dst = self.CT[64 * (h % 2):64 * (h % 2) + 64, 6 + h // 2, tq * 512:(tq + 1) * 512]
                P.op("dve", lambda e, r=r, po=po, dst=dst: e.tensor_tensor(dst, self.PS[po][0:64, :], r, ALU.mult),
                     [self.PB[po], rb], [self.CTb[6 + h // 2][tq]])
        P.barrier()

    def mixer_conv(self, l):
        P, C, I, S = self.P, self.C, self.I, self.S
        NQ = self.NQ
        win = I[f"win{l}"].rearrange("(k p) n -> p k n", p=128)
        self.proj_xq(win, 1536)
        UW = 30 + S
        U = C.bf(128, 6, UW); Ub = [Buf() for _ in range(6)]
        cb = C.f32(128, 12); cdw = C.f32(128, 186); cdb = C.f32(128, 6); clg = C.f32(128, 6); clb = C.f32(128, 6)
        pb = Buf()
        for t_, n_ in ((cb, "cbin"), (cdw, "cdw"), (cdb, "cdb"), (clg, "clg"), (clb, "clb")):
            P.dma("sp", t_, I[f"{n_}{l}"], writes=[pb])
        mark = C.off
        Wc = [C.bf(128, 8, 256) for _ in range(2)]; Wcb = [Buf() for _ in range(2)]
        sig = [C.f32(128, 512) for _ in range(2)]; sigb = [Buf() for _ in range(2)]
        for c in range(6):
            P.op("pool", lambda e, c=c: e.memset(U[:, c, 0:30], 0.0), [], [Ub[c]])
        for c in range(6):
            W_ = Wc[c % 2]; Wb_ = Wcb[c % 2]
            P.dma("pool", W_[:, :, 0:128], win[:, :, c * 128:(c + 1) * 128], writes=[Wb_])
            P.dma("pool", W_[:, :, 128:256], win[:, :, 768 + c * 128:768 + (c + 1) * 128], writes=[Wb_])
            for tq in range(NQ):
                p1, p2 = 2 + (tq % 2) * 2, 3 + (tq % 2) * 2
                self.proj_chunk(p1, 128, W_, Wb_, 0, tq)
                self.proj_chunk(p2, 128, W_, Wb_, 128, tq)
                sg_ = sig[tq % 2]; sb_ = sigb[tq % 2]
                self.act(sg_, self.PS[p2][:], AF.Sigmoid, [self.PB[p2], pb], [sb_], bias=cb[:, 6 + c:7 + c], scale=1.0)
                dst = U[:, c, 30 + tq * 512:30 + (tq + 1) * 512]
                P.op("dve", lambda e, dst=dst, p1=p1, c=c, sg_=sg_: e.scalar_tensor_tensor(dst, self.PS[p1][:], cb[:, c:c + 1], sg_, ALU.add, ALU.mult),
                     [self.PB[p1], sb_, pb], [Ub[c]])
        P.barrier()
        C.off = mark
        diag = self.A_t[:, 0:186 * 128].rearrange("p (j m) -> p j m", m=128)
        dgb = Buf()
        for idx in range(186):
            eng = "dve" if idx % 2 == 0 else "pool"
            P.op(eng, lambda e, idx=idx: e.tensor_scalar(diag[:, idx, :], self.identB, cdw[:, idx:idx + 1], None, ALU.mult),
                 [self.cB, pb] , [dgb] + self.XTb)
        ysb = [C.f32(128, 512) for _ in range(2)]; ysbb = [Buf() for _ in range(2)]
        ysq = [C.f32(128, 512) for _ in range(2)]; ysqb = [Buf() for _ in range(2)]
        mean = C.f32(128, 512); rstd = C.f32(128, 512); msq = C.f32(128, 512); stb = Buf()
        tmp = [C.f32(128, 512) for _ in range(2)]; tmpb = [Buf() for _ in range(2)]
        n2 = 0
        for tq in range(NQ):
            for c in range(6):
                for j in range(31):
                    self.mm(self.PS[c][:], diag[:, c * 31 + j, :], U[:, c, tq * 512 + j:tq * 512 + j + 512], j == 0, j == 30,
                            [dgb, Ub[c]], [self.PB[c]])
                y_ = ysb[n2 % 2]; yb_ = ysbb[n2 % 2]; q_ = ysq[n2 % 2]; qb_ = ysqb[n2 % 2]; n2 += 1
                self.act(y_, self.PS[c][:], AF.Identity, [self.PB[c], pb], [yb_], bias=cdb[:, c:c + 1], scale=1.0)
                P.op("pool", lambda e, y_=y_, q_=q_: e.tensor_tensor(q_, y_, y_, ALU.mult), [yb_], [qb_])
                self.mm(self.PS[6][:], self.onesF, y_, c == 0, c == 5, [yb_, self.cB], [self.PB[6]])
                self.mm(self.PS[7][:], self.onesF, q_, c == 0, c == 5, [qb_, self.cB], [self.PB[7]])
            P.op("dve", lambda e: e.tensor_scalar(mean, self.PS[6][:], 1.0 / 768, None, ALU.mult), [self.PB[6]], [stb])
            P.op("dve", lambda e: e.tensor_tensor(msq, mean, mean, ALU.mult), [stb], [stb])
            P.op("dve", lambda e: e.scalar_tensor_tensor(rstd, self.PS[7][:], 1.0 / 768, msq, ALU.mult, ALU.subtract), [self.PB[7], stb], [stb])
            P.op("act", lambda e: e.activation(rstd, rstd, AF.Sqrt, bias=self.eps_t[:, 0:1], scale=1.0), [stb, self.cB], [stb])
            P.op("dve", lambda e: e.reciprocal(rstd, rstd), [stb], [stb])
            for c in range(6):
                t_ = tmp[c % 2]; tb_ = tmpb[c % 2]
                P.op("dve", lambda e, t_=t_, c=c: e.scalar_tensor_tensor(t_, self.PS[c][:], cdb[:, c:c + 1], mean, ALU.add, ALU.subtract),
                     [self.PB[c], stb, pb], [tb_])
                P.op("dve", lambda e, t_=t_: e.tensor_tensor(t_, t_, rstd, ALU.mult), [tb_, stb], [tb_])
                dst = self.CT[:, c, tq * 512:(tq + 1) * 512]
                self.act(dst, t_, AF.Silu, [tb_, pb], [self.CTb[c][tq]], bias=clb[:, c:c + 1], scale=clg[:, c:c + 1])
        P.barrier()
        self.xattn(l)


def _consts(S):
    c = {}
    c["c_ident"] = np.eye(128, dtype=np.float32)
    cr = np.zeros((128, 4 * 128 + 4), np.float32)
    inv_n = (np.float32(500000.0) ** (-np.arange(0, 16, 2, dtype=np.float32) / np.float32(16))).astype(np.float32)
    inv_m = (np.float32(10000.0) ** (-np.arange(0, 32, 2, dtype=np.float32) / np.float32(32))).astype(np.float32)
    for base in (0, 64):
        for i in range(8):
            cr[base + i + 8, base + i] = -1.0
            cr[base + i, base + i + 8] = 1.0
            cr[base + i, 512] = inv_n[i]
            cr[base + i + 8, 512] = inv_n[i]
    for i in range(64, 80):
        cr[i + 16, 128 + i] = -1.0
        cr[i, 128 + i + 16] = 1.0
        cr[i, 513] = inv_m[i - 64]
        cr[i + 16, 513] = inv_m[i - 64]
    c["c_rope"] = cr
    eb = np.zeros((64, S), np.float32)
    for key in range(S):
        eb[(key // 64) % 64, key] = 1.0
    c["c_ebig"] = eb
    ov = np.zeros((256, 64), np.float32)
    ncmp = (S - 32) // 16 + 1
    for n in range(ncmp):
        for j in range(S // 64):
            if 16 * n < 64 * j + 64 and 16 * n + 32 > 64 * j:
                ov[n, j] = 1.0
    c["c_ov"] = ov
    fb = np.zeros((S, 64), np.float32)
    for t in range(S):
        cur = t // 64
        fb[t, cur + 1:] = -1.0e9
        fb[t, cur] = 1.0e9
        if cur >= 1:
            fb[t, cur - 1] = 2.0e9
        fb[t, 0] = 3.0e9
    c["c_fb"] = fb
    sel = np.zeros((3, 3 * 64), np.float32)
    for r in range(3):
        sel[r, r * 64:(r + 1) * 64] = 1.0
    c["c_sel"] = sel
    return c


def layer_inputs(l, kind, j, w):
    f = lambda a: np.ascontiguousarray(a, dtype=np.float32)
    d = {}
    d[f"wkv{l}"] = f(w["mem_w_kv"][l]); d[f"wo{l}"] = f(w["w_out"][l])
    d[f"lng{l}"] = f(w["ln_g"][l]); d[f"lnb{l}"] = f(w["ln_b"][l])
    d[f"wr{l}"] = f(np.concatenate([w["moe_w_grp"][l], w["moe_w_exp"][l]], axis=1))
    d[f"br{l}"] = f(np.concatenate([w["moe_b_grp"][l], w["moe_b_exp"][l]])[None, :])
    d[f"wg{l}"] = f(w["moe_w_gate"][l]); d[f"wu{l}"] = f(w["moe_w_up"][l]); d[f"wd{l}"] = f(w["moe_w_down"][l])
    if kind == 2:
        d[f"win{l}"] = f(w["conv_w_in"][j])
        d[f"cbin{l}"] = f(w["conv_b_in"][j].reshape(12, 128).T)
        d[f"cdw{l}"] = f(w["conv_dw_w"][j].T.reshape(6, 128, 31).transpose(1, 0, 2).reshape(128, 186))
        d[f"cdb{l}"] = f(w["conv_dw_b"][j].reshape(6, 128).T)
        d[f"clg{l}"] = f(w["conv_ln_g"][j].reshape(6, 128).T)
        d[f"clb{l}"] = f(w["conv_ln_b"][j].reshape(6, 128).T)
    elif kind == 1:
        d[f"win{l}"] = f(w["mla_w_in"][j])
        d[f"qn{l}"] = f(w["mla_q_norm"][j].reshape(2, 128).T); d[f"kvn{l}"] = f(w["mla_kv_norm"][j][:, None])
        d[f"wuq{l}"] = f(w["mla_w_uq"][j]); d[f"wukv{l}"] = f(w["mla_w_ukv"][j])
    else:
        d[f"win{l}"] = f(w["nsa_w_in"][j])
        d[f"cpe{l}"] = f(np.transpose(w["nsa_cmp_pe"][j], (0, 2, 1)))
        d[f"cw1{l}"] = f(w["nsa_cmp_w1"][j].reshape(2, 32, 64, 128).transpose(0, 2, 1, 3))
        d[f"cw2{l}"] = f(w["nsa_cmp_w2"][j])
    return d


_NC_CACHE = {}


def run_model(S, kinds, js, x, mem, positions, w, n_cores=NCORES):
    key = (S, tuple(kinds))
    if key not in _NC_CACHE:
        _NC_CACHE[key] = Builder(S, kinds).build()
    nc = _NC_CACHE[key]
    shared = _consts(S)
    for l, (kind, j) in enumerate(zip(kinds, js)):
        shared.update(layer_inputs(l, kind, j, w))
    in_maps = []
    for b in range(n_cores):
        m = dict(shared)
        m["x"] = np.ascontiguousarray(x[b], dtype=np.float32)
        m["mem"] = np.ascontiguousarray(mem[b], dtype=np.float32)
        m["pos"] = np.ascontiguousarray(positions[b][None, :], dtype=np.int32)
        pe = np.asarray(positions[b])[31::16]
        pe = np.concatenate([pe, np.repeat(pe[-1:], 256 - len(pe))])[:256]
        m["pos_end"] = np.ascontiguousarray(pe[None, :], dtype=np.int32)
        in_maps.append(m)
    import os
    if os.environ.get("K_TRACE"):
        res = run_bass_kernel_spmd(nc, in_maps, core_ids=list(range(n_cores)), trace=True)
        print("EXEC_TIME_NS", res.exec_time_ns)
        import pickle
        try:
            it = res.instructions_and_trace
            print("IT type", type(it), (len(it) if hasattr(it, "__len__") else ""))
            pj = res.profile_json
            print("PJ type", type(pj), (list(pj.keys())[:20] if isinstance(pj, dict) else str(pj)[:300]))
            pickle.dump({"it": it, "pj": pj}, open("trace_dump.pkl", "wb"))
        except Exception as ex:
            print("trace dump failed", ex)
    else:
        res = run_bass_kernel_spmd(nc, in_maps, core_ids=list(range(n_cores)))
    import os
    if os.environ.get("DBG_CT"):
        np.save("dbg_ct.npy", np.asarray(res.results[0]["dbg"]).astype(np.float32))
    if os.environ.get("DBG_GL"):
        a = np.asarray(res.results[0]["glT"]); print("glT", a.dtype, a.shape); np.save("d_glT.npy", a)
    if os.environ.get("DBG_NSA"):
        for nme in res.results[0]:
            if nme.startswith("d_"):
                a = np.asarray(res.results[0][nme])
                print(nme, a.dtype, a.shape)
                np.save(nme + ".npy", a.astype(np.float32))
    return np.stack([np.asarray(r["out"]) for r in res.results], axis=0)


def kernel(**inputs):
    x = np.asarray(inputs["x"]); mem = np.asarray(inputs["mem"]); positions = np.asarray(inputs["positions"])
    w = {k: np.asarray(v) for k, v in inputs.items() if k not in ("x", "mem", "positions")}
    kinds = [0, 1, 2, 0]
    js = [0, 0, 0, 1]
    out = run_model(x.shape[1], kinds, js, x, mem, positions, w)
    return out.astype(np.float32)


def _mixer_mla(self, l):
    P, C, I, S = self.P, self.C, self.I, self.S
    NT, NQ = self.NT, self.NQ
    win = I[f"win{l}"].rearrange("(k p) n -> p k n", p=128)
    self.proj_xq(win, 416)
    stg = [C.bf(128, 512) for _ in range(2)]; stgb = [Buf() for _ in range(2)]
    qn = C.f32(128, 2); kvn = C.f32(128, 2); nb = Buf()
    P.dma("sp", qn, I[f"qn{l}"], writes=[nb]); P.dma("sp", kvn[:, 0:1], I[f"kvn{l}"], writes=[nb])
    CQ = C.bf(128, 2, S); CKV = C.bf(128, S); KRr = C.bf(96, S)
    cqb = [Buf() for _ in range(NQ)]; krb = Buf()
    P.op("pool", lambda e: e.memset(KRr[0:64, :], 0.0), [], [krb])
    mark = C.off
    Win = C.bf(128, 8, 416); winb = Buf()
    P.dma("pool", Win, win[:, :, 0:416], writes=[winb])
    tk = [C.f32(128, 416) for _ in range(2)]; tkb = [Buf() for _ in range(2)]
    junk = C.f32(128, 256); ssq = [C.f32(128, 4) for _ in range(2)]
    for t in range(NT):
        pi = t % 2
        j = t % 2
        for k in range(8):
            self.mm(self.PS[pi][:, 0:416], self.XT[:, k, t * 128:(t + 1) * 128], Win[:, k, :], k == 0, k == 7,
                    [winb, self.XTb[t // 4]], [self.PB[pi]])
        sq = ssq[j]
        self.act(junk, self.PS[pi][:, 0:256], AF.Square, [self.PB[pi]], [tkb[j]], accum_out=sq[:, 0:1])
        self.act(junk[:, 0:128], self.PS[pi][:, 256:384], AF.Square, [self.PB[pi]], [tkb[j]], accum_out=sq[:, 1:2])
        self.act(sq[:, 0:1], sq[:, 0:1], AF.Sqrt, [tkb[j], self.cB], [tkb[j]], bias=self.eps_t[:, 1:2], scale=1.0 / 256)
        self.act(sq[:, 1:2], sq[:, 1:2], AF.Sqrt, [tkb[j], self.cB], [tkb[j]], bias=self.eps_t[:, 1:2], scale=1.0 / 128)
        P.op("dve", lambda e, sq=sq: e.reciprocal(sq[:, 0:2], sq[:, 0:2]), [tkb[j]], [tkb[j]])
        P.op("dve", lambda e, j=j, pi=pi, sq=sq: e.tensor_scalar(tk[j][:, 0:256], self.PS[pi][:, 0:256], sq[:, 0:1], None, ALU.mult), [self.PB[pi], tkb[j]], [tkb[j]])
        P.op("dve", lambda e, j=j, pi=pi, sq=sq: e.tensor_scalar(tk[j][:, 256:384], self.PS[pi][:, 256:384], sq[:, 1:2], None, ALU.mult), [self.PB[pi], tkb[j]], [tkb[j]])
        P.op("dve", lambda e, j=j, pi=pi: e.tensor_copy(tk[j][:, 384:416], self.PS[pi][:, 384:416]), [self.PB[pi], tkb[j]], [tkb[j]])
        pt = 2 + (t % 2)
        for bi, (c0, c1) in enumerate(((0, 128), (128, 256), (256, 384), (320, 416))):
            self.tr(self.PS[pt][0:c1 - c0, bi * 128:(bi + 1) * 128], tk[j][:, c0:c1], self.identF, [tkb[j], self.cB], [self.PB[pt]])
        tsl = slice(t * 128, (t + 1) * 128)
        q = t // 4
        self.act(CQ[:, 0, tsl], self.PS[pt][:, 0:128], AF.Copy, [self.PB[pt], nb], [cqb[q]], scale=qn[:, 0:1])
        self.act(CQ[:, 1, tsl], self.PS[pt][:, 128:256], AF.Copy, [self.PB[pt], nb], [cqb[q]], scale=qn[:, 1:2])
        self.act(CKV[:, tsl], self.PS[pt][:, 256:384], AF.Copy, [self.PB[pt], nb], [cqb[q]], scale=kvn[:, 0:1])
        P.op("dve", lambda e, pt=pt, tsl=tsl: e.tensor_copy(KRr[64:96, tsl], self.PS[pt][64:96, 384:512]), [self.PB[pt]], [cqb[q], krb])
    P.barrier()
    import os
    STOP = int(os.environ.get("MLA_STOP", "9"))
    if STOP == 1:
        self.xattn(l); return
    C.off = mark
    Wuq = C.bf(128, 2, 1152); Wukv = C.bf(128, 1536); ub = Buf()
    P.dma("pool", Wuq, I[f"wuq{l}"].rearrange("(k p) n -> p k n", p=128), writes=[ub])
    P.dma("pool", Wukv, I[f"wukv{l}"], writes=[ub])
    posb_t = C.f32(96, 512, dt=I32); posf = C.f32(96, 512); kf = C.f32(96, 512); ki = posb_t
    Ct = C.f32(96, 512); St = C.f32(96, 512); tb = Buf()
    qraw = [C.bf(96, 512) for _ in range(2)]; qrb = [Buf() for _ in range(2)]
    t1 = [C.f32(96, 512) for _ in range(2)]; t2 = [C.f32(96, 512) for _ in range(2)]; t12b = [Buf() for _ in range(2)]
    qo = [C.bf(96, 512) for _ in range(2)]; qob = [Buf() for _ in range(2)]
    vst = [C.bf(128, 768) for _ in range(2)]; vstb = [Buf() for _ in range(2)]
    Rm = self.RmB[0:96, 0:96]
    n = 0
    for tq in range(NQ):
        csl = slice(tq * 512, (tq + 1) * 512)
        SK = os.environ.get("MLA_SKIP", "")
        if "r" not in SK:
            self.rope_tables(tq, 96, self.invF[0:96, 1:2], posb_t, posf, kf, ki, Ct, St, tb)
        j = n % 2; n += 1
        if "k" not in SK:
            self.mm(self.PS[0][0:96, :], Rm, KRr[:, csl], True, True, [self.cB, cqb[tq], krb], [self.PB[0]])
            P.op("dve", lambda e, j=j, csl=csl: e.tensor_tensor(t1[j], KRr[:, csl], Ct, ALU.mult), [cqb[tq], krb, tb], [t12b[j]])
            P.op("dve", lambda e, j=j: e.tensor_tensor(t2[j], self.PS[0][0:96, :], St, ALU.mult), [self.PB[0], tb], [t12b[j]])
            P.op("pool", lambda e, j=j: e.tensor_tensor(qo[j], t1[j], t2[j], ALU.add), [t12b[j]], [qob[j]])
            P.dma("sp", self.pT[1920:1952, csl], qo[j][64:96, :], reads=[qob[j]], writes=[self.pT_b])
        for h in range(int(os.environ.get("MLA_NH", "12")) if "q" not in SK else 0):
            pq, pr = 1 + (h % 2) * 2, 2 + (h % 2) * 2
            for rc in range(2):
                self.mm(self.PS[pq][0:96, :], Wuq[:, rc, h * 96:(h + 1) * 96], CQ[:, rc, csl], rc == 0, rc == 1, [ub, cqb[tq]], [self.PB[pq]])
            j = n % 2; n += 1
            if "a" in SK:
                continue
            self.act(qraw[j], self.PS[pq][0:96, :], AF.Copy, [self.PB[pq]], [qrb[j]])
            if "b" in SK:
                continue
            self.mm(self.PS[pr][0:96, :], Rm, qraw[j], True, True, [self.cB, qrb[j]], [self.PB[pr]])
            if "c" in SK:
                continue
            VAR = os.environ.get("MLA_VAR", "0")
            if VAR == "0":
                P.op("dve", lambda e, j=j, pq=pq: e.tensor_tensor(t1[j], self.PS[pq][0:96, :], Ct, ALU.mult), [self.PB[pq], tb], [t12b[j]])
            elif VAR == "1":
                P.op("dve", lambda e, j=j, pq=pq: e.tensor_tensor(t1[j], self.PS[pq][0:96, :], St, ALU.mult), [self.PB[pq], tb], [t12b[j]])
            elif VAR == "2":
                P.op("dve", lambda e, j=j, pq=pq: e.tensor_tensor(t1[j], self.PS[pq][0:96, :], Ct, ALU.mult), [self.PB[pq], tb, qrb[j]], [t12b[j]])
            elif VAR == "3":
                P.op("dve", lambda e, j=j, pq=pq: e.tensor_tensor(t1[j], qraw[j], Ct, ALU.mult), [qrb[j], tb], [t12b[j]])
            if "e" in SK:
                continue
            P.op("dve", lambda e, j=j, pr=pr: e.tensor_tensor(t2[j], self.PS[pr][0:96, :], St, ALU.mult), [self.PB[pr], tb], [t12b[j]])
            if "f" in SK:
                continue
            P.op("dve" if "p" in SK else "pool", lambda e, j=j: e.tensor_tensor(qo[j], t1[j], t2[j], ALU.add), [t12b[j]], [qob[j]])
            if "d" not in SK:
                P.dma("sp", self.pT[h * 96:(h + 1) * 96, csl], qo[j], reads=[qob[j]], writes=[self.pT_b])
            if "n" in SK:
                continue
            pk = 5 + (h % 2)
            self.mm(self.PS[pk][0:64, :], Wukv[:, h * 128:h * 128 + 64], CKV[:, csl], True, True, [ub, cqb[tq]], [self.PB[pk]])
            sj = stg[h % 2]; sjb = stgb[h % 2]
            self.act(sj[0:64, :], self.PS[pk][0:64, :], AF.Copy, [self.PB[pk]], [sjb])
            P.dma("sp", self.pT[1152 + h * 64:1152 + (h + 1) * 64, csl], sj[0:64, :], reads=[sjb], writes=[self.pT_b])
        Wv = Wukv[:].rearrange("p (h c) -> p h c", c=128)
        for ts in range(4 if "v" not in SK else 0):
            t = tq * 4 + ts
            j = t % 2
            for hv in range(2):
                pv = 6 + hv
                self.mm(self.PS[pv][:, 0:384], CKV[:, t * 128:(t + 1) * 128], Wv[:, hv * 6:(hv + 1) * 6, 64:128], True, True, [ub, cqb[tq]], [self.PB[pv]])
                P.op("dve", lambda e, j=j, hv=hv, pv=pv: e.tensor_copy(vst[j][:, hv * 384:(hv + 1) * 384], self.PS[pv][:, 0:384]), [self.PB[pv]], [vstb[j]])
            P.dma("sp", self.vtok[t * 128:(t + 1) * 128, :], vst[j], reads=[vstb[j]], writes=[self.vtok_b])
    P.barrier()
    if STOP == 2:
        self.xattn(l); return
    C.reset()
    kT = [C.bf(96, S) for _ in range(2)]; qT = [C.bf(96, S) for _ in range(2)]
    vA = [C.bf(128, NT, 128) for _ in range(2)]
    hb = [Buf() for _ in range(2)]
    E = [C.bf(128, 512) for _ in range(6)]; Eb = [Buf() for _ in range(6)]
    rd = [C.f32(64, 512) for _ in range(2)]; rdb = [Buf() for _ in range(2)]
    for j in range(2):
        P.op("pool", lambda e, j=j: e.memset(vA[j][:, :, 64:128], 1.0), [], [hb[j]])
    scale = 1.0 / math.sqrt(96.0)
    ne = 0
    vt = self.vtok.rearrange("(t p) c -> p t c", p=128)

    def load_head(h):
        j = h % 2
        P.dma("sp", qT[j], self.pT[h * 96:(h + 1) * 96, :], reads=[self.pT_b], writes=[hb[j]])
        P.dma("sp", kT[j][0:64, :], self.pT[1152 + h * 64:1152 + (h + 1) * 64, :], reads=[self.pT_b], writes=[hb[j]])
        P.dma("sp", kT[j][64:96, :], self.pT[1920:1952, :], reads=[self.pT_b], writes=[hb[j]])
        with self.nc.allow_non_contiguous_dma(reason="per-head V gather (128B runs)"):
            for t0 in range(0, NT, 4):
                P.dma("sp", vA[j][:, t0:t0 + 4, 0:64], vt[:, t0:t0 + 4, h * 64:(h + 1) * 64], reads=[self.vtok_b], writes=[hb[j]])
    st_ = {"ne": 0, "ns": 0}

    def new_E():
        i = st_["ne"] % 6; st_["ne"] += 1
        return E[i], Eb[i]

    def new_S():
        i = st_["ns"] % 4; st_["ns"] += 1
        return i
    load_head(0)
    for h in range(12):
        if h + 1 < 12:
            load_head(h + 1)
        j = h % 2
        pairs = []
        for tq in range(NQ):
            po = 4 + (tq % 2)
            nk = 4 * tq + 4
            for kt in range(nk):
                def score(ps, kt=kt, tq=tq, j=j):
                    self.mm(self.PS[ps][:], kT[j][:, kt * 128:(kt + 1) * 128], qT[j][:, tq * 512:(tq + 1) * 512], True, True, [hb[j]], [self.PB[ps]])

                def post(ps, Ei, Ebi, kt=kt, tq=tq):
                    self.act(Ei, self.PS[ps][:], AF.Exp, [self.PB[ps]], [Ebi], scale=scale)
                    if kt >= 4 * tq:
                        base = tq * 512 - kt * 128
                        P.op("pool", lambda e: e.affine_select(Ei, Ei, [[1, 512]], ALU.is_ge, 0.0, base=base, channel_multiplier=-1), [Ebi], [Ebi])

                def pv(Ei, Ebi, kt=kt, nk=nk, po=po, j=j):
                    self.mm(self.PS[po][:], vA[j][:, kt, :], Ei, kt == 0, kt == nk - 1, [hb[j], Ebi], [self.PB[po]])
                d = {"score": score, "post": post, "pv": pv}
                if kt == nk - 1:
                    def after(tq=tq, po=po, h=h):
                        r = rd[tq % 2]; rb = rdb[tq % 2]
                        self.act(r, self.PS[po][64:128, :], AF.Ln, [self.PB[po]], [rb])
                        self.act(r, r, AF.Exp, [rb], [rb], scale=-1.0)
                        dst = self.CT[64 * (h % 2):64 * (h % 2) + 64, h // 2, tq * 512:(tq + 1) * 512]
                        P.op("dve", lambda e: e.tensor_tensor(dst, self.PS[po][0:64, :], r, ALU.mult),
                             [self.PB[po], rb], [self.CTb[h // 2][tq]])
                    d["after"] = after
                pairs.append(d)
        self.run_pipeline(pairs, new_S, new_E)
    P.barrier()
    self.xattn(l)


Builder.mixer_mla = _mixer_mla


def _mixer_nsa(self, l):
    P, C, I, S = self.P, self.C, self.I, self.S
    NT, NQ = self.NT, self.NQ
    NCMP = (S - 32) // 16 + 1
    NCT = (NCMP + 127) // 128
    NCP = NCT * 128
    win = I[f"win{l}"].rearrange("(k p) n -> p k n", p=128)
    self.proj_xq(win, 2340)
    stg = [C.bf(128, 512) for _ in range(2)]; stgb = [Buf() for _ in range(2)]
    Wq = C.bf(128, 8, 768); Wk = C.bf(128, 8, 1024); Wg = C.bf(128, 8, 36); Wv = C.bf(128, 8, 512); wb = Buf()
    P.dma("pool", Wq, win[:, :, 0:768], writes=[wb])
    P.dma("pool", Wk[:, :, 0:256], win[:, :, 1280:1536], writes=[wb])
    P.dma("pool", Wk[:, :, 256:512], win[:, :, 1792:2048], writes=[wb])
    P.dma("pool", Wk[:, :, 512:1024], win[:, :, 768:1280], writes=[wb])
    P.dma("pool", Wg, win[:, :, 2304:2340], writes=[wb])
    P.dma("pool", Wv[:, :, 0:256], win[:, :, 1536:1792], writes=[wb])
    P.dma("pool", Wv[:, :, 256:512], win[:, :, 2048:2304], writes=[wb])
    posb_t = C.f32(128, 512, dt=I32); posf = C.f32(128, 512); kf = C.f32(128, 512); ki = posb_t
    Ct = C.f32(128, 512); St = C.f32(128, 512); tb = Buf()
    qraw = [C.bf(128, 512) for _ in range(2)]; qrb = [Buf() for _ in range(2)]
    t1 = [C.f32(128, 512) for _ in range(2)]; t2 = [C.f32(128, 512) for _ in range(2)]; t12b = [Buf() for _ in range(2)]
    qo = [C.bf(128, 512) for _ in range(2)]; qob = [Buf() for _ in range(2)]
    vst = [C.bf(128, 512) for _ in range(2)]; vstb = [Buf() for _ in range(2)]
    gst = [C.f32(36, 512) for _ in range(2)]; gstb = [Buf() for _ in range(2)]
    n = 0
    for tq in range(NQ):
        csl = slice(tq * 512, (tq + 1) * 512)
        self.rope_tables(tq, 128, self.invF[:, 0:1], posb_t, posf, kf, ki, Ct, St, tb)
        for ci in range(10):
            Wsrc, col0 = (Wq, ci * 128) if ci < 6 else (Wk, (ci - 6) * 128)
            row0 = ci * 128
            pq, pr = 0 + (ci % 2) * 2, 1 + (ci % 2) * 2
            self.proj_chunk(pq, 128, Wsrc, wb, col0, tq)
            j = n % 2; n += 1
            self.act(qraw[j], self.PS[pq][:], AF.Copy, [self.PB[pq]], [qrb[j]])
            self.mm(self.PS[pr][:], self.RnB, qraw[j], True, True, [self.cB, qrb[j]], [self.PB[pr]])
            P.op("dve", lambda e, j=j, pq=pq: e.tensor_tensor(t1[j], self.PS[pq][:], Ct, ALU.mult), [self.PB[pq], tb], [t12b[j]])
            P.op("dve", lambda e, j=j, pr=pr: e.tensor_tensor(t2[j], self.PS[pr][:], St, ALU.mult), [self.PB[pr], tb], [t12b[j]])
            P.op("pool", lambda e, j=j: e.tensor_tensor(qo[j], t1[j], t2[j], ALU.add), [t12b[j]], [qob[j]])
            P.dma("sp", self.pT[row0:row0 + 128, csl], qo[j], reads=[qob[j]], writes=[self.pT_b])
        for ci in range(4):
            pi = 4 + (ci % 2)
            self.proj_chunk(pi, 128, Wk, wb, 512 + ci * 128, tq)
            sj = stg[ci % 2]; sjb = stgb[ci % 2]
            self.act(sj, self.PS[pi][:], AF.Copy, [self.PB[pi]], [sjb])
            P.dma("sp", self.pT[1280 + ci * 128:1280 + (ci + 1) * 128, csl], sj, reads=[sjb], writes=[self.pT_b])
        self.proj_chunk(6, 36, Wg, wb, 0, tq)
        gj = gst[tq % 2]; gjb = gstb[tq % 2]
        self.act(gj, self.PS[6][0:36, :], AF.Copy, [self.PB[6]], [gjb])
        P.dma("sp", self.glT[:, csl], gj, reads=[gjb], writes=[self.glT_b])
        for ts in range(4):
            t = tq * 4 + ts
            j = t % 2
            pv = 5 if ts % 2 == 0 else 7
            for k in range(8):
                self.mm(self.PS[pv][:], self.XT[:, k, t * 128:(t + 1) * 128], Wv[:, k, :], k == 0, k == 7,
                        [wb, self.XTb[tq]], [self.PB[pv]])
            P.op("dve", lambda e, j=j, pv=pv: e.tensor_copy(vst[j], self.PS[pv][:]), [self.PB[pv]], [vstb[j]])
            P.dma("sp", self.vtok[t * 128:(t + 1) * 128, 0:512], vst[j], reads=[vstb[j]], writes=[self.vtok_b])
    P.barrier()
    C.reset()
    kcmpT = C.bf(64, 4, NCP); vcA = C.bf(128, 4, NCT, 128); cmb = Buf()
    Ebig = self.A_t[64:128, 0:S]
    Ov = C.bf(128, NCT, 64); Sel = C.bf(3, 3, 64); kb = Buf()
    P.dma("pool", Ebig, I["c_ebig"], writes=[kb])
    P.dma("pool", Ov, I["c_ov"][0:NCP, :].rearrange("(t p) j -> p t j", p=128), writes=[kb])
    P.dma("pool", Sel, I["c_sel"].rearrange("r (a m) -> r a m", m=64), writes=[kb])
    P.op("pool", lambda e: e.memset(vcA[:, :, :, 0:64], 0.0), [], [cmb])
    P.op("pool", lambda e: e.memset(vcA[:, :, :, 64:128], 1.0), [], [cmb])
    P.op("pool", lambda e: e.memset(kcmpT, 0.0), [], [cmb])
    mark = C.off
    cw1 = C.bf(64, 2, 32, 128); cpe = C.bf(64, 2, 32); cw2 = C.bf(128, 2, 64); cwb = Buf()
    P.dma("pool", cw1, I[f"cw1{l}"].rearrange("a d l f -> d a l f"), writes=[cwb])
    P.dma("pool", cpe, I[f"cpe{l}"].rearrange("a d l -> d a l"), writes=[cwb])
    P.dma("pool", cw2, I[f"cw2{l}"].rearrange("a f d -> f a d"), writes=[cwb])
    kcT = [C.bf(64, S) for _ in range(2)]; kcb = [Buf() for _ in range(2)]
    hbias = C.f32(128, 2); hbb = Buf()
    hT = [C.bf(128, NCP) for _ in range(2)]; hTb = [Buf() for _ in range(2)]
    posb2 = C.f32(64, NCP, dt=I32); posf2 = C.f32(64, NCP); kf2 = C.f32(64, NCP); ki2 = posb2
    Ct2 = C.f32(64, NCP); St2 = C.f32(64, NCP); tb2 = Buf()
    kraw = C.bf(64, NCP); krb_ = Buf(); u1 = C.f32(64, NCP); u2 = C.f32(64, NCP)
    self.rope_tables(0, 64, self.invF[0:64, 0:1], posb2, posf2, kf2, ki2, Ct2, St2, tb2,
                     pos_src=I["pos_end"][0:1, 0:NCP], width=NCP)
    for a in range(2):
        for ll in range(32):
            self.mm(self.PS[6][:, a:a + 1], cw1[:, a, ll, :], cpe[:, a, ll:ll + 1], ll == 0, ll == 31, [cwb], [self.PB[6]])
    P.op("dve", lambda e: e.tensor_copy(hbias, self.PS[6][:, 0:2]), [self.PB[6]], [hbb])
    nn = 0
    for a in range(2):
        for k in range(4):
            jj = nn % 2; nn += 1
            if a == 0:
                P.dma("sp", kcT[jj], self.pT[1280 + 64 * k:1280 + 64 * (k + 1), :], reads=[self.pT_b], writes=[kcb[jj]])
            else:
                P.dma("sp", kcT[jj], self.pT[1536 + 64 * k:1536 + 64 * (k + 1), :], reads=[self.pT_b], writes=[kcb[jj]])
            ph = jj
            src = kcT[jj]
            for ll in range(32):
                rhs = src[:, ll:ll + 16 * (NCMP - 1) + 1:16]
                self.mm(self.PS[ph][:, 0:NCMP], cw1[:, a, ll, :], rhs, ll == 0, ll == 31, [cwb, kcb[jj]], [self.PB[ph]])
            h_ = hT[jj]; hb_ = hTb[jj]
            if NCMP < NCP:
                P.op("pool", lambda e, h_=h_: e.memset(h_[:, NCMP:NCP], 0.0), [], [hb_])
            self.act(h_[:, 0:NCMP], self.PS[ph][:, 0:NCMP], AF.Silu, [self.PB[ph], hbb], [hb_], bias=hbias[:, a:a + 1], scale=1.0)
            if a == 0:
                self.mm(self.PS[2][0:64, 0:NCP], cw2[:, 0, :], h_, True, True, [cwb, hb_], [self.PB[2]])
                self.act(kraw, self.PS[2][0:64, 0:NCP], AF.Copy, [self.PB[2]], [krb_])
                self.mm(self.PS[3][0:64, 0:NCP], self.RnB[0:64, 0:64], kraw, True, True, [self.cB, krb_], [self.PB[3]])
                P.op("dve", lambda e: e.tensor_tensor(u1, self.PS[2][0:64, 0:NCP], Ct2, ALU.mult), [self.PB[2], tb2], [krb_])
                P.op("dve", lambda e: e.tensor_tensor(u2, self.PS[3][0:64, 0:NCP], St2, ALU.mult), [self.PB[3], tb2], [krb_])
                P.op("dve", lambda e, k=k: e.tensor_tensor(kcmpT[:, k, 0:NCMP], u1[:, 0:NCMP], u2[:, 0:NCMP], ALU.add), [krb_], [cmb])
            else:
                for nt in range(NCT):
                    m_ = min(128, NCMP - nt * 128)
                    self.mm(self.PS[4 + nt][0:m_, 0:64], h_[:, nt * 128:nt * 128 + m_], cw2[:, 1, :], True, True, [cwb, hb_], [self.PB[4 + nt]])
                    P.op("dve", lambda e, k=k, nt=nt, m_=m_: e.tensor_copy(vcA[0:m_, k, nt, 0:64], self.PS[4 + nt][0:m_, 0:64]), [self.PB[4 + nt]], [cmb])
    P.barrier()
    self.nsa_attention(l, kcmpT, vcA, cmb, Ebig, Ov, Sel, kb, mark, NCMP, NCT)


Builder.mixer_nsa = _mixer_nsa


def _nsa_attention(self, l, kcmpT, vcA, cmb, Ebig, Ov, Sel, kb, mark, NCMP, NCT):
    P, C, I, S = self.P, self.C, self.I, self.S
    NT, NQ = self.NT, self.NQ
    C.off = mark
    A = self.A_t
    ksT = A[0:64, 0:S]; kwT = A[0:64, S:2 * S]
    ksE = A[0:128, 0:S]
    vsA = A[:, 2 * S:3 * S].rearrange("p (t c) -> p t c", c=128)
    vwA = A[:, 3 * S:4 * S].rearrange("p (t c) -> p t c", c=128)
    kvb = Buf()
    P.op("pool", lambda e: e.memset(vsA[:, :, 64:128], 1.0), [], [kvb])
    P.op("pool", lambda e: e.memset(vwA[:, :, 64:128], 1.0), [], [kvb])
    qc = [[C.bf(128, 512) for _ in range(3)] for _ in range(2)]; qcb = [[Buf() for _ in range(3)] for _ in range(2)]
    qsb = [[Buf() for _ in range(3)] for _ in range(2)]
    Gl = [C.f32(3, 512) for _ in range(3)]; glf = [Buf() for _ in range(3)]
    Glh = [[C.bf(3, 512) for _ in range(3)] for _ in range(2)]
    Gll = [[C.bf(3, 512) for _ in range(3)] for _ in range(2)]
    Gtmp = C.f32(3, 512); gtb = Buf()
    glb = [[Buf() for _ in range(3)] for _ in range(2)]
    FBt = [C.f32(128, 4, 64) for _ in range(2)]; CBt = [C.f32(128, 4, 64) for _ in range(2)]; fbb = [Buf() for _ in range(2)]
    E = [C.bf(128, 512) for _ in range(6)]; Eb = [Buf() for _ in range(6)]
    rd = [C.f32(64, 512) for _ in range(3)]; rdb = [Buf() for _ in range(3)]
    gs = [C.f32(64, 512) for _ in range(3)]; gsb = [Buf() for _ in range(3)]
    acc = [C.f32(64, 512) for _ in range(3)]; accb = [Buf() for _ in range(3)]
    impacc = C.f32(64, 512); impb = Buf()
    selbT = C.bf(64, 512); selTb = Buf()
    impm = [C.f32(128, 64) for _ in range(4)]; work = [C.f32(128, 64) for _ in range(4)]
    m8a = [C.f32(128, 8) for _ in range(4)]; m8b = [C.f32(128, 8) for _ in range(4)]
    selt = [C.f32(128, 64) for _ in range(4)]; selbf = [C.bf(128, 64) for _ in range(4)]; slb = [Buf() for _ in range(4)]
    PS, PB = self.PS, self.PB
    self.dbg_off = {"selbT": selbT.offset, "qc00": qc[0][0].offset, "impacc": impacc.offset, "selbf0": selbf[0].offset,
                    "CBt0": CBt[0].offset, "Glh00": Glh[0][0].offset, "E0": E[0].offset}
    PS7b = PS[7][:].bitcast(BF16)
    vt = self.vtok.rearrange("(t p) c -> p t c", p=128)
    fbv = I["c_fb"].rearrange("(q ts p) j -> q p ts j", p=128, ts=4)
    st = {"ne": 0, "nr": 0, "ns": 0}

    def new_E():
        i = st["ne"] % 6; st["ne"] += 1
        return E[i], Eb[i]

    def new_S():
        i = st["ns"] % 4; st["ns"] += 1
        return i

    def finish(k, g, tq, po, b, first, last, par, with_imp=False):
        h = 3 * k + g
        i = st["nr"] % 3; st["nr"] += 1
        r_, rb_ = rd[i], rdb[i]
        g_, gb_ = gs[i], gsb[i]
        P.op("dve", lambda e: e.tensor_scalar(r_, PS[po][64:128, :], 1.0e-30, None, ALU.max), [PB[po]], [rb_])
        if with_imp:
            self.act(g_, r_, AF.Ln, [rb_], [gb_])
            self.act(g_, g_, AF.Exp, [gb_], [gb_], scale=-1.0)
            if g == 0:
                P.op("dve", lambda e: e.tensor_tensor(impacc, PS[7][0:64, :], g_, ALU.mult), [PB[7], gb_], [impb])
            else:
                P.op("dve", lambda e: e.tensor_tensor(g_, PS[7][0:64, :], g_, ALU.mult), [PB[7], gb_], [gb_])
                P.op("dve", lambda e: e.tensor_tensor(impacc, impacc, g_, ALU.add), [gb_, impb], [impb])
        self.mm(PS[6][0:64, :], Sel[:, b, :], Glh[par][g], True, False, [kb, glb[par][g]], [PB[6]])
        self.mm(PS[6][0:64, :], Sel[:, b, :], Gll[par][g], False, True, [kb, glb[par][g]], [PB[6]])
        self.act(g_, PS[6][0:64, :], AF.Exp, [PB[6]], [gb_], scale=-1.0)
        P.op("dve", lambda e: e.scalar_tensor_tensor(g_, g_, 1.0, r_, ALU.add, ALU.mult), [gb_, rb_], [gb_])
        self.act(g_, g_, AF.Ln, [gb_], [gb_])
        self.act(g_, g_, AF.Exp, [gb_], [gb_], scale=-1.0)
        import os
        SKB = os.environ.get("NSA_SKIPB", "")
        if str(b) in SKB:
            P.op("dve", lambda e: e.memset(g_, 0.0), [], [gb_])
        if first:
            P.op("dve", lambda e: e.tensor_tensor(acc[g], PS[po][0:64, :], g_, ALU.mult), [PB[po], gb_], [accb[g]])
        else:
            P.op("dve", lambda e: e.tensor_tensor(r_, PS[po][0:64, :], g_, ALU.mult), [PB[po], gb_], [rb_])
            if not last:
                P.op("dve", lambda e: e.tensor_tensor(acc[g], acc[g], r_, ALU.add), [rb_, accb[g]], [accb[g]])
            else:
                dst = self.CT[64 * (h % 2):64 * (h % 2) + 64, h // 2, tq * 512:(tq + 1) * 512]
                P.op("dve", lambda e: e.tensor_tensor(dst, acc[g], r_, ALU.add), [rb_, accb[g]], [self.CTb[h // 2][tq]])

    for k in range(4):
        P.dma("sp", ksT, self.pT[768 + 64 * k:768 + 64 * (k + 1), :], reads=[self.pT_b], writes=[kvb])
        P.dma("sp", kwT, self.pT[1024 + 64 * k:1024 + 64 * (k + 1), :], reads=[self.pT_b], writes=[kvb])
        with self.nc.allow_non_contiguous_dma(reason="per-head V gather (128B runs)"):
            for t0 in range(0, NT, 4):
                P.dma("sp", vsA[:, t0:t0 + 4, 0:64], vt[:, t0:t0 + 4, 64 * k:64 * (k + 1)], reads=[self.vtok_b], writes=[kvb])
                P.dma("sp", vwA[:, t0:t0 + 4, 0:64], vt[:, t0:t0 + 4, 256 + 64 * k:256 + 64 * (k + 1)], reads=[self.vtok_b], writes=[kvb])
        for tq in range(NQ):
            par = tq % 2
            q0 = tq * 512
            csl = slice(q0, q0 + 512)
            for g in range(3):
                h = 3 * k + g
                P.dma("sp", qc[par][g][0:64, :], self.pT[64 * h:64 * (h + 1), csl], reads=[self.pT_b], writes=[qcb[par][g]])
                P.dma("sp", Gl[g], self.glT[3 * h:3 * h + 3, csl], reads=[self.glT_b], writes=[glf[g]])
                P.op("act", lambda e, par=par, g=g: e.copy(Glh[par][g], Gl[g]), [glf[g]], [glb[par][g]])
                P.op("dve", lambda e, par=par, g=g: e.tensor_tensor(Gtmp, Gl[g], Glh[par][g], ALU.subtract), [glf[g], glb[par][g]], [gtb])
                P.op("dve", lambda e, par=par, g=g: e.tensor_copy(Gll[par][g], Gtmp), [gtb], [glb[par][g]])
            P.dma("sp", FBt[par], fbv[tq], writes=[fbb[par]])
            P.op("dve", lambda e, par=par: e.tensor_scalar(CBt[par], FBt[par], 0.0, 3.0e-5, ALU.min, ALU.mult), [fbb[par]], [fbb[par]])
            trivial = (q0 + 511 < 16 * 64)

            def selection_part1(par=par, trivial=trivial):
                if trivial:
                    for ts in range(4):
                        P.op("dve", lambda e, ts=ts: e.tensor_copy(selbf[ts], CBt[par][:, ts, :]), [fbb[par]], [slb[ts]])
                    return
                for ts in range(4):
                    self.tr(PS[6][:, ts * 64:(ts + 1) * 64], impacc[:, ts * 128:(ts + 1) * 128], self.identF[0:64, 0:64], [impb, self.cB], [PB[6]])
                for ts in range(4):
                    i = ts
                    P.op("dve", lambda e, i=i, ts=ts: e.tensor_tensor(impm[i], PS[6][:, ts * 64:(ts + 1) * 64], FBt[par][:, ts, :], ALU.add), [PB[6], fbb[par]], [slb[i]])
                    P.op("dve", lambda e, i=i: e.max(out=m8a[i], in_=impm[i]), [slb[i]], [slb[i]])
                    P.op("dve", lambda e, i=i: e.match_replace(out=work[i], in_to_replace=m8a[i], in_values=impm[i], imm_value=-3.0e9), [slb[i]], [slb[i]])
                    P.op("dve", lambda e, i=i: e.max(out=m8b[i], in_=work[i]), [slb[i]], [slb[i]])
                    P.op("dve", lambda e, i=i: e.tensor_scalar(selt[i], impm[i], m8b[i][:, 7:8], None, ALU.is_ge), [slb[i]], [slb[i]])
                    P.op("dve", lambda e, i=i: e.tensor_scalar(selt[i], selt[i], 1.0, 30000.0, ALU.subtract, ALU.mult), [slb[i]], [slb[i]])
                    P.op("dve", lambda e, i=i, ts=ts: e.tensor_tensor(selbf[i], selt[i], CBt[par][:, ts, :], ALU.add), [slb[i], fbb[par]], [slb[i]])

            tiles = [nt for nt in range(NCT) if nt * 2048 + 31 <= q0 + 511]
            kts_w = list(range(max(0, 4 * tq - 4), 4 * tq + 4))
            pairsA = []
            grp = 0
            for g in range(3):
                po = 4 + (grp % 2); grp += 1
                for ii, nt in enumerate(tiles):
                    def score(ps, nt=nt, g=g):
                        self.mm(PS[ps][:], kcmpT[:, k, nt * 128:(nt + 1) * 128], qc[par][g][0:64, :], True, True, [cmb, qcb[par][g]], [PB[ps]])

                    def post(ps, Ei, Ebi, nt=nt):
                        self.act(Ei, PS[ps][:], AF.Exp, [PB[ps]], [Ebi], scale=0.125)
                        base = q0 - 16 * nt * 128 - 31
                        P.op("pool", lambda e: e.affine_select(Ei, Ei, [[1, 512]], ALU.is_ge, 0.0, base=base, channel_multiplier=-16), [Ebi], [Ebi])

                    def pv(Ei, Ebi, nt=nt, ii=ii, po=po):
                        self.mm(PS[po][:], vcA[:, k, nt, :], Ei, ii == 0, ii == len(tiles) - 1, [cmb, Ebi], [PB[po]])
                        self.mm(PS[7][0:64, :], Ov[:, nt, :], Ei, ii == 0, ii == len(tiles) - 1, [kb, Ebi], [PB[7]])
                    d = {"score": score, "post": post, "pv": pv}
                    if ii == len(tiles) - 1:
                        if g < 2:
                            d["after"] = (lambda g=g, po=po: finish(k, g, tq, po, 0, True, False, par, with_imp=True))
                        else:
                            d["after"] = (lambda g=g, po=po: (finish(k, g, tq, po, 0, True, False, par, with_imp=True), selection_part1()))
                    pairsA.append(d)
            for g in range(3):
                po = 4 + (grp % 2); grp += 1
                for ii, kt in enumerate(kts_w):
                    def score(ps, kt=kt, g=g):
                        self.mm(PS[ps][:], kwT[:, kt * 128:(kt + 1) * 128], qc[par][g][0:64, :], True, True, [kvb, qcb[par][g]], [PB[ps]])

                    def post(ps, Ei, Ebi, kt=kt):
                        self.act(Ei, PS[ps][:], AF.Exp, [PB[ps]], [Ebi], scale=0.125)
                        if kt >= 4 * tq:
                            base = q0 - kt * 128
                            P.op("pool", lambda e: e.affine_select(Ei, Ei, [[1, 512]], ALU.is_ge, 0.0, base=base, channel_multiplier=-1), [Ebi], [Ebi])
                        else:
                            base = 511 - q0 + kt * 128
                            P.op("pool", lambda e: e.affine_select(Ei, Ei, [[-1, 512]], ALU.is_ge, 0.0, base=base, channel_multiplier=1), [Ebi], [Ebi])

                    def pv(Ei, Ebi, kt=kt, ii=ii, po=po):
                        self.mm(PS[po][:], vwA[:, kt, :], Ei, ii == 0, ii == len(kts_w) - 1, [kvb, Ebi], [PB[po]])
                    d = {"score": score, "post": post, "pv": pv}
                    if ii == len(kts_w) - 1:
                        d["after"] = (lambda g=g, po=po: finish(k, g, tq, po, 2, False, False, par))
                    pairsA.append(d)
            self.run_pipeline(pairsA, new_S, new_E)
            for ts in range(4):
                self.tr(PS7b[0:64, ts * 128:(ts + 1) * 128], selbf[ts], self.identB, [slb[ts], self.cB], [PB[7]])
            for g in range(3):
                P.op("act", lambda e, g=g, par=par: e.copy(qc[par][g][64:128, :], PS7b[0:64, 0:512]), [PB[7]], [qsb[par][g]])
            pairsB = []
            nk = 4 * tq + 4
            for g in range(3):
                po = 4 + (grp % 2); grp += 1
                for kt in range(nk):
                    def score(ps, kt=kt, g=g):
                        self.mm(PS[ps][:], ksE[:, kt * 128:(kt + 1) * 128], qc[par][g], True, True,
                                [kvb, kb, qcb[par][g], qsb[par][g]], [PB[ps]])

                    def post(ps, Ei, Ebi, kt=kt):
                        self.act(Ei, PS[ps][:], AF.Exp, [PB[ps]], [Ebi], scale=0.125)
                        if kt >= 4 * tq:
                            base = q0 - kt * 128
                            P.op("pool", lambda e: e.affine_select(Ei, Ei, [[1, 512]], ALU.is_ge, 0.0, base=base, channel_multiplier=-1), [Ebi], [Ebi])

                    def pv(Ei, Ebi, kt=kt, po=po):
                        self.mm(PS[po][:], vsA[:, kt, :], Ei, kt == 0, kt == nk - 1, [kvb, Ebi], [PB[po]])
                    d = {"score": score, "post": post, "pv": pv}
                    if kt == nk - 1:
                        d["after"] = (lambda g=g, po=po: finish(k, g, tq, po, 1, False, True, par))
                    pairsB.append(d)
            self.run_pipeline(pairsB, new_S, new_E)
    P.barrier()
    self.xattn(l)


Builder.nsa_attention = _nsa_attention
```

```python
import contextlib
import math
import numpy as np
import concourse.bass as bass
import concourse.mybir as mybir
from concourse.bass_utils import run_bass_kernel_spmd

F32 = mybir.dt.float32
BF16 = mybir.dt.bfloat16
I32 = mybir.dt.int32
AF = mybir.ActivationFunctionType
ALU = mybir.AluOpType
AX = mybir.AxisListType

D = 1024
NCORES = 8
N_MEM = 256
ALPHA = (2.0 * 4) ** 0.25
LN_EPS = 1e-5
RMS_EPS = 1e-6
NEG = -30000.0
EPOCH = 16000
ENGS = ("pe", "act", "dve", "pool", "sp")


class Buf:
    __slots__ = ("name", "w", "r", "excl")

    def __init__(self, name="", excl=False):
        self.name = name
        self.w = None
        self.r = {}
        self.excl = excl


class Prog:
    def __init__(self, nc, stack, n_dma_slots=(("sp", 16), ("pool", 16), ("act", 4))):
        self.nc = nc
        self.stack = stack
        self.streams = {e: [] for e in ENGS}
        self.cnt = {e: 0 for e in ENGS}
        self.sems = []
        self.eng_sems = {e: [] for e in ENGS}
        self.waited = {e: {} for e in ENGS}
        self.dma_slots = {}
        for q, n in n_dma_slots:
            self.dma_slots[q] = [[self._new_sem(f"d{q}{i}"), 0] for i in range(n)]
        self.dma_rr = {q: 0 for q, _ in n_dma_slots}
        self.ninstr = 0

    def _new_sem(self, name):
        h = self.stack.enter_context(self.nc.semaphore(name))
        self.sems.append(h)
        return len(self.sems) - 1

    def _eng_sem(self, eng, epoch):
        lst = self.eng_sems[eng]
        while len(lst) <= epoch:
            lst.append(self._new_sem(f"s{eng}{len(lst)}"))
        return lst[epoch]

    def _need(self, eng, deps):
        w = self.waited[eng]
        for si, val in deps.items():
            if w.get(si, 0) >= val:
                continue
            w[si] = val
            h = self.sems[si]
            self.streams[eng].append(lambda e, h=h, val=val: e.wait_ge(h, val))

    def _collect(self, reads, writes, eng=None):
        deps = {}
        own = self.eng_sems.get(eng, ()) if eng else ()
        for b in reads:
            ev = b.w
            if ev is not None and deps.get(ev[0], 0) < ev[1]:
                deps[ev[0]] = ev[1]
            if b.excl:
                for si, v in b.r.items():
                    if si not in own and deps.get(si, 0) < v:
                        deps[si] = v
        for b in writes:
            ev = b.w
            if ev is not None and deps.get(ev[0], 0) < ev[1]:
                deps[ev[0]] = ev[1]
            for si, v in b.r.items():
                if deps.get(si, 0) < v:
                    deps[si] = v
        return deps

    @staticmethod
    def _mark(ev, reads, writes):
        si, v = ev
        for b in writes:
            b.w = ev
            b.r = {}
        for b in reads:
            if b.r.get(si, 0) < v:
                b.r[si] = v

    def op(self, eng, fn, reads=(), writes=()):
        deps = self._collect(reads, writes, eng)
        n = self.cnt[eng]
        epoch, within = divmod(n, EPOCH)
        si = self._eng_sem(eng, epoch)
        if eng == "pe":
            for s in self.eng_sems["pe"]:
                deps.pop(s, None)
        self._need(eng, deps)
        h = self.sems[si]
        self.streams[eng].append(lambda e, fn=fn, h=h: fn(e).then_inc(h, 1))
        self.cnt[eng] = n + 1
        ev = (si, within + 1)
        self._mark(ev, reads, writes)
        self.ninstr += 1
        return ev

    def dma(self, q, out, in_, reads=(), writes=(), **kw):
        deps = self._collect(reads, writes)
        slots = self.dma_slots[q]
        k = self.dma_rr[q]
        self.dma_rr[q] = (k + 1) % len(slots)
        slot = slots[k]
        si = slot[0]
        if slot[1] > 0:
            deps[si] = max(deps.get(si, 0), slot[1])
        self._need(q, deps)
        slot[1] += 16
        val = slot[1]
        h = self.sems[si]
        self.streams[q].append(
            lambda e, out=out, in_=in_, h=h, kw=kw: e.dma_start(out=out, in_=in_, **kw).then_inc(h, 16))
        ev = (si, val)
        self._mark(ev, reads, writes)
        self.ninstr += 1
        return ev

    def barrier(self):
        deps = {}
        for e in ENGS:
            n = self.cnt[e]
            if n == 0:
                continue
            epoch, within = divmod(n - 1, EPOCH)
            deps[self.eng_sems[e][epoch]] = within + 1
            for ep in range(epoch):
                deps[self.eng_sems[e][ep]] = EPOCH
        for q in self.dma_slots:
            for si, v in self.dma_slots[q]:
                if v:
                    deps[si] = v
        for e in ENGS:
            self._need(e, dict(deps))

    def emit(self):
        nc = self.nc
        with nc.Block() as block:
            @block.tensor
            def _(e):
                for f in self.streams["pe"]:
                    f(e)

            @block.scalar
            def _(e):
                for f in self.streams["act"]:
                    f(e)

            @block.vector
            def _(e):
                for f in self.streams["dve"]:
                    f(e)

            @block.gpsimd
            def _(e):
                for f in self.streams["pool"]:
                    f(e)

            @block.sync
            def _(e):
                for f in self.streams["sp"]:
                    f(e)


class Arena:
    def __init__(self, ap, nbf16):
        self.ap = ap
        self.n = nbf16
        self.off = 0

    def reset(self):
        self.off = 0

    def bf(self, parts, *shape):
        self.off = (self.off + 31) // 32 * 32
        n = int(np.prod(shape))
        n2 = (n + 1) // 2 * 2
        assert self.off + n2 <= self.n, ("arena overflow", self.off, n2, self.n)
        v = self.ap[0:parts, self.off:self.off + n]
        self.off += n2
        if len(shape) == 2:
            v = v.rearrange("p (a b) -> p a b", b=shape[1])
        elif len(shape) == 3:
            v = v.rearrange("p (a b c) -> p a b c", b=shape[1], c=shape[2])
        return v

    def f32(self, parts, *shape, dt=F32):
        self.off = (self.off + 31) // 32 * 32
        n = int(np.prod(shape))
        assert self.off + 2 * n <= self.n, ("arena overflow", self.off, 2 * n, self.n)
        v = self.ap[0:parts, self.off:self.off + 2 * n].bitcast(dt)
        self.off += 2 * n
        if len(shape) == 2:
            v = v.rearrange("p (a b) -> p a b", b=shape[1])
        elif len(shape) == 3:
            v = v.rearrange("p (a b c) -> p a b c", b=shape[1], c=shape[2])
        return v


NSA_IN = 2596
MLA_IN = 672
CONV_IN = 1792


class Builder:
    def __init__(self, S, kinds, last_only_out=True):
        self.S = S
        self.kinds = list(kinds)
        self.NT = S // 128
        self.NQ = S // 512
        self.SC = min(2048, S // 2)

    def mm(self, out, lhsT, rhs, start, stop, reads, writes):
        return self.P.op("pe", lambda e: e.matmul(out, lhsT, rhs, start=start, stop=stop), reads, writes)

    def tr(self, out, in_, ident, reads, writes):
        return self.P.op("pe", lambda e: e.transpose(out, in_, ident), reads, writes)

    def act(self, out, in_, func, reads, writes, **kw):
        return self.P.op("act", lambda e: e.activation(out, in_, func, **kw), reads, writes)

    def dbg_dump(self, name, ap, shape, dt, reads):
        import os
        if not os.environ.get("DBG_NSA"):
            return
        self.dbg_names = getattr(self, "dbg_names", [])
        d = self.dram(name, shape, dt, kind="ExternalOutput")
        self.P.dma("sp", d, ap, reads=reads)
        self.dbg_names.append(name)

    def dram(self, name, shape, dt, kind="Internal"):
        return self.nc.dram_tensor(name, list(shape), dt, kind=kind).ap()

    def build(self):
        S = self.S
        nc = self.nc = bass.Bass("TRN2", target_bir_lowering=False)
        I = self.I = {}

        def inp(name, shape, dt=F32):
            I[name] = self.dram(name, shape, dt, kind="ExternalInput")
            return I[name]

        inp("x", [S, D]); inp("mem", [N_MEM, D]); inp("pos", [1, S], I32); inp("pos_end", [1, 256], I32)
        inp("c_ident", [128, 128]); inp("c_rope", [128, 4 * 128 + 4])
        inp("c_ebig", [64, S]); inp("c_ov", [256, 64]); inp("c_fb", [S, 64]); inp("c_sel", [3, 3 * 64])
        for l, kind in enumerate(self.kinds):
            nin = (NSA_IN, MLA_IN, CONV_IN)[kind]
            inp(f"win{l}", [D, nin]); inp(f"wkv{l}", [D, 512]); inp(f"wo{l}", [D, D])
            inp(f"lng{l}", [2, D]); inp(f"lnb{l}", [2, D])
            inp(f"wr{l}", [D, 20]); inp(f"br{l}", [1, 20])
            inp(f"wg{l}", [16, D, 512]); inp(f"wu{l}", [16, D, 512]); inp(f"wd{l}", [16, 512, D])
            if kind == 0:
                inp(f"cpe{l}", [2, 64, 32]); inp(f"cw1{l}", [2, 64, 32, 128]); inp(f"cw2{l}", [2, 128, 64])
            elif kind == 1:
                inp(f"qn{l}", [128, 2]); inp(f"kvn{l}", [128, 1]); inp(f"wuq{l}", [256, 1152]); inp(f"wukv{l}", [128, 1536])
            else:
                inp(f"cbin{l}", [128, 12]); inp(f"cdw{l}", [128, 6 * 31]); inp(f"cdb{l}", [128, 6])
                inp(f"clg{l}", [128, 6]); inp(f"clb{l}", [128, 6])
        self.out = self.dram("out", [S, D], F32, kind="ExternalOutput")
        self.xres = self.dram("xres", [S, D], F32)
        self.pT = self.dram("pT", [2304, S], BF16)
        self.vtok = self.dram("vtok", [S, 768], BF16)
        import os
        self.glT = self.dram("glT", [36, S], F32, kind=("ExternalOutput" if os.environ.get("DBG_GL") else "Internal"))

        with contextlib.ExitStack() as st:
            P = self.P = Prog(nc, st)
            sb = lambda name, shape, dt: st.enter_context(nc.sbuf_tensor(name, shape, dt))
            self.A_t = sb("arenaA", [128, max(8 * S, 186 * 128)], BF16)
            self.B_t = sb("arenaB", [128, 8 * S], BF16)
            ZN = 4608
            self.Z = Arena(sb("arenaZ", [128, ZN], BF16)[:], ZN)
            rem = int(nc.sbuf_bytes_remaining) - 1024
            CN = min(rem // 2, 37000)
            self.C = Arena(sb("arenaC", [128, CN], BF16)[:], CN)
            self.PS = [st.enter_context(nc.psum_tensor(f"ps{i}", [128, 512], F32)) for i in range(8)]
            self.PB = [Buf(f"ps{i}", excl=True) for i in range(8)]
            self.XT = self.A_t[:, 0:8 * S].rearrange("p (k s) -> p k s", s=S)
            self.CT = self.B_t[:].rearrange("p (k s) -> p k s", s=S)
            self.XTb = [Buf(f"xt{q}") for q in range(self.NQ)]
            self.CTb = [[Buf(f"ct{k}_{q}") for q in range(self.NQ)] for k in range(8)]
            self.xres_b = [Buf(f"xr{t}") for t in range(self.NT)]
            self.pT_b = Buf("pT"); self.vtok_b = Buf("vtok"); self.glT_b = Buf("glT")
            self.setup_consts()
            for l, kind in enumerate(self.kinds):
                self.layer(l, kind)
            P.barrier()
            P.emit()
        return nc

    def setup_consts(self):
        P, Z, I = self.P, self.Z, self.I
        self.identF = Z.f32(128, 128); self.identB = Z.bf(128, 128)
        self.onesB = Z.bf(128, 128)
        self.onesF = Z.f32(128, 128)
        self.eps_t = Z.f32(128, 2)
        self.RnB = Z.bf(128, 128); self.RmB = Z.bf(128, 128); self.invF = Z.f32(128, 4)
        self.memT = Z.bf(128, 8, 256)
        self.gates = Z.f32(128, self.NT, 16)
        self.cB = Buf("consts"); self.memT_b = Buf("memT"); self.gates_b = [Buf() for _ in range(self.NT)]
        P.dma("sp", self.identF, I["c_ident"], writes=[self.cB])
        P.dma("pool", self.identB, I["c_ident"], writes=[self.cB])
        P.op("dve", lambda e: e.memset(self.onesB, 1.0), writes=[self.cB])
        P.op("dve", lambda e: e.memset(self.onesF, 1.0), writes=[self.cB])
        P.op("dve", lambda e: e.memset(self.eps_t[:, 0:1], LN_EPS), writes=[self.cB])
        P.op("dve", lambda e: e.memset(self.eps_t[:, 1:2], RMS_EPS), writes=[self.cB])
        P.dma("pool", self.RnB, I["c_rope"][:, 0:128], writes=[self.cB])
        P.dma("pool", self.RmB, I["c_rope"][:, 128:256], writes=[self.cB])
        P.dma("sp", self.invF, I["c_rope"][:, 512:516], writes=[self.cB])
        C = self.C
        C.reset()
        m = C.f32(128, 2, D)
        mb = Buf()
        P.dma("sp", m, I["mem"].rearrange("(t p) d -> p t d", p=128), writes=[mb])
        for t in range(2):
            for half in range(2):
                pi = t * 2 + half
                for k4 in range(4):
                    k = half * 4 + k4
                    self.tr(self.PS[pi][:, k4 * 128:(k4 + 1) * 128], m[:, t, k * 128:(k + 1) * 128], self.identF,
                            [mb, self.cB], [self.PB[pi]])
                src = self.PS[pi][:].rearrange("p (k s) -> p k s", s=128)
                dst = self.memT[:, half * 4:half * 4 + 4, t * 128:(t + 1) * 128]
                P.op("act", lambda e, dst=dst, src=src: e.copy(dst, src), [self.PB[pi]], [self.memT_b])
        P.barrier()

    def layer(self, l, kind):
        first = (l == 0)
        last = (l == len(self.kinds) - 1)
        self.xsrc = self.I["x"] if first else self.xres
        if first:
            self.load_xt_from_x()
        if kind == 2:
            self.mixer_conv(l)
        elif kind == 1:
            self.mixer_mla(l)
        else:
            self.mixer_nsa(l)
        import os
        if os.environ.get("DBG_CT") and l == 0:
            dbg = self.dram("dbg", [128, 8 * self.S], BF16, kind="ExternalOutput")
            self.P.dma("sp", dbg, self.B_t[:, 0:8 * self.S])
            self.P.barrier()
        self.phase_out(l)
        self.phase_moe(l, last)

    def load_xt_from_x(self):
        P, C = self.P, self.C
        C.reset()
        xt = [C.f32(128, D) for _ in range(3)]
        xb = [Buf() for _ in range(3)]
        x = self.I["x"]
        for t in range(self.NT):
            j = t % 3
            P.dma("sp", xt[j], x[t * 128:(t + 1) * 128, :], writes=[xb[j]])
            self.transpose_to_xt(xt[j], xb[j], t, bank0=(t % 2) * 2)
        P.barrier()

    def transpose_to_xt(self, src, srcb, t, bank0, scale=None):
        P = self.P
        q = t // 4
        for half in range(2):
            pi = bank0 + half
            for k4 in range(4):
                k = half * 4 + k4
                self.tr(self.PS[pi][:, k4 * 128:(k4 + 1) * 128], src[:, k * 128:(k + 1) * 128], self.identF,
                        [srcb, self.cB], [self.PB[pi]])
            s_ = self.PS[pi][:].rearrange("p (k s) -> p k s", s=128)
            d_ = self.XT[:, half * 4:half * 4 + 4, t * 128:(t + 1) * 128]
            P.op("act", lambda e, d_=d_, s_=s_: e.copy(d_, s_), [self.PB[pi]], [self.XTb[q]])

    def ln_tile(self, v, vb, gbc, bbc, gbb, scr, j):
        P = self.P
        st_, mv, rs = scr["st"][j], scr["mv"][j], scr["rs"][j][:, 0:1]
        sb_ = scr["b"][j]
        P.op("dve", lambda e: e.bn_stats(st_[:, 0, :], v[:, 0:512]), [vb], [sb_])
        P.op("dve", lambda e: e.bn_stats(st_[:, 1, :], v[:, 512:1024]), [vb], [sb_])
        P.op("dve", lambda e: e.bn_aggr(mv, st_[:].rearrange("p a b -> p (a b)")), [sb_], [sb_])
        P.op("act", lambda e: e.activation(rs, mv[:, 1:2], AF.Sqrt, bias=self.eps_t[:, 0:1], scale=1.0), [sb_, self.cB], [sb_])
        P.op("dve", lambda e: e.reciprocal(rs, rs), [sb_], [sb_])
        P.op("dve", lambda e: e.tensor_scalar(v, v, mv[:, 0:1], rs, ALU.subtract, ALU.mult), [vb, sb_], [vb])
        P.op("dve", lambda e: e.tensor_tensor(v, v, gbc, ALU.mult), [vb, gbb], [vb])
        P.op("dve", lambda e: e.tensor_tensor(v, v, bbc, ALU.add), [vb, gbb], [vb])

    def ln_scratch(self, n=2):
        C = self.C
        return {"st": [C.f32(128, 2, 6) for _ in range(n)], "mv": [C.f32(128, 2) for _ in range(n)],
                "rs": [C.f32(128, 2) for _ in range(n)], "b": [Buf() for _ in range(n)]}

    def phase_out(self, l):
        P, C, I = self.P, self.C, self.I
        C.reset()
        Wo = C.bf(128, 8, D); wob = Buf()
        P.dma("pool", Wo, I[f"wo{l}"].rearrange("(k p) n -> p k n", p=128), writes=[wob])
        gbc = C.f32(128, D); bbc = C.f32(128, D); gbb = Buf()
        P.dma("sp", gbc, I[f"lng{l}"][0:1, :].broadcast_to([128, D]), writes=[gbb])
        P.dma("sp", bbc, I[f"lnb{l}"][0:1, :].broadcast_to([128, D]), writes=[gbb])
        scr = self.ln_scratch()
        NB = 3
        xo = [C.f32(128, D) for _ in range(NB)]
        xb = [Buf() for _ in range(NB)]

        def load(t):
            P.dma("sp", xo[t % NB], self.xsrc[t * 128:(t + 1) * 128, :], reads=[self.xres_b[t]], writes=[xb[t % NB]])
        load(0)
        for t in range(self.NT):
            if t + 1 < self.NT:
                load(t + 1)
            j = t % NB
            q = t // 4
            for half in range(2):
                pi = (t % 2) * 2 + half
                for k in range(8):
                    self.mm(self.PS[pi][:], self.CT[:, k, t * 128:(t + 1) * 128], Wo[:, k, half * 512:(half + 1) * 512],
                            k == 0, k == 7, [self.CTb[k][q], wob], [self.PB[pi]])
                xs = xo[j][:, half * 512:(half + 1) * 512]
                P.op("dve", lambda e, xs=xs, pi=pi: e.scalar_tensor_tensor(xs, xs, ALPHA, self.PS[pi][:], ALU.mult, ALU.add),
                     [xb[j], self.PB[pi]], [xb[j]])
            self.ln_tile(xo[j], xb[j], gbc, bbc, gbb, scr, t % 2)
            P.dma("pool", self.xres[t * 128:(t + 1) * 128, :], xo[j], reads=[xb[j]], writes=[self.xres_b[t]])
            self.transpose_to_xt(xo[j], xb[j], t, bank0=4 + (t % 2) * 2)
        P.barrier()

    def phase_moe(self, l, last):
        P, C, I, S = self.P, self.C, self.I, self.S
        C.reset()
        NT, SC = self.NT, self.SC
        nsc = S // SC
        tps = SC // 128
        cps = SC // 512
        self.router(l)
        C.reset()
        gbc = C.f32(128, D); bbc = C.f32(128, D); gbb = Buf()
        P.dma("sp", gbc, I[f"lng{l}"][1:2, :].broadcast_to([128, D]), writes=[gbb])
        P.dma("sp", bbc, I[f"lnb{l}"][1:2, :].broadcast_to([128, D]), writes=[gbb])
        scr = self.ln_scratch()
        wg = [C.bf(128, 8, 512) for _ in range(2)]; wgb = [Buf() for _ in range(2)]
        wu = [C.bf(128, 8, 512) for _ in range(2)]; wub = [Buf() for _ in range(2)]
        wd = [C.bf(128, 4, D) for _ in range(1)]; wdb = [Buf() for _ in range(1)]
        hd = [C.bf(128, 4, 512) for _ in range(2)]; hdb = [Buf() for _ in range(2)]
        sg = [C.bf(128, 512) for _ in range(2)]; sgb = [Buf() for _ in range(2)]
        xo = [C.f32(128, D) for _ in range(2)]; xb = [Buf() for _ in range(2)]
        facc = self.B_t[:].bitcast(F32).rearrange("p (t d) -> p t d", d=D)
        fab = [Buf() for _ in range(tps)]

        def fa_bufs(tl):
            lo, hi = 2048 * tl, 2048 * tl + 2048
            res = [fab[tl]]
            for k in range(lo // S, (hi - 1) // S + 1):
                s0 = max(lo, k * S) - k * S
                s1 = min(hi, (k + 1) * S) - k * S
                for q in range(s0 // 512, (s1 - 1) // 512 + 1):
                    res.append(self.CTb[k][q])
            return res

        wgv = I[f"wg{l}"]; wuv = I[f"wu{l}"]; wdv = I[f"wd{l}"]

        def load_w(e):
            P.dma("pool", wg[e % 2], wgv[e].rearrange("(k p) n -> p k n", p=128), writes=[wgb[e % 2]])
            P.dma("pool", wu[e % 2], wuv[e].rearrange("(k p) n -> p k n", p=128), writes=[wub[e % 2]])

        def load_wd(e):
            P.dma("pool", wd[0], wdv[e].rearrange("(k p) n -> p k n", p=128), writes=[wdb[0]])

        for sc in range(nsc):
            tok0 = sc * SC
            load_w(0)
            load_wd(0)
            for e in range(16):
                if e + 1 < 16:
                    load_w(e + 1)
                for c in range(cps):
                    q = (tok0 // 512) + c
                    hb = hd[c % 2]
                    for f in range(4):
                        pg, pu = (f % 2) * 2, (f % 2) * 2 + 1
                        for k in range(8):
                            self.mm(self.PS[pg][:], wg[e % 2][:, k, f * 128:(f + 1) * 128], self.XT[:, k, q * 512:(q + 1) * 512],
                                    k == 0, k == 7, [wgb[e % 2], self.XTb[q]], [self.PB[pg]])
                        for k in range(8):
                            self.mm(self.PS[pu][:], wu[e % 2][:, k, f * 128:(f + 1) * 128], self.XT[:, k, q * 512:(q + 1) * 512],
                                    k == 0, k == 7, [wub[e % 2], self.XTb[q]], [self.PB[pu]])
                        s_ = sg[f % 2]
                        self.act(s_, self.PS[pg][:], AF.Silu, [self.PB[pg]], [sgb[f % 2]])
                        P.op("dve", lambda e_, f=f, s_=s_, pu=pu, hb=hb: e_.tensor_tensor(hb[:, f, :], s_, self.PS[pu][:], ALU.mult),
                             [sgb[f % 2], self.PB[pu]], [hdb[c % 2]])
                    for ts in range(4):
                        tl = c * 4 + ts
                        tg = tok0 // 128 + tl
                        for half in range(2):
                            po = 4 + ((ts * 2 + half) % 4)
                            for f in range(4):
                                self.mm(self.PS[po][:], hb[:, f, ts * 128:(ts + 1) * 128], wd[0][:, f, half * 512:(half + 1) * 512],
                                        f == 0, f == 3, [hdb[c % 2], wdb[0]], [self.PB[po]])
                            fa = facc[:, tl, half * 512:(half + 1) * 512]
                            gsc = self.gates[:, tg, e:e + 1]
                            if e == 0:
                                P.op("dve", lambda e_, fa=fa, po=po, gsc=gsc: e_.tensor_scalar(fa, self.PS[po][:], gsc, None, ALU.mult),
                                     [self.PB[po], self.gates_b[tg]], fa_bufs(tl))
                            else:
                                P.op("dve", lambda e_, fa=fa, po=po, gsc=gsc: e_.scalar_tensor_tensor(fa, self.PS[po][:], gsc, fa, ALU.mult, ALU.add),
                                     [self.PB[po], self.gates_b[tg]], fa_bufs(tl))
                if e + 1 < 16:
                    load_wd(e + 1)
            dst = self.out if last else self.xres

            def load(tl):
                tg = tok0 // 128 + tl
                P.dma("sp", xo[tl % 2], self.xres[tg * 128:(tg + 1) * 128, :], reads=[self.xres_b[tg]], writes=[xb[tl % 2]])
            load(0)
            for tl in range(tps):
                if tl + 1 < tps:
                    load(tl + 1)
                tg = tok0 // 128 + tl
                j = tl % 2
                P.op("dve", lambda e_, j=j, tl=tl: e_.scalar_tensor_tensor(xo[j], xo[j], ALPHA, facc[:, tl, :], ALU.mult, ALU.add),
                     [xb[j]] + fa_bufs(tl), [xb[j]])
                self.ln_tile(xo[j], xb[j], gbc, bbc, gbb, scr, j)
                P.dma("pool", dst[tg * 128:(tg + 1) * 128, :], xo[j], reads=[xb[j]], writes=[self.xres_b[tg]])
                if not last:
                    self.transpose_to_xt(xo[j], xb[j], tg, bank0=(tl % 2) * 2)
        P.barrier()

    def router(self, l):
        P, C, I = self.P, self.C, self.I
        NT = self.NT
        C.reset()
        Wr = C.bf(128, 8, 20); wrb = Buf()
        P.dma("pool", Wr, I[f"wr{l}"].rearrange("(k p) n -> p k n", p=128), writes=[wrb])
        brc = C.f32(128, 20)
        P.dma("sp", brc, I[f"br{l}"][0:1, :].broadcast_to([128, 20]), writes=[wrb])
        lg = C.f32(128, NT, 20); rb = Buf()
        for t in range(NT):
            q = t // 4
            pi = t % 4
            lgp = self.PS[pi][:, 0:20]
            for k in range(8):
                self.mm(lgp, self.XT[:, k, t * 128:(t + 1) * 128], Wr[:, k, :], k == 0, k == 7,
                        [self.XTb[q], wrb], [self.PB[pi]])
            P.op("dve", lambda e, t=t, lgp=lgp: e.tensor_tensor(lg[:, t, :], lgp, brc, ALU.add), [self.PB[pi], wrb], [rb])
        lgg = lg[:, :, 0:4]
        le = lg[:, :, 4:20].rearrange("p t (g j) -> p t g j", j=4)
        m = C.f32(128, NT); oh = C.f32(128, NT, 4); eg = C.f32(128, NT, 4); gs = C.f32(128, NT)
        m1 = C.f32(128, NT, 4); is1 = C.f32(128, NT, 4, 4); le2 = C.f32(128, NT, 4, 4); m2 = C.f32(128, NT, 4)
        sel2 = C.f32(128, NT, 4, 4); ee = C.f32(128, NT, 4, 4); ss = C.f32(128, NT, 4); w = C.f32(128, NT, 4)
        bc3 = lambda a: a.unsqueeze(2).broadcast_to([128, NT, 4])
        bc4 = lambda a: a.unsqueeze(3).broadcast_to([128, NT, 4, 4])
        R = [rb]
        dv = lambda fn: P.op("dve", fn, R, R)
        dv(lambda e: e.tensor_reduce(m, lgg, AX.X, ALU.max))
        dv(lambda e: e.tensor_tensor(oh, lgg, bc3(m), ALU.is_equal))
        dv(lambda e: e.tensor_tensor(eg, lgg, bc3(m), ALU.subtract))
        P.op("act", lambda e: e.activation(eg, eg, AF.Exp), R, R)
        dv(lambda e: e.tensor_reduce(gs, eg, AX.X, ALU.add))
        dv(lambda e: e.reciprocal(gs, gs))
        dv(lambda e: e.tensor_reduce(m1, le, AX.X, ALU.max))
        dv(lambda e: e.tensor_tensor(is1, le, bc4(m1), ALU.is_equal))
        dv(lambda e: e.scalar_tensor_tensor(le2, is1, -1.0e9, le, ALU.mult, ALU.add))
        dv(lambda e: e.tensor_reduce(m2, le2, AX.X, ALU.max))
        dv(lambda e: e.tensor_tensor(sel2, le, bc4(m2), ALU.is_ge))
        dv(lambda e: e.tensor_tensor(ee, le, bc4(m1), ALU.subtract))
        P.op("act", lambda e: e.activation(ee, ee, AF.Exp), R, R)
        dv(lambda e: e.tensor_tensor(ee, ee, sel2, ALU.mult))
        dv(lambda e: e.tensor_reduce(ss, ee, AX.X, ALU.add))
        dv(lambda e: e.reciprocal(ss, ss))
        dv(lambda e: e.tensor_tensor(w, ss, oh, ALU.mult))
        dv(lambda e: e.tensor_tensor(w, w, bc3(gs), ALU.mult))
        gv = self.gates[:].rearrange("p t (g j) -> p t g j", j=4)
        P.op("dve", lambda e: e.tensor_tensor(gv, ee, bc4(w), ALU.mult), R, R + self.gates_b)
        P.barrier()

    def run_pipeline(self, pairs, new_S, new_E, depth=3):
        n = len(pairs)
        slots = [None] * n

        def do_score(i):
            if pairs[i].get("pre"):
                pairs[i]["pre"]()
            slots[i] = new_S()
            pairs[i]["score"](slots[i])
        for i in range(min(depth, n)):
            do_score(i)
        for i in range(n):
            Ei, Ebi = new_E()
            pairs[i]["post"](slots[i], Ei, Ebi)
            if i + depth < n:
                do_score(i + depth)
            pairs[i]["pv"](Ei, Ebi)
            if pairs[i].get("after"):
                pairs[i]["after"]()

    def proj_chunk(self, pi, M, Wb, wbuf, col0, tq):
        for k in range(8):
            self.mm(self.PS[pi][0:M, :], Wb[:, k, col0:col0 + M], self.XT[:, k, tq * 512:(tq + 1) * 512],
                    k == 0, k == 7, [wbuf, self.XTb[tq]], [self.PB[pi]])

    def proj_xq(self, win, col0):
        P, C = self.P, self.C
        C.reset()
        stg = [C.bf(128, 512) for _ in range(2)]; stgb = [Buf() for _ in range(2)]
        Wx = C.bf(128, 8, 256); Wxb = Buf()
        P.dma("pool", Wx, win[:, :, col0:col0 + 256], writes=[Wxb])
        ns = 0
        for hh in range(2):
            for tq in range(self.NQ):
                pi = ns % 2
                self.proj_chunk(pi, 128, Wx, Wxb, hh * 128, tq)
                sg_ = stg[ns % 2]; sb_ = stgb[ns % 2]; ns += 1
                P.op("act", lambda e, sg_=sg_, pi=pi: e.copy(sg_, self.PS[pi][:]), [self.PB[pi]], [sb_])
                P.dma("sp", self.pT[2048 + hh * 128:2048 + (hh + 1) * 128, tq * 512:(tq + 1) * 512], sg_, reads=[sb_], writes=[self.pT_b])
        P.barrier()
        C.reset()

    def rope_tables(self, tq, npart, inv, posb_t, posf, kf, ki, Ct, St, tb, pos_src=None, width=512):
        P = self.P
        src = pos_src if pos_src is not None else self.I["pos"][0:1, tq * 512:(tq + 1) * 512]
        P.dma("sp", posb_t, src.broadcast_to([npart, width]), writes=[tb])
        P.op("dve", lambda e: e.tensor_copy(posf, posb_t), [tb], [tb])
        P.op("dve", lambda e: e.tensor_scalar(posf, posf, inv, None, ALU.mult), [tb, self.cB], [tb])
        TWO_PI = 2.0 * math.pi
        C1 = 6.28125
        C2 = TWO_PI - C1
        for out_t, shift in ((St, 0.0), (Ct, math.pi / 2)):
            P.op("dve", lambda e, shift=shift: e.tensor_scalar(kf, posf, shift, 1.0 / TWO_PI, ALU.add, ALU.mult), [tb], [tb])
            P.op("dve", lambda e: e.tensor_copy(ki, kf), [tb], [tb])
            P.op("dve", lambda e: e.tensor_copy(kf, ki), [tb], [tb])
            P.op("dve", lambda e, out_t=out_t, shift=shift: e.tensor_scalar(out_t, posf, shift, None, ALU.add), [tb], [tb])
            P.op("dve", lambda e, out_t=out_t: e.scalar_tensor_tensor(out_t, kf, -C1, out_t, ALU.mult, ALU.add), [tb], [tb])
            P.op("dve", lambda e, out_t=out_t: e.scalar_tensor_tensor(out_t, kf, -C2, out_t, ALU.mult, ALU.add), [tb], [tb])
            P.op("dve", lambda e, out_t=out_t: e.tensor_scalar(kf, out_t, math.pi, -TWO_PI, ALU.is_gt, ALU.mult), [tb], [tb])
            P.op("dve", lambda e, out_t=out_t: e.tensor_tensor(out_t, out_t, kf, ALU.add), [tb], [tb])
            P.op("dve", lambda e, out_t=out_t: e.tensor_scalar(kf, out_t, -math.pi, TWO_PI, ALU.is_lt, ALU.mult), [tb], [tb])
            P.op("dve", lambda e, out_t=out_t: e.tensor_tensor(out_t, out_t, kf, ALU.add), [tb], [tb])
            P.op("dve", lambda e, out_t=out_t: e.tensor_scalar(out_t, out_t, 3.1415925, -3.1415925, ALU.min, ALU.max), [tb], [tb])
            P.op("act", lambda e, out_t=out_t: e.activation(out_t, out_t, AF.Sin), [tb], [tb])

    def xattn(self, l):
        P, C, I, S = self.P, self.C, self.I, self.S
        C.reset()
        Wkv = C.bf(128, 8, 512); wb = Buf()
        P.dma("pool", Wkv, I[f"wkv{l}"].rearrange("(k p) n -> p k n", p=128), writes=[wb])
        mkT = C.bf(64, 4, 256); mkb = Buf()
        mv = C.bf(128, 2, 4, 128); mvb = Buf()
        xq = [C.bf(64, S) for _ in range(2)]; xqb = [Buf() for _ in range(2)]
        E = [C.bf(128, 512) for _ in range(3)]; Eb = [Buf() for _ in range(3)]
        rd = [C.f32(64, 512) for _ in range(2)]; rdb = [Buf() for _ in range(2)]
        P.op("pool", lambda e: e.memset(mv, 1.0), [], [mvb])
        for h in range(4):
            pi = h % 2
            for k in range(8):
                self.mm(self.PS[pi][0:64, 0:256], Wkv[:, k, h * 64:(h + 1) * 64], self.memT[:, k, :], k == 0, k == 7,
                        [wb, self.memT_b], [self.PB[pi]])
            P.op("act", lambda e, h=h, pi=pi: e.copy(mkT[:, h, :], self.PS[pi][0:64, 0:256]), [self.PB[pi]], [mkb])
        for mt in range(2):
            pi = 2 + mt
            for k in range(8):
                self.mm(self.PS[pi][:, 0:256], self.memT[:, k, mt * 128:(mt + 1) * 128], Wkv[:, k, 256:512], k == 0, k == 7,
                        [wb, self.memT_b], [self.PB[pi]])
            src = self.PS[pi][:, 0:256].rearrange("p (h d) -> p h d", d=64)
            P.op("act", lambda e, mt=mt, src=src: e.copy(mv[:, mt, :, 0:64], src), [self.PB[pi]], [mvb])
        ne = 0
        for h in range(4):
            xh = xq[h % 2]; xhb = xqb[h % 2]
            P.dma("sp", xh, self.pT[2048 + 64 * h:2048 + 64 * (h + 1), :], reads=[self.pT_b], writes=[xhb])
            for tq in range(self.NQ):
                po = 4 + (tq % 2)
                for mt in range(2):
                    ps = mt + 2 * (tq % 2)
                    self.mm(self.PS[ps][:], mkT[:, h, mt * 128:(mt + 1) * 128], xh[:, tq * 512:(tq + 1) * 512], True, True,
                            [mkb, xhb], [self.PB[ps]])
                    Ei = E[ne % 3]; Ebi = Eb[ne % 3]; ne += 1
                    self.act(Ei, self.PS[ps][:], AF.Exp, [self.PB[ps]], [Ebi], scale=0.125)
                    self.mm(self.PS[po][:], mv[:, mt, h, :], Ei, mt == 0, mt == 1, [mvb, Ebi], [self.PB[po]])
                r = rd[tq % 2]; rb = rdb[tq % 2]
                self.act(r, self.PS[po][64:128, :], AF.Ln, [self.PB[po]], [rb])
                self.act(r, r, AF.Exp, [rb], [rb], scale=-1.0)
                dst = self.CT[64 * (h % 2):64 * (h % 2) + 64, 6 + h // 2, tq * 512:(tq + 1) * 512]
                P.op("dve", lambda e, r=r, po=po, dst=dst: e.tensor_tensor(dst, self.PS[po][0:64, :], r, ALU.mult),
                     [self.PB[po], rb], [self.CTb[6 + h // 2][tq]])
        P.barrier()

    def mixer_conv(self, l):
        P, C, I, S = self.P, self.C, self.I, self.S
        NQ = self.NQ
        win = I[f"win{l}"].rearrange("(k p) n -> p k n", p=128)
        self.proj_xq(win, 1536)
        UW = 30 + S
        U = C.bf(128, 6, UW); Ub = [Buf() for _ in range(6)]
        cb = C.f32(128, 12); cdw = C.f32(128, 186); cdb = C.f32(128, 6); clg = C.f32(128, 6); clb = C.f32(128, 6)
        pb = Buf()
        for t_, n_ in ((cb, "cbin"), (cdw, "cdw"), (cdb, "cdb"), (clg, "clg"), (clb, "clb")):
            P.dma("sp", t_, I[f"{n_}{l}"], writes=[pb])
        mark = C.off
        Wc = [C.bf(128, 8, 256) for _ in range(2)]; Wcb = [Buf() for _ in range(2)]
        sig = [C.f32(128, 512) for _ in range(2)]; sigb = [Buf() for _ in range(2)]
        for c in range(6):
            P.op("pool", lambda e, c=c: e.memset(U[:, c, 0:30], 0.0), [], [Ub[c]])
        for c in range(6):
            W_ = Wc[c % 2]; Wb_ = Wcb[c % 2]
            P.dma("pool", W_[:, :, 0:128], win[:, :, c * 128:(c + 1) * 128], writes=[Wb_])
            P.dma("pool", W_[:, :, 128:256], win[:, :, 768 + c * 128:768 + (c + 1) * 128], writes=[Wb_])
            for tq in range(NQ):
                p1, p2 = 2 + (tq % 2) * 2, 3 + (tq % 2) * 2
                self.proj_chunk(p1, 128, W_, Wb_, 0, tq)
                self.proj_chunk(p2, 128, W_, Wb_, 128, tq)
                sg_ = sig[tq % 2]; sb_ = sigb[tq % 2]
                self.act(sg_, self.PS[p2][:], AF.Sigmoid, [self.PB[p2], pb], [sb_], bias=cb[:, 6 + c:7 + c], scale=1.0)
                dst = U[:, c, 30 + tq * 512:30 + (tq + 1) * 512]
                P.op("dve", lambda e, dst=dst, p1=p1, c=c, sg_=sg_: e.scalar_tensor_tensor(dst, self.PS[p1][:], cb[:, c:c + 1], sg_, ALU.add, ALU.mult),
                     [self.PB[p1], sb_, pb], [Ub[c]])
        P.barrier()
        C.off = mark
        diag = self.A_t[:, 0:186 * 128].rearrange("p (j m) -> p j m", m=128)
        dgb = Buf()
        for idx in range(186):
            eng = "dve" if idx % 2 == 0 else "pool"
            P.op(eng, lambda e, idx=idx: e.tensor_scalar(diag[:, idx, :], self.identB, cdw[:, idx:idx + 1], None, ALU.mult),
                 [self.cB, pb] , [dgb] + self.XTb)
        ysb = [C.f32(128, 512) for _ in range(2)]; ysbb = [Buf() for _ in range(2)]
        ysq = [C.f32(128, 512) for _ in range(2)]; ysqb = [Buf() for _ in range(2)]
        mean = C.f32(128, 512); rstd = C.f32(128, 512); msq = C.f32(128, 512); stb = Buf()
        tmp = [C.f32(128, 512) for _ in range(2)]; tmpb = [Buf() for _ in range(2)]
        n2 = 0
        for tq in range(NQ):
            for c in range(6):
                for j in range(31):
                    self.mm(self.PS[c][:], diag[:, c * 31 + j, :], U[:, c, tq * 512 + j:tq * 512 + j + 512], j == 0, j == 30,
                            [dgb, Ub[c]], [self.PB[c]])
                y_ = ysb[n2 % 2]; yb_ = ysbb[n2 % 2]; q_ = ysq[n2 % 2]; qb_ = ysqb[n2 % 2]; n2 += 1
                self.act(y_, self.PS[c][:], AF.Identity, [self.PB[c], pb], [yb_], bias=cdb[:, c:c + 1], scale=1.0)
                P.op("pool", lambda e, y_=y_, q_=q_: e.tensor_tensor(q_, y_, y_, ALU.mult), [yb_], [qb_])
                self.mm(self.PS[6][:], self.onesF, y_, c == 0, c == 5, [yb_, self.cB], [self.PB[6]])
                self.mm(self.PS[7][:], self.onesF, q_, c == 0, c == 5, [qb_, self.cB], [self.PB[7]])
            P.op("dve", lambda e: e.tensor_scalar(mean, self.PS[6][:], 1.0 / 768, None, ALU.mult), [self.PB[6]], [stb])
            P.op("dve", lambda e: e.tensor_tensor(msq, mean, mean, ALU.mult), [stb], [stb])
            P.op("dve", lambda e: e.scalar_tensor_tensor(rstd, self.PS[7][:], 1.0 / 768, msq, ALU.mult, ALU.subtract), [self.PB[7], stb], [stb])
            P.op("act", lambda e: e.activation(rstd, rstd, AF.Sqrt, bias=self.eps_t[:, 0:1], scale=1.0), [stb, self.cB], [stb])
            P.op("dve", lambda e: e.reciprocal(rstd, rstd), [stb], [stb])
            for c in range(6):
                t_ = tmp[c % 2]; tb_ = tmpb[c % 2]
                P.op("dve", lambda e, t_=t_, c=c: e.scalar_tensor_tensor(t_, self.PS[c][:], cdb[:, c:c + 1], mean, ALU.add, ALU.subtract),
                     [self.PB[c], stb, pb], [tb_])
                P.op("dve", lambda e, t_=t_: e.tensor_tensor(t_, t_, rstd, ALU.mult), [tb_, stb], [tb_])
                dst = self.CT[:, c, tq * 512:(tq + 1) * 512]
                self.act(dst, t_, AF.Silu, [tb_, pb], [self.CTb[c][tq]], bias=clb[:, c:c + 1], scale=clg[:, c:c + 1])
        P.barrier()
        self.xattn(l)


def _consts(S):
    c = {}
    c["c_ident"] = np.eye(128, dtype=np.float32)
    cr = np.zeros((128, 4 * 128 + 4), np.float32)
    inv_n = (np.float32(500000.0) ** (-np.arange(0, 16, 2, dtype=np.float32) / np.float32(16))).astype(np.float32)
    inv_m = (np.float32(10000.0) ** (-np.arange(0, 32, 2, dtype=np.float32) / np.float32(32))).astype(np.float32)
    for base in (0, 64):
        for i in range(8):
            cr[base + i + 8, base + i] = -1.0
            cr[base + i, base + i + 8] = 1.0
            cr[base + i, 512] = inv_n[i]
            cr[base + i + 8, 512] = inv_n[i]
    for i in range(64, 80):
        cr[i + 16, 128 + i] = -1.0
        cr[i, 128 + i + 16] = 1.0
        cr[i, 513] = inv_m[i - 64]
        cr[i + 16, 513] = inv_m[i - 64]
    c["c_rope"] = cr
    eb = np.zeros((64, S), np.float32)
    for key in range(S):
        eb[(key // 64) % 64, key] = 1.0
    c["c_ebig"] = eb
    ov = np.zeros((256, 64), np.float32)
    ncmp = (S - 32) // 16 + 1
    for n in range(ncmp):
        for j in range(S // 64):
            if 16 * n < 64 * j + 64 and 16 * n + 32 > 64 * j:
                ov[n, j] = 1.0
    c["c_ov"] = ov
    fb = np.zeros((S, 64), np.float32)
    for t in range(S):
        cur = t // 64
        fb[t, cur + 1:] = -1.0e9
        fb[t, cur] = 1.0e9
        if cur >= 1:
            fb[t, cur - 1] = 2.0e9
        fb[t, 0] = 3.0e9
    c["c_fb"] = fb
    sel = np.zeros((3, 3 * 64), np.float32)
    for r in range(3):
        sel[r, r * 64:(r + 1) * 64] = 1.0
    c["c_sel"] = sel
    return c


def layer_inputs(l, kind, j, w):
    f = lambda a: np.ascontiguousarray(a, dtype=np.float32)
    d = {}
    d[f"wkv{l}"] = f(w["mem_w_kv"][l]); d[f"wo{l}"] = f(w["w_out"][l])
    d[f"lng{l}"] = f(w["ln_g"][l]); d[f"lnb{l}"] = f(w["ln_b"][l])
    d[f"wr{l}"] = f(np.concatenate([w["moe_w_grp"][l], w["moe_w_exp"][l]], axis=1))
    d[f"br{l}"] = f(np.concatenate([w["moe_b_grp"][l], w["moe_b_exp"][l]])[None, :])
    d[f"wg{l}"] = f(w["moe_w_gate"][l]); d[f"wu{l}"] = f(w["moe_w_up"][l]); d[f"wd{l}"] = f(w["moe_w_down"][l])
    if kind == 2:
        d[f"win{l}"] = f(w["conv_w_in"][j])
        d[f"cbin{l}"] = f(w["conv_b_in"][j].reshape(12, 128).T)
        d[f"cdw{l}"] = f(w["conv_dw_w"][j].T.reshape(6, 128, 31).transpose(1, 0, 2).reshape(128, 186))
        d[f"cdb{l}"] = f(w["conv_dw_b"][j].reshape(6, 128).T)
        d[f"clg{l}"] = f(w["conv_ln_g"][j].reshape(6, 128).T)
        d[f"clb{l}"] = f(w["conv_ln_b"][j].reshape(6, 128).T)
    elif kind == 1:
        d[f"win{l}"] = f(w["mla_w_in"][j])
        d[f"qn{l}"] = f(w["mla_q_norm"][j].reshape(2, 128).T); d[f"kvn{l}"] = f(w["mla_kv_norm"][j][:, None])
        d[f"wuq{l}"] = f(w["mla_w_uq"][j]); d[f"wukv{l}"] = f(w["mla_w_ukv"][j])
    else:
        d[f"win{l}"] = f(w["nsa_w_in"][j])
        d[f"cpe{l}"] = f(np.transpose(w["nsa_cmp_pe"][j], (0, 2, 1)))
        d[f"cw1{l}"] = f(w["nsa_cmp_w1"][j].reshape(2, 32, 64, 128).transpose(0, 2, 1, 3))
        d[f"cw2{l}"] = f(w["nsa_cmp_w2"][j])
    return d


_NC_CACHE = {}


def run_model(S, kinds, js, x, mem, positions, w, n_cores=NCORES):
    key = (S, tuple(kinds))
    if key not in _NC_CACHE:
        _NC_CACHE[key] = Builder(S, kinds).build()
    nc = _NC_CACHE[key]
    shared = _consts(S)
    for l, (kind, j) in enumerate(zip(kinds, js)):
        shared.update(layer_inputs(l, kind, j, w))
    in_maps = []
    for b in range(n_cores):
        m = dict(shared)
        m["x"] = np.ascontiguousarray(x[b], dtype=np.float32)
        m["mem"] = np.ascontiguousarray(mem[b], dtype=np.float32)
        m["pos"] = np.ascontiguousarray(positions[b][None, :], dtype=np.int32)
        pe = np.asarray(positions[b])[31::16]
        pe = np.concatenate([pe, np.repeat(pe[-1:], 256 - len(pe))])[:256]
        m["pos_end"] = np.ascontiguousarray(pe[None, :], dtype=np.int32)
        in_maps.append(m)
    import os
    if os.environ.get("K_TRACE"):
        res = run_bass_kernel_spmd(nc, in_maps, core_ids=list(range(n_cores)), trace=True)
        print("EXEC_TIME_NS", res.exec_time_ns)
        import pickle
        try:
            it = res.instructions_and_trace
            print("IT type", type(it), (len(it) if hasattr(it, "__len__") else ""))
            pj = res.profile_json
            print("PJ type", type(pj), (list(pj.keys())[:20] if isinstance(pj, dict) else str(pj)[:300]))
            pickle.dump({"it": it, "pj": pj}, open("trace_dump.pkl", "wb"))
        except Exception as ex:
            print("trace dump failed", ex)
    else:
        res = run_bass_kernel_spmd(nc, in_maps, core_ids=list(range(n_cores)))
    import os
    if os.environ.get("DBG_CT"):
        np.save("dbg_ct.npy", np.asarray(res.results[0]["dbg"]).astype(np.float32))
    if os.environ.get("DBG_GL"):
        a = np.asarray(res.results[0]["glT"]); print("glT", a.dtype, a.shape); np.save("d_glT.npy", a)
    if os.environ.get("DBG_NSA"):
        for nme in res.results[0]:
            if nme.startswith("d_"):
                a = np.asarray(res.results[0][nme])
                print(nme, a.dtype, a.shape)
                np.save(nme + ".npy", a.astype(np.float32))
    return np.stack([np.asarray(r["out"]) for r in res.results], axis=0)


def kernel(**inputs):
    x = np.asarray(inputs["x"]); mem = np.asarray(inputs["mem"]); positions = np.asarray(inputs["positions"])
    w = {k: np.asarray(v) for k, v in inputs.items() if k not in ("x", "mem", "positions")}
    kinds = [0, 1, 2, 0]
    js = [0, 0, 0, 1]
    out = run_model(x.shape[1], kinds, js, x, mem, positions, w)
    return out.astype(np.float32)


def _mixer_mla(self, l):
    P, C, I, S = self.P, self.C, self.I, self.S
    NT, NQ = self.NT, self.NQ
    win = I[f"win{l}"].rearrange("(k p) n -> p k n", p=128)
    self.proj_xq(win, 416)
    stg = [C.bf(128, 512) for _ in range(2)]; stgb = [Buf() for _ in range(2)]
    qn = C.f32(128, 2); kvn = C.f32(128, 2); nb = Buf()
    P.dma("sp", qn, I[f"qn{l}"], writes=[nb]); P.dma("sp", kvn[:, 0:1], I[f"kvn{l}"], writes=[nb])
    CQ = C.bf(128, 2, S); CKV = C.bf(128, S); KRr = C.bf(96, S)
    cqb = [Buf() for _ in range(NQ)]; krb = Buf()
    P.op("pool", lambda e: e.memset(KRr[0:64, :], 0.0), [], [krb])
    mark = C.off
    Win = C.bf(128, 8, 416); winb = Buf()
    P.dma("pool", Win, win[:, :, 0:416], writes=[winb])
    tk = [C.f32(128, 416) for _ in range(2)]; tkb = [Buf() for _ in range(2)]
    junk = C.f32(128, 256); ssq = [C.f32(128, 4) for _ in range(2)]
    for t in range(NT):
        pi = t % 2
        j = t % 2
        for k in range(8):
            self.mm(self.PS[pi][:, 0:416], self.XT[:, k, t * 128:(t + 1) * 128], Win[:, k, :], k == 0, k == 7,
                    [winb, self.XTb[t // 4]], [self.PB[pi]])
        sq = ssq[j]
        self.act(junk, self.PS[pi][:, 0:256], AF.Square, [self.PB[pi]], [tkb[j]], accum_out=sq[:, 0:1])
        self.act(junk[:, 0:128], self.PS[pi][:, 256:384], AF.Square, [self.PB[pi]], [tkb[j]], accum_out=sq[:, 1:2])
        self.act(sq[:, 0:1], sq[:, 0:1], AF.Sqrt, [tkb[j], self.cB], [tkb[j]], bias=self.eps_t[:, 1:2], scale=1.0 / 256)
        self.act(sq[:, 1:2], sq[:, 1:2], AF.Sqrt, [tkb[j], self.cB], [tkb[j]], bias=self.eps_t[:, 1:2], scale=1.0 / 128)
        P.op("dve", lambda e, sq=sq: e.reciprocal(sq[:, 0:2], sq[:, 0:2]), [tkb[j]], [tkb[j]])
        P.op("dve", lambda e, j=j, pi=pi, sq=sq: e.tensor_scalar(tk[j][:, 0:256], self.PS[pi][:, 0:256], sq[:, 0:1], None, ALU.mult), [self.PB[pi], tkb[j]], [tkb[j]])
        P.op("dve", lambda e, j=j, pi=pi, sq=sq: e.tensor_scalar(tk[j][:, 256:384], self.PS[pi][:, 256:384], sq[:, 1:2], None, ALU.mult), [self.PB[pi], tkb[j]], [tkb[j]])
        P.op("dve", lambda e, j=j, pi=pi: e.tensor_copy(tk[j][:, 384:416], self.PS[pi][:, 384:416]), [self.PB[pi], tkb[j]], [tkb[j]])
        pt = 2 + (t % 2)
        for bi, (c0, c1) in enumerate(((0, 128), (128, 256), (256, 384), (320, 416))):
            self.tr(self.PS[pt][0:c1 - c0, bi * 128:(bi + 1) * 128], tk[j][:, c0:c1], self.identF, [tkb[j], self.cB], [self.PB[pt]])
        tsl = slice(t * 128, (t + 1) * 128)
        q = t // 4
        self.act(CQ[:, 0, tsl], self.PS[pt][:, 0:128], AF.Copy, [self.PB[pt], nb], [cqb[q]], scale=qn[:, 0:1])
        self.act(CQ[:, 1, tsl], self.PS[pt][:, 128:256], AF.Copy, [self.PB[pt], nb], [cqb[q]], scale=qn[:, 1:2])
        self.act(CKV[:, tsl], self.PS[pt][:, 256:384], AF.Copy, [self.PB[pt], nb], [cqb[q]], scale=kvn[:, 0:1])
        P.op("dve", lambda e, pt=pt, tsl=tsl: e.tensor_copy(KRr[64:96, tsl], self.PS[pt][64:96, 384:512]), [self.PB[pt]], [cqb[q], krb])
    P.barrier()
    import os
    STOP = int(os.environ.get("MLA_STOP", "9"))
    if STOP == 1:
        self.xattn(l); return
    C.off = mark
    Wuq = C.bf(128, 2, 1152); Wukv = C.bf(128, 1536); ub = Buf()
    P.dma("pool", Wuq, I[f"wuq{l}"].rearrange("(k p) n -> p k n", p=128), writes=[ub])
    P.dma("pool", Wukv, I[f"wukv{l}"], writes=[ub])
    posb_t = C.f32(96, 512, dt=I32); posf = C.f32(96, 512); kf = C.f32(96, 512); ki = posb_t
    Ct = C.f32(96, 512); St = C.f32(96, 512); tb = Buf()
    qraw = [C.bf(96, 512) for _ in range(2)]; qrb = [Buf() for _ in range(2)]
    t1 = [C.f32(96, 512) for _ in range(2)]; t2 = [C.f32(96, 512) for _ in range(2)]; t12b = [Buf() for _ in range(2)]
    qo = [C.bf(96, 512) for _ in range(2)]; qob = [Buf() for _ in range(2)]
    vst = [C.bf(128, 768) for _ in range(2)]; vstb = [Buf() for _ in range(2)]
    Rm = self.RmB[0:96, 0:96]
    n = 0
    for tq in range(NQ):
        csl = slice(tq * 512, (tq + 1) * 512)
        SK = os.environ.get("MLA_SKIP", "")
        if "r" not in SK:
            self.rope_tables(tq, 96, self.invF[0:96, 1:2], posb_t, posf, kf, ki, Ct, St, tb)
        j = n % 2; n += 1
        if "k" not in SK:
            self.mm(self.PS[0][0:96, :], Rm, KRr[:, csl], True, True, [self.cB, cqb[tq], krb], [self.PB[0]])
            P.op("dve", lambda e, j=j, csl=csl: e.tensor_tensor(t1[j], KRr[:, csl], Ct, ALU.mult), [cqb[tq], krb, tb], [t12b[j]])
            P.op("dve", lambda e, j=j: e.tensor_tensor(t2[j], self.PS[0][0:96, :], St, ALU.mult), [self.PB[0], tb], [t12b[j]])
            P.op("pool", lambda e, j=j: e.tensor_tensor(qo[j], t1[j], t2[j], ALU.add), [t12b[j]], [qob[j]])
            P.dma("sp", self.pT[1920:1952, csl], qo[j][64:96, :], reads=[qob[j]], writes=[self.pT_b])
        for h in range(int(os.environ.get("MLA_NH", "12")) if "q" not in SK else 0):
            pq, pr = 1 + (h % 2) * 2, 2 + (h % 2) * 2
            for rc in range(2):
                self.mm(self.PS[pq][0:96, :], Wuq[:, rc, h * 96:(h + 1) * 96], CQ[:, rc, csl], rc == 0, rc == 1, [ub, cqb[tq]], [self.PB[pq]])
            j = n % 2; n += 1
            if "a" in SK:
                continue
            self.act(qraw[j], self.PS[pq][0:96, :], AF.Copy, [self.PB[pq]], [qrb[j]])
            if "b" in SK:
                continue
            self.mm(self.PS[pr][0:96, :], Rm, qraw[j], True, True, [self.cB, qrb[j]], [self.PB[pr]])
            if "c" in SK:
                continue
            VAR = os.environ.get("MLA_VAR", "0")
            if VAR == "0":
                P.op("dve", lambda e, j=j, pq=pq: e.tensor_tensor(t1[j], self.PS[pq][0:96, :], Ct, ALU.mult), [self.PB[pq], tb], [t12b[j]])
            elif VAR == "1":
                P.op("dve", lambda e, j=j, pq=pq: e.tensor_tensor(t1[j], self.PS[pq][0:96, :], St, ALU.mult), [self.PB[pq], tb], [t12b[j]])
            elif VAR == "2":
                P.op("dve", lambda e, j=j, pq=pq: e.tensor_tensor(t1[j], self.PS[pq][0:96, :], Ct, ALU.mult), [self.PB[pq], tb, qrb[j]], [t12b[j]])
            elif VAR == "3":
                P.op("dve", lambda e, j=j, pq=pq: e.tensor_tensor(t1[j], qraw[j], Ct, ALU.mult), [qrb[j], tb], [t12b[j]])
            if "e" in SK:
                continue
            P.op("dve", lambda e, j=j, pr=pr: e.tensor_tensor(t2[j], self.PS[pr][0:96, :], St, ALU.mult), [self.PB[pr], tb], [t12b[j]])
            if "f" in SK:
                continue
            P.op("dve" if "p" in SK else "pool", lambda e, j=j: e.tensor_tensor(qo[j], t1[j], t2[j], ALU.add), [t12b[j]], [qob[j]])
            if "d" not in SK:
                P.dma("sp", self.pT[h * 96:(h + 1) * 96, csl], qo[j], reads=[qob[j]], writes=[self.pT_b])
            if "n" in SK:
                continue
            pk = 5 + (h % 2)
            self.mm(self.PS[pk][0:64, :], Wukv[:, h * 128:h * 128 + 64], CKV[:, csl], True, True, [ub, cqb[tq]], [self.PB[pk]])
            sj = stg[h % 2]; sjb = stgb[h % 2]
            self.act(sj[0:64, :], self.PS[pk][0:64, :], AF.Copy, [self.PB[pk]], [sjb])
            P.dma("sp", self.pT[1152 + h * 64:1152 + (h + 1) * 64, csl], sj[0:64, :], reads=[sjb], writes=[self.pT_b])
        Wv = Wukv[:].rearrange("p (h c) -> p h c", c=128)
        for ts in range(4 if "v" not in SK else 0):
            t = tq * 4 + ts
            j = t % 2
            for hv in range(2):
                pv = 6 + hv
                self.mm(self.PS[pv][:, 0:384], CKV[:, t * 128:(t + 1) * 128], Wv[:, hv * 6:(hv + 1) * 6, 64:128], True, True, [ub, cqb[tq]], [self.PB[pv]])
                P.op("dve", lambda e, j=j, hv=hv, pv=pv: e.tensor_copy(vst[j][:, hv * 384:(hv + 1) * 384], self.PS[pv][:, 0:384]), [self.PB[pv]], [vstb[j]])
            P.dma("sp", self.vtok[t * 128:(t + 1) * 128, :], vst[j], reads=[vstb[j]], writes=[self.vtok_b])
    P.barrier()
    if STOP == 2:
        self.xattn(l); return
    C.reset()
    kT = [C.bf(96, S) for _ in range(2)]; qT = [C.bf(96, S) for _ in range(2)]
    vA = [C.bf(128, NT, 128) for _ in range(2)]
    hb = [Buf() for _ in range(2)]
    E = [C.bf(128, 512) for _ in range(6)]; Eb = [Buf() for _ in range(6)]
    rd = [C.f32(64, 512) for _ in range(2)]; rdb = [Buf() for _ in range(2)]
    for j in range(2):
        P.op("pool", lambda e, j=j: e.memset(vA[j][:, :, 64:128], 1.0), [], [hb[j]])
    scale = 1.0 / math.sqrt(96.0)
    ne = 0
    vt = self.vtok.rearrange("(t p) c -> p t c", p=128)

    def load_head(h):
        j = h % 2
        P.dma("sp", qT[j], self.pT[h * 96:(h + 1) * 96, :], reads=[self.pT_b], writes=[hb[j]])
        P.dma("sp", kT[j][0:64, :], self.pT[1152 + h * 64:1152 + (h + 1) * 64, :], reads=[self.pT_b], writes=[hb[j]])
        P.dma("sp", kT[j][64:96, :], self.pT[1920:1952, :], reads=[self.pT_b], writes=[hb[j]])
        with self.nc.allow_non_contiguous_dma(reason="per-head V gather (128B runs)"):
            for t0 in range(0, NT, 4):
                P.dma("sp", vA[j][:, t0:t0 + 4, 0:64], vt[:, t0:t0 + 4, h * 64:(h + 1) * 64], reads=[self.vtok_b], writes=[hb[j]])
    st_ = {"ne": 0, "ns": 0}

    def new_E():
        i = st_["ne"] % 6; st_["ne"] += 1
        return E[i], Eb[i]

    def new_S():
        i = st_["ns"] % 4; st_["ns"] += 1
        return i
    load_head(0)
    for h in range(12):
        if h + 1 < 12:
            load_head(h + 1)
        j = h % 2
        pairs = []
        for tq in range(NQ):
            po = 4 + (tq % 2)
            nk = 4 * tq + 4
            for kt in range(nk):
                def score(ps, kt=kt, tq=tq, j=j):
                    self.mm(self.PS[ps][:], kT[j][:, kt * 128:(kt + 1) * 128], qT[j][:, tq * 512:(tq + 1) * 512], True, True, [hb[j]], [self.PB[ps]])

                def post(ps, Ei, Ebi, kt=kt, tq=tq):
                    self.act(Ei, self.PS[ps][:], AF.Exp, [self.PB[ps]], [Ebi], scale=scale)
                    if kt >= 4 * tq:
                        base = tq * 512 - kt * 128
                        P.op("pool", lambda e: e.affine_select(Ei, Ei, [[1, 512]], ALU.is_ge, 0.0, base=base, channel_multiplier=-1), [Ebi], [Ebi])

                def pv(Ei, Ebi, kt=kt, nk=nk, po=po, j=j):
                    self.mm(self.PS[po][:], vA[j][:, kt, :], Ei, kt == 0, kt == nk - 1, [hb[j], Ebi], [self.PB[po]])
                d = {"score": score, "post": post, "pv": pv}
                if kt == nk - 1:
                    def after(tq=tq, po=po, h=h):
                        r = rd[tq % 2]; rb = rdb[tq % 2]
                        self.act(r, self.PS[po][64:128, :], AF.Ln, [self.PB[po]], [rb])
                        self.act(r, r, AF.Exp, [rb], [rb], scale=-1.0)
                        dst = self.CT[64 * (h % 2):64 * (h % 2) + 64, h // 2, tq * 512:(tq + 1) * 512]
                        P.op("dve", lambda e: e.tensor_tensor(dst, self.PS[po][0:64, :], r, ALU.mult),
                             [self.PB[po], rb], [self.CTb[h // 2][tq]])
                    d["after"] = after
                pairs.append(d)
        self.run_pipeline(pairs, new_S, new_E)
    P.barrier()
    self.xattn(l)


Builder.mixer_mla = _mixer_mla


def _mixer_nsa(self, l):
    P, C, I, S = self.P, self.C, self.I, self.S
    NT, NQ = self.NT, self.NQ
    NCMP = (S - 32) // 16 + 1
    NCT = (NCMP + 127) // 128
    NCP = NCT * 128
    win = I[f"win{l}"].rearrange("(k p) n -> p k n", p=128)
    self.proj_xq(win, 2340)
    stg = [C.bf(128, 512) for _ in range(2)]; stgb = [Buf() for _ in range(2)]
    Wq = C.bf(128, 8, 768); Wk = C.bf(128, 8, 1024); Wg = C.bf(128, 8, 36); Wv = C.bf(128, 8, 512); wb = Buf()
    P.dma("pool", Wq, win[:, :, 0:768], writes=[wb])
    P.dma("pool", Wk[:, :, 0:256], win[:, :, 1280:1536], writes=[wb])
    P.dma("pool", Wk[:, :, 256:512], win[:, :, 1792:2048], writes=[wb])
    P.dma("pool", Wk[:, :, 512:1024], win[:, :, 768:1280], writes=[wb])
    P.dma("pool", Wg, win[:, :, 2304:2340], writes=[wb])
    P.dma("pool", Wv[:, :, 0:256], win[:, :, 1536:1792], writes=[wb])
    P.dma("pool", Wv[:, :, 256:512], win[:, :, 2048:2304], writes=[wb])
    posb_t = C.f32(128, 512, dt=I32); posf = C.f32(128, 512); kf = C.f32(128, 512); ki = posb_t
    Ct = C.f32(128, 512); St = C.f32(128, 512); tb = Buf()
    qraw = [C.bf(128, 512) for _ in range(2)]; qrb = [Buf() for _ in range(2)]
    t1 = [C.f32(128, 512) for _ in range(2)]; t2 = [C.f32(128, 512) for _ in range(2)]; t12b = [Buf() for _ in range(2)]
    qo = [C.bf(128, 512) for _ in range(2)]; qob = [Buf() for _ in range(2)]
    vst = [C.bf(128, 512) for _ in range(2)]; vstb = [Buf() for _ in range(2)]
    gst = [C.f32(36, 512) for _ in range(2)]; gstb = [Buf() for _ in range(2)]
    n = 0
    for tq in range(NQ):
        csl = slice(tq * 512, (tq + 1) * 512)
        self.rope_tables(tq, 128, self.invF[:, 0:1], posb_t, posf, kf, ki, Ct, St, tb)
        for ci in range(10):
            Wsrc, col0 = (Wq, ci * 128) if ci < 6 else (Wk, (ci - 6) * 128)
            row0 = ci * 128
            pq, pr = 0 + (ci % 2) * 2, 1 + (ci % 2) * 2
            self.proj_chunk(pq, 128, Wsrc, wb, col0, tq)
            j = n % 2; n += 1
            self.act(qraw[j], self.PS[pq][:], AF.Copy, [self.PB[pq]], [qrb[j]])
            self.mm(self.PS[pr][:], self.RnB, qraw[j], True, True, [self.cB, qrb[j]], [self.PB[pr]])
            P.op("dve", lambda e, j=j, pq=pq: e.tensor_tensor(t1[j], self.PS[pq][:], Ct, ALU.mult), [self.PB[pq], tb], [t12b[j]])
            P.op("dve", lambda e, j=j, pr=pr: e.tensor_tensor(t2[j], self.PS[pr][:], St, ALU.mult), [self.PB[pr], tb], [t12b[j]])
            P.op("pool", lambda e, j=j: e.tensor_tensor(qo[j], t1[j], t2[j], ALU.add), [t12b[j]], [qob[j]])
            P.dma("sp", self.pT[row0:row0 + 128, csl], qo[j], reads=[qob[j]], writes=[self.pT_b])
        for ci in range(4):
            pi = 4 + (ci % 2)
            self.proj_chunk(pi, 128, Wk, wb, 512 + ci * 128, tq)
            sj = stg[ci % 2]; sjb = stgb[ci % 2]
            self.act(sj, self.PS[pi][:], AF.Copy, [self.PB[pi]], [sjb])
            P.dma("sp", self.pT[1280 + ci * 128:1280 + (ci + 1) * 128, csl], sj, reads=[sjb], writes=[self.pT_b])
        self.proj_chunk(6, 36, Wg, wb, 0, tq)
        gj = gst[tq % 2]; gjb = gstb[tq % 2]
        self.act(gj, self.PS[6][0:36, :], AF.Copy, [self.PB[6]], [gjb])
        P.dma("sp", self.glT[:, csl], gj, reads=[gjb], writes=[self.glT_b])
        for ts in range(4):
            t = tq * 4 + ts
            j = t % 2
            pv = 5 if ts % 2 == 0 else 7
            for k in range(8):
                self.mm(self.PS[pv][:], self.XT[:, k, t * 128:(t + 1) * 128], Wv[:, k, :], k == 0, k == 7,
                        [wb, self.XTb[tq]], [self.PB[pv]])
            P.op("dve", lambda e, j=j, pv=pv: e.tensor_copy(vst[j], self.PS[pv][:]), [self.PB[pv]], [vstb[j]])
            P.dma("sp", self.vtok[t * 128:(t + 1) * 128, 0:512], vst[j], reads=[vstb[j]], writes=[self.vtok_b])
    P.barrier()
    C.reset()
    kcmpT = C.bf(64, 4, NCP); vcA = C.bf(128, 4, NCT, 128); cmb = Buf()
    Ebig = self.A_t[64:128, 0:S]
    Ov = C.bf(128, NCT, 64); Sel = C.bf(3, 3, 64); kb = Buf()
    P.dma("pool", Ebig, I["c_ebig"], writes=[kb])
    P.dma("pool", Ov, I["c_ov"][0:NCP, :].rearrange("(t p) j -> p t j", p=128), writes=[kb])
    P.dma("pool", Sel, I["c_sel"].rearrange("r (a m) -> r a m", m=64), writes=[kb])
    P.op("pool", lambda e: e.memset(vcA[:, :, :, 0:64], 0.0), [], [cmb])
    P.op("pool", lambda e: e.memset(vcA[:, :, :, 64:128], 1.0), [], [cmb])
    P.op("pool", lambda e: e.memset(kcmpT, 0.0), [], [cmb])
    mark = C.off
    cw1 = C.bf(64, 2, 32, 128); cpe = C.bf(64, 2, 32); cw2 = C.bf(128, 2, 64); cwb = Buf()
    P.dma("pool", cw1, I[f"cw1{l}"].rearrange("a d l f -> d a l f"), writes=[cwb])
    P.dma("pool", cpe, I[f"cpe{l}"].rearrange("a d l -> d a l"), writes=[cwb])
    P.dma("pool", cw2, I[f"cw2{l}"].rearrange("a f d -> f a d"), writes=[cwb])
    kcT = [C.bf(64, S) for _ in range(2)]; kcb = [Buf() for _ in range(2)]
    hbias = C.f32(128, 2); hbb = Buf()
    hT = [C.bf(128, NCP) for _ in range(2)]; hTb = [Buf() for _ in range(2)]
    posb2 = C.f32(64, NCP, dt=I32); posf2 = C.f32(64, NCP); kf2 = C.f32(64, NCP); ki2 = posb2
    Ct2 = C.f32(64, NCP); St2 = C.f32(64, NCP); tb2 = Buf()
    kraw = C.bf(64, NCP); krb_ = Buf(); u1 = C.f32(64, NCP); u2 = C.f32(64, NCP)
    self.rope_tables(0, 64, self.invF[0:64, 0:1], posb2, posf2, kf2, ki2, Ct2, St2, tb2,
                     pos_src=I["pos_end"][0:1, 0:NCP], width=NCP)
    for a in range(2):
        for ll in range(32):
            self.mm(self.PS[6][:, a:a + 1], cw1[:, a, ll, :], cpe[:, a, ll:ll + 1], ll == 0, ll == 31, [cwb], [self.PB[6]])
    P.op("dve", lambda e: e.tensor_copy(hbias, self.PS[6][:, 0:2]), [self.PB[6]], [hbb])
    nn = 0
    for a in range(2):
        for k in range(4):
            jj = nn % 2; nn += 1
            if a == 0:
                P.dma("sp", kcT[jj], self.pT[1280 + 64 * k:1280 + 64 * (k + 1), :], reads=[self.pT_b], writes=[kcb[jj]])
            else:
                P.dma("sp", kcT[jj], self.pT[1536 + 64 * k:1536 + 64 * (k + 1), :], reads=[self.pT_b], writes=[kcb[jj]])
            ph = jj
            src = kcT[jj]
            for ll in range(32):
                rhs = src[:, ll:ll + 16 * (NCMP - 1) + 1:16]
                self.mm(self.PS[ph][:, 0:NCMP], cw1[:, a, ll, :], rhs, ll == 0, ll == 31, [cwb, kcb[jj]], [self.PB[ph]])
            h_ = hT[jj]; hb_ = hTb[jj]
            if NCMP < NCP:
                P.op("pool", lambda e, h_=h_: e.memset(h_[:, NCMP:NCP], 0.0), [], [hb_])
            self.act(h_[:, 0:NCMP], self.PS[ph][:, 0:NCMP], AF.Silu, [self.PB[ph], hbb], [hb_], bias=hbias[:, a:a + 1], scale=1.0)
            if a == 0:
                self.mm(self.PS[2][0:64, 0:NCP], cw2[:, 0, :], h_, True, True, [cwb, hb_], [self.PB[2]])
                self.act(kraw, self.PS[2][0:64, 0:NCP], AF.Copy, [self.PB[2]], [krb_])
                self.mm(self.PS[3][0:64, 0:NCP], self.RnB[0:64, 0:64], kraw, True, True, [self.cB, krb_], [self.PB[3]])
                P.op("dve", lambda e: e.tensor_tensor(u1, self.PS[2][0:64, 0:NCP], Ct2, ALU.mult), [self.PB[2], tb2], [krb_])
                P.op("dve", lambda e: e.tensor_tensor(u2, self.PS[3][0:64, 0:NCP], St2, ALU.mult), [self.PB[3], tb2], [krb_])
                P.op("dve", lambda e, k=k: e.tensor_tensor(kcmpT[:, k, 0:NCMP], u1[:, 0:NCMP], u2[:, 0:NCMP], ALU.add), [krb_], [cmb])
            else:
                for nt in range(NCT):
                    m_ = min(128, NCMP - nt * 128)
                    self.mm(self.PS[4 + nt][0:m_, 0:64], h_[:, nt * 128:nt * 128 + m_], cw2[:, 1, :], True, True, [cwb, hb_], [self.PB[4 + nt]])
                    P.op("dve", lambda e, k=k, nt=nt, m_=m_: e.tensor_copy(vcA[0:m_, k, nt, 0:64], self.PS[4 + nt][0:m_, 0:64]), [self.PB[4 + nt]], [cmb])
    P.barrier()
    self.nsa_attention(l, kcmpT, vcA, cmb, Ebig, Ov, Sel, kb, mark, NCMP, NCT)


Builder.mixer_nsa = _mixer_nsa


def _nsa_attention(self, l, kcmpT, vcA, cmb, Ebig, Ov, Sel, kb, mark, NCMP, NCT):
    P, C, I, S = self.P, self.C, self.I, self.S
    NT, NQ = self.NT, self.NQ
    C.off = mark
    A = self.A_t
    ksT = A[0:64, 0:S]; kwT = A[0:64, S:2 * S]
    ksE = A[0:128, 0:S]
    vsA = A[:, 2 * S:3 * S].rearrange("p (t c) -> p t c", c=128)
    vwA = A[:, 3 * S:4 * S].rearrange("p (t c) -> p t c", c=128)
    kvb = Buf()
    P.op("pool", lambda e: e.memset(vsA[:, :, 64:128], 1.0), [], [kvb])
    P.op("pool", lambda e: e.memset(vwA[:, :, 64:128], 1.0), [], [kvb])
    qc = [[C.bf(128, 512) for _ in range(3)] for _ in range(2)]; qcb = [[Buf() for _ in range(3)] for _ in range(2)]
    qsb = [[Buf() for _ in range(3)] for _ in range(2)]
    Gl = [C.f32(3, 512) for _ in range(3)]; glf = [Buf() for _ in range(3)]
    Glh = [[C.bf(3, 512) for _ in range(3)] for _ in range(2)]
    Gll = [[C.bf(3, 512) for _ in range(3)] for _ in range(2)]
    Gtmp = C.f32(3, 512); gtb = Buf()
    glb = [[Buf() for _ in range(3)] for _ in range(2)]
    FBt = [C.f32(128, 4, 64) for _ in range(2)]; CBt = [C.f32(128, 4, 64) for _ in range(2)]; fbb = [Buf() for _ in range(2)]
    E = [C.bf(128, 512) for _ in range(6)]; Eb = [Buf() for _ in range(6)]
    rd = [C.f32(64, 512) for _ in range(3)]; rdb = [Buf() for _ in range(3)]
    gs = [C.f32(64, 512) for _ in range(3)]; gsb = [Buf() for _ in range(3)]
    acc = [C.f32(64, 512) for _ in range(3)]; accb = [Buf() for _ in range(3)]
    impacc = C.f32(64, 512); impb = Buf()
    selbT = C.bf(64, 512); selTb = Buf()
    impm = [C.f32(128, 64) for _ in range(4)]; work = [C.f32(128, 64) for _ in range(4)]
    m8a = [C.f32(128, 8) for _ in range(4)]; m8b = [C.f32(128, 8) for _ in range(4)]
    selt = [C.f32(128, 64) for _ in range(4)]; selbf = [C.bf(128, 64) for _ in range(4)]; slb = [Buf() for _ in range(4)]
    PS, PB = self.PS, self.PB
    self.dbg_off = {"selbT": selbT.offset, "qc00": qc[0][0].offset, "impacc": impacc.offset, "selbf0": selbf[0].offset,
                    "CBt0": CBt[0].offset, "Glh00": Glh[0][0].offset, "E0": E[0].offset}
    PS7b = PS[7][:].bitcast(BF16)
    vt = self.vtok.rearrange("(t p) c -> p t c", p=128)
    fbv = I["c_fb"].rearrange("(q ts p) j -> q p ts j", p=128, ts=4)
    st = {"ne": 0, "nr": 0, "ns": 0, "grp": 0}

    def new_E():
        i = st["ne"] % 6; st["ne"] += 1
        return E[i], Eb[i]

    def new_S():
        i = st["ns"] % 4; st["ns"] += 1
        return i

    def finish(k, g, tq, po, b, first, last, par, with_imp=False):
        h = 3 * k + g
        i = st["nr"] % 3; st["nr"] += 1
        r_, rb_ = rd[i], rdb[i]
        g_, gb_ = gs[i], gsb[i]
        P.op("dve", lambda e: e.tensor_scalar(r_, PS[po][64:128, :], 1.0e-30, None, ALU.max), [PB[po]], [rb_])
        if with_imp:
            self.act(g_, r_, AF.Ln, [rb_], [gb_])
            self.act(g_, g_, AF.Exp, [gb_], [gb_], scale=-1.0)
            if g == 0:
                P.op("dve", lambda e: e.tensor_tensor(impacc, PS[7][0:64, :], g_, ALU.mult), [PB[7], gb_], [impb])
            else:
                P.op("dve", lambda e: e.tensor_tensor(g_, PS[7][0:64, :], g_, ALU.mult), [PB[7], gb_], [gb_])
                P.op("dve", lambda e: e.tensor_tensor(impacc, impacc, g_, ALU.add), [gb_, impb], [impb])
        self.mm(PS[6][0:64, :], Sel[:, b, :], Glh[par][g], True, False, [kb, glb[par][g]], [PB[6]])
        self.mm(PS[6][0:64, :], Sel[:, b, :], Gll[par][g], False, True, [kb, glb[par][g]], [PB[6]])
        self.act(g_, PS[6][0:64, :], AF.Exp, [PB[6]], [gb_], scale=-1.0)
        P.op("dve", lambda e: e.scalar_tensor_tensor(g_, g_, 1.0, r_, ALU.add, ALU.mult), [gb_, rb_], [gb_])
        self.act(g_, g_, AF.Ln, [gb_], [gb_])
        self.act(g_, g_, AF.Exp, [gb_], [gb_], scale=-1.0)
        import os
        SKB = os.environ.get("NSA_SKIPB", "")
        if str(b) in SKB:
            P.op("dve", lambda e: e.memset(g_, 0.0), [], [gb_])
        if first:
            P.op("dve", lambda e: e.tensor_tensor(acc[g], PS[po][0:64, :], g_, ALU.mult), [PB[po], gb_], [accb[g]])
        else:
            P.op("dve", lambda e: e.tensor_tensor(r_, PS[po][0:64, :], g_, ALU.mult), [PB[po], gb_], [rb_])
            if not last:
                P.op("dve", lambda e: e.tensor_tensor(acc[g], acc[g], r_, ALU.add), [rb_, accb[g]], [accb[g]])
            else:
                dst = self.CT[64 * (h % 2):64 * (h % 2) + 64, h // 2, tq * 512:(tq + 1) * 512]
                P.op("dve", lambda e: e.tensor_tensor(dst, acc[g], r_, ALU.add), [rb_, accb[g]], [self.CTb[h // 2][tq]])

    for k in range(4):
        P.dma("sp", ksT, self.pT[768 + 64 * k:768 + 64 * (k + 1), :], reads=[self.pT_b], writes=[kvb])
        P.dma("sp", kwT, self.pT[1024 + 64 * k:1024 + 64 * (k + 1), :], reads=[self.pT_b], writes=[kvb])
        with self.nc.allow_non_contiguous_dma(reason="per-head V gather (128B runs)"):
            for t0 in range(0, NT, 4):
                P.dma("sp", vsA[:, t0:t0 + 4, 0:64], vt[:, t0:t0 + 4, 64 * k:64 * (k + 1)], reads=[self.vtok_b], writes=[kvb])
                P.dma("sp", vwA[:, t0:t0 + 4, 0:64], vt[:, t0:t0 + 4, 256 + 64 * k:256 + 64 * (k + 1)], reads=[self.vtok_b], writes=[kvb])
        allpairs = []

        def add_tq(k, tq, allpairs):
            par = tq % 2
            q0 = tq * 512
            csl = slice(q0, q0 + 512)

            def setup():
              for g in range(3):
                  h = 3 * k + g
                  P.dma("sp", qc[par][g][0:64, :], self.pT[64 * h:64 * (h + 1), csl], reads=[self.pT_b], writes=[qcb[par][g]])
                  P.dma("sp", Gl[g], self.glT[3 * h:3 * h + 3, csl], reads=[self.glT_b], writes=[glf[g]])
                  P.op("act", lambda e, par=par, g=g: e.copy(Glh[par][g], Gl[g]), [glf[g]], [glb[par][g]])
                  P.op("dve", lambda e, par=par, g=g: e.tensor_tensor(Gtmp, Gl[g], Glh[par][g], ALU.subtract), [glf[g], glb[par][g]], [gtb])
                  P.op("dve", lambda e, par=par, g=g: e.tensor_copy(Gll[par][g], Gtmp), [gtb], [glb[par][g]])
              P.dma("sp", FBt[par], fbv[tq], writes=[fbb[par]])
              P.op("dve", lambda e, par=par: e.tensor_scalar(CBt[par], FBt[par], 0.0, 3.0e-5, ALU.min, ALU.mult), [fbb[par]], [fbb[par]])
            trivial = (q0 + 511 < 16 * 64)

            def selection_part1(par=par, trivial=trivial):
                if trivial:
                    for ts in range(4):
                        P.op("dve", lambda e, ts=ts: e.tensor_copy(selbf[ts], CBt[par][:, ts, :]), [fbb[par]], [slb[ts]])
                    return
                for ts in range(4):
                    self.tr(PS[6][:, ts * 64:(ts + 1) * 64], impacc[:, ts * 128:(ts + 1) * 128], self.identF[0:64, 0:64], [impb, self.cB], [PB[6]])
                for ts in range(4):
                    i = ts
                    P.op("dve", lambda e, i=i, ts=ts: e.tensor_tensor(impm[i], PS[6][:, ts * 64:(ts + 1) * 64], FBt[par][:, ts, :], ALU.add), [PB[6], fbb[par]], [slb[i]])
                    P.op("dve", lambda e, i=i: e.max(out=m8a[i], in_=impm[i]), [slb[i]], [slb[i]])
                    P.op("dve", lambda e, i=i: e.match_replace(out=work[i], in_to_replace=m8a[i], in_values=impm[i], imm_value=-3.0e9), [slb[i]], [slb[i]])
                    P.op("dve", lambda e, i=i: e.max(out=m8b[i], in_=work[i]), [slb[i]], [slb[i]])
                    P.op("dve", lambda e, i=i: e.tensor_scalar(selt[i], impm[i], m8b[i][:, 7:8], None, ALU.is_ge), [slb[i]], [slb[i]])
                    P.op("dve", lambda e, i=i: e.tensor_scalar(selt[i], selt[i], 1.0, 30000.0, ALU.subtract, ALU.mult), [slb[i]], [slb[i]])
                    P.op("dve", lambda e, i=i, ts=ts: e.tensor_tensor(selbf[i], selt[i], CBt[par][:, ts, :], ALU.add), [slb[i], fbb[par]], [slb[i]])

            tiles = [nt for nt in range(NCT) if nt * 2048 + 31 <= q0 + 511]
            kts_w = list(range(max(0, 4 * tq - 4), 4 * tq + 4))
            pairsA = []
            for g in range(3):
                po = 4 + (st["grp"] % 2); st["grp"] += 1
                for ii, nt in enumerate(tiles):
                    def score(ps, nt=nt, g=g):
                        self.mm(PS[ps][:], kcmpT[:, k, nt * 128:(nt + 1) * 128], qc[par][g][0:64, :], True, True, [cmb, qcb[par][g]], [PB[ps]])

                    def post(ps, Ei, Ebi, nt=nt):
                        self.act(Ei, PS[ps][:], AF.Exp, [PB[ps]], [Ebi], scale=0.125)
                        base = q0 - 16 * nt * 128 - 31
                        P.op("pool", lambda e: e.affine_select(Ei, Ei, [[1, 512]], ALU.is_ge, 0.0, base=base, channel_multiplier=-16), [Ebi], [Ebi])

                    def pv(Ei, Ebi, nt=nt, ii=ii, po=po):
                        self.mm(PS[po][:], vcA[:, k, nt, :], Ei, ii == 0, ii == len(tiles) - 1, [cmb, Ebi], [PB[po]])
                        self.mm(PS[7][0:64, :], Ov[:, nt, :], Ei, ii == 0, ii == len(tiles) - 1, [kb, Ebi], [PB[7]])
                    d = {"score": score, "post": post, "pv": pv}
                    if ii == len(tiles) - 1:
                        if g < 2:
                            d["after"] = (lambda g=g, po=po: finish(k, g, tq, po, 0, True, False, par, with_imp=True))
                        else:
                            d["after"] = (lambda g=g, po=po: (finish(k, g, tq, po, 0, True, False, par, with_imp=True), selection_part1()))
                    pairsA.append(d)
            for g in range(3):
                po = 4 + (st["grp"] % 2); st["grp"] += 1
                for ii, kt in enumerate(kts_w):
                    def score(ps, kt=kt, g=g):
                        self.mm(PS[ps][:], kwT[:, kt * 128:(kt + 1) * 128], qc[par][g][0:64, :], True, True, [kvb, qcb[par][g]], [PB[ps]])

                    def post(ps, Ei, Ebi, kt=kt):
                        self.act(Ei, PS[ps][:], AF.Exp, [PB[ps]], [Ebi], scale=0.125)
                        if kt >= 4 * tq:
                            base = q0 - kt * 128
                            P.op("pool", lambda e: e.affine_select(Ei, Ei, [[1, 512]], ALU.is_ge, 0.0, base=base, channel_multiplier=-1), [Ebi], [Ebi])
                        else:
                            base = 511 - q0 + kt * 128
                            P.op("pool", lambda e: e.affine_select(Ei, Ei, [[-1, 512]], ALU.is_ge, 0.0, base=base, channel_multiplier=1), [Ebi], [Ebi])

                    def pv(Ei, Ebi, kt=kt, ii=ii, po=po):
                        self.mm(PS[po][:], vwA[:, kt, :], Ei, ii == 0, ii == len(kts_w) - 1, [kvb, Ebi], [PB[po]])
                    d = {"score": score, "post": post, "pv": pv}
                    if ii == len(kts_w) - 1:
                        d["after"] = (lambda g=g, po=po: finish(k, g, tq, po, 2, False, False, par))
                    pairsA.append(d)
            pairsA[0]["pre"] = setup
            def selection_part2():
                for ts in range(4):
                    self.tr(PS7b[0:64, ts * 128:(ts + 1) * 128], selbf[ts], self.identB, [slb[ts], self.cB], [PB[7]])
                for g in range(3):
                    P.op("act", lambda e, g=g: e.copy(qc[par][g][64:128, :], PS7b[0:64, 0:512]), [PB[7]], [qsb[par][g]])
            pairsB = []
            nk = 4 * tq + 4
            for g in range(3):
                po = 4 + (st["grp"] % 2); st["grp"] += 1
                for kt in range(nk):
                    def score(ps, kt=kt, g=g):
                        self.mm(PS[ps][:], ksE[:, kt * 128:(kt + 1) * 128], qc[par][g], True, True,
                                [kvb, kb, qcb[par][g], qsb[par][g]], [PB[ps]])

                    def post(ps, Ei, Ebi, kt=kt):
                        self.act(Ei, PS[ps][:], AF.Exp, [PB[ps]], [Ebi], scale=0.125)
                        if kt >= 4 * tq:
                            base = q0 - kt * 128
                            P.op("pool", lambda e: e.affine_select(Ei, Ei, [[1, 512]], ALU.is_ge, 0.0, base=base, channel_multiplier=-1), [Ebi], [Ebi])

                    def pv(Ei, Ebi, kt=kt, po=po):
                        self.mm(PS[po][:], vsA[:, kt, :], Ei, kt == 0, kt == nk - 1, [kvb, Ebi], [PB[po]])
                    d = {"score": score, "post": post, "pv": pv}
                    if kt == nk - 1:
                        d["after"] = (lambda g=g, po=po: finish(k, g, tq, po, 1, False, True, par))
                    pairsB.append(d)
            pairsB[0]["pre"] = selection_part2
            allpairs.extend(pairsA); allpairs.extend(pairsB)
        for tq in range(NQ):
            add_tq(k, tq, allpairs)
        self.run_pipeline(allpairs, new_S, new_E)
    P.barrier()
    self.xattn(l)


Builder.nsa_attention = _nsa_attention
```

```python
import contextlib
import math
import numpy as np
import concourse.bass as bass
import concourse.mybir as mybir
from concourse.bass_utils import run_bass_kernel_spmd

F32 = mybir.dt.float32
BF16 = mybir.dt.bfloat16
I32 = mybir.dt.int32
AF = mybir.ActivationFunctionType
ALU = mybir.AluOpType
AX = mybir.AxisListType

D = 1024
NCORES = 8
N_MEM = 256
ALPHA = (2.0 * 4) ** 0.25
LN_EPS = 1e-5
RMS_EPS = 1e-6
NEG = -30000.0
EPOCH = 16000
ENGS = ("pe", "act", "dve", "pool", "sp")


class Buf:
    __slots__ = ("name", "w", "r", "excl")

    def __init__(self, name="", excl=False):
        self.name = name
        self.w = None
        self.r = {}
        self.excl = excl


class Prog:
    def __init__(self, nc, stack, n_dma_slots=(("sp", 16), ("pool", 16), ("act", 4))):
        self.nc = nc
        self.stack = stack
        self.streams = {e: [] for e in ENGS}
        self.cnt = {e: 0 for e in ENGS}
        self.sems = []
        self.eng_sems = {e: [] for e in ENGS}
        self.waited = {e: {} for e in ENGS}
        self.dma_slots = {}
        for q, n in n_dma_slots:
            self.dma_slots[q] = [[self._new_sem(f"d{q}{i}"), 0] for i in range(n)]
        self.dma_rr = {q: 0 for q, _ in n_dma_slots}
        self.ninstr = 0

    def _new_sem(self, name):
        h = self.stack.enter_context(self.nc.semaphore(name))
        self.sems.append(h)
        return len(self.sems) - 1

    def _eng_sem(self, eng, epoch):
        lst = self.eng_sems[eng]
        while len(lst) <= epoch:
            lst.append(self._new_sem(f"s{eng}{len(lst)}"))
        return lst[epoch]

    def _need(self, eng, deps):
        w = self.waited[eng]
        for si, val in deps.items():
            if w.get(si, 0) >= val:
                continue
            w[si] = val
            h = self.sems[si]
            self.streams[eng].append(lambda e, h=h, val=val: e.wait_ge(h, val))

    def _collect(self, reads, writes, eng=None):
        deps = {}
        own = self.eng_sems.get(eng, ()) if eng else ()
        for b in reads:
            ev = b.w
            if ev is not None and deps.get(ev[0], 0) < ev[1]:
                deps[ev[0]] = ev[1]
            if b.excl:
                for si, v in b.r.items():
                    if si not in own and deps.get(si, 0) < v:
                        deps[si] = v
        for b in writes:
            ev = b.w
            if ev is not None and deps.get(ev[0], 0) < ev[1]:
                deps[ev[0]] = ev[1]
            for si, v in b.r.items():
                if deps.get(si, 0) < v:
                    deps[si] = v
        return deps

    @staticmethod
    def _mark(ev, reads, writes):
        si, v = ev
        for b in writes:
            b.w = ev
            b.r = {}
        for b in reads:
            if b.r.get(si, 0) < v:
                b.r[si] = v

    def op(self, eng, fn, reads=(), writes=()):
        deps = self._collect(reads, writes, eng)
        n = self.cnt[eng]
        epoch, within = divmod(n, EPOCH)
        si = self._eng_sem(eng, epoch)
        if eng == "pe":
            for s in self.eng_sems["pe"]:
                deps.pop(s, None)
        self._need(eng, deps)
        h = self.sems[si]
        self.streams[eng].append(lambda e, fn=fn, h=h: fn(e).then_inc(h, 1))
        self.cnt[eng] = n + 1
        ev = (si, within + 1)
        self._mark(ev, reads, writes)
        self.ninstr += 1
        return ev

    def dma(self, q, out, in_, reads=(), writes=(), **kw):
        deps = self._collect(reads, writes)
        slots = self.dma_slots[q]
        k = self.dma_rr[q]
        self.dma_rr[q] = (k + 1) % len(slots)
        slot = slots[k]
        si = slot[0]
        if slot[1] > 0:
            deps[si] = max(deps.get(si, 0), slot[1])
        self._need(q, deps)
        slot[1] += 16
        val = slot[1]
        h = self.sems[si]
        self.streams[q].append(
            lambda e, out=out, in_=in_, h=h, kw=kw: e.dma_start(out=out, in_=in_, **kw).then_inc(h, 16))
        ev = (si, val)
        self._mark(ev, reads, writes)
        self.ninstr += 1
        return ev

    def barrier(self):
        deps = {}
        for e in ENGS:
            n = self.cnt[e]
            if n == 0:
                continue
            epoch, within = divmod(n - 1, EPOCH)
            deps[self.eng_sems[e][epoch]] = within + 1
            for ep in range(epoch):
                deps[self.eng_sems[e][ep]] = EPOCH
        for q in self.dma_slots:
            for si, v in self.dma_slots[q]:
                if v:
                    deps[si] = v
        for e in ENGS:
            self._need(e, dict(deps))

    def emit(self):
        nc = self.nc
        with nc.Block() as block:
            @block.tensor
            def _(e):
                for f in self.streams["pe"]:
                    f(e)

            @block.scalar
            def _(e):
                for f in self.streams["act"]:
                    f(e)

            @block.vector
            def _(e):
                for f in self.streams["dve"]:
                    f(e)

            @block.gpsimd
            def _(e):
                for f in self.streams["pool"]:
                    f(e)

            @block.sync
            def _(e):
                for f in self.streams["sp"]:
                    f(e)


class Arena:
    def __init__(self, ap, nbf16):
        self.ap = ap
        self.n = nbf16
        self.off = 0

    def reset(self):
        self.off = 0

    def bf(self, parts, *shape):
        self.off = (self.off + 31) // 32 * 32
        n = int(np.prod(shape))
        n2 = (n + 1) // 2 * 2
        assert self.off + n2 <= self.n, ("arena overflow", self.off, n2, self.n)
        v = self.ap[0:parts, self.off:self.off + n]
        self.off += n2
        if len(shape) == 2:
            v = v.rearrange("p (a b) -> p a b", b=shape[1])
        elif len(shape) == 3:
            v = v.rearrange("p (a b c) -> p a b c", b=shape[1], c=shape[2])
        return v

    def f32(self, parts, *shape, dt=F32):
        self.off = (self.off + 31) // 32 * 32
        n = int(np.prod(shape))
        assert self.off + 2 * n <= self.n, ("arena overflow", self.off, 2 * n, self.n)
        v = self.ap[0:parts, self.off:self.off + 2 * n].bitcast(dt)
        self.off += 2 * n
        if len(shape) == 2:
            v = v.rearrange("p (a b) -> p a b", b=shape[1])
        elif len(shape) == 3:
            v = v.rearrange("p (a b c) -> p a b c", b=shape[1], c=shape[2])
        return v


NSA_IN = 2596
MLA_IN = 672
CONV_IN = 1792


class Builder:
    def __init__(self, S, kinds, last_only_out=True):
        self.S = S
        self.kinds = list(kinds)
        self.NT = S // 128
        self.NQ = S // 512
        self.SC = min(2048, S // 2)

    def mm(self, out, lhsT, rhs, start, stop, reads, writes, sgc=False):
        if sgc:
            return self.P.op("pe", lambda e: e.matmul(out, lhsT, rhs, start=start, stop=stop, skip_group_check=True), reads, writes)
        return self.P.op("pe", lambda e: e.matmul(out, lhsT, rhs, start=start, stop=stop), reads, writes)

    def tr(self, out, in_, ident, reads, writes):
        return self.P.op("pe", lambda e: e.transpose(out, in_, ident), reads, writes)

    def act(self, out, in_, func, reads, writes, **kw):
        return self.P.op("act", lambda e: e.activation(out, in_, func, **kw), reads, writes)

    def dbg_dump(self, name, ap, shape, dt, reads):
        import os
        if not os.environ.get("DBG_NSA"):
            return
        self.dbg_names = getattr(self, "dbg_names", [])
        d = self.dram(name, shape, dt, kind="ExternalOutput")
        self.P.dma("sp", d, ap, reads=reads)
        self.dbg_names.append(name)

    def dram(self, name, shape, dt, kind="Internal"):
        return self.nc.dram_tensor(name, list(shape), dt, kind=kind).ap()

    def build(self):
        S = self.S
        nc = self.nc = bass.Bass("TRN2", target_bir_lowering=False)
        I = self.I = {}

        def inp(name, shape, dt=F32):
            I[name] = self.dram(name, shape, dt, kind="ExternalInput")
            return I[name]

        inp("x", [S, D]); inp("mem", [N_MEM, D]); inp("pos", [1, S], I32); inp("pos_end", [1, 256], I32)
        inp("c_ident", [128, 128]); inp("c_rope", [128, 4 * 128 + 4])
        inp("c_ebig", [64, S]); inp("c_ov", [256, 64]); inp("c_fb", [S, 64]); inp("c_sel", [3, 3 * 64])
        for l, kind in enumerate(self.kinds):
            nin = (NSA_IN, MLA_IN, CONV_IN)[kind]
            inp(f"win{l}", [D, nin]); inp(f"wkv{l}", [D, 512]); inp(f"wo{l}", [D, D])
            inp(f"lng{l}", [2, D]); inp(f"lnb{l}", [2, D])
            inp(f"wr{l}", [D, 20]); inp(f"br{l}", [1, 20])
            inp(f"wg{l}", [16, D, 512]); inp(f"wu{l}", [16, D, 512]); inp(f"wd{l}", [16, 512, D])
            if kind == 0:
                inp(f"cpe{l}", [2, 64, 32]); inp(f"cw1{l}", [2, 64, 32, 128]); inp(f"cw2{l}", [2, 128, 64])
            elif kind == 1:
                inp(f"qn{l}", [128, 2]); inp(f"kvn{l}", [128, 1]); inp(f"wuq{l}", [256, 1152]); inp(f"wukv{l}", [128, 1536])
            else:
                inp(f"cbin{l}", [128, 12]); inp(f"cdw{l}", [128, 6 * 31]); inp(f"cdb{l}", [128, 6])
                inp(f"clg{l}", [128, 6]); inp(f"clb{l}", [128, 6])
        self.out = self.dram("out", [S, D], F32, kind="ExternalOutput")
        self.xres = self.dram("xres", [S, D], F32)
        self.pT = self.dram("pT", [2304, S], BF16)
        self.vtok = self.dram("vtok", [S, 768], BF16)
        import os
        self.glT = self.dram("glT", [36, S], F32, kind=("ExternalOutput" if os.environ.get("DBG_GL") else "Internal"))

        with contextlib.ExitStack() as st:
            P = self.P = Prog(nc, st)
            sb = lambda name, shape, dt: st.enter_context(nc.sbuf_tensor(name, shape, dt))
            self.A_t = sb("arenaA", [128, max(8 * S, 186 * 128)], BF16)
            self.B_t = sb("arenaB", [128, 8 * S], BF16)
            ZN = 4608
            self.Z = Arena(sb("arenaZ", [128, ZN], BF16)[:], ZN)
            rem = int(nc.sbuf_bytes_remaining) - 1024
            CN = min(rem // 2, 37000)
            self.C = Arena(sb("arenaC", [128, CN], BF16)[:], CN)
            self.PS = [st.enter_context(nc.psum_tensor(f"ps{i}", [128, 512], F32)) for i in range(8)]
            self.PB = [Buf(f"ps{i}", excl=True) for i in range(8)]
            self.XT = self.A_t[:, 0:8 * S].rearrange("p (k s) -> p k s", s=S)
            self.CT = self.B_t[:].rearrange("p (k s) -> p k s", s=S)
            self.XTb = [Buf(f"xt{q}") for q in range(self.NQ)]
            self.CTb = [[Buf(f"ct{k}_{q}") for q in range(self.NQ)] for k in range(8)]
            self.xres_b = [Buf(f"xr{t}") for t in range(self.NT)]
            self.pT_b = Buf("pT"); self.vtok_b = Buf("vtok"); self.glT_b = Buf("glT")
            self.setup_consts()
            for l, kind in enumerate(self.kinds):
                self.layer(l, kind)
            P.barrier()
            P.emit()
        return nc

    def setup_consts(self):
        P, Z, I = self.P, self.Z, self.I
        self.identF = Z.f32(128, 128); self.identB = Z.bf(128, 128)
        self.onesB = Z.bf(128, 128)
        self.onesF = Z.f32(128, 128)
        self.eps_t = Z.f32(128, 2)
        self.RnB = Z.bf(128, 128); self.RmB = Z.bf(128, 128); self.invF = Z.f32(128, 4)
        self.memT = Z.bf(128, 8, 256)
        self.gates = Z.f32(128, self.NT, 16)
        self.cB = Buf("consts"); self.memT_b = Buf("memT"); self.gates_b = [Buf() for _ in range(self.NT)]
        P.dma("sp", self.identF, I["c_ident"], writes=[self.cB])
        P.dma("pool", self.identB, I["c_ident"], writes=[self.cB])
        P.op("dve", lambda e: e.memset(self.onesB, 1.0), writes=[self.cB])
        P.op("dve", lambda e: e.memset(self.onesF, 1.0), writes=[self.cB])
        P.op("dve", lambda e: e.memset(self.eps_t[:, 0:1], LN_EPS), writes=[self.cB])
        P.op("dve", lambda e: e.memset(self.eps_t[:, 1:2], RMS_EPS), writes=[self.cB])
        P.dma("pool", self.RnB, I["c_rope"][:, 0:128], writes=[self.cB])
        P.dma("pool", self.RmB, I["c_rope"][:, 128:256], writes=[self.cB])
        P.dma("sp", self.invF, I["c_rope"][:, 512:516], writes=[self.cB])
        C = self.C
        C.reset()
        m = C.f32(128, 2, D)
        mb = Buf()
        P.dma("sp", m, I["mem"].rearrange("(t p) d -> p t d", p=128), writes=[mb])
        for t in range(2):
            for half in range(2):
                pi = t * 2 + half
                for k4 in range(4):
                    k = half * 4 + k4
                    self.tr(self.PS[pi][:, k4 * 128:(k4 + 1) * 128], m[:, t, k * 128:(k + 1) * 128], self.identF,
                            [mb, self.cB], [self.PB[pi]])
                src = self.PS[pi][:].rearrange("p (k s) -> p k s", s=128)
                dst = self.memT[:, half * 4:half * 4 + 4, t * 128:(t + 1) * 128]
                P.op("act", lambda e, dst=dst, src=src: e.copy(dst, src), [self.PB[pi]], [self.memT_b])
        P.barrier()

    def layer(self, l, kind):
        first = (l == 0)
        last = (l == len(self.kinds) - 1)
        self.xsrc = self.I["x"] if first else self.xres
        if first:
            self.load_xt_from_x()
        if kind == 2:
            self.mixer_conv(l)
        elif kind == 1:
            self.mixer_mla(l)
        else:
            self.mixer_nsa(l)
        import os
        if os.environ.get("DBG_CT") and l == 0:
            dbg = self.dram("dbg", [128, 8 * self.S], BF16, kind="ExternalOutput")
            self.P.dma("sp", dbg, self.B_t[:, 0:8 * self.S])
            self.P.barrier()
        self.phase_out(l)
        self.phase_moe(l, last)

    def load_xt_from_x(self):
        P, C = self.P, self.C
        C.reset()
        xt = [C.f32(128, D) for _ in range(3)]
        xb = [Buf() for _ in range(3)]
        x = self.I["x"]
        for t in range(self.NT):
            j = t % 3
            P.dma("sp", xt[j], x[t * 128:(t + 1) * 128, :], writes=[xb[j]])
            self.transpose_to_xt(xt[j], xb[j], t, bank0=(t % 2) * 2)
        P.barrier()

    def transpose_to_xt(self, src, srcb, t, bank0, scale=None):
        P = self.P
        q = t // 4
        for half in range(2):
            pi = bank0 + half
            for k4 in range(4):
                k = half * 4 + k4
                self.tr(self.PS[pi][:, k4 * 128:(k4 + 1) * 128], src[:, k * 128:(k + 1) * 128], self.identF,
                        [srcb, self.cB], [self.PB[pi]])
            s_ = self.PS[pi][:].rearrange("p (k s) -> p k s", s=128)
            d_ = self.XT[:, half * 4:half * 4 + 4, t * 128:(t + 1) * 128]
            P.op("act", lambda e, d_=d_, s_=s_: e.copy(d_, s_), [self.PB[pi]], [self.XTb[q]])

    def ln_tile(self, v, vb, gbc, bbc, gbb, scr, j):
        P = self.P
        st_, mv, rs = scr["st"][j], scr["mv"][j], scr["rs"][j][:, 0:1]
        sb_ = scr["b"][j]
        P.op("dve", lambda e: e.bn_stats(st_[:, 0, :], v[:, 0:512]), [vb], [sb_])
        P.op("dve", lambda e: e.bn_stats(st_[:, 1, :], v[:, 512:1024]), [vb], [sb_])
        P.op("dve", lambda e: e.bn_aggr(mv, st_[:].rearrange("p a b -> p (a b)")), [sb_], [sb_])
        P.op("act", lambda e: e.activation(rs, mv[:, 1:2], AF.Sqrt, bias=self.eps_t[:, 0:1], scale=1.0), [sb_, self.cB], [sb_])
        P.op("dve", lambda e: e.reciprocal(rs, rs), [sb_], [sb_])
        P.op("dve", lambda e: e.tensor_scalar(v, v, mv[:, 0:1], rs, ALU.subtract, ALU.mult), [vb, sb_], [vb])
        P.op("dve", lambda e: e.tensor_tensor(v, v, gbc, ALU.mult), [vb, gbb], [vb])
        P.op("dve", lambda e: e.tensor_tensor(v, v, bbc, ALU.add), [vb, gbb], [vb])

    def ln_scratch(self, n=2):
        C = self.C
        return {"st": [C.f32(128, 2, 6) for _ in range(n)], "mv": [C.f32(128, 2) for _ in range(n)],
                "rs": [C.f32(128, 2) for _ in range(n)], "b": [Buf() for _ in range(n)]}

    def phase_out(self, l):
        P, C, I = self.P, self.C, self.I
        C.reset()
        Wo = C.bf(128, 8, D); wob = Buf()
        P.dma("pool", Wo, I[f"wo{l}"].rearrange("(k p) n -> p k n", p=128), writes=[wob])
        gbc = C.f32(128, D); bbc = C.f32(128, D); gbb = Buf()
        P.dma("sp", gbc, I[f"lng{l}"][0:1, :].broadcast_to([128, D]), writes=[gbb])
        P.dma("sp", bbc, I[f"lnb{l}"][0:1, :].broadcast_to([128, D]), writes=[gbb])
        scr = self.ln_scratch()
        NB = 3
        xo = [C.f32(128, D) for _ in range(NB)]
        xb = [Buf() for _ in range(NB)]

        def load(t):
            P.dma("sp", xo[t % NB], self.xsrc[t * 128:(t + 1) * 128, :], reads=[self.xres_b[t]], writes=[xb[t % NB]])
        load(0)
        for t in range(self.NT):
            if t + 1 < self.NT:
                load(t + 1)
            j = t % NB
            q = t // 4
            for half in range(2):
                pi = (t % 2) * 2 + half
                for k in range(8):
                    self.mm(self.PS[pi][:], self.CT[:, k, t * 128:(t + 1) * 128], Wo[:, k, half * 512:(half + 1) * 512],
                            k == 0, k == 7, [self.CTb[k][q], wob], [self.PB[pi]])
                xs = xo[j][:, half * 512:(half + 1) * 512]
                P.op("dve", lambda e, xs=xs, pi=pi: e.scalar_tensor_tensor(xs, xs, ALPHA, self.PS[pi][:], ALU.mult, ALU.add),
                     [xb[j], self.PB[pi]], [xb[j]])
            self.ln_tile(xo[j], xb[j], gbc, bbc, gbb, scr, t % 2)
            P.dma("pool", self.xres[t * 128:(t + 1) * 128, :], xo[j], reads=[xb[j]], writes=[self.xres_b[t]])
            self.transpose_to_xt(xo[j], xb[j], t, bank0=4 + (t % 2) * 2)
        P.barrier()

    def phase_moe(self, l, last):
        P, C, I, S = self.P, self.C, self.I, self.S
        C.reset()
        NT, SC = self.NT, self.SC
        nsc = S // SC
        tps = SC // 128
        cps = SC // 512
        self.router(l)
        C.reset()
        gbc = C.f32(128, D); bbc = C.f32(128, D); gbb = Buf()
        P.dma("sp", gbc, I[f"lng{l}"][1:2, :].broadcast_to([128, D]), writes=[gbb])
        P.dma("sp", bbc, I[f"lnb{l}"][1:2, :].broadcast_to([128, D]), writes=[gbb])
        scr = self.ln_scratch()
        wg = [C.bf(128, 8, 512) for _ in range(2)]; wgb = [Buf() for _ in range(2)]
        wu = [C.bf(128, 8, 512) for _ in range(2)]; wub = [Buf() for _ in range(2)]
        wd = [C.bf(128, 4, D) for _ in range(1)]; wdb = [Buf() for _ in range(1)]
        hd = [C.bf(128, 4, 512) for _ in range(2)]; hdb = [Buf() for _ in range(2)]
        sg = [C.bf(128, 512) for _ in range(2)]; sgb = [Buf() for _ in range(2)]
        xo = [C.f32(128, D) for _ in range(2)]; xb = [Buf() for _ in range(2)]
        facc = self.B_t[:].bitcast(F32).rearrange("p (t d) -> p t d", d=D)
        fab = [Buf() for _ in range(tps)]

        def fa_bufs(tl):
            lo, hi = 2048 * tl, 2048 * tl + 2048
            res = [fab[tl]]
            for k in range(lo // S, (hi - 1) // S + 1):
                s0 = max(lo, k * S) - k * S
                s1 = min(hi, (k + 1) * S) - k * S
                for q in range(s0 // 512, (s1 - 1) // 512 + 1):
                    res.append(self.CTb[k][q])
            return res

        wgv = I[f"wg{l}"]; wuv = I[f"wu{l}"]; wdv = I[f"wd{l}"]

        def load_w(e):
            P.dma("pool", wg[e % 2], wgv[e].rearrange("(k p) n -> p k n", p=128), writes=[wgb[e % 2]])
            P.dma("pool", wu[e % 2], wuv[e].rearrange("(k p) n -> p k n", p=128), writes=[wub[e % 2]])

        def load_wd(e):
            P.dma("pool", wd[0], wdv[e].rearrange("(k p) n -> p k n", p=128), writes=[wdb[0]])

        for sc in range(nsc):
            tok0 = sc * SC
            load_w(0)
            load_wd(0)
            for e in range(16):
                if e + 1 < 16:
                    load_w(e + 1)
                for c in range(cps):
                    q = (tok0 // 512) + c
                    hb = hd[c % 2]
                    for f in range(4):
                        pg, pu = (f % 2) * 2, (f % 2) * 2 + 1
                        for k in range(8):
                            self.mm(self.PS[pg][:], wg[e % 2][:, k, f * 128:(f + 1) * 128], self.XT[:, k, q * 512:(q + 1) * 512],
                                    k == 0, k == 7, [wgb[e % 2], self.XTb[q]], [self.PB[pg]])
                        for k in range(8):
                            self.mm(self.PS[pu][:], wu[e % 2][:, k, f * 128:(f + 1) * 128], self.XT[:, k, q * 512:(q + 1) * 512],
                                    k == 0, k == 7, [wub[e % 2], self.XTb[q]], [self.PB[pu]])
                        s_ = sg[f % 2]
                        self.act(s_, self.PS[pg][:], AF.Silu, [self.PB[pg]], [sgb[f % 2]])
                        P.op("dve", lambda e_, f=f, s_=s_, pu=pu, hb=hb: e_.tensor_tensor(hb[:, f, :], s_, self.PS[pu][:], ALU.mult),
                             [sgb[f % 2], self.PB[pu]], [hdb[c % 2]])
                    for ts in range(4):
                        tl = c * 4 + ts
                        tg = tok0 // 128 + tl
                        for half in range(2):
                            po = 4 + ((ts * 2 + half) % 4)
                            for f in range(4):
                                self.mm(self.PS[po][:], hb[:, f, ts * 128:(ts + 1) * 128], wd[0][:, f, half * 512:(half + 1) * 512],
                                        f == 0, f == 3, [hdb[c % 2], wdb[0]], [self.PB[po]])
                            fa = facc[:, tl, half * 512:(half + 1) * 512]
                            gsc = self.gates[:, tg, e:e + 1]
                            if e == 0:
                                P.op("dve", lambda e_, fa=fa, po=po, gsc=gsc: e_.tensor_scalar(fa, self.PS[po][:], gsc, None, ALU.mult),
                                     [self.PB[po], self.gates_b[tg]], fa_bufs(tl))
                            else:
                                P.op("dve", lambda e_, fa=fa, po=po, gsc=gsc: e_.scalar_tensor_tensor(fa, self.PS[po][:], gsc, fa, ALU.mult, ALU.add),
                                     [self.PB[po], self.gates_b[tg]], fa_bufs(tl))
                if e + 1 < 16:
                    load_wd(e + 1)
            dst = self.out if last else self.xres

            def load(tl):
                tg = tok0 // 128 + tl
                P.dma("sp", xo[tl % 2], self.xres[tg * 128:(tg + 1) * 128, :], reads=[self.xres_b[tg]], writes=[xb[tl % 2]])
            load(0)
            for tl in range(tps):
                if tl + 1 < tps:
                    load(tl + 1)
                tg = tok0 // 128 + tl
                j = tl % 2
                P.op("dve", lambda e_, j=j, tl=tl: e_.scalar_tensor_tensor(xo[j], xo[j], ALPHA, facc[:, tl, :], ALU.mult, ALU.add),
                     [xb[j]] + fa_bufs(tl), [xb[j]])
                self.ln_tile(xo[j], xb[j], gbc, bbc, gbb, scr, j)
                P.dma("pool", dst[tg * 128:(tg + 1) * 128, :], xo[j], reads=[xb[j]], writes=[self.xres_b[tg]])
                if not last:
                    self.transpose_to_xt(xo[j], xb[j], tg, bank0=(tl % 2) * 2)
        P.barrier()

    def router(self, l):
        P, C, I = self.P, self.C, self.I
        NT = self.NT
        C.reset()
        Wr = C.bf(128, 8, 20); wrb = Buf()
        P.dma("pool", Wr, I[f"wr{l}"].rearrange("(k p) n -> p k n", p=128), writes=[wrb])
        brc = C.f32(128, 20)
        P.dma("sp", brc, I[f"br{l}"][0:1, :].broadcast_to([128, 20]), writes=[wrb])
        lg = C.f32(128, NT, 20); rb = Buf()
        for t in range(NT):
            q = t // 4
            pi = t % 4
            lgp = self.PS[pi][:, 0:20]
            for k in range(8):
                self.mm(lgp, self.XT[:, k, t * 128:(t + 1) * 128], Wr[:, k, :], k == 0, k == 7,
                        [self.XTb[q], wrb], [self.PB[pi]])
            P.op("dve", lambda e, t=t, lgp=lgp: e.tensor_tensor(lg[:, t, :], lgp, brc, ALU.add), [self.PB[pi], wrb], [rb])
        lgg = lg[:, :, 0:4]
        le = lg[:, :, 4:20].rearrange("p t (g j) -> p t g j", j=4)
        m = C.f32(128, NT); oh = C.f32(128, NT, 4); eg = C.f32(128, NT, 4); gs = C.f32(128, NT)
        m1 = C.f32(128, NT, 4); is1 = C.f32(128, NT, 4, 4); le2 = C.f32(128, NT, 4, 4); m2 = C.f32(128, NT, 4)
        sel2 = C.f32(128, NT, 4, 4); ee = C.f32(128, NT, 4, 4); ss = C.f32(128, NT, 4); w = C.f32(128, NT, 4)
        bc3 = lambda a: a.unsqueeze(2).broadcast_to([128, NT, 4])
        bc4 = lambda a: a.unsqueeze(3).broadcast_to([128, NT, 4, 4])
        R = [rb]
        dv = lambda fn: P.op("dve", fn, R, R)
        dv(lambda e: e.tensor_reduce(m, lgg, AX.X, ALU.max))
        dv(lambda e: e.tensor_tensor(oh, lgg, bc3(m), ALU.is_equal))
        dv(lambda e: e.tensor_tensor(eg, lgg, bc3(m), ALU.subtract))
        P.op("act", lambda e: e.activation(eg, eg, AF.Exp), R, R)
        dv(lambda e: e.tensor_reduce(gs, eg, AX.X, ALU.add))
        dv(lambda e: e.reciprocal(gs, gs))
        dv(lambda e: e.tensor_reduce(m1, le, AX.X, ALU.max))
        dv(lambda e: e.tensor_tensor(is1, le, bc4(m1), ALU.is_equal))
        dv(lambda e: e.scalar_tensor_tensor(le2, is1, -1.0e9, le, ALU.mult, ALU.add))
        dv(lambda e: e.tensor_reduce(m2, le2, AX.X, ALU.max))
        dv(lambda e: e.tensor_tensor(sel2, le, bc4(m2), ALU.is_ge))
        dv(lambda e: e.tensor_tensor(ee, le, bc4(m1), ALU.subtract))
        P.op("act", lambda e: e.activation(ee, ee, AF.Exp), R, R)
        dv(lambda e: e.tensor_tensor(ee, ee, sel2, ALU.mult))
        dv(lambda e: e.tensor_reduce(ss, ee, AX.X, ALU.add))
        dv(lambda e: e.reciprocal(ss, ss))
        dv(lambda e: e.tensor_tensor(w, ss, oh, ALU.mult))
        dv(lambda e: e.tensor_tensor(w, w, bc3(gs), ALU.mult))
        gv = self.gates[:].rearrange("p t (g j) -> p t g j", j=4)
        P.op("dve", lambda e: e.tensor_tensor(gv, ee, bc4(w), ALU.mult), R, R + self.gates_b)
        P.barrier()

    def run_pipeline(self, pairs, new_S, new_E, depth=3):
        n = len(pairs)
        slots = [None] * n

        def do_score(i):
            if pairs[i].get("pre"):
                pairs[i]["pre"]()
            slots[i] = new_S()
            pairs[i]["score"](slots[i])
        for i in range(min(depth, n)):
            do_score(i)
        for i in range(n):
            Ei, Ebi = new_E()
            pairs[i]["post"](slots[i], Ei, Ebi)
            if i + depth < n:
                do_score(i + depth)
            pairs[i]["pv"](Ei, Ebi)
            if pairs[i].get("after"):
                pairs[i]["after"]()

    def proj_chunk(self, pi, M, Wb, wbuf, col0, tq):
        for k in range(8):
            self.mm(self.PS[pi][0:M, :], Wb[:, k, col0:col0 + M], self.XT[:, k, tq * 512:(tq + 1) * 512],
                    k == 0, k == 7, [wbuf, self.XTb[tq]], [self.PB[pi]])

    def proj_xq(self, win, col0):
        P, C = self.P, self.C
        C.reset()
        stg = [C.bf(128, 512) for _ in range(2)]; stgb = [Buf() for _ in range(2)]
        Wx = C.bf(128, 8, 256); Wxb = Buf()
        P.dma("pool", Wx, win[:, :, col0:col0 + 256], writes=[Wxb])
        ns = 0
        for hh in range(2):
            for tq in range(self.NQ):
                pi = ns % 2
                self.proj_chunk(pi, 128, Wx, Wxb, hh * 128, tq)
                sg_ = stg[ns % 2]; sb_ = stgb[ns % 2]; ns += 1
                P.op("act", lambda e, sg_=sg_, pi=pi: e.copy(sg_, self.PS[pi][:]), [self.PB[pi]], [sb_])
                P.dma("sp", self.pT[2048 + hh * 128:2048 + (hh + 1) * 128, tq * 512:(tq + 1) * 512], sg_, reads=[sb_], writes=[self.pT_b])
        P.barrier()
        C.reset()

    def rope_tables(self, tq, npart, inv, posb_t, posf, kf, ki, Ct, St, tb, pos_src=None, width=512):
        P = self.P
        src = pos_src if pos_src is not None else self.I["pos"][0:1, tq * 512:(tq + 1) * 512]
        P.dma("sp", posb_t, src.broadcast_to([npart, width]), writes=[tb])
        P.op("dve", lambda e: e.tensor_copy(posf, posb_t), [tb], [tb])
        P.op("dve", lambda e: e.tensor_scalar(posf, posf, inv, None, ALU.mult), [tb, self.cB], [tb])
        TWO_PI = 2.0 * math.pi
        C1 = 6.28125
        C2 = TWO_PI - C1
        for out_t, shift in ((St, 0.0), (Ct, math.pi / 2)):
            P.op("dve", lambda e, shift=shift: e.tensor_scalar(kf, posf, shift, 1.0 / TWO_PI, ALU.add, ALU.mult), [tb], [tb])
            P.op("dve", lambda e: e.tensor_copy(ki, kf), [tb], [tb])
            P.op("dve", lambda e: e.tensor_copy(kf, ki), [tb], [tb])
            P.op("dve", lambda e, out_t=out_t, shift=shift: e.tensor_scalar(out_t, posf, shift, None, ALU.add), [tb], [tb])
            P.op("dve", lambda e, out_t=out_t: e.scalar_tensor_tensor(out_t, kf, -C1, out_t, ALU.mult, ALU.add), [tb], [tb])
            P.op("dve", lambda e, out_t=out_t: e.scalar_tensor_tensor(out_t, kf, -C2, out_t, ALU.mult, ALU.add), [tb], [tb])
            P.op("dve", lambda e, out_t=out_t: e.tensor_scalar(kf, out_t, math.pi, -TWO_PI, ALU.is_gt, ALU.mult), [tb], [tb])
            P.op("dve", lambda e, out_t=out_t: e.tensor_tensor(out_t, out_t, kf, ALU.add), [tb], [tb])
            P.op("dve", lambda e, out_t=out_t: e.tensor_scalar(kf, out_t, -math.pi, TWO_PI, ALU.is_lt, ALU.mult), [tb], [tb])
            P.op("dve", lambda e, out_t=out_t: e.tensor_tensor(out_t, out_t, kf, ALU.add), [tb], [tb])
            P.op("dve", lambda e, out_t=out_t: e.tensor_scalar(out_t, out_t, 3.1415925, -3.1415925, ALU.min, ALU.max), [tb], [tb])
            P.op("act", lambda e, out_t=out_t: e.activation(out_t, out_t, AF.Sin), [tb], [tb])

    def xattn(self, l):
        P, C, I, S = self.P, self.C, self.I, self.S
        C.reset()
        Wkv = C.bf(128, 8, 512); wb = Buf()
        P.dma("pool", Wkv, I[f"wkv{l}"].rearrange("(k p) n -> p k n", p=128), writes=[wb])
        mkT = C.bf(64, 4, 256); mkb = Buf()
        mv = C.bf(128, 2, 4, 128); mvb = Buf()
        xq = [C.bf(64, S) for _ in range(2)]; xqb = [Buf() for _ in range(2)]
        E = [C.bf(128, 512) for _ in range(3)]; Eb = [Buf() for _ in range(3)]
        rd = [C.f32(64, 512) for _ in range(2)]; rdb = [Buf() for _ in range(2)]
        P.op("pool", lambda e: e.memset(mv, 1.0), [], [mvb])
        for h in range(4):
            pi = h % 2
            for k in range(8):
                self.mm(self.PS[pi][0:64, 0:256], Wkv[:, k, h * 64:(h + 1) * 64], self.memT[:, k, :], k == 0, k == 7,
                        [wb, self.memT_b], [self.PB[pi]])
            P.op("act", lambda e, h=h, pi=pi: e.copy(mkT[:, h, :], self.PS[pi][0:64, 0:256]), [self.PB[pi]], [mkb])
        for mt in range(2):
            pi = 2 + mt
            for k in range(8):
                self.mm(self.PS[pi][:, 0:256], self.memT[:, k, mt * 128:(mt + 1) * 128], Wkv[:, k, 256:512], k == 0, k == 7,
                        [wb, self.memT_b], [self.PB[pi]])
            src = self.PS[pi][:, 0:256].rearrange("p (h d) -> p h d", d=64)
            P.op("act", lambda e, mt=mt, src=src: e.copy(mv[:, mt, :, 0:64], src), [self.PB[pi]], [mvb])
        ne = 0
        for h in range(4):
            xh = xq[h % 2]; xhb = xqb[h % 2]
            P.dma("sp", xh, self.pT[2048 + 64 * h:2048 + 64 * (h + 1), :], reads=[self.pT_b], writes=[xhb])
            for tq in range(self.NQ):
                po = 4 + (tq % 2)
                for mt in range(2):
                    ps = mt + 2 * (tq % 2)
                    self.mm(self.PS[ps][:], mkT[:, h, mt * 128:(mt + 1) * 128], xh[:, tq * 512:(tq + 1) * 512], True, True,
                            [mkb, xhb], [self.PB[ps]])
                    Ei = E[ne % 3]; Ebi = Eb[ne % 3]; ne += 1
                    self.act(Ei, self.PS[ps][:], AF.Exp, [self.PB[ps]], [Ebi], scale=0.125)
                    self.mm(self.PS[po][:], mv[:, mt, h, :], Ei, mt == 0, mt == 1, [mvb, Ebi], [self.PB[po]])
                r = rd[tq % 2]; rb = rdb[tq % 2]
                self.act(r, self.PS[po][64:128, :], AF.Ln, [self.PB[po]], [rb])
                self.act(r, r, AF.Exp, [rb], [rb], scale=-1.0)
                dst = self.CT[64 * (h % 2):64 * (h % 2) + 64, 6 + h // 2, tq * 512:(tq + 1) * 512]
                P.op("dve", lambda e, r=r, po=po, dst=dst: e.tensor_tensor(dst, self.PS[po][0:64, :], r, ALU.mult),
                     [self.PB[po], rb], [self.CTb[6 + h // 2][tq]])
        P.barrier()

    def mixer_conv(self, l):
        P, C, I, S = self.P, self.C, self.I, self.S
        NQ = self.NQ
        win = I[f"win{l}"].rearrange("(k p) n -> p k n", p=128)
        self.proj_xq(win, 1536)
        UW = 30 + S
        U = C.bf(128, 6, UW); Ub = [Buf() for _ in range(6)]
        cb = C.f32(128, 12); cdw = C.f32(128, 186); cdb = C.f32(128, 6); clg = C.f32(128, 6); clb = C.f32(128, 6)
        pb = Buf()
        for t_, n_ in ((cb, "cbin"), (cdw, "cdw"), (cdb, "cdb"), (clg, "clg"), (clb, "clb")):
            P.dma("sp", t_, I[f"{n_}{l}"], writes=[pb])
        mark = C.off
        Wc = [C.bf(128, 8, 256) for _ in range(2)]; Wcb = [Buf() for _ in range(2)]
        sig = [C.f32(128, 512) for _ in range(2)]; sigb = [Buf() for _ in range(2)]
        for c in range(6):
            P.op("pool", lambda e, c=c: e.memset(U[:, c, 0:30], 0.0), [], [Ub[c]])
        for c in range(6):
            W_ = Wc[c % 2]; Wb_ = Wcb[c % 2]
            P.dma("pool", W_[:, :, 0:128], win[:, :, c * 128:(c + 1) * 128], writes=[Wb_])
            P.dma("pool", W_[:, :, 128:256], win[:, :, 768 + c * 128:768 + (c + 1) * 128], writes=[Wb_])
            for tq in range(NQ):
                p1, p2 = 2 + (tq % 2) * 2, 3 + (tq % 2) * 2
                self.proj_chunk(p1, 128, W_, Wb_, 0, tq)
                self.proj_chunk(p2, 128, W_, Wb_, 128, tq)
                sg_ = sig[tq % 2]; sb_ = sigb[tq % 2]
                self.act(sg_, self.PS[p2][:], AF.Sigmoid, [self.PB[p2], pb], [sb_], bias=cb[:, 6 + c:7 + c], scale=1.0)
                dst = U[:, c, 30 + tq * 512:30 + (tq + 1) * 512]
                P.op("dve", lambda e, dst=dst, p1=p1, c=c, sg_=sg_: e.scalar_tensor_tensor(dst, self.PS[p1][:], cb[:, c:c + 1], sg_, ALU.add, ALU.mult),
                     [self.PB[p1], sb_, pb], [Ub[c]])
        P.barrier()
        C.off = mark
        diag = self.A_t[:, 0:186 * 128].rearrange("p (j m) -> p j m", m=128)
        dgb = Buf()
        for idx in range(186):
            eng = "dve" if idx % 2 == 0 else "pool"
            P.op(eng, lambda e, idx=idx: e.tensor_scalar(diag[:, idx, :], self.identB, cdw[:, idx:idx + 1], None, ALU.mult),
                 [self.cB, pb] , [dgb] + self.XTb)
        ysb = [C.f32(128, 512) for _ in range(2)]; ysbb = [Buf() for _ in range(2)]
        ysq = [C.f32(128, 512) for _ in range(2)]; ysqb = [Buf() for _ in range(2)]
        mean = C.f32(128, 512); rstd = C.f32(128, 512); msq = C.f32(128, 512); stb = Buf()
        tmp = [C.f32(128, 512) for _ in range(2)]; tmpb = [Buf() for _ in range(2)]
        n2 = 0
        for tq in range(NQ):
            for c in range(6):
                for j in range(31):
                    self.mm(self.PS[c][:], diag[:, c * 31 + j, :], U[:, c, tq * 512 + j:tq * 512 + j + 512], j == 0, j == 30,
                            [dgb, Ub[c]], [self.PB[c]])
                y_ = ysb[n2 % 2]; yb_ = ysbb[n2 % 2]; q_ = ysq[n2 % 2]; qb_ = ysqb[n2 % 2]; n2 += 1
                self.act(y_, self.PS[c][:], AF.Identity, [self.PB[c], pb], [yb_], bias=cdb[:, c:c + 1], scale=1.0)
                P.op("pool", lambda e, y_=y_, q_=q_: e.tensor_tensor(q_, y_, y_, ALU.mult), [yb_], [qb_])
                self.mm(self.PS[6][:], self.onesF, y_, c == 0, c == 5, [yb_, self.cB], [self.PB[6]])
                self.mm(self.PS[7][:], self.onesF, q_, c == 0, c == 5, [qb_, self.cB], [self.PB[7]])
            P.op("dve", lambda e: e.tensor_scalar(mean, self.PS[6][:], 1.0 / 768, None, ALU.mult), [self.PB[6]], [stb])
            P.op("dve", lambda e: e.tensor_tensor(msq, mean, mean, ALU.mult), [stb], [stb])
            P.op("dve", lambda e: e.scalar_tensor_tensor(rstd, self.PS[7][:], 1.0 / 768, msq, ALU.mult, ALU.subtract), [self.PB[7], stb], [stb])
            P.op("act", lambda e: e.activation(rstd, rstd, AF.Sqrt, bias=self.eps_t[:, 0:1], scale=1.0), [stb, self.cB], [stb])
            P.op("dve", lambda e: e.reciprocal(rstd, rstd), [stb], [stb])
            for c in range(6):
                t_ = tmp[c % 2]; tb_ = tmpb[c % 2]
                P.op("dve", lambda e, t_=t_, c=c: e.scalar_tensor_tensor(t_, self.PS[c][:], cdb[:, c:c + 1], mean, ALU.add, ALU.subtract),
                     [self.PB[c], stb, pb], [tb_])
                P.op("dve", lambda e, t_=t_: e.tensor_tensor(t_, t_, rstd, ALU.mult), [tb_, stb], [tb_])
                dst = self.CT[:, c, tq * 512:(tq + 1) * 512]
                self.act(dst, t_, AF.Silu, [tb_, pb], [self.CTb[c][tq]], bias=clb[:, c:c + 1], scale=clg[:, c:c + 1])
        P.barrier()
        self.xattn(l)


def _consts(S):
    c = {}
    c["c_ident"] = np.eye(128, dtype=np.float32)
    cr = np.zeros((128, 4 * 128 + 4), np.float32)
    inv_n = (np.float32(500000.0) ** (-np.arange(0, 16, 2, dtype=np.float32) / np.float32(16))).astype(np.float32)
    inv_m = (np.float32(10000.0) ** (-np.arange(0, 32, 2, dtype=np.float32) / np.float32(32))).astype(np.float32)
    for base in (0, 64):
        for i in range(8):
            cr[base + i + 8, base + i] = -1.0
            cr[base + i, base + i + 8] = 1.0
            cr[base + i, 512] = inv_n[i]
            cr[base + i + 8, 512] = inv_n[i]
    for i in range(64, 80):
        cr[i + 16, 128 + i] = -1.0
        cr[i, 128 + i + 16] = 1.0
        cr[i, 513] = inv_m[i - 64]
        cr[i + 16, 513] = inv_m[i - 64]
    c["c_rope"] = cr
    eb = np.zeros((64, S), np.float32)
    for key in range(S):
        eb[(key // 64) % 64, key] = 1.0
    c["c_ebig"] = eb
    ov = np.zeros((256, 64), np.float32)
    ncmp = (S - 32) // 16 + 1
    for n in range(ncmp):
        for j in range(S // 64):
            if 16 * n < 64 * j + 64 and 16 * n + 32 > 64 * j:
                ov[n, j] = 1.0
    c["c_ov"] = ov
    fb = np.zeros((S, 64), np.float32)
    for t in range(S):
        cur = t // 64
        fb[t, cur + 1:] = -1.0e9
        fb[t, cur] = 1.0e9
        if cur >= 1:
            fb[t, cur - 1] = 2.0e9
        fb[t, 0] = 3.0e9
    c["c_fb"] = fb
    sel = np.zeros((3, 3 * 64), np.float32)
    for r in range(3):
        sel[r, r * 64:(r + 1) * 64] = 1.0
    c["c_sel"] = sel
    return c


def layer_inputs(l, kind, j, w):
    f = lambda a: np.ascontiguousarray(a, dtype=np.float32)
    d = {}
    d[f"wkv{l}"] = f(w["mem_w_kv"][l]); d[f"wo{l}"] = f(w["w_out"][l])
    d[f"lng{l}"] = f(w["ln_g"][l]); d[f"lnb{l}"] = f(w["ln_b"][l])
    d[f"wr{l}"] = f(np.concatenate([w["moe_w_grp"][l], w["moe_w_exp"][l]], axis=1))
    d[f"br{l}"] = f(np.concatenate([w["moe_b_grp"][l], w["moe_b_exp"][l]])[None, :])
    d[f"wg{l}"] = f(w["moe_w_gate"][l]); d[f"wu{l}"] = f(w["moe_w_up"][l]); d[f"wd{l}"] = f(w["moe_w_down"][l])
    if kind == 2:
        d[f"win{l}"] = f(w["conv_w_in"][j])
        d[f"cbin{l}"] = f(w["conv_b_in"][j].reshape(12, 128).T)
        d[f"cdw{l}"] = f(w["conv_dw_w"][j].T.reshape(6, 128, 31).transpose(1, 0, 2).reshape(128, 186))
        d[f"cdb{l}"] = f(w["conv_dw_b"][j].reshape(6, 128).T)
        d[f"clg{l}"] = f(w["conv_ln_g"][j].reshape(6, 128).T)
        d[f"clb{l}"] = f(w["conv_ln_b"][j].reshape(6, 128).T)
    elif kind == 1:
        d[f"win{l}"] = f(w["mla_w_in"][j])
        d[f"qn{l}"] = f(w["mla_q_norm"][j].reshape(2, 128).T); d[f"kvn{l}"] = f(w["mla_kv_norm"][j][:, None])
        d[f"wuq{l}"] = f(w["mla_w_uq"][j]); d[f"wukv{l}"] = f(w["mla_w_ukv"][j])
    else:
        d[f"win{l}"] = f(w["nsa_w_in"][j])
        d[f"cpe{l}"] = f(np.transpose(w["nsa_cmp_pe"][j], (0, 2, 1)))
        d[f"cw1{l}"] = f(w["nsa_cmp_w1"][j].reshape(2, 32, 64, 128).transpose(0, 2, 1, 3))
        d[f"cw2{l}"] = f(w["nsa_cmp_w2"][j])
    return d


_NC_CACHE = {}


def run_model(S, kinds, js, x, mem, positions, w, n_cores=NCORES):
    key = (S, tuple(kinds))
    if key not in _NC_CACHE:
        _NC_CACHE[key] = Builder(S, kinds).build()
    nc = _NC_CACHE[key]
    shared = _consts(S)
    for l, (kind, j) in enumerate(zip(kinds, js)):
        shared.update(layer_inputs(l, kind, j, w))
    in_maps = []
    for b in range(n_cores):
        m = dict(shared)
        m["x"] = np.ascontiguousarray(x[b], dtype=np.float32)
        m["mem"] = np.ascontiguousarray(mem[b], dtype=np.float32)
        m["pos"] = np.ascontiguousarray(positions[b][None, :], dtype=np.int32)
        pe = np.asarray(positions[b])[31::16]
        pe = np.concatenate([pe, np.repeat(pe[-1:], 256 - len(pe))])[:256]
        m["pos_end"] = np.ascontiguousarray(pe[None, :], dtype=np.int32)
        in_maps.append(m)
    import os
    if os.environ.get("K_TRACE"):
        res = run_bass_kernel_spmd(nc, in_maps, core_ids=list(range(n_cores)), trace=True)
        print("EXEC_TIME_NS", res.exec_time_ns)
        import pickle
        try:
            it = res.instructions_and_trace
            print("IT type", type(it), (len(it) if hasattr(it, "__len__") else ""))
            pj = res.profile_json
            print("PJ type", type(pj), (list(pj.keys())[:20] if isinstance(pj, dict) else str(pj)[:300]))
            pickle.dump({"it": it, "pj": pj}, open("trace_dump.pkl", "wb"))
        except Exception as ex:
            print("trace dump failed", ex)
    else:
        res = run_bass_kernel_spmd(nc, in_maps, core_ids=list(range(n_cores)))
    import os
    if os.environ.get("DBG_CT"):
        np.save("dbg_ct.npy", np.asarray(res.results[0]["dbg"]).astype(np.float32))
    if os.environ.get("DBG_GL"):
        a = np.asarray(res.results[0]["glT"]); print("glT", a.dtype, a.shape); np.save("d_glT.npy", a)
    if os.environ.get("DBG_NSA"):
        for nme in res.results[0]:
            if nme.startswith("d_"):
                a = np.asarray(res.results[0][nme])
                print(nme, a.dtype, a.shape)
                np.save(nme + ".npy", a.astype(np.float32))
    return np.stack([np.asarray(r["out"]) for r in res.results], axis=0)


def kernel(**inputs):
    x = np.asarray(inputs["x"]); mem = np.asarray(inputs["mem"]); positions = np.asarray(inputs["positions"])
    w = {k: np.asarray(v) for k, v in inputs.items() if k not in ("x", "mem", "positions")}
    kinds = [0, 1, 2, 0]
    js = [0, 0, 0, 1]
    out = run_model(x.shape[1], kinds, js, x, mem, positions, w)
    return out.astype(np.float32)


def _mixer_mla(self, l):
    P, C, I, S = self.P, self.C, self.I, self.S
    NT, NQ = self.NT, self.NQ
    win = I[f"win{l}"].rearrange("(k p) n -> p k n", p=128)
    self.proj_xq(win, 416)
    stg = [C.bf(128, 512) for _ in range(2)]; stgb = [Buf() for _ in range(2)]
    qn = C.f32(128, 2); kvn = C.f32(128, 2); nb = Buf()
    P.dma("sp", qn, I[f"qn{l}"], writes=[nb]); P.dma("sp", kvn[:, 0:1], I[f"kvn{l}"], writes=[nb])
    CQ = C.bf(128, 2, S); CKV = C.bf(128, S); KRr = C.bf(96, S)
    cqb = [Buf() for _ in range(NQ)]; krb = Buf()
    P.op("pool", lambda e: e.memset(KRr[0:64, :], 0.0), [], [krb])
    mark = C.off
    Win = C.bf(128, 8, 416); winb = Buf()
    P.dma("pool", Win, win[:, :, 0:416], writes=[winb])
    tk = [C.f32(128, 416) for _ in range(2)]; tkb = [Buf() for _ in range(2)]
    junk = C.f32(128, 256); ssq = [C.f32(128, 4) for _ in range(2)]
    for t in range(NT):
        pi = t % 2
        j = t % 2
        for k in range(8):
            self.mm(self.PS[pi][:, 0:416], self.XT[:, k, t * 128:(t + 1) * 128], Win[:, k, :], k == 0, k == 7,
                    [winb, self.XTb[t // 4]], [self.PB[pi]])
        sq = ssq[j]
        self.act(junk, self.PS[pi][:, 0:256], AF.Square, [self.PB[pi]], [tkb[j]], accum_out=sq[:, 0:1])
        self.act(junk[:, 0:128], self.PS[pi][:, 256:384], AF.Square, [self.PB[pi]], [tkb[j]], accum_out=sq[:, 1:2])
        self.act(sq[:, 0:1], sq[:, 0:1], AF.Sqrt, [tkb[j], self.cB], [tkb[j]], bias=self.eps_t[:, 1:2], scale=1.0 / 256)
        self.act(sq[:, 1:2], sq[:, 1:2], AF.Sqrt, [tkb[j], self.cB], [tkb[j]], bias=self.eps_t[:, 1:2], scale=1.0 / 128)
        P.op("dve", lambda e, sq=sq: e.reciprocal(sq[:, 0:2], sq[:, 0:2]), [tkb[j]], [tkb[j]])
        P.op("dve", lambda e, j=j, pi=pi, sq=sq: e.tensor_scalar(tk[j][:, 0:256], self.PS[pi][:, 0:256], sq[:, 0:1], None, ALU.mult), [self.PB[pi], tkb[j]], [tkb[j]])
        P.op("dve", lambda e, j=j, pi=pi, sq=sq: e.tensor_scalar(tk[j][:, 256:384], self.PS[pi][:, 256:384], sq[:, 1:2], None, ALU.mult), [self.PB[pi], tkb[j]], [tkb[j]])
        P.op("dve", lambda e, j=j, pi=pi: e.tensor_copy(tk[j][:, 384:416], self.PS[pi][:, 384:416]), [self.PB[pi], tkb[j]], [tkb[j]])
        pt = 2 + (t % 2)
        for bi, (c0, c1) in enumerate(((0, 128), (128, 256), (256, 384), (320, 416))):
            self.tr(self.PS[pt][0:c1 - c0, bi * 128:(bi + 1) * 128], tk[j][:, c0:c1], self.identF, [tkb[j], self.cB], [self.PB[pt]])
        tsl = slice(t * 128, (t + 1) * 128)
        q = t // 4
        self.act(CQ[:, 0, tsl], self.PS[pt][:, 0:128], AF.Copy, [self.PB[pt], nb], [cqb[q]], scale=qn[:, 0:1])
        self.act(CQ[:, 1, tsl], self.PS[pt][:, 128:256], AF.Copy, [self.PB[pt], nb], [cqb[q]], scale=qn[:, 1:2])
        self.act(CKV[:, tsl], self.PS[pt][:, 256:384], AF.Copy, [self.PB[pt], nb], [cqb[q]], scale=kvn[:, 0:1])
        P.op("dve", lambda e, pt=pt, tsl=tsl: e.tensor_copy(KRr[64:96, tsl], self.PS[pt][64:96, 384:512]), [self.PB[pt]], [cqb[q], krb])
    P.barrier()
    import os
    STOP = int(os.environ.get("MLA_STOP", "9"))
    if STOP == 1:
        self.xattn(l); return
    C.off = mark
    Wuq = C.bf(128, 2, 1152); Wukv = C.bf(128, 1536); ub = Buf()
    P.dma("pool", Wuq, I[f"wuq{l}"].rearrange("(k p) n -> p k n", p=128), writes=[ub])
    P.dma("pool", Wukv, I[f"wukv{l}"], writes=[ub])
    posb_t = C.f32(96, 512, dt=I32); posf = C.f32(96, 512); kf = C.f32(96, 512); ki = posb_t
    Ct = C.f32(96, 512); St = C.f32(96, 512); tb = Buf()
    qraw = [C.bf(96, 512) for _ in range(2)]; qrb = [Buf() for _ in range(2)]
    t1 = [C.f32(96, 512) for _ in range(2)]; t2 = [C.f32(96, 512) for _ in range(2)]; t12b = [Buf() for _ in range(2)]
    qo = [C.bf(96, 512) for _ in range(2)]; qob = [Buf() for _ in range(2)]
    vst = [C.bf(128, 768) for _ in range(2)]; vstb = [Buf() for _ in range(2)]
    Rm = self.RmB[0:96, 0:96]
    n = 0
    for tq in range(NQ):
        csl = slice(tq * 512, (tq + 1) * 512)
        SK = os.environ.get("MLA_SKIP", "")
        if "r" not in SK:
            self.rope_tables(tq, 96, self.invF[0:96, 1:2], posb_t, posf, kf, ki, Ct, St, tb)
        j = n % 2; n += 1
        if "k" not in SK:
            self.mm(self.PS[0][0:96, :], Rm, KRr[:, csl], True, True, [self.cB, cqb[tq], krb], [self.PB[0]])
            P.op("dve", lambda e, j=j, csl=csl: e.tensor_tensor(t1[j], KRr[:, csl], Ct, ALU.mult), [cqb[tq], krb, tb], [t12b[j]])
            P.op("dve", lambda e, j=j: e.tensor_tensor(t2[j], self.PS[0][0:96, :], St, ALU.mult), [self.PB[0], tb], [t12b[j]])
            P.op("pool", lambda e, j=j: e.tensor_tensor(qo[j], t1[j], t2[j], ALU.add), [t12b[j]], [qob[j]])
            P.dma("sp", self.pT[1920:1952, csl], qo[j][64:96, :], reads=[qob[j]], writes=[self.pT_b])
        for h in range(int(os.environ.get("MLA_NH", "12")) if "q" not in SK else 0):
            pq, pr = 1 + (h % 2) * 2, 2 + (h % 2) * 2
            for rc in range(2):
                self.mm(self.PS[pq][0:96, :], Wuq[:, rc, h * 96:(h + 1) * 96], CQ[:, rc, csl], rc == 0, rc == 1, [ub, cqb[tq]], [self.PB[pq]])
            j = n % 2; n += 1
            if "a" in SK:
                continue
            self.act(qraw[j], self.PS[pq][0:96, :], AF.Copy, [self.PB[pq]], [qrb[j]])
            if "b" in SK:
                continue
            self.mm(self.PS[pr][0:96, :], Rm, qraw[j], True, True, [self.cB, qrb[j]], [self.PB[pr]])
            if "c" in SK:
                continue
            VAR = os.environ.get("MLA_VAR", "0")
            if VAR == "0":
                P.op("dve", lambda e, j=j, pq=pq: e.tensor_tensor(t1[j], self.PS[pq][0:96, :], Ct, ALU.mult), [self.PB[pq], tb], [t12b[j]])
            elif VAR == "1":
                P.op("dve", lambda e, j=j, pq=pq: e.tensor_tensor(t1[j], self.PS[pq][0:96, :], St, ALU.mult), [self.PB[pq], tb], [t12b[j]])
            elif VAR == "2":
                P.op("dve", lambda e, j=j, pq=pq: e.tensor_tensor(t1[j], self.PS[pq][0:96, :], Ct, ALU.mult), [self.PB[pq], tb, qrb[j]], [t12b[j]])
            elif VAR == "3":
                P.op("dve", lambda e, j=j, pq=pq: e.tensor_tensor(t1[j], qraw[j], Ct, ALU.mult), [qrb[j], tb], [t12b[j]])
            if "e" in SK:
                continue
            P.op("dve", lambda e, j=j, pr=pr: e.tensor_tensor(t2[j], self.PS[pr][0:96, :], St, ALU.mult), [self.PB[pr], tb], [t12b[j]])
            if "f" in SK:
                continue
            P.op("dve" if "p" in SK else "pool", lambda e, j=j: e.tensor_tensor(qo[j], t1[j], t2[j], ALU.add), [t12b[j]], [qob[j]])
            if "d" not in SK:
                P.dma("sp", self.pT[h * 96:(h + 1) * 96, csl], qo[j], reads=[qob[j]], writes=[self.pT_b])
            if "n" in SK:
                continue
            pk = 5 + (h % 2)
            self.mm(self.PS[pk][0:64, :], Wukv[:, h * 128:h * 128 + 64], CKV[:, csl], True, True, [ub, cqb[tq]], [self.PB[pk]])
            sj = stg[h % 2]; sjb = stgb[h % 2]
            self.act(sj[0:64, :], self.PS[pk][0:64, :], AF.Copy, [self.PB[pk]], [sjb])
            P.dma("sp", self.pT[1152 + h * 64:1152 + (h + 1) * 64, csl], sj[0:64, :], reads=[sjb], writes=[self.pT_b])
        Wv = Wukv[:].rearrange("p (h c) -> p h c", c=128)
        for ts in range(4 if "v" not in SK else 0):
            t = tq * 4 + ts
            j = t % 2
            for hv in range(2):
                pv = 6 + hv
                self.mm(self.PS[pv][:, 0:384], CKV[:, t * 128:(t + 1) * 128], Wv[:, hv * 6:(hv + 1) * 6, 64:128], True, True, [ub, cqb[tq]], [self.PB[pv]])
                P.op("dve", lambda e, j=j, hv=hv, pv=pv: e.tensor_copy(vst[j][:, hv * 384:(hv + 1) * 384], self.PS[pv][:, 0:384]), [self.PB[pv]], [vstb[j]])
            P.dma("sp", self.vtok[t * 128:(t + 1) * 128, :], vst[j], reads=[vstb[j]], writes=[self.vtok_b])
    P.barrier()
    if STOP == 2:
        self.xattn(l); return
    C.reset()
    kT = [C.bf(96, S) for _ in range(2)]; qT = [C.bf(96, S) for _ in range(2)]
    vA = [C.bf(128, NT, 128) for _ in range(2)]
    hb = [Buf() for _ in range(2)]
    E = [C.bf(128, 512) for _ in range(6)]; Eb = [Buf() for _ in range(6)]
    rd = [C.f32(64, 512) for _ in range(2)]; rdb = [Buf() for _ in range(2)]
    for j in range(2):
        P.op("pool", lambda e, j=j: e.memset(vA[j][:, :, 64:128], 1.0), [], [hb[j]])
    scale = 1.0 / math.sqrt(96.0)
    ne = 0
    vt = self.vtok.rearrange("(t p) c -> p t c", p=128)

    def load_head(h):
        j = h % 2
        P.dma("sp", qT[j], self.pT[h * 96:(h + 1) * 96, :], reads=[self.pT_b], writes=[hb[j]])
        P.dma("sp", kT[j][0:64, :], self.pT[1152 + h * 64:1152 + (h + 1) * 64, :], reads=[self.pT_b], writes=[hb[j]])
        P.dma("sp", kT[j][64:96, :], self.pT[1920:1952, :], reads=[self.pT_b], writes=[hb[j]])
        with self.nc.allow_non_contiguous_dma(reason="per-head V gather (128B runs)"):
            for t0 in range(0, NT, 4):
                P.dma("sp", vA[j][:, t0:t0 + 4, 0:64], vt[:, t0:t0 + 4, h * 64:(h + 1) * 64], reads=[self.vtok_b], writes=[hb[j]])
    st_ = {"ne": 0, "ns": 0}

    def new_E():
        i = st_["ne"] % 6; st_["ne"] += 1
        return E[i], Eb[i]

    def new_S():
        i = st_["ns"] % 4; st_["ns"] += 1
        return i
    load_head(0)
    for h in range(12):
        if h + 1 < 12:
            load_head(h + 1)
        j = h % 2
        pairs = []
        for tq in range(NQ):
            po = 4 + (tq % 2)
            nk = 4 * tq + 4
            for kt in range(nk):
                c0 = max(0, 128 * (kt - 4 * tq))

                def score(ps, kt=kt, tq=tq, j=j, c0=c0):
                    self.mm(self.PS[ps][:, c0:512], kT[j][:, kt * 128:(kt + 1) * 128], qT[j][:, tq * 512 + c0:(tq + 1) * 512], True, True, [hb[j]], [self.PB[ps]])

                def post(ps, Ei, Ebi, kt=kt, tq=tq, c0=c0):
                    self.act(Ei[:, c0:512], self.PS[ps][:, c0:512], AF.Exp, [self.PB[ps]], [Ebi], scale=scale)
                    if kt >= 4 * tq:
                        base = tq * 512 + c0 - kt * 128
                        P.op("pool", lambda e: e.affine_select(Ei[:, c0:512], Ei[:, c0:512], [[1, 512 - c0]], ALU.is_ge, 0.0, base=base, channel_multiplier=-1), [Ebi], [Ebi])

                def pv(Ei, Ebi, kt=kt, nk=nk, po=po, j=j, c0=c0):
                    self.mm(self.PS[po][:, c0:512], vA[j][:, kt, :], Ei[:, c0:512], kt == 0, kt == nk - 1, [hb[j], Ebi], [self.PB[po]], sgc=True)
                d = {"score": score, "post": post, "pv": pv}
                if kt == nk - 1:
                    def after(tq=tq, po=po, h=h):
                        r = rd[tq % 2]; rb = rdb[tq % 2]
                        self.act(r, self.PS[po][64:128, :], AF.Ln, [self.PB[po]], [rb])
                        self.act(r, r, AF.Exp, [rb], [rb], scale=-1.0)
                        dst = self.CT[64 * (h % 2):64 * (h % 2) + 64, h // 2, tq * 512:(tq + 1) * 512]
                        P.op("dve", lambda e: e.tensor_tensor(dst, self.PS[po][0:64, :], r, ALU.mult),
                             [self.PB[po], rb], [self.CTb[h // 2][tq]])
                    d["after"] = after
                pairs.append(d)
        self.run_pipeline(pairs, new_S, new_E)
    P.barrier()
    self.xattn(l)


Builder.mixer_mla = _mixer_mla


def _mixer_nsa(self, l):
    P, C, I, S = self.P, self.C, self.I, self.S
    NT, NQ = self.NT, self.NQ
    NCMP = (S - 32) // 16 + 1
    NCT = (NCMP + 127) // 128
    NCP = NCT * 128
    win = I[f"win{l}"].rearrange("(k p) n -> p k n", p=128)
    self.proj_xq(win, 2340)
    stg = [C.bf(128, 512) for _ in range(2)]; stgb = [Buf() for _ in range(2)]
    Wq = C.bf(128, 8, 768); Wk = C.bf(128, 8, 1024); Wg = C.bf(128, 8, 36); Wv = C.bf(128, 8, 512); wb = Buf()
    P.dma("pool", Wq, win[:, :, 0:768], writes=[wb])
    P.dma("pool", Wk[:, :, 0:256], win[:, :, 1280:1536], writes=[wb])
    P.dma("pool", Wk[:, :, 256:512], win[:, :, 1792:2048], writes=[wb])
    P.dma("pool", Wk[:, :, 512:1024], win[:, :, 768:1280], writes=[wb])
    P.dma("pool", Wg, win[:, :, 2304:2340], writes=[wb])
    P.dma("pool", Wv[:, :, 0:256], win[:, :, 1536:1792], writes=[wb])
    P.dma("pool", Wv[:, :, 256:512], win[:, :, 2048:2304], writes=[wb])
    posb_t = C.f32(128, 512, dt=I32); posf = C.f32(128, 512); kf = C.f32(128, 512); ki = posb_t
    Ct = C.f32(128, 512); St = C.f32(128, 512); tb = Buf()
    qraw = [C.bf(128, 512) for _ in range(2)]; qrb = [Buf() for _ in range(2)]
    t1 = [C.f32(128, 512) for _ in range(2)]; t2 = [C.f32(128, 512) for _ in range(2)]; t12b = [Buf() for _ in range(2)]
    qo = [C.bf(128, 512) for _ in range(2)]; qob = [Buf() for _ in range(2)]
    vst = [C.bf(128, 512) for _ in range(2)]; vstb = [Buf() for _ in range(2)]
    gst = [C.f32(36, 512) for _ in range(2)]; gstb = [Buf() for _ in range(2)]
    n = 0
    for tq in range(NQ):
        csl = slice(tq * 512, (tq + 1) * 512)
        self.rope_tables(tq, 128, self.invF[:, 0:1], posb_t, posf, kf, ki, Ct, St, tb)
        for ci in range(10):
            Wsrc, col0 = (Wq, ci * 128) if ci < 6 else (Wk, (ci - 6) * 128)
            row0 = ci * 128
            pq, pr = 0 + (ci % 2) * 2, 1 + (ci % 2) * 2
            self.proj_chunk(pq, 128, Wsrc, wb, col0, tq)
            j = n % 2; n += 1
            self.act(qraw[j], self.PS[pq][:], AF.Copy, [self.PB[pq]], [qrb[j]])
            self.mm(self.PS[pr][:], self.RnB, qraw[j], True, True, [self.cB, qrb[j]], [self.PB[pr]])
            P.op("dve", lambda e, j=j, pq=pq: e.tensor_tensor(t1[j], self.PS[pq][:], Ct, ALU.mult), [self.PB[pq], tb], [t12b[j]])
            P.op("dve", lambda e, j=j, pr=pr: e.tensor_tensor(t2[j], self.PS[pr][:], St, ALU.mult), [self.PB[pr], tb], [t12b[j]])
            P.op("pool", lambda e, j=j: e.tensor_tensor(qo[j], t1[j], t2[j], ALU.add), [t12b[j]], [qob[j]])
            P.dma("sp", self.pT[row0:row0 + 128, csl], qo[j], reads=[qob[j]], writes=[self.pT_b])
        for ci in range(4):
            pi = 4 + (ci % 2)
            self.proj_chunk(pi, 128, Wk, wb, 512 + ci * 128, tq)
            sj = stg[ci % 2]; sjb = stgb[ci % 2]
            self.act(sj, self.PS[pi][:], AF.Copy, [self.PB[pi]], [sjb])
            P.dma("sp", self.pT[1280 + ci * 128:1280 + (ci + 1) * 128, csl], sj, reads=[sjb], writes=[self.pT_b])
        self.proj_chunk(6, 36, Wg, wb, 0, tq)
        gj = gst[tq % 2]; gjb = gstb[tq % 2]
        self.act(gj, self.PS[6][0:36, :], AF.Copy, [self.PB[6]], [gjb])
        P.dma("sp", self.glT[:, csl], gj, reads=[gjb], writes=[self.glT_b])
        for ts in range(4):
            t = tq * 4 + ts
            j = t % 2
            pv = 5 if ts % 2 == 0 else 7
            for k in range(8):
                self.mm(self.PS[pv][:], self.XT[:, k, t * 128:(t + 1) * 128], Wv[:, k, :], k == 0, k == 7,
                        [wb, self.XTb[tq]], [self.PB[pv]])
            P.op("dve", lambda e, j=j, pv=pv: e.tensor_copy(vst[j], self.PS[pv][:]), [self.PB[pv]], [vstb[j]])
            P.dma("sp", self.vtok[t * 128:(t + 1) * 128, 0:512], vst[j], reads=[vstb[j]], writes=[self.vtok_b])
    P.barrier()
    C.reset()
    kcmpT = C.bf(64, 4, NCP); vcA = C.bf(128, 4, NCT, 128); cmb = Buf()
    Ebig = self.A_t[64:128, 0:S]
    Ov = C.bf(128, NCT, 64); Sel = C.bf(3, 3, 64); kb = Buf()
    P.dma("pool", Ebig, I["c_ebig"], writes=[kb])
    P.dma("pool", Ov, I["c_ov"][0:NCP, :].rearrange("(t p) j -> p t j", p=128), writes=[kb])
    P.dma("pool", Sel, I["c_sel"].rearrange("r (a m) -> r a m", m=64), writes=[kb])
    P.op("pool", lambda e: e.memset(vcA[:, :, :, 0:64], 0.0), [], [cmb])
    P.op("pool", lambda e: e.memset(vcA[:, :, :, 64:128], 1.0), [], [cmb])
    P.op("pool", lambda e: e.memset(kcmpT, 0.0), [], [cmb])
    mark = C.off
    cw1 = C.bf(64, 2, 32, 128); cpe = C.bf(64, 2, 32); cw2 = C.bf(128, 2, 64); cwb = Buf()
    P.dma("pool", cw1, I[f"cw1{l}"].rearrange("a d l f -> d a l f"), writes=[cwb])
    P.dma("pool", cpe, I[f"cpe{l}"].rearrange("a d l -> d a l"), writes=[cwb])
    P.dma("pool", cw2, I[f"cw2{l}"].rearrange("a f d -> f a d"), writes=[cwb])
    kcT = [C.bf(64, S) for _ in range(2)]; kcb = [Buf() for _ in range(2)]
    hbias = C.f32(128, 2); hbb = Buf()
    hT = [C.bf(128, NCP) for _ in range(2)]; hTb = [Buf() for _ in range(2)]
    posb2 = C.f32(64, NCP, dt=I32); posf2 = C.f32(64, NCP); kf2 = C.f32(64, NCP); ki2 = posb2
    Ct2 = C.f32(64, NCP); St2 = C.f32(64, NCP); tb2 = Buf()
    kraw = C.bf(64, NCP); krb_ = Buf(); u1 = C.f32(64, NCP); u2 = C.f32(64, NCP)
    self.rope_tables(0, 64, self.invF[0:64, 0:1], posb2, posf2, kf2, ki2, Ct2, St2, tb2,
                     pos_src=I["pos_end"][0:1, 0:NCP], width=NCP)
    for a in range(2):
        for ll in range(32):
            self.mm(self.PS[6][:, a:a + 1], cw1[:, a, ll, :], cpe[:, a, ll:ll + 1], ll == 0, ll == 31, [cwb], [self.PB[6]])
    P.op("dve", lambda e: e.tensor_copy(hbias, self.PS[6][:, 0:2]), [self.PB[6]], [hbb])
    nn = 0
    for a in range(2):
        for k in range(4):
            jj = nn % 2; nn += 1
            if a == 0:
                P.dma("sp", kcT[jj], self.pT[1280 + 64 * k:1280 + 64 * (k + 1), :], reads=[self.pT_b], writes=[kcb[jj]])
            else:
                P.dma("sp", kcT[jj], self.pT[1536 + 64 * k:1536 + 64 * (k + 1), :], reads=[self.pT_b], writes=[kcb[jj]])
            ph = jj
            src = kcT[jj]
            for ll in range(32):
                rhs = src[:, ll:ll + 16 * (NCMP - 1) + 1:16]
                self.mm(self.PS[ph][:, 0:NCMP], cw1[:, a, ll, :], rhs, ll == 0, ll == 31, [cwb, kcb[jj]], [self.PB[ph]])
            h_ = hT[jj]; hb_ = hTb[jj]
            if NCMP < NCP:
                P.op("pool", lambda e, h_=h_: e.memset(h_[:, NCMP:NCP], 0.0), [], [hb_])
            self.act(h_[:, 0:NCMP], self.PS[ph][:, 0:NCMP], AF.Silu, [self.PB[ph], hbb], [hb_], bias=hbias[:, a:a + 1], scale=1.0)
            if a == 0:
                self.mm(self.PS[2][0:64, 0:NCP], cw2[:, 0, :], h_, True, True, [cwb, hb_], [self.PB[2]])
                self.act(kraw, self.PS[2][0:64, 0:NCP], AF.Copy, [self.PB[2]], [krb_])
                self.mm(self.PS[3][0:64, 0:NCP], self.RnB[0:64, 0:64], kraw, True, True, [self.cB, krb_], [self.PB[3]])
                P.op("dve", lambda e: e.tensor_tensor(u1, self.PS[2][0:64, 0:NCP], Ct2, ALU.mult), [self.PB[2], tb2], [krb_])
                P.op("dve", lambda e: e.tensor_tensor(u2, self.PS[3][0:64, 0:NCP], St2, ALU.mult), [self.PB[3], tb2], [krb_])
                P.op("dve", lambda e, k=k: e.tensor_tensor(kcmpT[:, k, 0:NCMP], u1[:, 0:NCMP], u2[:, 0:NCMP], ALU.add), [krb_], [cmb])
            else:
                for nt in range(NCT):
                    m_ = min(128, NCMP - nt * 128)
                    self.mm(self.PS[4 + nt][0:m_, 0:64], h_[:, nt * 128:nt * 128 + m_], cw2[:, 1, :], True, True, [cwb, hb_], [self.PB[4 + nt]])
                    P.op("dve", lambda e, k=k, nt=nt, m_=m_: e.tensor_copy(vcA[0:m_, k, nt, 0:64], self.PS[4 + nt][0:m_, 0:64]), [self.PB[4 + nt]], [cmb])
    P.barrier()
    self.nsa_attention(l, kcmpT, vcA, cmb, Ebig, Ov, Sel, kb, mark, NCMP, NCT)


Builder.mixer_nsa = _mixer_nsa


def _nsa_attention(self, l, kcmpT, vcA, cmb, Ebig, Ov, Sel, kb, mark, NCMP, NCT):
    P, C, I, S = self.P, self.C, self.I, self.S
    NT, NQ = self.NT, self.NQ
    C.off = mark
    A = self.A_t
    ksT = A[0:64, 0:S]; kwT = A[0:64, S:2 * S]
    ksE = A[0:128, 0:S]
    vsA = A[:, 2 * S:3 * S].rearrange("p (t c) -> p t c", c=128)
    vwA = A[:, 3 * S:4 * S].rearrange("p (t c) -> p t c", c=128)
    kvb = Buf()
    P.op("pool", lambda e: e.memset(vsA[:, :, 64:128], 1.0), [], [kvb])
    P.op("pool", lambda e: e.memset(vwA[:, :, 64:128], 1.0), [], [kvb])
    qc = [[C.bf(128, 512) for _ in range(3)] for _ in range(2)]; qcb = [[Buf() for _ in range(3)] for _ in range(2)]
    qsb = [[Buf() for _ in range(3)] for _ in range(2)]
    Gl = [C.f32(3, 512) for _ in range(3)]; glf = [Buf() for _ in range(3)]
    Glh = [[C.bf(3, 512) for _ in range(3)] for _ in range(2)]
    Gll = [[C.bf(3, 512) for _ in range(3)] for _ in range(2)]
    Gtmp = C.f32(3, 512); gtb = Buf()
    glb = [[Buf() for _ in range(3)] for _ in range(2)]
    FBt = [C.f32(128, 4, 64) for _ in range(2)]; CBt = [C.f32(128, 4, 64) for _ in range(2)]; fbb = [Buf() for _ in range(2)]
    E = [C.bf(128, 512) for _ in range(6)]; Eb = [Buf() for _ in range(6)]
    rd = [C.f32(64, 512) for _ in range(3)]; rdb = [Buf() for _ in range(3)]
    gs = [C.f32(64, 512) for _ in range(3)]; gsb = [Buf() for _ in range(3)]
    acc = [C.f32(64, 512) for _ in range(3)]; accb = [Buf() for _ in range(3)]
    impacc = C.f32(64, 512); impb = Buf()
    selbT = C.bf(64, 512); selTb = Buf()
    impm = [C.f32(128, 64) for _ in range(4)]; work = [C.f32(128, 64) for _ in range(4)]
    m8a = [C.f32(128, 8) for _ in range(4)]; m8b = [C.f32(128, 8) for _ in range(4)]
    selt = [C.f32(128, 64) for _ in range(4)]; selbf = [C.bf(128, 64) for _ in range(4)]; slb = [Buf() for _ in range(4)]
    PS, PB = self.PS, self.PB
    self.dbg_off = {"selbT": selbT.offset, "qc00": qc[0][0].offset, "impacc": impacc.offset, "selbf0": selbf[0].offset,
                    "CBt0": CBt[0].offset, "Glh00": Glh[0][0].offset, "E0": E[0].offset}
    PS7b = PS[7][:].bitcast(BF16)
    vt = self.vtok.rearrange("(t p) c -> p t c", p=128)
    fbv = I["c_fb"].rearrange("(q ts p) j -> q p ts j", p=128, ts=4)
    st = {"ne": 0, "nr": 0, "ns": 0, "grp": 0}

    def new_E():
        i = st["ne"] % 6; st["ne"] += 1
        return E[i], Eb[i]

    def new_S():
        i = st["ns"] % 4; st["ns"] += 1
        return i

    def finish(k, g, tq, po, b, first, last, par, with_imp=False):
        h = 3 * k + g
        i = st["nr"] % 3; st["nr"] += 1
        r_, rb_ = rd[i], rdb[i]
        g_, gb_ = gs[i], gsb[i]
        P.op("dve", lambda e: e.tensor_scalar(r_, PS[po][64:128, :], 1.0e-30, None, ALU.max), [PB[po]], [rb_])
        if with_imp:
            self.act(g_, r_, AF.Ln, [rb_], [gb_])
            self.act(g_, g_, AF.Exp, [gb_], [gb_], scale=-1.0)
            if g == 0:
                P.op("dve", lambda e: e.tensor_tensor(impacc, PS[7][0:64, :], g_, ALU.mult), [PB[7], gb_], [impb])
            else:
                P.op("dve", lambda e: e.tensor_tensor(g_, PS[7][0:64, :], g_, ALU.mult), [PB[7], gb_], [gb_])
                P.op("dve", lambda e: e.tensor_tensor(impacc, impacc, g_, ALU.add), [gb_, impb], [impb])
        self.mm(PS[6][0:64, :], Sel[:, b, :], Glh[par][g], True, False, [kb, glb[par][g]], [PB[6]])
        self.mm(PS[6][0:64, :], Sel[:, b, :], Gll[par][g], False, True, [kb, glb[par][g]], [PB[6]])
        self.act(g_, PS[6][0:64, :], AF.Exp, [PB[6]], [gb_], scale=-1.0)
        P.op("dve", lambda e: e.scalar_tensor_tensor(g_, g_, 1.0, r_, ALU.add, ALU.mult), [gb_, rb_], [gb_])
        self.act(g_, g_, AF.Ln, [gb_], [gb_])
        self.act(g_, g_, AF.Exp, [gb_], [gb_], scale=-1.0)
        import os
        SKB = os.environ.get("NSA_SKIPB", "")
        if str(b) in SKB:
            P.op("dve", lambda e: e.memset(g_, 0.0), [], [gb_])
        if first:
            P.op("dve", lambda e: e.tensor_tensor(acc[g], PS[po][0:64, :], g_, ALU.mult), [PB[po], gb_], [accb[g]])
        else:
            P.op("dve", lambda e: e.tensor_tensor(r_, PS[po][0:64, :], g_, ALU.mult), [PB[po], gb_], [rb_])
            if not last:
                P.op("dve", lambda e: e.tensor_tensor(acc[g], acc[g], r_, ALU.add), [rb_, accb[g]], [accb[g]])
            else:
                dst = self.CT[64 * (h % 2):64 * (h % 2) + 64, h // 2, tq * 512:(tq + 1) * 512]
                P.op("dve", lambda e: e.tensor_tensor(dst, acc[g], r_, ALU.add), [rb_, accb[g]], [self.CTb[h // 2][tq]])

    for k in range(4):
        P.dma("sp", ksT, self.pT[768 + 64 * k:768 + 64 * (k + 1), :], reads=[self.pT_b], writes=[kvb])
        P.dma("sp", kwT, self.pT[1024 + 64 * k:1024 + 64 * (k + 1), :], reads=[self.pT_b], writes=[kvb])
        with self.nc.allow_non_contiguous_dma(reason="per-head V gather (128B runs)"):
            for t0 in range(0, NT, 4):
                P.dma("sp", vsA[:, t0:t0 + 4, 0:64], vt[:, t0:t0 + 4, 64 * k:64 * (k + 1)], reads=[self.vtok_b], writes=[kvb])
                P.dma("sp", vwA[:, t0:t0 + 4, 0:64], vt[:, t0:t0 + 4, 256 + 64 * k:256 + 64 * (k + 1)], reads=[self.vtok_b], writes=[kvb])
        allpairs = []

        def add_tq(k, tq, allpairs):
            par = tq % 2
            q0 = tq * 512
            csl = slice(q0, q0 + 512)

            def setup():
              for g in range(3):
                  h = 3 * k + g
                  P.dma("sp", qc[par][g][0:64, :], self.pT[64 * h:64 * (h + 1), csl], reads=[self.pT_b], writes=[qcb[par][g]])
                  P.dma("sp", Gl[g], self.glT[3 * h:3 * h + 3, csl], reads=[self.glT_b], writes=[glf[g]])
                  P.op("act", lambda e, par=par, g=g: e.copy(Glh[par][g], Gl[g]), [glf[g]], [glb[par][g]])
                  P.op("dve", lambda e, par=par, g=g: e.tensor_tensor(Gtmp, Gl[g], Glh[par][g], ALU.subtract), [glf[g], glb[par][g]], [gtb])
                  P.op("dve", lambda e, par=par, g=g: e.tensor_copy(Gll[par][g], Gtmp), [gtb], [glb[par][g]])
              P.dma("sp", FBt[par], fbv[tq], writes=[fbb[par]])
              P.op("dve", lambda e, par=par: e.tensor_scalar(CBt[par], FBt[par], 0.0, 3.0e-5, ALU.min, ALU.mult), [fbb[par]], [fbb[par]])
            trivial = (q0 + 511 < 16 * 64)

            def selection_part1(par=par, trivial=trivial):
                if trivial:
                    for ts in range(4):
                        P.op("dve", lambda e, ts=ts: e.tensor_copy(selbf[ts], CBt[par][:, ts, :]), [fbb[par]], [slb[ts]])
                    return
                for ts in range(4):
                    self.tr(PS[6][:, ts * 64:(ts + 1) * 64], impacc[:, ts * 128:(ts + 1) * 128], self.identF[0:64, 0:64], [impb, self.cB], [PB[6]])
                for ts in range(4):
                    i = ts
                    P.op("dve", lambda e, i=i, ts=ts: e.tensor_tensor(impm[i], PS[6][:, ts * 64:(ts + 1) * 64], FBt[par][:, ts, :], ALU.add), [PB[6], fbb[par]], [slb[i]])
                    P.op("dve", lambda e, i=i: e.max(out=m8a[i], in_=impm[i]), [slb[i]], [slb[i]])
                    P.op("dve", lambda e, i=i: e.match_replace(out=work[i], in_to_replace=m8a[i], in_values=impm[i], imm_value=-3.0e9), [slb[i]], [slb[i]])
                    P.op("dve", lambda e, i=i: e.max(out=m8b[i], in_=work[i]), [slb[i]], [slb[i]])
                    P.op("dve", lambda e, i=i: e.tensor_scalar(selt[i], impm[i], m8b[i][:, 7:8], None, ALU.is_ge), [slb[i]], [slb[i]])
                    P.op("dve", lambda e, i=i: e.tensor_scalar(selt[i], selt[i], 1.0, 30000.0, ALU.subtract, ALU.mult), [slb[i]], [slb[i]])
                    P.op("dve", lambda e, i=i, ts=ts: e.tensor_tensor(selbf[i], selt[i], CBt[par][:, ts, :], ALU.add), [slb[i], fbb[par]], [slb[i]])

            tiles = [nt for nt in range(NCT) if nt * 2048 + 31 <= q0 + 511]
            kts_w = list(range(max(0, 4 * tq - 4), 4 * tq + 4))
            pairsA = []
            for g in range(3):
                po = 4 + (st["grp"] % 2); st["grp"] += 1
                for ii, nt in enumerate(tiles):
                    def score(ps, nt=nt, g=g):
                        self.mm(PS[ps][:], kcmpT[:, k, nt * 128:(nt + 1) * 128], qc[par][g][0:64, :], True, True, [cmb, qcb[par][g]], [PB[ps]])

                    def post(ps, Ei, Ebi, nt=nt):
                        self.act(Ei, PS[ps][:], AF.Exp, [PB[ps]], [Ebi], scale=0.125)
                        base = q0 - 16 * nt * 128 - 31
                        P.op("pool", lambda e: e.affine_select(Ei, Ei, [[1, 512]], ALU.is_ge, 0.0, base=base, channel_multiplier=-16), [Ebi], [Ebi])

                    def pv(Ei, Ebi, nt=nt, ii=ii, po=po):
                        self.mm(PS[po][:], vcA[:, k, nt, :], Ei, ii == 0, ii == len(tiles) - 1, [cmb, Ebi], [PB[po]])
                        self.mm(PS[7][0:64, :], Ov[:, nt, :], Ei, ii == 0, ii == len(tiles) - 1, [kb, Ebi], [PB[7]])
                    d = {"score": score, "post": post, "pv": pv}
                    if ii == len(tiles) - 1:
                        if g < 2:
                            d["after"] = (lambda g=g, po=po: finish(k, g, tq, po, 0, True, False, par, with_imp=True))
                        else:
                            d["after"] = (lambda g=g, po=po: (finish(k, g, tq, po, 0, True, False, par, with_imp=True), selection_part1()))
                    pairsA.append(d)
            for g in range(3):
                po = 4 + (st["grp"] % 2); st["grp"] += 1
                for ii, kt in enumerate(kts_w):
                    r_ = kt - 4 * tq
                    c0, c1 = (0, min(512, 128 * (r_ + 5))) if r_ < 0 else (128 * r_, 512)

                    def score(ps, kt=kt, g=g, c0=c0, c1=c1):
                        self.mm(PS[ps][:, c0:c1], kwT[:, kt * 128:(kt + 1) * 128], qc[par][g][0:64, c0:c1], True, True, [kvb, qcb[par][g]], [PB[ps]])

                    def post(ps, Ei, Ebi, kt=kt, c0=c0, c1=c1):
                        self.act(Ei[:, c0:c1], PS[ps][:, c0:c1], AF.Exp, [PB[ps]], [Ebi], scale=0.125)
                        if kt >= 4 * tq:
                            base = q0 + c0 - kt * 128
                            P.op("pool", lambda e: e.affine_select(Ei[:, c0:c1], Ei[:, c0:c1], [[1, c1 - c0]], ALU.is_ge, 0.0, base=base, channel_multiplier=-1), [Ebi], [Ebi])
                        else:
                            base = 511 - q0 - c0 + kt * 128
                            P.op("pool", lambda e: e.affine_select(Ei[:, c0:c1], Ei[:, c0:c1], [[-1, c1 - c0]], ALU.is_ge, 0.0, base=base, channel_multiplier=1), [Ebi], [Ebi])

                    def pv(Ei, Ebi, kt=kt, ii=ii, po=po, c0=c0, c1=c1):
                        self.mm(PS[po][:, c0:c1], vwA[:, kt, :], Ei[:, c0:c1], ii == 0, ii == len(kts_w) - 1, [kvb, Ebi], [PB[po]], sgc=True)
                    d = {"score": score, "post": post, "pv": pv}
                    if ii == len(kts_w) - 1:
                        d["after"] = (lambda g=g, po=po: finish(k, g, tq, po, 2, False, False, par))
                    pairsA.append(d)
            pairsA[0]["pre"] = setup
            def selection_part2():
                for ts in range(4):
                    self.tr(PS7b[0:64, ts * 128:(ts + 1) * 128], selbf[ts], self.identB, [slb[ts], self.cB], [PB[7]])
                for g in range(3):
                    P.op("act", lambda e, g=g: e.copy(qc[par][g][64:128, :], PS7b[0:64, 0:512]), [PB[7]], [qsb[par][g]])
            pairsB = []
            nk = 4 * tq + 4
            for g in range(3):
                po = 4 + (st["grp"] % 2); st["grp"] += 1
                for kt in range(nk):
                    c0 = max(0, 128 * (kt - 4 * tq))
                    c1 = 512

                    def score(ps, kt=kt, g=g, c0=c0, c1=c1):
                        self.mm(PS[ps][:, c0:c1], ksE[:, kt * 128:(kt + 1) * 128], qc[par][g][:, c0:c1], True, True,
                                [kvb, kb, qcb[par][g], qsb[par][g]], [PB[ps]])

                    def post(ps, Ei, Ebi, kt=kt, c0=c0, c1=c1):
                        self.act(Ei[:, c0:c1], PS[ps][:, c0:c1], AF.Exp, [PB[ps]], [Ebi], scale=0.125)
                        if kt >= 4 * tq:
                            base = q0 + c0 - kt * 128
                            P.op("pool", lambda e: e.affine_select(Ei[:, c0:c1], Ei[:, c0:c1], [[1, c1 - c0]], ALU.is_ge, 0.0, base=base, channel_multiplier=-1), [Ebi], [Ebi])

                    def pv(Ei, Ebi, kt=kt, po=po, c0=c0, c1=c1):
                        self.mm(PS[po][:, c0:c1], vsA[:, kt, :], Ei[:, c0:c1], kt == 0, kt == nk - 1, [kvb, Ebi], [PB[po]], sgc=True)
                    d = {"score": score, "post": post, "pv": pv}
                    if kt == nk - 1:
                        d["after"] = (lambda g=g, po=po: finish(k, g, tq, po, 1, False, True, par))
                    pairsB.append(d)
            pairsB[0]["pre"] = selection_part2
            allpairs.extend(pairsA); allpairs.extend(pairsB)
        for tq in range(NQ):
            add_tq(k, tq, allpairs)
        self.run_pipeline(allpairs, new_S, new_E)
    P.barrier()
    self.xattn(l)


Builder.nsa_attention = _nsa_attention
```
